# Optimizing a Trainium2 kernel written in Bass

```python
import jax, jax.numpy as jnp
from jax import lax
import numpy as np

D_MODEL = 1024
BATCH = 4
SEQ = 8192
DEPTH = 2

CHUNK = 64
N_MOD = 6
EPS = 1e-6

M_HEADS = 4
M_QK_DIM = 128
M_V_DIM = 256
M_QK_WIDTH = M_HEADS * M_QK_DIM
M_V_WIDTH = M_HEADS * M_V_DIM
CONV_WIDTH = 4

A_HEADS = 8
Q_LORA = 384
KV_LORA = 256
QK_NOPE = 128
QK_ROPE = 64
V_HEAD = 128
A_V_WIDTH = A_HEADS * V_HEAD
ROPE_THETA = 10000.0
Q_BLOCK = 128

N_BRANCHES = 2
IN_SIZES = (M_QK_WIDTH, M_QK_WIDTH, M_V_WIDTH, M_V_WIDTH, M_HEADS, M_HEADS,
            Q_LORA, KV_LORA, QK_ROPE, N_BRANCHES * D_MODEL)
D_IN = sum(IN_SIZES)
IN_SPLIT_POINTS = tuple(sum(IN_SIZES[:i + 1]) for i in range(len(IN_SIZES) - 1))

N_GROUPS = 4
EXPERTS_PER_GROUP = 8
N_EXPERTS = N_GROUPS * EXPERTS_PER_GROUP
TOP_K = 2
D_EXPERT = 256
MOE_BLOCK = 128

kernel_name = "hybrid_mlstm_mla_hmoe_adaln"


def rms_norm(x, g):
    xf = x.astype(jnp.float32)
    y = xf * lax.rsqrt(jnp.mean(xf * xf, axis=-1, keepdims=True) + EPS)
    return (y * g.astype(jnp.float32)).astype(x.dtype)


def modulate(h, shift, scale):
    return h * (1.0 + scale[:, None, :]) + shift[:, None, :]


def apply_rope(x, cos, sin):
    x1, x2 = jnp.split(x, 2, axis=-1)
    return jnp.concatenate([x1 * cos - x2 * sin, x1 * sin + x2 * cos], axis=-1)


def causal_depthwise_conv(x, w, b):
    C = x.shape[-1]
    y = lax.conv_general_dilated(x, w[:, None, :], window_strides=(1,),
                                 padding=((CONV_WIDTH - 1, 0),),
                                 dimension_numbers=('NWC', 'WIO', 'NWC'),
                                 feature_group_count=C)
    return y + b


def mlstm_chunkwise(q, k, v, log_i, log_f):
    B, S, NH, DQK = q.shape
    DV = v.shape[-1]
    nc = S // CHUNK

    def to_chunks(t):
        t = t.reshape(B, nc, CHUNK, NH, *t.shape[3:])
        return jnp.moveaxis(t, (1, 3), (0, 2))

    causal = jnp.tril(jnp.ones((CHUNK, CHUNK), dtype=bool))

    def step(carry, xs):
        C, n, m = carry
        qc, kc, vc, lic, lfc = xs
        b = jnp.cumsum(lfc, axis=-1)
        dmat = b[..., :, None] - b[..., None, :] + lic[..., None, :]
        dmat = jnp.where(causal, dmat, -jnp.inf)
        m_inter = b + m[..., None]
        m_t = jnp.maximum(jnp.max(dmat, axis=-1), m_inter)
        s = jnp.einsum('bhtd,bhsd->bhts', qc, kc) * jnp.exp(dmat - m_t[..., None])
        w_inter = jnp.exp(m_inter - m_t)
        num = (jnp.einsum('bhts,bhsv->bhtv', s, vc)
               + w_inter[..., None] * jnp.einsum('bhtd,bhvd->bhtv', qc, C))
        den = jnp.sum(s, axis=-1) + w_inter * jnp.einsum('bhtd,bhd->bht', qc, n)
        h = num / jnp.maximum(jnp.abs(den), jnp.exp(-m_t))[..., None]
        b_last = b[..., -1]
        a = b_last[..., None] - b + lic
        m_new = jnp.maximum(b_last + m, jnp.max(a, axis=-1))
        decay = jnp.exp(b_last + m - m_new)
        wk = jnp.exp(a - m_new[..., None])
        C_new = decay[..., None, None] * C + jnp.einsum('bhs,bhsv,bhsd->bhvd', wk, vc, kc)
        n_new = decay[..., None] * n + jnp.einsum('bhs,bhsd->bhd', wk, kc)
        return (C_new, n_new, m_new), h

    init = (jnp.zeros((B, NH, DV, DQK), jnp.float32),
            jnp.zeros((B, NH, DQK), jnp.float32),
            jnp.zeros((B, NH), jnp.float32))
    _, h = lax.scan(step, init, (to_chunks(q), to_chunks(k), to_chunks(v),
                                 to_chunks(log_i), to_chunks(log_f)))
    return jnp.moveaxis(h, (0, 2), (1, 3)).reshape(B, S, NH, DV)


def chunk_causal_mla_attention(q_nope, q_rope, k_nope, k_rope, v):
    B, S, H, _ = q_nope.shape
    nqb = S // Q_BLOCK
    key_chunk = jnp.arange(S) // CHUNK
    scale = (QK_NOPE + QK_ROPE) ** -0.5

    def to_blocks(t):
        return t.reshape(B, nqb, Q_BLOCK, *t.shape[2:]).swapaxes(0, 1)

    def one_block(args):
        qn, qr, blk = args
        s = (jnp.einsum('bqhd,bkhd->bhqk', qn, k_nope)
             + jnp.einsum('bqhr,bkr->bhqk', qr, k_rope)).astype(jnp.float32) * scale
        q_chunk = (blk * Q_BLOCK + jnp.arange(Q_BLOCK)) // CHUNK
        mask = key_chunk[None, :] <= q_chunk[:, None]
        s = jnp.where(mask, s, -jnp.inf)
        p = jax.nn.softmax(s, axis=-1).astype(v.dtype)
        return jnp.einsum('bhqk,bkhd->bqhd', p, v)

    out = lax.map(one_block, (to_blocks(q_nope), to_blocks(q_rope), jnp.arange(nqb)))
    return out.swapaxes(0, 1).reshape(B, S, H * V_HEAD)


def mixer_sublayer(h, cos, sin, w_in, conv_w, conv_b, igate_b, fgate_b, q_norm_g,
                   kv_norm_g, w_uq, w_ukv, w_branch_m, w_branch_a, w_out):
    B, S, _ = h.shape
    proj = h @ w_in
    (q_m, k_m, v_m, o_m, i_m, f_m, c_q, c_kv, k_r, gate_pre) = jnp.split(
        proj, IN_SPLIT_POINTS, axis=-1)

    qk = jax.nn.silu(causal_depthwise_conv(jnp.concatenate([q_m, k_m], axis=-1), conv_w, conv_b))
    q_m, k_m = jnp.split(qk.astype(jnp.float32), 2, axis=-1)
    q_m = q_m.reshape(B, S, M_HEADS, M_QK_DIM) * (M_QK_DIM ** -0.5)
    k_m = k_m.reshape(B, S, M_HEADS, M_QK_DIM)
    v_m = v_m.astype(jnp.float32).reshape(B, S, M_HEADS, M_V_DIM)
    log_i = (i_m + igate_b).astype(jnp.float32)
    log_f = jax.nn.log_sigmoid((f_m + fgate_b).astype(jnp.float32))
    y_m = mlstm_chunkwise(q_m, k_m, v_m, log_i, log_f).reshape(B, S, M_V_WIDTH).astype(h.dtype)
    y_m = jax.nn.sigmoid(o_m) * y_m

    q_a = (rms_norm(c_q, q_norm_g) @ w_uq).reshape(B, S, A_HEADS, QK_NOPE + QK_ROPE)
    q_nope = q_a[..., :QK_NOPE]
    q_rope = apply_rope(q_a[..., QK_NOPE:], cos[:, None, :], sin[:, None, :])
    kv = (rms_norm(c_kv, kv_norm_g) @ w_ukv).reshape(B, S, A_HEADS, QK_NOPE + V_HEAD)
    k_nope, v_a = kv[..., :QK_NOPE], kv[..., QK_NOPE:]
    k_rope = apply_rope(k_r, cos, sin)
    y_a = chunk_causal_mla_attention(q_nope, q_rope, k_nope, k_rope, v_a)

    g_m, g_a = jnp.split(jax.nn.sigmoid(gate_pre), 2, axis=-1)
    merged = g_m * (y_m @ w_branch_m) + g_a * (y_a @ w_branch_a)
    return merged @ w_out


def hierarchical_moe(h, w_group, b_group, w_router, b_router, w_gate, w_up, w_down):
    B, S, D = h.shape
    T = B * S
    ht = h.reshape(T, D)
    hf = ht.astype(jnp.float32)
    grp_probs = jax.nn.softmax(hf @ w_group.astype(jnp.float32) + b_group.astype(jnp.float32), axis=-1)
    grp = jnp.argmax(grp_probs, axis=-1)
    p_grp = jnp.take_along_axis(grp_probs, grp[:, None], axis=-1)[:, 0]
    e_logits = (hf @ w_router.astype(jnp.float32) + b_router.astype(jnp.float32)).reshape(
        T, N_GROUPS, EXPERTS_PER_GROUP)
    in_grp = jnp.take_along_axis(e_logits, grp[:, None, None], axis=1)[:, 0]
    top_p, top_e = lax.top_k(jax.nn.softmax(in_grp, axis=-1), TOP_K)
    weights = p_grp[:, None] * top_p / jnp.sum(top_p, axis=-1, keepdims=True)
    expert_id = grp[:, None] * EXPERTS_PER_GROUP + top_e

    n_assign = T * TOP_K
    flat_e = expert_id.reshape(-1).astype(jnp.int32)
    flat_t = jnp.repeat(jnp.arange(T, dtype=jnp.int32), TOP_K)
    flat_w = weights.reshape(-1)
    order = jnp.argsort(flat_e)
    se, st, sw = flat_e[order], flat_t[order], flat_w[order]
    counts = jnp.bincount(flat_e, length=N_EXPERTS)
    padded = (counts + MOE_BLOCK - 1) // MOE_BLOCK * MOE_BLOCK
    starts = jnp.cumsum(counts) - counts
    p_ends = jnp.cumsum(padded)
    p_starts = p_ends - padded
    dest = p_starts[se] + jnp.arange(n_assign, dtype=jnp.int32) - starts[se]
    cap = n_assign + N_EXPERTS * MOE_BLOCK
    n_blocks = cap // MOE_BLOCK
    slot_tok = jnp.zeros((cap,), jnp.int32).at[dest].set(st)
    slot_w = jnp.zeros((cap,), jnp.float32).at[dest].set(sw)
    blk_start = jnp.arange(n_blocks) * MOE_BLOCK
    blk_expert = jnp.minimum(jnp.sum(p_ends[None, :] <= blk_start[:, None], axis=1), N_EXPERTS - 1)
    xg = ht[slot_tok].reshape(n_blocks, MOE_BLOCK, D)

    def run_block(args):
        xb, e, wb = args
        a = jax.nn.silu(xb @ w_gate[e]) * (xb @ w_up[e])
        return (a @ w_down[e]) * wb[:, None].astype(xb.dtype)

    out = lax.map(run_block, (xg, blk_expert, slot_w.reshape(n_blocks, MOE_BLOCK)))
    y = jnp.zeros((T, D), h.dtype).at[slot_tok].add(out.reshape(cap, D))
    return y.reshape(B, S, D)


def setup_inputs(seed: int = 0) -> dict:
    key = jax.random.key(seed)
    ks = jax.random.split(key, 26)
    f32 = jnp.float32

    def nrm(k, shape, scale):
        return jax.random.normal(k, shape, f32) * scale

    def gain(k, shape):
        return 1.0 + 0.02 * jax.random.normal(k, shape, f32)

    L, D = DEPTH, D_MODEL
    return {
        "x": nrm(ks[0], (BATCH, SEQ, D), 1.0),
        "c": nrm(ks[1], (BATCH, D), 1.0),
        "mod_w": nrm(ks[2], (L, D, N_MOD * D), 0.5 * D ** -0.5),
        "mod_b": nrm(ks[3], (L, N_MOD * D), 0.02),
        "norm1_g": gain(ks[4], (L, D)),
        "w_in": nrm(ks[5], (L, D, D_IN), D ** -0.5),
        "conv_w": nrm(ks[6], (L, CONV_WIDTH, 2 * M_QK_WIDTH), CONV_WIDTH ** -0.5),
        "conv_b": nrm(ks[7], (L, 2 * M_QK_WIDTH), 0.02),
        "igate_b": nrm(ks[8], (L, M_HEADS), 0.1),
        "fgate_b": 3.0 + nrm(ks[9], (L, M_HEADS), 0.5),
        "q_norm_g": gain(ks[10], (L, Q_LORA)),
        "kv_norm_g": gain(ks[11], (L, KV_LORA)),
        "w_uq": nrm(ks[12], (L, Q_LORA, A_HEADS * (QK_NOPE + QK_ROPE)), Q_LORA ** -0.5),
        "w_ukv": nrm(ks[13], (L, KV_LORA, A_HEADS * (QK_NOPE + V_HEAD)), KV_LORA ** -0.5),
        "w_branch_m": nrm(ks[14], (L, M_V_WIDTH, D), M_V_WIDTH ** -0.5),
        "w_branch_a": nrm(ks[15], (L, A_V_WIDTH, D), A_V_WIDTH ** -0.5),
        "w_out": nrm(ks[16], (L, D, D), D ** -0.5),
        "norm2_g": gain(ks[17], (L, D)),
        "w_group": nrm(ks[18], (L, D, N_GROUPS), D ** -0.5),
        "b_group": nrm(ks[19], (L, N_GROUPS), 0.01),
        "w_router": nrm(ks[20], (L, D, N_EXPERTS), D ** -0.5),
        "b_router": nrm(ks[21], (L, N_EXPERTS), 0.01),
        "w_expert_gate": nrm(ks[22], (L, N_EXPERTS, D, D_EXPERT), D ** -0.5),
        "w_expert_up": nrm(ks[23], (L, N_EXPERTS, D, D_EXPERT), D ** -0.5),
        "w_expert_down": nrm(ks[24], (L, N_EXPERTS, D_EXPERT, D), D_EXPERT ** -0.5),
        "final_norm_g": gain(ks[25], (D,)),
    }


def reference(x, c, mod_w, mod_b, norm1_g, w_in, conv_w, conv_b, igate_b, fgate_b,
              q_norm_g, kv_norm_g, w_uq, w_ukv, w_branch_m, w_branch_a, w_out, norm2_g,
              w_group, b_group, w_router, b_router, w_expert_gate, w_expert_up,
              w_expert_down, final_norm_g):
    B, S, D = x.shape
    pos = jnp.arange(S, dtype=jnp.float32)
    inv_freq = 1.0 / (ROPE_THETA ** (jnp.arange(0, QK_ROPE, 2, dtype=jnp.float32) / QK_ROPE))
    ang = pos[:, None] * inv_freq[None, :]
    cos = jnp.cos(ang).astype(x.dtype)
    sin = jnp.sin(ang).astype(x.dtype)
    cond = jax.nn.silu(c)
    for l in range(DEPTH):
        mod = cond @ mod_w[l] + mod_b[l]
        shift1, scale1, gate1, shift2, scale2, gate2 = jnp.split(mod, N_MOD, axis=-1)
        h = modulate(rms_norm(x, norm1_g[l]), shift1, scale1)
        x = x + gate1[:, None, :] * mixer_sublayer(
            h, cos, sin, w_in[l], conv_w[l], conv_b[l], igate_b[l], fgate_b[l],
            q_norm_g[l], kv_norm_g[l], w_uq[l], w_ukv[l], w_branch_m[l], w_branch_a[l], w_out[l])
        h = modulate(rms_norm(x, norm2_g[l]), shift2, scale2)
        x = x + gate2[:, None, :] * hierarchical_moe(
            h, w_group[l], b_group[l], w_router[l], b_router[l],
            w_expert_gate[l], w_expert_up[l], w_expert_down[l])
    return rms_norm(x, final_norm_g)
```

```python
import math
import numpy as np
import ml_dtypes
import concourse.bass as bass
import concourse.mybir as mybir
from concourse.bass_utils import run_bass_kernel_spmd

F32 = mybir.dt.float32
BF16 = mybir.dt.bfloat16
AF = mybir.ActivationFunctionType
ALU = mybir.AluOpType
AX = mybir.AxisListType

D = 1024
NCORES = 8
EPS = 1e-6
BIG = 30000.0


class Tok:
    __slots__ = ("w", "r", "wsem", "rsem", "name")

    def __init__(self, name=""):
        self.w = None
        self.r = []
        self.wsem = None
        self.rsem = None
        self.name = name


class _Sem:
    __slots__ = ("h", "cnt", "key")

    def __init__(self, h, key):
        self.h = h
        self.cnt = 0
        self.key = key


class _Eng:
    def __init__(self, name, e, sem):
        self.name = name
        self.e = e
        self.sem = sem
        self.seen = {}


class KB:
    def __init__(self):
        self.nc = bass.Bass("TRN2", target_bir_lowering=False)
        nc = self.nc
        self._nsem = 0
        self.eng = {}
        for name in ("tensor", "vector", "scalar", "gpsimd", "sync"):
            self.eng[name] = _Eng(name, getattr(nc, name), self._newsem("e_" + name))
        self._stack = []
        self._dsems = []
        self._free_dsems = []
        self._live_dsems = []
        self.ninst = 0

    def _newsem(self, name):
        if not name.startswith("e_") and self._free_dsems:
            sem = self._free_dsems.pop()
            self._live_dsems.append(sem)
            return sem
        self._nsem += 1
        h = self.nc.semaphore(name + "_%d" % self._nsem).__enter__()
        sem = _Sem(h, self._nsem)
        if not name.startswith("e_"):
            self._dsems.append(sem)
            self._live_dsems.append(sem)
        return sem

    def dram_in(self, name, shape, dt):
        return self.nc.dram_tensor(name, list(shape), dt, kind="ExternalInput").ap()

    def dram_out(self, name, shape, dt):
        return self.nc.dram_tensor(name, list(shape), dt, kind="ExternalOutput").ap()

    def dram_tmp(self, name, shape, dt):
        return self.nc.dram_tensor(name, list(shape), dt, kind="Internal").ap()

    def sbuf(self, name, shape, dt):
        self._uid = getattr(self, "_uid", 0) + 1
        cm = self.nc.sbuf_tensor("%s_u%d" % (name, self._uid), list(shape), dt)
        t = cm.__enter__()
        self._stack.append(cm)
        return t

    def psum(self, name, shape, dt):
        self._uid = getattr(self, "_uid", 0) + 1
        cm = self.nc.psum_tensor("%s_u%d" % (name, self._uid), list(shape), dt)
        t = cm.__enter__()
        self._stack.append(cm)
        return t

    def mark(self):
        return (len(self._stack), len(self._live_dsems))

    def release(self, mark):
        self.barrier()
        while len(self._stack) > mark[0]:
            self._stack.pop().__exit__(None, None, None)
        while len(self._live_dsems) > mark[1]:
            self._free_dsems.append(self._live_dsems.pop())

    def barrier(self):
        sems = [E.sem for E in self.eng.values()] + self._dsems
        for E in self.eng.values():
            for sem in sems:
                if sem is E.sem or sem.cnt == 0:
                    continue
                if E.seen.get(sem.key, 0) >= sem.cnt:
                    continue
                E.e.wait_ge(sem.h, sem.cnt)
                E.seen[sem.key] = sem.cnt
                self.ninst += 1

    def _wait(self, E, evs):
        need = {}
        for ev in evs:
            if ev is None:
                continue
            sem, val = ev
            if sem is E.sem and E.name == "tensor":
                continue
            if need.get(sem.key, (None, 0))[1] < val:
                need[sem.key] = (sem, val)
        for key, (sem, val) in need.items():
            if E.seen.get(key, 0) >= val:
                continue
            assert val <= sem.cnt, "waiting on an event that is never signalled"
            E.e.wait_ge(sem.h, val)
            E.seen[key] = val
            self.ninst += 1

    def _deps(self, reads, writes):
        evs = []
        for t in reads:
            if hasattr(t, "_toks"):
                evs.extend(x.w for x in t._toks)
            else:
                evs.append(t.w)
        for t in writes:
            evs.append(t.w)
            evs.extend(t.r)
        return evs

    def op(self, engname, fn, reads=(), writes=(), sig=True):
        E = self.eng[engname]
        self._wait(E, self._deps(reads, writes))
        ins = fn(E.e)
        self.ninst += 1
        if sig:
            E.sem.cnt += 1
            ins.then_inc(E.sem.h, 1)
            ev = (E.sem, E.sem.cnt)
        else:
            assert engname == "tensor"
            ev = (E.sem, E.sem.cnt + 1)
        for t in reads:
            for tt_ in (t._toks if hasattr(t, "_toks") else (t,)):
                tt_.r.append(ev)
                if len(tt_.r) > 24:
                    tt_.r = self._compact(tt_.r)
        for t in writes:
            t.w = ev
            t.r = []
        return ins

    @staticmethod
    def _compact(r):
        best = {}
        for sem, val in r:
            if best.get(sem.key, (None, 0))[1] < val:
                best[sem.key] = (sem, val)
        return list(best.values())

    def dma(self, queue, out, in_, reads=(), writes=(), **kw):
        E = self.eng[queue]
        self._wait(E, self._deps(reads, writes))
        ins = E.e.dma_start(out=out, in_=in_, **kw)
        self.ninst += 1
        if writes and writes[0].wsem is None and not getattr(writes[0], "_dram", False):
            pass
        tok = None
        for t in writes:
            if not getattr(t, "name", "").startswith("dram:"):
                tok = t
                if tok.wsem is None:
                    tok.wsem = self._newsem("dw")
                sem = tok.wsem
                break
        if tok is None:
            for t in reads:
                if not getattr(t, "name", "").startswith("dram:"):
                    tok = t
                    if tok.rsem is None:
                        tok.rsem = self._newsem("dr")
                    sem = tok.rsem
                    break
        assert tok is not None, "dma needs an sbuf-side token"
        sem.cnt += 16
        ins.then_inc(sem.h, 16)
        ev = (sem, sem.cnt)
        for t in reads:
            for tt_ in (t._toks if hasattr(t, "_toks") else (t,)):
                tt_.r.append(ev)
                if len(tt_.r) > 24:
                    tt_.r = self._compact(tt_.r)
        for t in writes:
            t.w = ev
            t.r = []
        return ins

    def finish(self, toks):
        E = self.eng["sync"]
        evs = []
        for t in toks:
            evs.append(t.w)
            evs.extend(t.r)
        self._wait(E, evs)


def load_consts(kb):
    nc = kb.nc
    c = {}
    c["tok"] = Tok("consts")
    ones_f = kb.sbuf("c_ones_f", [128, 128], F32)
    ones_b = kb.sbuf("c_ones_b", [128, 128], BF16)
    id_f = kb.sbuf("c_id_f", [128, 128], F32)
    id_b = kb.sbuf("c_id_b", [128, 128], BF16)
    tri_f = kb.sbuf("c_tri_f", [128, 128], F32)
    tris_f = kb.sbuf("c_tris_f", [128, 128], F32)
    tri_b = kb.sbuf("c_tri_b", [128, 128], BF16)
    t = c["tok"]
    kb.op("gpsimd", lambda e: e.memset(ones_f[:], 1.0), writes=[t])
    kb.op("gpsimd", lambda e: e.memset(ones_b[:], 1.0), writes=[t])
    kb.op("gpsimd", lambda e: e.affine_select(out=id_f[:], in_=ones_f[:], pattern=[[-1, 128]],
                                             compare_op=ALU.is_equal, fill=0.0, base=0,
                                             channel_multiplier=1), reads=[t], writes=[t])
    kb.op("gpsimd", lambda e: e.tensor_copy(out=id_b[:], in_=id_f[:]), reads=[t], writes=[t])
    kb.op("gpsimd", lambda e: e.affine_select(out=tri_f[:], in_=ones_f[:], pattern=[[1, 128]],
                                             compare_op=ALU.is_ge, fill=0.0, base=0,
                                             channel_multiplier=-1), reads=[t], writes=[t])
    kb.op("gpsimd", lambda e: e.tensor_copy(out=tri_b[:], in_=tri_f[:]), reads=[t], writes=[t])
    kb.op("gpsimd", lambda e: e.affine_select(out=tris_f[:], in_=ones_f[:], pattern=[[-1, 128]],
                                             compare_op=ALU.is_gt, fill=0.0, base=0,
                                             channel_multiplier=1), reads=[t], writes=[t])
    eps = kb.sbuf("c_eps", [128, 1], F32)
    kb.op("gpsimd", lambda e: e.memset(eps[:], EPS), writes=[t])
    c["eps"] = eps
    one = kb.sbuf("c_one", [128, 1], F32)
    kb.op("gpsimd", lambda e: e.memset(one[:], 1.0), writes=[t])
    c["one"] = one
    ones_b512 = kb.sbuf("c_ones_b512", [128, 512], BF16)
    kb.op("gpsimd", lambda e: e.memset(ones_b512[:], 1.0), writes=[t])
    c["ones_b512"] = ones_b512
    c.update(ones_f=ones_f, ones_b=ones_b, id_f=id_f, id_b=id_b, tri_f=tri_f, tris_f=tris_f,
             tri_b=tri_b)
    return c


def emit_mod(kb, consts, modw, modb, cvec, nvec, outs, ps_tag="modps"):
    nc = kb.nc
    mk = kb.mark()
    cpc = kb.sbuf("mod_cpc", [128, 8], F32)
    cond = kb.sbuf("mod_cond", [128, 8], F32)
    bc = kb.sbuf("mod_bc", [128, 8, 128], F32)
    wt = kb.sbuf("mod_w", [128, 8, 1024], F32)
    bt = kb.sbuf("mod_b", [128, 1024], F32)
    ps = kb.psum(ps_tag, [128, 512], F32)
    t_c, t_cond, t_bc, t_w, t_b, t_ps = (Tok("m%d" % i) for i in range(6))
    kb.dma("sync", cpc[:], cvec.rearrange("(p c) -> p c", c=8), writes=[t_c])
    kb.op("scalar", lambda e: e.activation(out=cond[:], in_=cpc[:], func=AF.Silu),
          reads=[t_c], writes=[t_cond])
    for c in range(8):
        kb.op("vector", lambda e, c=c: e.tensor_scalar_mul(out=bc[:, c, :], in0=consts["ones_f"][:],
                                                           scalar1=cond[:, c:c + 1]),
              reads=[t_cond, consts["tok"]], writes=[t_bc])
    for v in range(nvec):
        out_t, out_tok = outs[v]
        kb.dma("sync", wt[:], modw[:, v * 1024:(v + 1) * 1024].rearrange("(p c) n -> p c n", c=8),
               writes=[t_w])
        kb.dma("sync", bt[:], modb[v * 1024:(v + 1) * 1024].partition_broadcast(128), writes=[t_b])
        for n2 in range(2):
            for c in range(8):
                kb.op("tensor", lambda e, c=c, n2=n2: e.matmul(ps[:], lhsT=bc[:, c, :],
                                                               rhs=wt[:, c, n2 * 512:(n2 + 1) * 512],
                                                               start=(c == 0), stop=(c == 7)),
                      reads=[t_bc, t_w], writes=[t_ps], sig=(c == 7))
            kb.op("vector", lambda e, n2=n2: e.tensor_tensor(out=out_t[:, n2 * 512:(n2 + 1) * 512],
                                                             in0=ps[:], in1=bt[:, n2 * 512:(n2 + 1) * 512],
                                                             op=ALU.add),
                  reads=[t_ps, t_b], writes=[out_tok])
    kb.release(mk)


def emit_norm_mod(kb, xt, t_x, gmul, t_g, shift, t_s, hb, t_h, scr, t_scr, stat, t_stat, eps_ap):
    kb.op("gpsimd", lambda e: e.memset(stat[:, 0:1], 0.0), writes=[t_stat])
    kb.op("scalar", lambda e: e.activation(out=scr[:], in_=xt[:], func=AF.Square,
                                           accum_out=stat[:, 0:1]),
          reads=[t_x], writes=[t_scr, t_stat])
    kb.op("scalar", lambda e: e.activation(out=stat[:, 1:2], in_=stat[:, 0:1], func=AF.Sqrt,
                                           bias=eps_ap, scale=1.0 / D),
          reads=[t_stat], writes=[t_stat])
    kb.op("vector", lambda e: e.reciprocal(out=stat[:, 2:3], in_=stat[:, 1:2]),
          reads=[t_stat], writes=[t_stat])
    if shift is None:
        kb.op("vector", lambda e: e.scalar_tensor_tensor(out=hb[:], in0=xt[:], scalar=stat[:, 2:3],
                                                         in1=gmul[:], op0=ALU.mult, op1=ALU.mult),
              reads=[t_x, t_stat, t_g], writes=[t_h])
        return
    kb.op("vector", lambda e: e.scalar_tensor_tensor(out=scr[:], in0=xt[:], scalar=stat[:, 2:3],
                                                     in1=gmul[:], op0=ALU.mult, op1=ALU.mult),
          reads=[t_x, t_stat, t_g], writes=[t_scr])
    kb.op("gpsimd", lambda e: e.tensor_tensor(out=hb[:], in0=scr[:], in1=shift[:], op=ALU.add),
          reads=[t_scr, t_s], writes=[t_h])


def build_k1(Sh):
    kb = KB()
    nc = kb.nc
    x = kb.dram_in("x", [Sh, D], F32)
    cvec = kb.dram_in("cvec", [D], F32)
    modw = kb.dram_in("modw", [D, 2 * D], F32)
    modb = kb.dram_in("modb", [2 * D], F32)
    g = kb.dram_in("g", [D], F32)
    hT = kb.dram_out("hT", [8, 128, Sh], BF16)
    consts = load_consts(kb)
    emit_k1_body(kb, consts, x, cvec, modw, modb, g, hT, Sh)
    kb.finish([kb._out_tok])
    return kb


def emit_k1_body(kb, consts, x, cvec, modw, modb, g, hT, Sh, hT_tok=None, x_tok=None):
    shift = kb.sbuf("k1_shift", [128, D], F32)
    gmul = kb.sbuf("k1_gmul", [128, D], F32)
    gt = kb.sbuf("k1_g", [128, D], F32)
    t_shift, t_gmul, t_g = Tok("shift"), Tok("gmul"), Tok("g")
    emit_mod(kb, consts, modw, modb, cvec, 2, [(shift, t_shift), (gmul, t_gmul)])
    kb.dma("sync", gt[:], g.partition_broadcast(128), writes=[t_g])
    kb.op("vector", lambda e: e.scalar_tensor_tensor(out=gmul[:], in0=gmul[:], scalar=1.0, in1=gt[:],
                                                     op0=ALU.add, op1=ALU.mult),
          reads=[t_gmul, t_g], writes=[t_gmul])
    NB = 2
    xt = [kb.sbuf("k1_x%d" % i, [128, D], F32) for i in range(NB)]
    scr = [kb.sbuf("k1_scr%d" % i, [128, D], F32) for i in range(NB)]
    hb = [kb.sbuf("k1_hb%d" % i, [128, D], BF16) for i in range(NB)]
    stat = [kb.sbuf("k1_st%d" % i, [128, 4], F32) for i in range(NB)]
    hTs = [kb.sbuf("k1_hT%d" % i, [128, 8, 512], BF16) for i in range(2)]
    pst = [kb.psum("k1_pst%d" % i, [128, 8, 128], BF16) for i in range(2)]
    t_x = [Tok("x%d" % i) for i in range(NB)]
    t_scr = [Tok("scr%d" % i) for i in range(NB)]
    t_hb = [Tok("hb%d" % i) for i in range(NB)]
    t_st = [Tok("st%d" % i) for i in range(NB)]
    t_hT = [Tok("hT%d" % i) for i in range(2)]
    t_ps = [Tok("ps%d" % i) for i in range(2)]
    t_out = hT_tok if hT_tok is not None else Tok("dram:hT")
    kb._out_tok = t_out
    ntile = Sh // 128
    for i in range(ntile):
        s = i % NB
        grp = (i // 4) % 2
        kb.dma("sync", xt[s][:], x[i * 128:(i + 1) * 128, :], reads=([x_tok] if x_tok is not None else []), writes=[t_x[s]])
        emit_norm_mod(kb, xt[s], t_x[s], gmul, t_gmul, shift, t_shift, hb[s], t_hb[s],
                      scr[s], t_scr[s], stat[s], t_st[s], consts["eps"][:, 0:1])
        p = i % 2
        for c in range(8):
            kb.op("tensor", lambda e, c=c, s=s, p=p: e.transpose(out=pst[p][:, c, :],
                                                                 in_=hb[s][:, c * 128:(c + 1) * 128],
                                                                 identity=consts["id_b"][:]),
                  reads=[t_hb[s], consts["tok"]], writes=[t_ps[p]], sig=(c == 7))
        j = i % 4
        kb.op("scalar", lambda e, p=p, grp=grp, j=j: e.copy(out=hTs[grp][:, :, j * 128:(j + 1) * 128],
                                                            in_=pst[p][:]),
              reads=[t_ps[p]], writes=[t_hT[grp]])
        if j == 3:
            t0 = (i - 3) * 128
            kb.dma("gpsimd", hT[:, :, t0:t0 + 512].rearrange("c p t -> p c t"), hTs[grp][:],
                   reads=[t_hT[grp]], writes=[t_out])


def _mm(kb, out, lhsT, rhs, start, stop, reads, writes):
    return kb.op("tensor", lambda e: e.matmul(out, lhsT=lhsT, rhs=rhs, start=start, stop=stop),
                 reads=reads, writes=writes, sig=stop)


def load_w_bf16(kb, dst, src_rows_cols, tok, nchunk, queue="gpsimd"):
    for c in range(nchunk):
        kb.dma(queue, dst[:, c, :], src_rows_cols[c * 128:(c + 1) * 128, :], writes=[tok])


def emit_mlstm_pass(kb, consts, S, hT, t_hT, w, ymT, t_ymT):
    NT = S // 512
    mk = kb.mark()
    ct = consts["tok"]
    wqk = kb.sbuf("a_wqk", [128, 8, 1024], BF16)
    wtm = kb.sbuf("a_wtm", [128, 8, 2056], BF16)
    cw = kb.sbuf("a_cw", [128, 8, 4], F32)
    cb = kb.sbuf("a_cb", [128, 8], F32)
    ifb = kb.sbuf("a_ifb", [128, 8], F32)
    t_w = Tok("a_w")
    t_wt = Tok("a_wt")
    t_cw = Tok("a_cw")
    load_w_bf16(kb, wqk, w["wqk"], t_w, 8)
    load_w_bf16(kb, wtm, w["wtm"], t_wt, 8)
    kb.dma("sync", cw[:], w["cw"], writes=[t_cw])
    kb.dma("sync", cb[:], w["cb"], writes=[t_cw])
    kb.dma("sync", ifb[:], w["ifb"].partition_broadcast(128), writes=[t_cw])

    hTs = [kb.sbuf("a_hT%d" % i, [128, 8, 512], BF16) for i in range(2)]
    t_hTs = [Tok("a_hT%d" % i) for i in range(2)]
    pre = kb.sbuf("a_pre", [128, 8, 515], F32)
    t_pre = [Tok("a_pre%d" % g) for g in range(8)]
    cacc = [kb.sbuf("a_cacc%d" % i, [128, 512], F32) for i in range(2)]
    t_cacc = [Tok("a_cacc%d" % i) for i in range(2)]
    qkT = kb.sbuf("a_qkT", [128, 8, 512], BF16)
    t_qk = [Tok("a_qk%d" % g) for g in range(8)]
    Vp = [kb.sbuf("a_vp%d" % i, [128, 4, 257], BF16) for i in range(2)]
    t_vp = [Tok("a_vp%d" % i) for i in range(2)]
    sgo = [kb.sbuf("a_sgo%d" % i, [128, 1024], F32) for i in range(2)]
    t_sgo = [Tok("a_sgo%d" % i) for i in range(2)]
    gsb = [kb.sbuf("a_g%d" % i, [128, 48], F32) for i in range(2)]
    t_g = [Tok("a_g%d" % i) for i in range(2)]
    Zf = kb.sbuf("a_zf", [128, 4, 257], F32)
    Zb = kb.sbuf("a_zb", [128, 4, 257], BF16)
    t_zf = [Tok("a_zf%d" % h) for h in range(4)]
    t_zb = [Tok("a_zb%d" % h) for h in range(4)]
    s0sb = [kb.sbuf("a_s0%d" % i, [128, 128], BF16) for i in range(2)]
    t_s0 = [Tok("a_s0%d" % i) for i in range(2)]
    ytok = [kb.sbuf("a_yt%d" % i, [128, 256], BF16) for i in range(2)]
    t_yt = [Tok("a_yt%d" % i) for i in range(2)]
    khat = [kb.sbuf("a_kh%d" % i, [128, 128], BF16) for i in range(2)]
    t_kh = [Tok("a_kh%d" % i) for i in range(2)]
    dsm = [kb.sbuf("a_d%d" % i, [128, 8], F32) for i in range(2)]
    t_d = [Tok("a_d%d" % i) for i in range(2)]
    yst = [kb.sbuf("a_yst%d" % i, [128, 8, 512], BF16) for i in range(2)]
    t_yst = [Tok("a_yst%d" % i) for i in range(2)]

    ps_fm = [kb.psum("a_psfm%d" % i, [128, 512], F32) for i in range(2)]
    t_psfm = [Tok("a_psfm%d" % i) for i in range(2)]
    ps_tm = [kb.psum("a_pstm%d" % i, [128, 512], F32) for i in range(2)]
    t_pstm = [Tok("a_pstm%d" % i) for i in range(2)]
    ps_m = kb.psum("a_psm", [128, 512], F32)
    t_ps_s0, t_ps_o, t_ps_if, t_ps_cs = Tok("ps_s0"), Tok("ps_o"), Tok("ps_if"), Tok("ps_cs")
    ps_u = kb.psum("a_psu", [128, 257], F32)
    t_ps_u = Tok("ps_u")
    ps_t = kb.psum("a_pst", [128, 3, 128], BF16)
    t_ps_yt, t_ps_kt = Tok("ps_yt"), Tok("ps_kt")

    kb.op("gpsimd", lambda e: e.memset(pre[:, :, 0:3], 0.0), writes=t_pre)
    kb.op("gpsimd", lambda e: e.memset(Zf[:], 0.0), writes=t_zf)
    kb.op("gpsimd", lambda e: e.memset(Zb[:], 0.0), writes=t_zb)
    for i in range(2):
        kb.op("gpsimd", lambda e, i=i: e.memset(Vp[i][:, :, 256:257], 1.0), writes=[t_vp[i]])

    fmi = 0
    tmi = 0
    hi = 0
    for tt in range(NT):
        s = tt % 2
        kb.dma("sync", hTs[s][:], hT[:, :, tt * 512:(tt + 1) * 512].rearrange("c p t -> p c t"),
               reads=[t_hT], writes=[t_hTs[s]])
        for g in range(8):
            p = fmi % 2
            fmi += 1
            for c in range(8):
                _mm(kb, ps_fm[p][:], wqk[:, c, g * 128:(g + 1) * 128], hTs[s][:, c, :], c == 0, c == 7,
                    [t_w, t_hTs[s]], [t_psfm[p]])
            kb.op("scalar", lambda e, p=p, g=g: e.copy(out=pre[:, g, 3:515], in_=ps_fm[p][:]),
                  reads=[t_psfm[p]], writes=[t_pre[g]])
            a = g % 2
            kb.op("vector", lambda e, a=a, g=g: e.tensor_scalar_mul(out=cacc[a][:], in0=pre[:, g, 0:512],
                                                                    scalar1=cw[:, g, 0:1]),
                  reads=[t_pre[g], t_cw], writes=[t_cacc[a]])
            for j in range(1, 4):
                kb.op("vector", lambda e, a=a, g=g, j=j: e.scalar_tensor_tensor(
                    out=cacc[a][:], in0=pre[:, g, j:j + 512], scalar=cw[:, g, j:j + 1], in1=cacc[a][:],
                    op0=ALU.mult, op1=ALU.add), reads=[t_pre[g], t_cw, t_cacc[a]], writes=[t_cacc[a]])
            kb.op("scalar", lambda e, a=a, g=g: e.activation(out=qkT[:, g, :], in_=cacc[a][:], func=AF.Silu,
                                                             bias=cb[:, g:g + 1]),
                  reads=[t_cacc[a], t_cw], writes=[t_qk[g]])
            kb.op("gpsimd", lambda e, g=g: e.tensor_copy(out=pre[:, g, 0:3], in_=pre[:, g, 512:515]),
                  reads=[t_pre[g]], writes=[t_pre[g]])
        for j in range(4):
            vs = (tt * 4 + j) % 2
            tsl = slice(j * 128, (j + 1) * 128)
            for half in range(2):
                p = tmi % 2
                tmi += 1
                for c in range(8):
                    _mm(kb, ps_tm[p][:], hTs[s][:, c, tsl], wtm[:, c, half * 512:(half + 1) * 512],
                        c == 0, c == 7, [t_wt, t_hTs[s]], [t_pstm[p]])
                kb.op("scalar", lambda e, p=p, vs=vs, half=half: e.copy(
                    out=Vp[vs][:, 2 * half:2 * half + 2, 0:256],
                    in_=ps_tm[p][:].rearrange("p (h v) -> p h v", h=2)),
                    reads=[t_pstm[p]], writes=[t_vp[vs]])
            for half in range(2):
                p = tmi % 2
                tmi += 1
                for c in range(8):
                    _mm(kb, ps_tm[p][:], hTs[s][:, c, tsl], wtm[:, c, 1024 + half * 512:1024 + (half + 1) * 512],
                        c == 0, c == 7, [t_wt, t_hTs[s]], [t_pstm[p]])
                kb.op("scalar", lambda e, p=p, vs=vs, half=half: e.activation(
                    out=sgo[vs][:, half * 512:(half + 1) * 512], in_=ps_tm[p][:], func=AF.Sigmoid),
                    reads=[t_pstm[p]], writes=[t_sgo[vs]])
            for c in range(8):
                _mm(kb, ps_m[:, 385:393], hTs[s][:, c, tsl], wtm[:, c, 2048:2056], c == 0, c == 7,
                    [t_wt, t_hTs[s]], [t_ps_if])
            G = gsb[vs]
            tg = t_g[vs]
            kb.op("vector", lambda e, G=G: e.tensor_tensor(out=G[:, 0:8], in0=ps_m[:, 385:393], in1=ifb[:],
                                                           op=ALU.add),
                  reads=[t_ps_if, t_cw], writes=[tg])
            kb.op("scalar", lambda e, G=G: e.activation(out=G[:, 8:12], in_=G[:, 4:8], func=AF.Exp, scale=-1.0),
                  reads=[tg], writes=[tg])
            kb.op("scalar", lambda e, G=G: e.activation(out=G[:, 8:12], in_=G[:, 8:12], func=AF.Ln,
                                                        bias=consts["one"][:, 0:1]),
                  reads=[tg, ct], writes=[tg])
            kb.op("vector", lambda e, G=G: e.tensor_scalar_mul(out=G[:, 8:12], in0=G[:, 8:12], scalar1=-1.0),
                  reads=[tg], writes=[tg])
            _mm(kb, ps_m[:, 393:397], consts["tri_f"][:], G[:, 8:12], True, True, [ct, tg], [t_ps_cs])
            _mm(kb, ps_m[:, 397:401], consts["tris_f"][:], G[:, 8:12], True, True, [ct, tg], [t_ps_cs])
            _mm(kb, ps_m[:, 401:405], consts["ones_f"][:], G[:, 8:12], True, True, [ct, tg], [t_ps_cs])
            kb.op("vector", lambda e, G=G: e.tensor_copy(out=G[:, 16:20], in_=ps_m[:, 393:397]),
                  reads=[t_ps_cs], writes=[tg])
            kb.op("vector", lambda e, G=G: e.tensor_tensor(out=G[:, 20:24], in0=G[:, 0:4], in1=ps_m[:, 393:397],
                                                           op=ALU.subtract),
                  reads=[t_ps_cs, tg], writes=[tg])
            kb.op("vector", lambda e, G=G: e.tensor_tensor(out=G[:, 24:28], in0=G[:, 0:4], in1=ps_m[:, 397:401],
                                                           op=ALU.add),
                  reads=[t_ps_cs, tg], writes=[tg])
            kb.op("vector", lambda e, G=G: e.tensor_copy(out=G[:, 28:32], in_=ps_m[:, 401:405]),
                  reads=[t_ps_cs], writes=[tg])
            kb.op("scalar", lambda e, G=G: e.activation(out=G[:, 32:48], in_=G[:, 16:32], func=AF.Exp),
                  reads=[tg], writes=[tg])
            kb.op("vector", lambda e, G=G: e.tensor_scalar_mul(out=G[:, 12:16], in0=G[:, 32:36],
                                                               scalar1=128.0 ** -0.5),
                  reads=[tg], writes=[tg])
            for h in range(4):
                q = hi % 2
                hi += 1
                qT = qkT[:, h, tsl]
                kT = qkT[:, 4 + h, tsl]
                _mm(kb, ps_m[:, 0:128], kT, qT, True, True, [t_qk[h], t_qk[4 + h]], [t_ps_s0])
                kb.op("vector", lambda e, q=q, G=G, h=h: e.scalar_tensor_tensor(
                    out=s0sb[q][:], in0=ps_m[:, 0:128], scalar=G[:, 36 + h:37 + h], in1=consts["tri_f"][:],
                    op0=ALU.mult, op1=ALU.mult), reads=[t_ps_s0, tg, ct], writes=[t_s0[q]])
                _mm(kb, ps_m[:, 128:385], s0sb[q][:], Vp[vs][:, h, :], True, False, [t_s0[q], t_vp[vs]], [t_ps_o])
                _mm(kb, ps_m[:, 128:385], qT, Zb[:, h, :], False, True, [t_qk[h], t_zb[h]], [t_ps_o])
                dd = dsm[q]
                td = t_d[q]
                kb.op("vector", lambda e, dd=dd, G=G, h=h: e.tensor_tensor(
                    out=dd[:, 0:1], in0=ps_m[:, 384:385], in1=G[:, 12 + h:13 + h], op=ALU.mult),
                    reads=[t_ps_o, tg], writes=[td])
                kb.op("vector", lambda e, dd=dd: e.tensor_scalar(out=dd[:, 1:2], in0=dd[:, 0:1], scalar1=-1.0,
                                                                 scalar2=1.0, op0=ALU.mult, op1=ALU.max),
                      reads=[td], writes=[td])
                kb.op("vector", lambda e, dd=dd: e.tensor_tensor(out=dd[:, 2:3], in0=dd[:, 1:2], in1=dd[:, 0:1],
                                                                 op=ALU.max), reads=[td], writes=[td])
                kb.op("vector", lambda e, dd=dd: e.reciprocal(out=dd[:, 3:4], in_=dd[:, 2:3]),
                      reads=[td], writes=[td])
                kb.op("vector", lambda e, dd=dd, G=G, h=h: e.tensor_tensor(
                    out=dd[:, 4:5], in0=dd[:, 3:4], in1=G[:, 12 + h:13 + h], op=ALU.mult),
                    reads=[td, tg], writes=[td])
                kb.op("vector", lambda e, q=q, dd=dd, vs=vs, h=h: e.scalar_tensor_tensor(
                    out=ytok[q][:], in0=ps_m[:, 128:384], scalar=dd[:, 4:5],
                    in1=sgo[vs][:, h * 256:(h + 1) * 256], op0=ALU.mult, op1=ALU.mult),
                    reads=[t_ps_o, td, t_sgo[vs]], writes=[t_yt[q]])
                for k in range(2):
                    kb.op("tensor", lambda e, q=q, k=k: e.transpose(out=ps_t[:, k, :],
                                                                    in_=ytok[q][:, k * 128:(k + 1) * 128],
                                                                    identity=consts["id_b"][:]),
                          reads=[t_yt[q], ct], writes=[t_ps_yt], sig=(k == 1))
                kb.op("scalar", lambda e, s=s, h=h, tsl=tsl: e.copy(out=yst[s][:, 2 * h:2 * h + 2, tsl],
                                                                    in_=ps_t[:, 0:2, :]),
                      reads=[t_ps_yt], writes=[t_yst[s]])
                kb.op("tensor", lambda e, kT=kT: e.transpose(out=ps_t[:, 2, :], in_=kT, identity=consts["id_b"][:]),
                      reads=[t_qk[4 + h], ct], writes=[t_ps_kt])
                kb.op("scalar", lambda e, q=q, G=G, h=h: e.activation(out=khat[q][:], in_=ps_t[:, 2, :],
                                                                      func=AF.Copy, scale=G[:, 40 + h:41 + h]),
                      reads=[t_ps_kt, tg], writes=[t_kh[q]])
                _mm(kb, ps_u[:], khat[q][:], Vp[vs][:, h, :], True, True, [t_kh[q], t_vp[vs]], [t_ps_u])
                kb.op("vector", lambda e, G=G, h=h: e.scalar_tensor_tensor(
                    out=Zf[:, h, :], in0=Zf[:, h, :], scalar=G[:, 44 + h:45 + h], in1=ps_u[:],
                    op0=ALU.mult, op1=ALU.add), reads=[t_zf[h], tg, t_ps_u], writes=[t_zf[h]])
                kb.op("gpsimd", lambda e, h=h: e.tensor_copy(out=Zb[:, h, :], in_=Zf[:, h, :]),
                      reads=[t_zf[h]], writes=[t_zb[h]])
        kb.dma("gpsimd", ymT[:, :, tt * 512:(tt + 1) * 512].rearrange("c p t -> p c t"), yst[s][:],
               reads=[t_yst[s]], writes=[t_ymT])
    kb.release(mk)


def emit_mla_proj_pass(kb, consts, S, hT, t_hT, w, scr):
    NT = S // 512
    mk = kb.mark()
    ct = consts["tok"]
    wmla = kb.sbuf("b_wmla", [128, 8, 768], BF16)
    wuqn = kb.sbuf("b_wuqn", [128, 3, 1024], BF16)
    wuqr = kb.sbuf("b_wuqr", [128, 3, 512], BF16)
    wuqs = kb.sbuf("b_wuqs", [128, 3, 512], BF16)
    wuk = kb.sbuf("b_wuk", [128, 2, 1024], BF16)
    wuv = kb.sbuf("b_wuv", [128, 2, 1024], BF16)
    gq = kb.sbuf("b_gq", [128, 5], F32)
    t_w = Tok("b_w")
    load_w_bf16(kb, wmla, w["wmla"], t_w, 8)
    load_w_bf16(kb, wuqn, w["wuqn"], t_w, 3)
    load_w_bf16(kb, wuqr, w["wuqr"], t_w, 3)
    load_w_bf16(kb, wuqs, w["wuqs"], t_w, 3)
    load_w_bf16(kb, wuk, w["wuk"], t_w, 2)
    load_w_bf16(kb, wuv, w["wuv"], t_w, 2)
    kb.dma("sync", gq[:], w["gqkv"], writes=[t_w])
    epsq = consts["eps"]

    hTs = [kb.sbuf("b_hT%d" % i, [128, 8, 512], BF16) for i in range(2)]
    t_hTs = [Tok("b_hT%d" % i) for i in range(2)]
    cs = [kb.sbuf("b_cs%d" % i, [64, 2, 512], F32) for i in range(2)]
    t_cs = [Tok("b_cs%d" % i) for i in range(2)]
    raw = kb.sbuf("b_raw", [128, 5, 512], F32)
    t_raw = [Tok("b_raw%d" % g) for g in range(5)]
    sq = kb.sbuf("b_sq", [128, 5, 512], BF16)
    t_sq = [Tok("b_sq%d" % g) for g in range(5)]
    rs = kb.sbuf("b_rs", [128, 2, 512], F32)
    t_rs = [Tok("b_rs%d" % g) for g in range(2)]
    cn = kb.sbuf("b_cn", [128, 5, 512], BF16)
    t_cn = [Tok("b_cn%d" % g) for g in range(5)]
    rt = [kb.sbuf("b_rt%d" % i, [64, 2, 512], F32) for i in range(2)]
    t_rt = [Tok("b_rt%d" % i) for i in range(2)]
    krs = kb.sbuf("b_krs", [64, 512], BF16)
    t_krs = Tok("b_krs")
    qn_st = kb.sbuf("b_qn", [128, 8, 512], BF16)
    qr_st = kb.sbuf("b_qr", [64, 8, 512], BF16)
    kn_st = kb.sbuf("b_kn", [128, 8, 512], BF16)
    t_qn, t_qr, t_kn = Tok("b_qn"), Tok("b_qr"), Tok("b_kn")
    v_st = [kb.sbuf("b_v%d" % i, [128, 1024], BF16) for i in range(2)]
    t_v = [Tok("b_v%d" % i) for i in range(2)]

    ps_fm = [kb.psum("b_psfm%d" % i, [128, 512], F32) for i in range(2)]
    t_psfm = [Tok("b_psfm%d" % i) for i in range(2)]
    ps_ss = [kb.psum("b_psss%d" % i, [128, 512], F32) for i in range(2)]
    t_psss = [Tok("b_psss%d" % i) for i in range(2)]
    ps_r = [kb.psum("b_psr%d" % i, [128, 512], F32) for i in range(2)]
    t_psr = [Tok("b_psr%d" % i) for i in range(2)]
    ps_tm = [kb.psum("b_pstm%d" % i, [128, 512], F32) for i in range(2)]
    t_pstm = [Tok("b_pstm%d" % i) for i in range(2)]
    d_QTn, d_QTr, d_KTn, d_KrT, d_Vd = scr["QTn"], scr["QTr"], scr["KTn"], scr["KrT"], scr["Vd"]
    t_scr = scr["tok"]

    fmi = 0
    ri = 0
    vi = 0

    def rope(psa, psb, t_pa, t_pb, cst, t_cst, out_ap, t_out):
        nonlocal ri
        k = ri % 2
        ri += 1
        kb.op("vector", lambda e: e.tensor_tensor(out=rt[k][:, 0, :], in0=psa, in1=cst[:, 0, :], op=ALU.mult),
              reads=[t_pa, t_cst], writes=[t_rt[k]])
        kb.op("vector", lambda e: e.tensor_tensor(out=rt[k][:, 1, :], in0=psb, in1=cst[:, 1, :], op=ALU.mult),
              reads=[t_pb, t_cst], writes=[t_rt[k]])
        kb.op("gpsimd", lambda e: e.tensor_tensor(out=out_ap, in0=rt[k][:, 0, :], in1=rt[k][:, 1, :], op=ALU.add),
              reads=[t_rt[k]], writes=[t_out])

    for tt in range(NT):
        s = tt % 2
        tok_sl = slice(tt * 512, (tt + 1) * 512)
        kb.dma("sync", hTs[s][:], hT[:, :, tok_sl].rearrange("c p t -> p c t"), reads=[t_hT], writes=[t_hTs[s]])
        kb.dma("sync", cs[s][:], w["cs"][:, :, tok_sl], writes=[t_cs[s]])
        for g in range(5):
            p = fmi % 2
            fmi += 1
            for c in range(8):
                _mm(kb, ps_fm[p][:], wmla[:, c, g * 128:(g + 1) * 128], hTs[s][:, c, :], c == 0, c == 7,
                    [t_w, t_hTs[s]], [t_psfm[p]])
            kb.op("scalar", lambda e, p=p, g=g: e.copy(out=raw[:, g, :], in_=ps_fm[p][:]),
                  reads=[t_psfm[p]], writes=[t_raw[g]])
            kb.op("gpsimd", lambda e, g=g: e.tensor_tensor(out=sq[:, g, :], in0=raw[:, g, :], in1=raw[:, g, :],
                                                           op=ALU.mult), reads=[t_raw[g]], writes=[t_sq[g]])
        for n, (g0, g1, dim) in enumerate(((0, 3, 384.0), (3, 5, 256.0))):
            for g in range(g0, g1):
                _mm(kb, ps_ss[n][:], consts["ones_b"][:], sq[:, g, :], g == g0, g == g1 - 1,
                    [ct, t_sq[g]], [t_psss[n]])
            kb.op("scalar", lambda e, n=n, dim=dim: e.activation(out=rs[:, n, :], in_=ps_ss[n][:], func=AF.Sqrt,
                                                                 bias=epsq[:, 0:1], scale=1.0 / dim),
                  reads=[t_psss[n], ct], writes=[t_rs[n]])
            kb.op("vector", lambda e, n=n: e.reciprocal(out=rs[:, n, :], in_=rs[:, n, :]),
                  reads=[t_rs[n]], writes=[t_rs[n]])
            for g in range(g0, g1):
                kb.op("vector", lambda e, n=n, g=g: e.scalar_tensor_tensor(
                    out=cn[:, g, :], in0=raw[:, g, :], scalar=gq[:, g:g + 1], in1=rs[:, n, :],
                    op0=ALU.mult, op1=ALU.mult), reads=[t_raw[g], t_w, t_rs[n]], writes=[t_cn[g]])
        for k2 in range(2):
            for c in range(8):
                _mm(kb, ps_r[k2][0:64, :], wmla[:, c, 640 + 64 * k2:704 + 64 * k2], hTs[s][:, c, :], c == 0, c == 7,
                    [t_w, t_hTs[s]], [t_psr[k2]])
        rope(ps_r[0][0:64, :], ps_r[1][0:64, :], t_psr[0], t_psr[1], cs[s], t_cs[s], krs[:], t_krs)
        kb.dma("gpsimd", d_KrT[:, tok_sl], krs[:], reads=[t_krs], writes=[t_scr])
        for h in range(8):
            p = fmi % 2
            fmi += 1
            for c in range(3):
                _mm(kb, ps_fm[p][:], wuqn[:, c, h * 128:(h + 1) * 128], cn[:, c, :], c == 0, c == 2,
                    [t_w, t_cn[c]], [t_psfm[p]])
            kb.op("scalar", lambda e, p=p, h=h: e.copy(out=qn_st[:, h, :], in_=ps_fm[p][:]),
                  reads=[t_psfm[p]], writes=[t_qn])
            for k2, wsrc in enumerate((wuqr, wuqs)):
                for c in range(3):
                    _mm(kb, ps_r[k2][0:64, :], wsrc[:, c, h * 64:(h + 1) * 64], cn[:, c, :], c == 0, c == 2,
                        [t_w, t_cn[c]], [t_psr[k2]])
            rope(ps_r[0][0:64, :], ps_r[1][0:64, :], t_psr[0], t_psr[1], cs[s], t_cs[s], qr_st[:, h, :], t_qr)
            p = fmi % 2
            fmi += 1
            for c in range(2):
                _mm(kb, ps_fm[p][:], wuk[:, c, h * 128:(h + 1) * 128], cn[:, 3 + c, :], c == 0, c == 1,
                    [t_w, t_cn[3 + c]], [t_psfm[p]])
            kb.op("scalar", lambda e, p=p, h=h: e.copy(out=kn_st[:, h, :], in_=ps_fm[p][:]),
                  reads=[t_psfm[p]], writes=[t_kn])
        kb.dma("gpsimd", d_QTn[:, :, tok_sl].rearrange("h p t -> p h t"), qn_st[:], reads=[t_qn], writes=[t_scr])
        kb.dma("gpsimd", d_QTr[:, :, tok_sl].rearrange("h p t -> p h t"), qr_st[:], reads=[t_qr], writes=[t_scr])
        kb.dma("gpsimd", d_KTn[:, :, tok_sl].rearrange("h p t -> p h t"), kn_st[:], reads=[t_kn], writes=[t_scr])
        for j in range(4):
            q = vi % 2
            vi += 1
            for half in range(2):
                p = half
                for c in range(2):
                    _mm(kb, ps_tm[p][:], cn[:, 3 + c, j * 128:(j + 1) * 128], wuv[:, c, half * 512:(half + 1) * 512],
                        c == 0, c == 1, [t_w, t_cn[3 + c]], [t_pstm[p]])
                kb.op("scalar", lambda e, p=p, q=q, half=half: e.copy(out=v_st[q][:, half * 512:(half + 1) * 512],
                                                                      in_=ps_tm[p][:]),
                      reads=[t_pstm[p]], writes=[t_v[q]])
            kb.dma("gpsimd", d_Vd[:, :, tt * 4 + j, :].rearrange("h p d -> p h d"),
                   v_st[q][:].rearrange("p (h d) -> p h d", h=8), reads=[t_v[q]], writes=[t_scr])
    kb.release(mk)


def emit_attn_pass(kb, consts, S, scr, yaT, t_yaT):
    NQ = S // 512
    NK = S // 128
    mk = kb.mark()
    ct = consts["tok"]
    scale = 192.0 ** -0.5
    t_scr = scr["tok"]
    masks = kb.sbuf("c_mask", [128, 4, 512], BF16)
    t_mask = Tok("c_mask")
    for r in range(4):
        for kbk in range(2):
            kb.op("gpsimd", lambda e, r=r, kbk=kbk: e.affine_select(
                out=masks[64 * kbk:64 * kbk + 64, r, :].rearrange("p (a b) -> p a b", a=8),
                in_=consts["ones_b512"][64 * kbk:64 * kbk + 64, :].rearrange("p (a b) -> p a b", a=8),
                pattern=[[1, 8], [0, 64]], compare_op=ALU.is_ge, fill=0.0, base=-(2 * r + kbk),
                channel_multiplier=0), reads=[ct], writes=[t_mask])
    krt = kb.sbuf("c_krt", [64, S], BF16)
    t_krt = Tok("c_krt")
    kb.dma("sync", krt[:], scr["KrT"][:, :], reads=[t_scr], writes=[t_krt])
    ktn = [kb.sbuf("c_ktn%d" % i, [128, S], BF16) for i in range(2)]
    t_ktn = [Tok("c_ktn%d" % i) for i in range(2)]
    vsb = [kb.sbuf("c_v%d" % i, [128, NK, 128], BF16) for i in range(2)]
    t_vsb = [Tok("c_v%d" % i) for i in range(2)]
    qn = [kb.sbuf("c_qn%d" % i, [128, 512], BF16) for i in range(2)]
    qr = [kb.sbuf("c_qr%d" % i, [64, 512], BF16) for i in range(2)]
    t_q = [Tok("c_q%d" % i) for i in range(2)]
    NPT = 4
    NPS = 3
    pT = [kb.sbuf("c_pT%d" % i, [128, 512], BF16) for i in range(NPT)]
    t_pT = [Tok("c_pT%d" % i) for i in range(NPT)]
    rden = [kb.sbuf("c_rd%d" % i, [128, 512], F32) for i in range(2)]
    t_rden = [Tok("c_rd%d" % i) for i in range(2)]
    yst = [kb.sbuf("c_y%d" % i, [128, 512], BF16) for i in range(2)]
    t_yst = [Tok("c_y%d" % i) for i in range(2)]
    ps_s = [kb.psum("c_pss%d" % i, [128, 512], F32) for i in range(NPS)]
    t_pss = [Tok("c_pss%d" % i) for i in range(NPS)]
    ps_a = [kb.psum("c_psa%d" % i, [128, 512], F32) for i in range(2)]
    t_psa = [Tok("c_psa%d" % i) for i in range(2)]
    ps_d = [kb.psum("c_psd%d" % i, [128, 512], F32) for i in range(2)]
    t_psd = [Tok("c_psd%d" % i) for i in range(2)]
    DEPTH_PIPE = 2
    gi = 0
    qi = 0
    for h in range(8):
        hs = h % 2
        kb.dma("sync", ktn[hs][:], scr["KTn"][h, :, :], reads=[t_scr], writes=[t_ktn[hs]])
        kb.dma("sync", vsb[hs][:], scr["Vd"][h, :, :, :], reads=[t_scr], writes=[t_vsb[hs]])
        for j in range(NQ):
            a = qi % 2
            qi += 1
            qsl = slice(j * 512, (j + 1) * 512)
            kb.dma("sync", qn[a][:], scr["QTn"][h, :, qsl], reads=[t_scr], writes=[t_q[a]])
            kb.dma("sync", qr[a][:], scr["QTr"][h, :, qsl], reads=[t_scr], writes=[t_q[a]])
            nkt = 4 * (j + 1)
            base = gi
            gi += nkt

            def qk(kt):
                p = (base + kt) % NPS
                ksl = slice(kt * 128, (kt + 1) * 128)
                _mm(kb, ps_s[p][:], ktn[hs][:, ksl], qn[a][:], True, False, [t_ktn[hs], t_q[a]], [t_pss[p]])
                _mm(kb, ps_s[p][:], krt[:, ksl], qr[a][:], False, True, [t_krt, t_q[a]], [t_pss[p]])

            def soft(kt):
                p = (base + kt) % NPS
                u = (base + kt) % NPT
                kb.op("scalar", lambda e: e.activation(out=pT[u][:], in_=ps_s[p][:], func=AF.Exp, scale=scale),
                      reads=[t_pss[p]], writes=[t_pT[u]])
                if kt >= 4 * j:
                    r = kt - 4 * j
                    kb.op("vector", lambda e: e.tensor_tensor(out=pT[u][:], in0=pT[u][:], in1=masks[:, r, :],
                                                              op=ALU.mult),
                          reads=[t_pT[u], t_mask], writes=[t_pT[u]])

            def pv(kt):
                u = (base + kt) % NPT
                _mm(kb, ps_a[a][:], vsb[hs][:, kt, :], pT[u][:], kt == 0, kt == nkt - 1,
                    [t_vsb[hs], t_pT[u]], [t_psa[a]])
                _mm(kb, ps_d[a][:], consts["ones_b"][:], pT[u][:], kt == 0, kt == nkt - 1,
                    [ct, t_pT[u]], [t_psd[a]])

            for kt in range(min(DEPTH_PIPE, nkt)):
                qk(kt)
            for kt in range(nkt):
                soft(kt)
                if kt + DEPTH_PIPE < nkt:
                    qk(kt + DEPTH_PIPE)
                pv(kt)
            kb.op("vector", lambda e, a=a: e.reciprocal(out=rden[a][:], in_=ps_d[a][:]),
                  reads=[t_psd[a]], writes=[t_rden[a]])
            kb.op("vector", lambda e, a=a: e.tensor_tensor(out=yst[a][:], in0=ps_a[a][:], in1=rden[a][:],
                                                           op=ALU.mult),
                  reads=[t_psa[a], t_rden[a]], writes=[t_yst[a]])
            kb.dma("gpsimd", yaT[h, :, qsl], yst[a][:], reads=[t_yst[a]], writes=[t_yaT])
    kb.release(mk)


def alloc_mla_scratch(kb, S):
    return dict(QTn=kb.dram_tmp("s_QTn", [8, 128, S], BF16), QTr=kb.dram_tmp("s_QTr", [8, 64, S], BF16),
                KTn=kb.dram_tmp("s_KTn", [8, 128, S], BF16), KrT=kb.dram_tmp("s_KrT", [64, S], BF16),
                Vd=kb.dram_tmp("s_Vd", [8, 128, S // 128, 128], BF16), tok=Tok("dram:mla_scr"))


def emit_merge_pass(kb, consts, S, hT, ymT, yaT, t_in, x, t_x, w, gate1, t_gate1, xmid, t_xmid):
    NT = S // 512
    mk = kb.mark()
    wg = kb.sbuf("d_wg", [128, 8, 2048], BF16)
    wbm = kb.sbuf("d_wbm", [128, 8, 1024], BF16)
    wba = kb.sbuf("d_wba", [128, 8, 1024], BF16)
    wo = kb.sbuf("d_wo", [128, 8, 1024], BF16)
    t_w = Tok("d_w")
    load_w_bf16(kb, wg, w["wgate"], t_w, 8)
    load_w_bf16(kb, wbm, w["wbm"], t_w, 8)
    load_w_bf16(kb, wba, w["wba"], t_w, 8)
    load_w_bf16(kb, wo, w["wout"], t_w, 8)
    hTs = [kb.sbuf("d_hT%d" % i, [128, 8, 512], BF16) for i in range(2)]
    ymS = [kb.sbuf("d_ym%d" % i, [128, 8, 512], BF16) for i in range(2)]
    yaS = [kb.sbuf("d_ya%d" % i, [128, 8, 512], BF16) for i in range(2)]
    t_ld = [Tok("d_ld%d" % i) for i in range(2)]
    sg = [kb.sbuf("d_sg%d" % i, [128, 2, 512], F32) for i in range(2)]
    t_sg = [Tok("d_sg%d" % i) for i in range(2)]
    mm_ = [kb.sbuf("d_mm%d" % i, [128, 2, 512], F32) for i in range(2)]
    t_mm = [Tok("d_mm%d" % i) for i in range(2)]
    mg = [kb.sbuf("d_mg%d" % i, [128, 8, 512], BF16) for i in range(2)]
    t_mg = [Tok("d_mg%d" % i) for i in range(2)]
    xt = [kb.sbuf("d_x%d" % i, [128, 1024], F32) for i in range(2)]
    t_xt = [Tok("d_x%d" % i) for i in range(2)]
    tmp = [kb.sbuf("d_tmp%d" % i, [128, 512], F32) for i in range(2)]
    t_tmp = [Tok("d_tmp%d" % i) for i in range(2)]
    ps4 = [kb.psum("d_ps%d" % i, [128, 512], F32) for i in range(4)]
    t_ps4 = [Tok("d_ps%d" % i) for i in range(4)]
    pso = [kb.psum("d_pso%d" % i, [128, 512], F32) for i in range(2)]
    t_pso = [Tok("d_pso%d" % i) for i in range(2)]
    oi = 0
    xi = 0
    for tt in range(NT):
        s = tt % 2
        tsl = slice(tt * 512, (tt + 1) * 512)
        kb.dma("sync", hTs[s][:], hT[:, :, tsl].rearrange("c p t -> p c t"), reads=[t_in], writes=[t_ld[s]])
        kb.dma("sync", ymS[s][:], ymT[:, :, tsl].rearrange("c p t -> p c t"), reads=[t_in], writes=[t_ld[s]])
        kb.dma("sync", yaS[s][:], yaT[:, :, tsl].rearrange("c p t -> p c t"), reads=[t_in], writes=[t_ld[s]])
        for oc in range(8):
            a = oc % 2
            osl = slice(oc * 128, (oc + 1) * 128)
            for c in range(8):
                _mm(kb, ps4[0][:], wbm[:, c, osl], ymS[s][:, c, :], c == 0, c == 7, [t_w, t_ld[s]], [t_ps4[0]])
            for c in range(8):
                _mm(kb, ps4[1][:], wba[:, c, osl], yaS[s][:, c, :], c == 0, c == 7, [t_w, t_ld[s]], [t_ps4[1]])
            for k in range(2):
                for c in range(8):
                    _mm(kb, ps4[2 + k][:], wg[:, c, k * 1024 + oc * 128:k * 1024 + (oc + 1) * 128], hTs[s][:, c, :],
                        c == 0, c == 7, [t_w, t_ld[s]], [t_ps4[2 + k]])
                kb.op("scalar", lambda e, a=a, k=k: e.activation(out=sg[a][:, k, :], in_=ps4[2 + k][:],
                                                                 func=AF.Sigmoid),
                      reads=[t_ps4[2 + k]], writes=[t_sg[a]])
            for k in range(2):
                kb.op("vector", lambda e, a=a, k=k: e.tensor_tensor(out=mm_[a][:, k, :], in0=ps4[k][:],
                                                                    in1=sg[a][:, k, :], op=ALU.mult),
                      reads=[t_ps4[k], t_sg[a]], writes=[t_mm[a]])
            kb.op("gpsimd", lambda e, a=a, s=s, oc=oc: e.tensor_tensor(out=mg[s][:, oc, :], in0=mm_[a][:, 0, :],
                                                                       in1=mm_[a][:, 1, :], op=ALU.add),
                  reads=[t_mm[a]], writes=[t_mg[s]])
        for j in range(4):
            xs = xi % 2
            xi += 1
            t0 = tt * 512 + j * 128
            kb.dma("sync", xt[xs][:], x[t0:t0 + 128, :], reads=[t_x], writes=[t_xt[xs]])
            for half in range(2):
                p = oi % 2
                oi += 1
                hsl = slice(half * 512, (half + 1) * 512)
                for c in range(8):
                    _mm(kb, pso[p][:], mg[s][:, c, j * 128:(j + 1) * 128], wo[:, c, hsl], c == 0, c == 7,
                        [t_w, t_mg[s]], [t_pso[p]])
                kb.op("vector", lambda e, p=p, hsl=hsl: e.tensor_tensor(out=tmp[p][:], in0=pso[p][:],
                                                                        in1=gate1[:, hsl], op=ALU.mult),
                      reads=[t_pso[p], t_gate1], writes=[t_tmp[p]])
                kb.op("gpsimd", lambda e, p=p, xs=xs, hsl=hsl: e.tensor_tensor(out=xt[xs][:, hsl], in0=xt[xs][:, hsl],
                                                                               in1=tmp[p][:], op=ALU.add),
                      reads=[t_tmp[p], t_xt[xs]], writes=[t_xt[xs]])
            kb.dma("gpsimd", xmid[t0:t0 + 128, :], xt[xs][:], reads=[t_xt[xs]], writes=[t_xmid])
    kb.release(mk)


def emit_moe_pass(kb, consts, S, xmid, t_xmid, w, gmul2, t_gmul2, shift2, t_shift2, gate2, t_gate2,
                  xout, t_xout, final_g=None):
    TS = min(2048, S)
    NST = S // TS
    NTL = TS // 128
    mk = kb.mark()
    ct = consts["tok"]
    h2T = kb.sbuf("e_h2T", [128, 8, TS], BF16)
    t_h2T = Tok("e_h2T")
    yacc = kb.sbuf("e_yacc", [128, NTL, 1024], F32)
    t_yacc = [Tok("e_yacc%d" % i) for i in range(NTL)]
    Wt = kb.sbuf("e_Wt", [128, NTL, 32], F32)
    t_Wt = [Tok("e_Wt%d" % i) for i in range(NTL)]
    wr = kb.sbuf("e_wr", [128, 8, 36], F32)
    rb = kb.sbuf("e_rb", [128, 36], F32)
    t_wr = Tok("e_wr")
    kb.dma("sync", wr[:], w["wr"].rearrange("(c p) n -> p c n", p=128), writes=[t_wr])
    kb.dma("sync", rb[:], w["rb"].partition_broadcast(128), writes=[t_wr])
    fg = None
    if final_g is not None:
        fg = kb.sbuf("e_fg", [128, 1024], F32)
        kb.dma("sync", fg[:], final_g.partition_broadcast(128), writes=[t_wr])
    weg = [kb.sbuf("e_weg%d" % i, [128, 8, 256], BF16) for i in range(2)]
    weu = [kb.sbuf("e_weu%d" % i, [128, 8, 256], BF16) for i in range(2)]
    wed = [kb.sbuf("e_wed%d" % i, [128, 2, 1024], BF16) for i in range(2)]
    t_we = [Tok("e_we%d" % i) for i in range(2)]
    xt = [kb.sbuf("e_x%d" % i, [128, 1024], F32) for i in range(2)]
    t_xt = [Tok("e_x%d" % i) for i in range(2)]
    scr = [kb.sbuf("e_scr%d" % i, [128, 1024], F32) for i in range(2)]
    t_scr = [Tok("e_scr%d" % i) for i in range(2)]
    h2f = [kb.sbuf("e_h2f%d" % i, [128, 1024], F32) for i in range(2)]
    t_h2f = [Tok("e_h2f%d" % i) for i in range(2)]
    stat = [kb.sbuf("e_st%d" % i, [128, 4], F32) for i in range(2)]
    t_st = [Tok("e_st%d" % i) for i in range(2)]
    h2Tf = kb.sbuf("e_h2Tf", [128, 8, 128], F32)
    t_h2Tf = Tok("e_h2Tf")
    R = [kb.sbuf("e_R%d" % i, [128, 160], F32) for i in range(2)]
    t_R = [Tok("e_R%d" % i) for i in range(2)]
    sgl = [kb.sbuf("e_sg%d" % i, [128, 512], F32) for i in range(2)]
    t_sgl = [Tok("e_sg%d" % i) for i in range(2)]
    aT = [kb.sbuf("e_aT%d" % i, [128, 2, 512], BF16) for i in range(2)]
    t_aT = [Tok("e_aT%d" % i) for i in range(2)]
    bank = [kb.psum("e_bank%d" % i, [128, 512], F32) for i in range(8)]
    t_bank = [Tok("e_bank%d" % i) for i in range(8)]
    wload = 0

    def load_expert(e):
        nonlocal wload
        k = wload % 2
        wload += 1
        for c in range(8):
            kb.dma("gpsimd", weg[k][:, c, :], w["weg"][e, c * 128:(c + 1) * 128, :], writes=[t_we[k]])
        for c in range(8):
            kb.dma("gpsimd", weu[k][:, c, :], w["weu"][e, c * 128:(c + 1) * 128, :], writes=[t_we[k]])
        for c in range(2):
            kb.dma("gpsimd", wed[k][:, c, :], w["wed"][e, c * 128:(c + 1) * 128, :], writes=[t_we[k]])
        return k

    for st in range(NST):
        for i in range(NTL):
            s = i % 2
            t0 = st * TS + i * 128
            kb.dma("sync", xt[s][:], xmid[t0:t0 + 128, :], reads=[t_xmid], writes=[t_xt[s]])
            emit_norm_mod(kb, xt[s], t_xt[s], gmul2, t_gmul2, shift2, t_shift2, h2f[s], t_h2f[s],
                          scr[s], t_scr[s], stat[s], t_st[s], consts["eps"][:, 0:1])
            for half in range(2):
                for c4 in range(4):
                    c = half * 4 + c4
                    kb.op("tensor", lambda e, s=s, c=c, half=half, c4=c4: e.transpose(
                        out=bank[half][:, c4 * 128:(c4 + 1) * 128], in_=h2f[s][:, c * 128:(c + 1) * 128],
                        identity=consts["id_f"][:]), reads=[t_h2f[s], ct], writes=[t_bank[half]], sig=(c4 == 3))
                kb.op("scalar", lambda e, half=half: e.copy(
                    out=h2Tf[:, half * 4:half * 4 + 4, :], in_=bank[half][:].rearrange("p (c t) -> p c t", c=4)),
                    reads=[t_bank[half]], writes=[t_h2Tf])
            kb.op("gpsimd", lambda e, i=i: e.tensor_copy(out=h2T[:, :, i * 128:(i + 1) * 128], in_=h2Tf[:]),
                  reads=[t_h2Tf], writes=[t_h2T])
            for c in range(8):
                _mm(kb, bank[2][:, 0:36], h2Tf[:, c, :], wr[:, c, :], c == 0, c == 7, [t_h2Tf, t_wr], [t_bank[2]])
            r = R[s]
            tr = t_R[s]
            V = lambda fn, reads, writes: kb.op("vector", fn, reads=reads, writes=writes)
            V(lambda e: e.tensor_tensor(out=r[:, 0:36], in0=bank[2][:, 0:36], in1=rb[:], op=ALU.add),
              [t_bank[2], t_wr], [tr])
            V(lambda e: e.reduce_max(out=r[:, 36:37], in_=r[:, 0:4], axis=AX.X), [tr], [tr])
            V(lambda e: e.tensor_scalar_mul(out=r[:, 37:38], in0=r[:, 36:37], scalar1=-1.0), [tr], [tr])
            kb.op("scalar", lambda e: e.activation(out=r[:, 40:44], in_=r[:, 0:4], func=AF.Exp, bias=r[:, 37:38]),
                  reads=[tr], writes=[tr])
            V(lambda e: e.reduce_sum(out=r[:, 44:45], in_=r[:, 40:44], axis=AX.X), [tr], [tr])
            V(lambda e: e.reciprocal(out=r[:, 45:46], in_=r[:, 44:45]), [tr], [tr])
            V(lambda e: e.tensor_scalar(out=r[:, 48:52], in0=r[:, 0:4], scalar1=r[:, 36:37], scalar2=None,
                                        op0=ALU.is_equal), [tr], [tr])
            V(lambda e: e.tensor_scalar(out=r[:, 52:56], in0=r[:, 48:52], scalar1=-1.0, scalar2=BIG,
                                        op0=ALU.add, op1=ALU.mult), [tr], [tr])
            V(lambda e: e.tensor_tensor(out=r[:, 64:96].rearrange("p (g k) -> p g k", g=4),
                                        in0=r[:, 4:36].rearrange("p (g k) -> p g k", g=4),
                                        in1=r[:, 52:56].unsqueeze(2).to_broadcast([128, 4, 8]), op=ALU.add),
              [tr], [tr])
            V(lambda e: e.reduce_max(out=r[:, 96:97], in_=r[:, 64:96], axis=AX.X), [tr], [tr])
            V(lambda e: e.tensor_scalar(out=r[:, 104:136], in0=r[:, 64:96], scalar1=r[:, 96:97], scalar2=None,
                                        op0=ALU.is_equal), [tr], [tr])
            V(lambda e: e.scalar_tensor_tensor(out=r[:, 64:96], in0=r[:, 104:136], scalar=-BIG, in1=r[:, 64:96],
                                               op0=ALU.mult, op1=ALU.add), [tr], [tr])
            V(lambda e: e.reduce_max(out=r[:, 97:98], in_=r[:, 64:96], axis=AX.X), [tr], [tr])
            V(lambda e: e.tensor_tensor(out=r[:, 98:99], in0=r[:, 97:98], in1=r[:, 96:97], op=ALU.subtract),
              [tr], [tr])
            kb.op("scalar", lambda e: e.activation(out=r[:, 99:100], in_=r[:, 98:99], func=AF.Exp),
                  reads=[tr], writes=[tr])
            V(lambda e: e.tensor_scalar_add(out=r[:, 100:101], in0=r[:, 99:100], scalar1=1.0), [tr], [tr])
            V(lambda e: e.reciprocal(out=r[:, 101:102], in_=r[:, 100:101]), [tr], [tr])
            V(lambda e: e.tensor_tensor(out=r[:, 102:103], in0=r[:, 101:102], in1=r[:, 45:46], op=ALU.mult),
              [tr], [tr])
            V(lambda e: e.tensor_tensor(out=r[:, 103:104], in0=r[:, 102:103], in1=r[:, 99:100], op=ALU.mult),
              [tr], [tr])
            V(lambda e, i=i: e.tensor_scalar_mul(out=Wt[:, i, :], in0=r[:, 104:136], scalar1=r[:, 102:103]),
              [tr], [t_Wt[i]])
            V(lambda e: e.tensor_scalar(out=r[:, 104:136], in0=r[:, 64:96], scalar1=r[:, 97:98], scalar2=None,
                                        op0=ALU.is_equal), [tr], [tr])
            V(lambda e, i=i: e.scalar_tensor_tensor(out=Wt[:, i, :], in0=r[:, 104:136], scalar=r[:, 103:104],
                                                    in1=Wt[:, i, :], op0=ALU.mult, op1=ALU.add),
              [tr, t_Wt[i]], [t_Wt[i]])
        steps = [(ex, tl) for ex in range(32) for tl in range(TS // 512)]
        wslot = {}
        gcount = [0]
        dcount = [0]

        def gu_group(si, hc, which):
            ex, tl = steps[si]
            k = wslot[ex]
            tsl = slice(tl * 512, (tl + 1) * 512)
            hsl = slice(hc * 128, (hc + 1) * 128)
            pg = hc
            if which == 0:
                for c in range(8):
                    _mm(kb, bank[pg][:], weg[k][:, c, hsl], h2T[:, c, tsl], c == 0, c == 7,
                        [t_we[k], t_h2T], [t_bank[pg]])
                kb.op("scalar", lambda e: e.activation(out=sgl[pg][:], in_=bank[pg][:], func=AF.Silu),
                      reads=[t_bank[pg]], writes=[t_sgl[pg]])
            else:
                a = si % 2
                for c in range(8):
                    _mm(kb, bank[2 + pg][:], weu[k][:, c, hsl], h2T[:, c, tsl], c == 0, c == 7,
                        [t_we[k], t_h2T], [t_bank[2 + pg]])
                kb.op("vector", lambda e: e.tensor_tensor(out=aT[a][:, hc, :], in0=bank[2 + pg][:], in1=sgl[pg][:],
                                                          op=ALU.mult),
                      reads=[t_bank[2 + pg], t_sgl[pg]], writes=[t_aT[a]])

        def d_group(si, j, half):
            ex, tl = steps[si]
            k = wslot[ex]
            a = si % 2
            ti = tl * 4 + j
            pd = 4 + dcount[0] % 4
            dcount[0] += 1
            osl = slice(half * 512, (half + 1) * 512)
            for hc in range(2):
                _mm(kb, bank[pd][:], aT[a][:, hc, j * 128:(j + 1) * 128], wed[k][:, hc, osl],
                    hc == 0, hc == 1, [t_aT[a], t_we[k]], [t_bank[pd]])
            if ex == 0:
                kb.op("vector", lambda e: e.tensor_scalar_mul(out=yacc[:, ti, osl], in0=bank[pd][:],
                                                              scalar1=Wt[:, ti, ex:ex + 1]),
                      reads=[t_bank[pd], t_Wt[ti]], writes=[t_yacc[ti]])
            else:
                kb.op("vector", lambda e: e.scalar_tensor_tensor(out=yacc[:, ti, osl], in0=bank[pd][:],
                                                                 scalar=Wt[:, ti, ex:ex + 1], in1=yacc[:, ti, osl],
                                                                 op0=ALU.mult, op1=ALU.add),
                      reads=[t_bank[pd], t_Wt[ti], t_yacc[ti]], writes=[t_yacc[ti]])

        dlist = [(j, half) for j in range(4) for half in range(2)]
        for si in range(len(steps) + 1):
            if si < len(steps):
                ex, tl = steps[si]
                if tl == 0:
                    wslot[ex] = load_expert(ex)
            gul = [(0, 0), (0, 1), (1, 0), (1, 1)]
            for gidx in range(4):
                if si < len(steps):
                    gu_group(si, gul[gidx][0], gul[gidx][1])
                if si > 0:
                    for (j, half) in dlist[2 * gidx:2 * gidx + 2]:
                        d_group(si - 1, j, half)
        for i in range(NTL):
            s = i % 2
            t0 = st * TS + i * 128
            kb.dma("sync", xt[s][:], xmid[t0:t0 + 128, :], reads=[t_xmid], writes=[t_xt[s]])
            kb.op("vector", lambda e, i=i: e.tensor_tensor(out=yacc[:, i, :], in0=yacc[:, i, :], in1=gate2[:],
                                                           op=ALU.mult),
                  reads=[t_yacc[i], t_gate2], writes=[t_yacc[i]])
            kb.op("gpsimd", lambda e, s=s, i=i: e.tensor_tensor(out=xt[s][:], in0=xt[s][:], in1=yacc[:, i, :],
                                                                op=ALU.add),
                  reads=[t_xt[s], t_yacc[i]], writes=[t_xt[s]])
            if final_g is None:
                kb.dma("gpsimd", xout[t0:t0 + 128, :], xt[s][:], reads=[t_xt[s]], writes=[t_xout])
            else:
                emit_norm_mod(kb, xt[s], t_xt[s], fg, t_wr, None, None, h2f[s], t_h2f[s],
                              scr[s], t_scr[s], stat[s], t_st[s], consts["eps"][:, 0:1])
                kb.dma("gpsimd", xout[t0:t0 + 128, :], h2f[s][:], reads=[t_h2f[s]], writes=[t_xout])
    kb.release(mk)


LAYER_SHAPES = dict(
    modw=[D, 6144], modb=[6144], g1=[D],
    wqk=[D, 1024], wtm=[D, 2056], cw=[128, 8, 4], cb=[128, 8], ifb=[8],
    wmla=[D, 768], gqkv=[128, 5], wuqn=[384, 1024], wuqr=[384, 512], wuqs=[384, 512],
    wuk=[256, 1024], wuv=[256, 1024],
    wgate=[D, 2048], wbm=[D, D], wba=[D, D], wout=[D, D], g2=[D], wr=[D, 36], rb=[36],
    weg=[32, D, 256], weu=[32, D, 256], wed=[32, 256, D])
DEPTH = 2


def build_full(S):
    kb = KB()
    x = kb.dram_in("x", [S, D], F32)
    cvec = kb.dram_in("cvec", [D], F32)
    cs = kb.dram_in("cs", [64, 2, S], F32)
    fg = kb.dram_in("fg", [D], F32)
    W = []
    for l in range(DEPTH):
        W.append({k: kb.dram_in("%s_%d" % (k, l), v, F32) for k, v in LAYER_SHAPES.items()})
        W[l]["cs"] = cs
    out = kb.dram_out("out", [S, D], F32)
    hT = kb.dram_tmp("s_hT", [8, 128, S], BF16)
    ymT = kb.dram_tmp("s_ymT", [8, 128, S], BF16)
    yaT = kb.dram_tmp("s_yaT", [8, 128, S], BF16)
    xmid = kb.dram_tmp("s_xmid", [S, D], F32)
    xnext = kb.dram_tmp("s_xnext", [S, D], F32)
    scr = alloc_mla_scratch(kb, S)
    t_hT, t_ymT, t_yaT = Tok("dram:hT"), Tok("dram:ymT"), Tok("dram:yaT")
    t_xmid, t_xnext, t_out, t_x = Tok("dram:xmid"), Tok("dram:xnext"), Tok("dram:out"), Tok("dram:x")
    consts = load_consts(kb)
    x_cur, t_xcur = x, t_x
    for l in range(DEPTH):
        w = W[l]
        mk = kb.mark()
        emit_k1_body(kb, consts, x_cur, cvec, w["modw"][:, 0:2048], w["modb"][0:2048], w["g1"], hT, S,
                     hT_tok=t_hT, x_tok=t_xcur)
        kb.release(mk)
        emit_mlstm_pass(kb, consts, S, hT, t_hT, w, ymT, t_ymT)
        emit_mla_proj_pass(kb, consts, S, hT, t_hT, w, scr)
        emit_attn_pass(kb, consts, S, scr, yaT, t_yaT)
        mk = kb.mark()
        mods = [kb.sbuf("mod%d" % i, [128, D], F32) for i in range(4)]
        tm = [Tok("mod%d" % i) for i in range(4)]
        g2t = kb.sbuf("g2t", [128, D], F32)
        tg2 = Tok("g2t")
        emit_mod(kb, consts, w["modw"][:, 2048:6144], w["modb"][2048:6144], cvec, 4, list(zip(mods, tm)))
        kb.dma("sync", g2t[:], w["g2"].partition_broadcast(128), writes=[tg2])
        kb.op("vector", lambda e: e.scalar_tensor_tensor(out=mods[2][:], in0=mods[2][:], scalar=1.0, in1=g2t[:],
                                                         op0=ALU.add, op1=ALU.mult),
              reads=[tm[2], tg2], writes=[tm[2]])
        t_in_all = _MultiTok([t_hT, t_ymT, t_yaT])
        emit_merge_pass(kb, consts, S, hT, ymT, yaT, t_in_all, x_cur, t_xcur, w, mods[0], tm[0], xmid, t_xmid)
        last = (l == DEPTH - 1)
        emit_moe_pass(kb, consts, S, xmid, t_xmid, w, mods[2], tm[2], mods[1], tm[1], mods[3], tm[3],
                      out if last else xnext, t_out if last else t_xnext, final_g=fg if last else None)
        kb.release(mk)
        x_cur, t_xcur = xnext, t_xnext
    kb.finish([t_out])
    return kb


class _MultiTok:
    def __init__(self, toks):
        self.name = "dram:multi"
        self._toks = toks


def _rope_tables(S):
    pos = np.arange(S, dtype=np.float32)
    inv_freq = (1.0 / (np.float32(10000.0) ** (np.arange(0, 64, 2, dtype=np.float32) / np.float32(64)))).astype(np.float32)
    ang = pos[:, None] * inv_freq[None, :]
    cos = np.cos(ang).astype(np.float32)
    sin = np.sin(ang).astype(np.float32)
    CC = np.concatenate([cos, cos], 1).T
    SS = np.concatenate([-sin, sin], 1).T
    return np.ascontiguousarray(np.stack([CC, SS], 1))


def _prep_layer(z, l):
    c = np.ascontiguousarray
    win = z["w_in"][l]
    d = {}
    d["modw"] = c(z["mod_w"][l])
    d["modb"] = c(z["mod_b"][l])
    d["g1"] = c(z["norm1_g"][l])
    d["wqk"] = c(win[:, :1024])
    d["wtm"] = c(win[:, 1024:3080])
    d["cw"] = c(z["conv_w"][l].reshape(4, 8, 128).transpose(2, 1, 0))
    d["cb"] = c(z["conv_b"][l].reshape(8, 128).T)
    d["ifb"] = c(np.concatenate([z["igate_b"][l], z["fgate_b"][l]]))
    kr = win[:, 3720:3784]
    d["wmla"] = c(np.concatenate([win[:, 3080:3784], kr[:, 32:], kr[:, :32]], 1))
    d["gqkv"] = c(np.concatenate([z["q_norm_g"][l].reshape(3, 128).T, z["kv_norm_g"][l].reshape(2, 128).T], 1))
    uq = z["w_uq"][l].reshape(384, 8, 192)
    d["wuqn"] = c(uq[:, :, :128].reshape(384, 1024))
    d["wuqr"] = c(uq[:, :, 128:].reshape(384, 512))
    d["wuqs"] = c(np.concatenate([uq[:, :, 160:], uq[:, :, 128:160]], 2).reshape(384, 512))
    ukv = z["w_ukv"][l].reshape(256, 8, 256)
    d["wuk"] = c(ukv[:, :, :128].reshape(256, 1024))
    d["wuv"] = c(ukv[:, :, 128:].reshape(256, 1024))
    d["wgate"] = c(win[:, 3784:5832])
    d["wbm"] = c(z["w_branch_m"][l])
    d["wba"] = c(z["w_branch_a"][l])
    d["wout"] = c(z["w_out"][l])
    d["g2"] = c(z["norm2_g"][l])
    d["wr"] = c(np.concatenate([z["w_group"][l], z["w_router"][l]], 1))
    d["rb"] = c(np.concatenate([z["b_group"][l], z["b_router"][l]]))
    d["weg"] = c(z["w_expert_gate"][l])
    d["weu"] = c(z["w_expert_up"][l])
    d["wed"] = c(z["w_expert_down"][l])
    return d


_PROGRAMS = {}


def kernel(**inputs):
    z = {k: np.asarray(v) for k, v in inputs.items()}
    x = z["x"].astype(np.float32, copy=False)
    B, S, _ = x.shape
    if S not in _PROGRAMS:
        _PROGRAMS[S] = build_full(S)
    kb = _PROGRAMS[S]
    shared = {"cs": _rope_tables(S), "fg": np.ascontiguousarray(z["final_norm_g"].astype(np.float32))}
    for l in range(DEPTH):
        for k, v in _prep_layer(z, l).items():
            assert list(v.shape) == LAYER_SHAPES[k], (k, v.shape)
            shared["%s_%d" % (k, l)] = v.astype(np.float32, copy=False)
    in_maps = []
    for b in range(B):
        m = dict(shared)
        m["x"] = np.ascontiguousarray(x[b])
        m["cvec"] = np.ascontiguousarray(z["c"][b].astype(np.float32))
        in_maps.append(m)
    res = run_bass_kernel_spmd(kb.nc, in_maps, core_ids=list(range(B)))
    return np.stack([np.asarray(r["out"]) for r in res.results], 0).astype(np.float32)
```

```python
import math
import numpy as np
import ml_dtypes
import concourse.bass as bass
import concourse.mybir as mybir
from concourse.bass_utils import run_bass_kernel_spmd

F32 = mybir.dt.float32
BF16 = mybir.dt.bfloat16
AF = mybir.ActivationFunctionType
ALU = mybir.AluOpType
AX = mybir.AxisListType

D = 1024
NCORES = 8
EPS = 1e-6
BIG = 30000.0


class Tok:
    __slots__ = ("w", "r", "wsem", "rsem", "name")

    def __init__(self, name=""):
        self.w = None
        self.r = []
        self.wsem = None
        self.rsem = None
        self.name = name


class _Sem:
    __slots__ = ("h", "cnt", "key")

    def __init__(self, h, key):
        self.h = h
        self.cnt = 0
        self.key = key


class _Eng:
    def __init__(self, name, e, sem):
        self.name = name
        self.e = e
        self.sem = sem
        self.seen = {}


class KB:
    def __init__(self):
        self.nc = bass.Bass("TRN2", target_bir_lowering=False)
        nc = self.nc
        self._nsem = 0
        self.eng = {}
        for name in ("tensor", "vector", "scalar", "gpsimd", "sync"):
            self.eng[name] = _Eng(name, getattr(nc, name), self._newsem("e_" + name))
        self._stack = []
        self._dsems = []
        self._free_dsems = []
        self._live_dsems = []
        self.ninst = 0

    def _newsem(self, name):
        if not name.startswith("e_") and self._free_dsems:
            sem = self._free_dsems.pop()
            self._live_dsems.append(sem)
            return sem
        self._nsem += 1
        h = self.nc.semaphore(name + "_%d" % self._nsem).__enter__()
        sem = _Sem(h, self._nsem)
        if not name.startswith("e_"):
            self._dsems.append(sem)
            self._live_dsems.append(sem)
        return sem

    def dram_in(self, name, shape, dt):
        return self.nc.dram_tensor(name, list(shape), dt, kind="ExternalInput").ap()

    def dram_out(self, name, shape, dt):
        return self.nc.dram_tensor(name, list(shape), dt, kind="ExternalOutput").ap()

    def dram_tmp(self, name, shape, dt):
        return self.nc.dram_tensor(name, list(shape), dt, kind="Internal").ap()

    def sbuf(self, name, shape, dt):
        self._uid = getattr(self, "_uid", 0) + 1
        cm = self.nc.sbuf_tensor("%s_u%d" % (name, self._uid), list(shape), dt)
        t = cm.__enter__()
        self._stack.append(cm)
        return t

    def psum(self, name, shape, dt):
        self._uid = getattr(self, "_uid", 0) + 1
        cm = self.nc.psum_tensor("%s_u%d" % (name, self._uid), list(shape), dt)
        t = cm.__enter__()
        self._stack.append(cm)
        return t

    def mark(self):
        return (len(self._stack), len(self._live_dsems))

    def release(self, mark):
        self.barrier()
        while len(self._stack) > mark[0]:
            self._stack.pop().__exit__(None, None, None)
        while len(self._live_dsems) > mark[1]:
            self._free_dsems.append(self._live_dsems.pop())

    def barrier(self):
        sems = [E.sem for E in self.eng.values()] + self._dsems
        for E in self.eng.values():
            for sem in sems:
                if sem is E.sem or sem.cnt == 0:
                    continue
                if E.seen.get(sem.key, 0) >= sem.cnt:
                    continue
                E.e.wait_ge(sem.h, sem.cnt)
                E.seen[sem.key] = sem.cnt
                self.ninst += 1

    def _wait(self, E, evs):
        need = {}
        for ev in evs:
            if ev is None:
                continue
            sem, val = ev
            if sem is E.sem and E.name == "tensor":
                continue
            if need.get(sem.key, (None, 0))[1] < val:
                need[sem.key] = (sem, val)
        for key, (sem, val) in need.items():
            if E.seen.get(key, 0) >= val:
                continue
            assert val <= sem.cnt, "waiting on an event that is never signalled"
            E.e.wait_ge(sem.h, val)
            E.seen[key] = val
            self.ninst += 1

    def _deps(self, reads, writes):
        evs = []
        for t in reads:
            if hasattr(t, "_toks"):
                evs.extend(x.w for x in t._toks)
            else:
                evs.append(t.w)
        for t in writes:
            evs.append(t.w)
            evs.extend(t.r)
        return evs

    def op(self, engname, fn, reads=(), writes=(), sig=True):
        E = self.eng[engname]
        self._wait(E, self._deps(reads, writes))
        ins = fn(E.e)
        self.ninst += 1
        if sig:
            E.sem.cnt += 1
            ins.then_inc(E.sem.h, 1)
            ev = (E.sem, E.sem.cnt)
        else:
            assert engname == "tensor"
            ev = (E.sem, E.sem.cnt + 1)
        for t in reads:
            for tt_ in (t._toks if hasattr(t, "_toks") else (t,)):
                tt_.r.append(ev)
                if len(tt_.r) > 24:
                    tt_.r = self._compact(tt_.r)
        for t in writes:
            t.w = ev
            t.r = []
        return ins

    @staticmethod
    def _compact(r):
        best = {}
        for sem, val in r:
            if best.get(sem.key, (None, 0))[1] < val:
                best[sem.key] = (sem, val)
        return list(best.values())

    def dma(self, queue, out, in_, reads=(), writes=(), **kw):
        E = self.eng[queue]
        evs = self._deps(reads, [])
        for t in writes:
            isdram = getattr(t, "name", "").startswith("dram:")
            if t.w is not None and not isdram and not (t.wsem is not None and t.w[0] is t.wsem):
                evs.append(t.w)
            evs.extend(t.r)
        self._wait(E, evs)
        ins = E.e.dma_start(out=out, in_=in_, **kw)
        self.ninst += 1
        if writes and writes[0].wsem is None and not getattr(writes[0], "_dram", False):
            pass
        tok = None
        for t in writes:
            if not getattr(t, "name", "").startswith("dram:"):
                tok = t
                if tok.wsem is None:
                    tok.wsem = self._newsem("dw")
                sem = tok.wsem
                break
        if tok is None:
            for t in reads:
                if not getattr(t, "name", "").startswith("dram:"):
                    tok = t
                    if tok.rsem is None:
                        tok.rsem = self._newsem("dr")
                    sem = tok.rsem
                    break
        assert tok is not None, "dma needs an sbuf-side token"
        sem.cnt += 16
        ins.then_inc(sem.h, 16)
        ev = (sem, sem.cnt)
        for t in reads:
            for tt_ in (t._toks if hasattr(t, "_toks") else (t,)):
                tt_.r.append(ev)
                if len(tt_.r) > 24:
                    tt_.r = self._compact(tt_.r)
        for t in writes:
            t.w = ev
            t.r = []
        return ins

    def finish(self, toks):
        E = self.eng["sync"]
        evs = []
        for t in toks:
            evs.append(t.w)
            evs.extend(t.r)
        self._wait(E, evs)


def load_consts(kb):
    nc = kb.nc
    c = {}
    c["tok"] = Tok("consts")
    ones_f = kb.sbuf("c_ones_f", [128, 128], F32)
    ones_b = kb.sbuf("c_ones_b", [128, 128], BF16)
    id_f = kb.sbuf("c_id_f", [128, 128], F32)
    id_b = kb.sbuf("c_id_b", [128, 128], BF16)
    tri_f = kb.sbuf("c_tri_f", [128, 128], F32)
    tris_f = kb.sbuf("c_tris_f", [128, 128], F32)
    tri_b = kb.sbuf("c_tri_b", [128, 128], BF16)
    t = c["tok"]
    kb.op("gpsimd", lambda e: e.memset(ones_f[:], 1.0), writes=[t])
    kb.op("gpsimd", lambda e: e.memset(ones_b[:], 1.0), writes=[t])
    kb.op("gpsimd", lambda e: e.affine_select(out=id_f[:], in_=ones_f[:], pattern=[[-1, 128]],
                                             compare_op=ALU.is_equal, fill=0.0, base=0,
                                             channel_multiplier=1), reads=[t], writes=[t])
    kb.op("gpsimd", lambda e: e.tensor_copy(out=id_b[:], in_=id_f[:]), reads=[t], writes=[t])
    kb.op("gpsimd", lambda e: e.affine_select(out=tri_f[:], in_=ones_f[:], pattern=[[1, 128]],
                                             compare_op=ALU.is_ge, fill=0.0, base=0,
                                             channel_multiplier=-1), reads=[t], writes=[t])
    kb.op("gpsimd", lambda e: e.tensor_copy(out=tri_b[:], in_=tri_f[:]), reads=[t], writes=[t])
    kb.op("gpsimd", lambda e: e.affine_select(out=tris_f[:], in_=ones_f[:], pattern=[[-1, 128]],
                                             compare_op=ALU.is_gt, fill=0.0, base=0,
                                             channel_multiplier=1), reads=[t], writes=[t])
    eps = kb.sbuf("c_eps", [128, 1], F32)
    kb.op("gpsimd", lambda e: e.memset(eps[:], EPS), writes=[t])
    c["eps"] = eps
    one = kb.sbuf("c_one", [128, 1], F32)
    kb.op("gpsimd", lambda e: e.memset(one[:], 1.0), writes=[t])
    c["one"] = one
    ones_b512 = kb.sbuf("c_ones_b512", [128, 512], BF16)
    kb.op("gpsimd", lambda e: e.memset(ones_b512[:], 1.0), writes=[t])
    c["ones_b512"] = ones_b512
    c.update(ones_f=ones_f, ones_b=ones_b, id_f=id_f, id_b=id_b, tri_f=tri_f, tris_f=tris_f,
             tri_b=tri_b)
    return c


def emit_mod(kb, consts, modw, modb, cvec, nvec, outs, ps_tag="modps"):
    nc = kb.nc
    mk = kb.mark()
    cpc = kb.sbuf("mod_cpc", [128, 8], F32)
    cond = kb.sbuf("mod_cond", [128, 8], F32)
    bc = kb.sbuf("mod_bc", [128, 8, 128], F32)
    wt = kb.sbuf("mod_w", [128, 8, 1024], F32)
    bt = kb.sbuf("mod_b", [128, 1024], F32)
    ps = kb.psum(ps_tag, [128, 512], F32)
    t_c, t_cond, t_bc, t_w, t_b, t_ps = (Tok("m%d" % i) for i in range(6))
    kb.dma("sync", cpc[:], cvec.rearrange("(p c) -> p c", c=8), writes=[t_c])
    kb.op("scalar", lambda e: e.activation(out=cond[:], in_=cpc[:], func=AF.Silu),
          reads=[t_c], writes=[t_cond])
    for c in range(8):
        kb.op("vector", lambda e, c=c: e.tensor_scalar_mul(out=bc[:, c, :], in0=consts["ones_f"][:],
                                                           scalar1=cond[:, c:c + 1]),
              reads=[t_cond, consts["tok"]], writes=[t_bc])
    for v in range(nvec):
        out_t, out_tok = outs[v]
        kb.dma("sync", wt[:], modw[:, v * 1024:(v + 1) * 1024].rearrange("(p c) n -> p c n", c=8),
               writes=[t_w])
        kb.dma("sync", bt[:], modb[v * 1024:(v + 1) * 1024].partition_broadcast(128), writes=[t_b])
        for n2 in range(2):
            for c in range(8):
                kb.op("tensor", lambda e, c=c, n2=n2: e.matmul(ps[:], lhsT=bc[:, c, :],
                                                               rhs=wt[:, c, n2 * 512:(n2 + 1) * 512],
                                                               start=(c == 0), stop=(c == 7)),
                      reads=[t_bc, t_w], writes=[t_ps], sig=(c == 7))
            kb.op("vector", lambda e, n2=n2: e.tensor_tensor(out=out_t[:, n2 * 512:(n2 + 1) * 512],
                                                             in0=ps[:], in1=bt[:, n2 * 512:(n2 + 1) * 512],
                                                             op=ALU.add),
                  reads=[t_ps, t_b], writes=[out_tok])
    kb.release(mk)


def emit_norm_mod(kb, xt, t_x, gmul, t_g, shift, t_s, hb, t_h, scr, t_scr, stat, t_stat, eps_ap):
    kb.op("gpsimd", lambda e: e.memset(stat[:, 0:1], 0.0), writes=[t_stat])
    kb.op("scalar", lambda e: e.activation(out=scr[:], in_=xt[:], func=AF.Square,
                                           accum_out=stat[:, 0:1]),
          reads=[t_x], writes=[t_scr, t_stat])
    kb.op("scalar", lambda e: e.activation(out=stat[:, 1:2], in_=stat[:, 0:1], func=AF.Sqrt,
                                           bias=eps_ap, scale=1.0 / D),
          reads=[t_stat], writes=[t_stat])
    kb.op("vector", lambda e: e.reciprocal(out=stat[:, 2:3], in_=stat[:, 1:2]),
          reads=[t_stat], writes=[t_stat])
    if shift is None:
        kb.op("vector", lambda e: e.scalar_tensor_tensor(out=hb[:], in0=xt[:], scalar=stat[:, 2:3],
                                                         in1=gmul[:], op0=ALU.mult, op1=ALU.mult),
              reads=[t_x, t_stat, t_g], writes=[t_h])
        return
    kb.op("vector", lambda e: e.scalar_tensor_tensor(out=scr[:], in0=xt[:], scalar=stat[:, 2:3],
                                                     in1=gmul[:], op0=ALU.mult, op1=ALU.mult),
          reads=[t_x, t_stat, t_g], writes=[t_scr])
    kb.op("gpsimd", lambda e: e.tensor_tensor(out=hb[:], in0=scr[:], in1=shift[:], op=ALU.add),
          reads=[t_scr, t_s], writes=[t_h])


def build_k1(Sh):
    kb = KB()
    nc = kb.nc
    x = kb.dram_in("x", [Sh, D], F32)
    cvec = kb.dram_in("cvec", [D], F32)
    modw = kb.dram_in("modw", [D, 2 * D], F32)
    modb = kb.dram_in("modb", [2 * D], F32)
    g = kb.dram_in("g", [D], F32)
    hT = kb.dram_out("hT", [8, 128, Sh], BF16)
    consts = load_consts(kb)
    emit_k1_body(kb, consts, x, cvec, modw, modb, g, hT, Sh)
    kb.finish([kb._out_tok])
    return kb


def emit_k1_body(kb, consts, x, cvec, modw, modb, g, hT, Sh, hT_tok=None, x_tok=None):
    shift = kb.sbuf("k1_shift", [128, D], F32)
    gmul = kb.sbuf("k1_gmul", [128, D], F32)
    gt = kb.sbuf("k1_g", [128, D], F32)
    t_shift, t_gmul, t_g = Tok("shift"), Tok("gmul"), Tok("g")
    emit_mod(kb, consts, modw, modb, cvec, 2, [(shift, t_shift), (gmul, t_gmul)])
    kb.dma("sync", gt[:], g.partition_broadcast(128), writes=[t_g])
    kb.op("vector", lambda e: e.scalar_tensor_tensor(out=gmul[:], in0=gmul[:], scalar=1.0, in1=gt[:],
                                                     op0=ALU.add, op1=ALU.mult),
          reads=[t_gmul, t_g], writes=[t_gmul])
    NB = 2
    xt = [kb.sbuf("k1_x%d" % i, [128, D], F32) for i in range(NB)]
    scr = [kb.sbuf("k1_scr%d" % i, [128, D], F32) for i in range(NB)]
    hb = [kb.sbuf("k1_hb%d" % i, [128, D], BF16) for i in range(NB)]
    stat = [kb.sbuf("k1_st%d" % i, [128, 4], F32) for i in range(NB)]
    hTs = [kb.sbuf("k1_hT%d" % i, [128, 8, 512], BF16) for i in range(2)]
    pst = [kb.psum("k1_pst%d" % i, [128, 8, 128], BF16) for i in range(2)]
    t_x = [Tok("x%d" % i) for i in range(NB)]
    t_scr = [Tok("scr%d" % i) for i in range(NB)]
    t_hb = [Tok("hb%d" % i) for i in range(NB)]
    t_st = [Tok("st%d" % i) for i in range(NB)]
    t_hT = [Tok("hT%d" % i) for i in range(2)]
    t_ps = [Tok("ps%d" % i) for i in range(2)]
    t_out = hT_tok if hT_tok is not None else Tok("dram:hT")
    kb._out_tok = t_out
    ntile = Sh // 128
    for i in range(ntile):
        s = i % NB
        grp = (i // 4) % 2
        kb.dma("sync", xt[s][:], x[i * 128:(i + 1) * 128, :], reads=([x_tok] if x_tok is not None else []), writes=[t_x[s]])
        emit_norm_mod(kb, xt[s], t_x[s], gmul, t_gmul, shift, t_shift, hb[s], t_hb[s],
                      scr[s], t_scr[s], stat[s], t_st[s], consts["eps"][:, 0:1])
        p = i % 2
        for c in range(8):
            kb.op("tensor", lambda e, c=c, s=s, p=p: e.transpose(out=pst[p][:, c, :],
                                                                 in_=hb[s][:, c * 128:(c + 1) * 128],
                                                                 identity=consts["id_b"][:]),
                  reads=[t_hb[s], consts["tok"]], writes=[t_ps[p]], sig=(c == 7))
        j = i % 4
        kb.op("scalar", lambda e, p=p, grp=grp, j=j: e.copy(out=hTs[grp][:, :, j * 128:(j + 1) * 128],
                                                            in_=pst[p][:]),
              reads=[t_ps[p]], writes=[t_hT[grp]])
        if j == 3:
            t0 = (i - 3) * 128
            kb.dma("gpsimd", hT[:, :, t0:t0 + 512].rearrange("c p t -> p c t"), hTs[grp][:],
                   reads=[t_hT[grp]], writes=[t_out])


def _mm(kb, out, lhsT, rhs, start, stop, reads, writes):
    return kb.op("tensor", lambda e: e.matmul(out, lhsT=lhsT, rhs=rhs, start=start, stop=stop),
                 reads=reads, writes=writes, sig=stop)


def load_w_bf16(kb, dst, src_rows_cols, tok, nchunk, queue="gpsimd"):
    for c in range(nchunk):
        kb.dma(queue, dst[:, c, :], src_rows_cols[c * 128:(c + 1) * 128, :], writes=[tok])


def emit_mlstm_pass(kb, consts, S, hT, t_hT, w, ymT, t_ymT):
    NT = S // 512
    mk = kb.mark()
    ct = consts["tok"]
    wqk = kb.sbuf("a_wqk", [128, 8, 1024], BF16)
    wtm = kb.sbuf("a_wtm", [128, 8, 2056], BF16)
    cw = kb.sbuf("a_cw", [128, 8, 4], F32)
    cb = kb.sbuf("a_cb", [128, 8], F32)
    ifb = kb.sbuf("a_ifb", [128, 8], F32)
    t_w = Tok("a_w")
    t_wt = Tok("a_wt")
    t_cw = Tok("a_cw")
    load_w_bf16(kb, wqk, w["wqk"], t_w, 8)
    load_w_bf16(kb, wtm, w["wtm"], t_wt, 8)
    kb.dma("sync", cw[:], w["cw"], writes=[t_cw])
    kb.dma("sync", cb[:], w["cb"], writes=[t_cw])
    kb.dma("sync", ifb[:], w["ifb"].partition_broadcast(128), writes=[t_cw])

    hTs = [kb.sbuf("a_hT%d" % i, [128, 8, 512], BF16) for i in range(2)]
    t_hTs = [Tok("a_hT%d" % i) for i in range(2)]
    pre = kb.sbuf("a_pre", [128, 8, 515], F32)
    t_pre = [Tok("a_pre%d" % g) for g in range(8)]
    cacc = [kb.sbuf("a_cacc%d" % i, [128, 512], F32) for i in range(2)]
    t_cacc = [Tok("a_cacc%d" % i) for i in range(2)]
    qkT = kb.sbuf("a_qkT", [128, 8, 512], BF16)
    t_qk = [Tok("a_qk%d" % g) for g in range(8)]
    Vp = [kb.sbuf("a_vp%d" % i, [128, 4, 257], BF16) for i in range(2)]
    t_vp = [Tok("a_vp%d" % i) for i in range(2)]
    sgo = [kb.sbuf("a_sgo%d" % i, [128, 1024], F32) for i in range(2)]
    t_sgo = [Tok("a_sgo%d" % i) for i in range(2)]
    gsb = [kb.sbuf("a_g%d" % i, [128, 48], F32) for i in range(2)]
    t_g = [Tok("a_g%d" % i) for i in range(2)]
    Zf = kb.sbuf("a_zf", [128, 4, 257], F32)
    Zb = kb.sbuf("a_zb", [128, 4, 257], BF16)
    t_zf = [Tok("a_zf%d" % h) for h in range(4)]
    t_zb = [Tok("a_zb%d" % h) for h in range(4)]
    s0sb = [kb.sbuf("a_s0%d" % i, [128, 128], BF16) for i in range(2)]
    t_s0 = [Tok("a_s0%d" % i) for i in range(2)]
    ytok = [kb.sbuf("a_yt%d" % i, [128, 256], BF16) for i in range(2)]
    t_yt = [Tok("a_yt%d" % i) for i in range(2)]
    khat = [kb.sbuf("a_kh%d" % i, [128, 128], BF16) for i in range(2)]
    t_kh = [Tok("a_kh%d" % i) for i in range(2)]
    dsm = [kb.sbuf("a_d%d" % i, [128, 8], F32) for i in range(2)]
    t_d = [Tok("a_d%d" % i) for i in range(2)]
    yst = [kb.sbuf("a_yst%d" % i, [128, 8, 512], BF16) for i in range(2)]
    t_yst = [Tok("a_yst%d" % i) for i in range(2)]

    ps_fm = [kb.psum("a_psfm%d" % i, [128, 512], F32) for i in range(2)]
    t_psfm = [Tok("a_psfm%d" % i) for i in range(2)]
    ps_tm = [kb.psum("a_pstm%d" % i, [128, 512], F32) for i in range(2)]
    t_pstm = [Tok("a_pstm%d" % i) for i in range(2)]
    ps_m = kb.psum("a_psm", [128, 512], F32)
    t_ps_s0, t_ps_o, t_ps_if, t_ps_cs = Tok("ps_s0"), Tok("ps_o"), Tok("ps_if"), Tok("ps_cs")
    ps_u = kb.psum("a_psu", [128, 257], F32)
    t_ps_u = Tok("ps_u")
    ps_t = kb.psum("a_pst", [128, 3, 128], BF16)
    t_ps_yt, t_ps_kt = Tok("ps_yt"), Tok("ps_kt")

    kb.op("gpsimd", lambda e: e.memset(pre[:, :, 0:3], 0.0), writes=t_pre)
    kb.op("gpsimd", lambda e: e.memset(Zf[:], 0.0), writes=t_zf)
    kb.op("gpsimd", lambda e: e.memset(Zb[:], 0.0), writes=t_zb)
    for i in range(2):
        kb.op("gpsimd", lambda e, i=i: e.memset(Vp[i][:, :, 256:257], 1.0), writes=[t_vp[i]])

    fmi = 0
    tmi = 0
    hi = 0
    for tt in range(NT):
        s = tt % 2
        kb.dma("sync", hTs[s][:], hT[:, :, tt * 512:(tt + 1) * 512].rearrange("c p t -> p c t"),
               reads=[t_hT], writes=[t_hTs[s]])
        for g in range(8):
            p = fmi % 2
            fmi += 1
            for c in range(8):
                _mm(kb, ps_fm[p][:], wqk[:, c, g * 128:(g + 1) * 128], hTs[s][:, c, :], c == 0, c == 7,
                    [t_w, t_hTs[s]], [t_psfm[p]])
            kb.op("scalar", lambda e, p=p, g=g: e.copy(out=pre[:, g, 3:515], in_=ps_fm[p][:]),
                  reads=[t_psfm[p]], writes=[t_pre[g]])
            a = g % 2
            kb.op("vector", lambda e, a=a, g=g: e.tensor_scalar_mul(out=cacc[a][:], in0=pre[:, g, 0:512],
                                                                    scalar1=cw[:, g, 0:1]),
                  reads=[t_pre[g], t_cw], writes=[t_cacc[a]])
            for j in range(1, 4):
                kb.op("vector", lambda e, a=a, g=g, j=j: e.scalar_tensor_tensor(
                    out=cacc[a][:], in0=pre[:, g, j:j + 512], scalar=cw[:, g, j:j + 1], in1=cacc[a][:],
                    op0=ALU.mult, op1=ALU.add), reads=[t_pre[g], t_cw, t_cacc[a]], writes=[t_cacc[a]])
            kb.op("scalar", lambda e, a=a, g=g: e.activation(out=qkT[:, g, :], in_=cacc[a][:], func=AF.Silu,
                                                             bias=cb[:, g:g + 1]),
                  reads=[t_cacc[a], t_cw], writes=[t_qk[g]])
            kb.op("gpsimd", lambda e, g=g: e.tensor_copy(out=pre[:, g, 0:3], in_=pre[:, g, 512:515]),
                  reads=[t_pre[g]], writes=[t_pre[g]])
        for j in range(4):
            vs = (tt * 4 + j) % 2
            tsl = slice(j * 128, (j + 1) * 128)
            for half in range(2):
                p = tmi % 2
                tmi += 1
                for c in range(8):
                    _mm(kb, ps_tm[p][:], hTs[s][:, c, tsl], wtm[:, c, half * 512:(half + 1) * 512],
                        c == 0, c == 7, [t_wt, t_hTs[s]], [t_pstm[p]])
                kb.op("scalar", lambda e, p=p, vs=vs, half=half: e.copy(
                    out=Vp[vs][:, 2 * half:2 * half + 2, 0:256],
                    in_=ps_tm[p][:].rearrange("p (h v) -> p h v", h=2)),
                    reads=[t_pstm[p]], writes=[t_vp[vs]])
            for half in range(2):
                p = tmi % 2
                tmi += 1
                for c in range(8):
                    _mm(kb, ps_tm[p][:], hTs[s][:, c, tsl], wtm[:, c, 1024 + half * 512:1024 + (half + 1) * 512],
                        c == 0, c == 7, [t_wt, t_hTs[s]], [t_pstm[p]])
                kb.op("scalar", lambda e, p=p, vs=vs, half=half: e.activation(
                    out=sgo[vs][:, half * 512:(half + 1) * 512], in_=ps_tm[p][:], func=AF.Sigmoid),
                    reads=[t_pstm[p]], writes=[t_sgo[vs]])
            for c in range(8):
                _mm(kb, ps_m[:, 385:393], hTs[s][:, c, tsl], wtm[:, c, 2048:2056], c == 0, c == 7,
                    [t_wt, t_hTs[s]], [t_ps_if])
            G = gsb[vs]
            tg = t_g[vs]
            kb.op("vector", lambda e, G=G: e.tensor_tensor(out=G[:, 0:8], in0=ps_m[:, 385:393], in1=ifb[:],
                                                           op=ALU.add),
                  reads=[t_ps_if, t_cw], writes=[tg])
            kb.op("scalar", lambda e, G=G: e.activation(out=G[:, 8:12], in_=G[:, 4:8], func=AF.Exp, scale=-1.0),
                  reads=[tg], writes=[tg])
            kb.op("scalar", lambda e, G=G: e.activation(out=G[:, 8:12], in_=G[:, 8:12], func=AF.Ln,
                                                        bias=consts["one"][:, 0:1]),
                  reads=[tg, ct], writes=[tg])
            kb.op("vector", lambda e, G=G: e.tensor_scalar_mul(out=G[:, 8:12], in0=G[:, 8:12], scalar1=-1.0),
                  reads=[tg], writes=[tg])
            _mm(kb, ps_m[:, 393:397], consts["tri_f"][:], G[:, 8:12], True, True, [ct, tg], [t_ps_cs])
            _mm(kb, ps_m[:, 397:401], consts["tris_f"][:], G[:, 8:12], True, True, [ct, tg], [t_ps_cs])
            _mm(kb, ps_m[:, 401:405], consts["ones_f"][:], G[:, 8:12], True, True, [ct, tg], [t_ps_cs])
            kb.op("vector", lambda e, G=G: e.tensor_copy(out=G[:, 16:20], in_=ps_m[:, 393:397]),
                  reads=[t_ps_cs], writes=[tg])
            kb.op("vector", lambda e, G=G: e.tensor_tensor(out=G[:, 20:24], in0=G[:, 0:4], in1=ps_m[:, 393:397],
                                                           op=ALU.subtract),
                  reads=[t_ps_cs, tg], writes=[tg])
            kb.op("vector", lambda e, G=G: e.tensor_tensor(out=G[:, 24:28], in0=G[:, 0:4], in1=ps_m[:, 397:401],
                                                           op=ALU.add),
                  reads=[t_ps_cs, tg], writes=[tg])
            kb.op("vector", lambda e, G=G: e.tensor_copy(out=G[:, 28:32], in_=ps_m[:, 401:405]),
                  reads=[t_ps_cs], writes=[tg])
            kb.op("scalar", lambda e, G=G: e.activation(out=G[:, 32:48], in_=G[:, 16:32], func=AF.Exp),
                  reads=[tg], writes=[tg])
            kb.op("vector", lambda e, G=G: e.tensor_scalar_mul(out=G[:, 12:16], in0=G[:, 32:36],
                                                               scalar1=128.0 ** -0.5),
                  reads=[tg], writes=[tg])
            for h in range(4):
                q = hi % 2
                hi += 1
                qT = qkT[:, h, tsl]
                kT = qkT[:, 4 + h, tsl]
                _mm(kb, ps_m[:, 0:128], kT, qT, True, True, [t_qk[h], t_qk[4 + h]], [t_ps_s0])
                kb.op("vector", lambda e, q=q, G=G, h=h: e.scalar_tensor_tensor(
                    out=s0sb[q][:], in0=ps_m[:, 0:128], scalar=G[:, 36 + h:37 + h], in1=consts["tri_f"][:],
                    op0=ALU.mult, op1=ALU.mult), reads=[t_ps_s0, tg, ct], writes=[t_s0[q]])
                _mm(kb, ps_m[:, 128:385], s0sb[q][:], Vp[vs][:, h, :], True, False, [t_s0[q], t_vp[vs]], [t_ps_o])
                _mm(kb, ps_m[:, 128:385], qT, Zb[:, h, :], False, True, [t_qk[h], t_zb[h]], [t_ps_o])
                dd = dsm[q]
                td = t_d[q]
                kb.op("vector", lambda e, dd=dd, G=G, h=h: e.tensor_tensor(
                    out=dd[:, 0:1], in0=ps_m[:, 384:385], in1=G[:, 12 + h:13 + h], op=ALU.mult),
                    reads=[t_ps_o, tg], writes=[td])
                kb.op("vector", lambda e, dd=dd: e.tensor_scalar(out=dd[:, 1:2], in0=dd[:, 0:1], scalar1=-1.0,
                                                                 scalar2=1.0, op0=ALU.mult, op1=ALU.max),
                      reads=[td], writes=[td])
                kb.op("vector", lambda e, dd=dd: e.tensor_tensor(out=dd[:, 2:3], in0=dd[:, 1:2], in1=dd[:, 0:1],
                                                                 op=ALU.max), reads=[td], writes=[td])
                kb.op("vector", lambda e, dd=dd: e.reciprocal(out=dd[:, 3:4], in_=dd[:, 2:3]),
                      reads=[td], writes=[td])
                kb.op("vector", lambda e, dd=dd, G=G, h=h: e.tensor_tensor(
                    out=dd[:, 4:5], in0=dd[:, 3:4], in1=G[:, 12 + h:13 + h], op=ALU.mult),
                    reads=[td, tg], writes=[td])
                kb.op("vector", lambda e, q=q, dd=dd, vs=vs, h=h: e.scalar_tensor_tensor(
                    out=ytok[q][:], in0=ps_m[:, 128:384], scalar=dd[:, 4:5],
                    in1=sgo[vs][:, h * 256:(h + 1) * 256], op0=ALU.mult, op1=ALU.mult),
                    reads=[t_ps_o, td, t_sgo[vs]], writes=[t_yt[q]])
                for k in range(2):
                    kb.op("tensor", lambda e, q=q, k=k: e.transpose(out=ps_t[:, k, :],
                                                                    in_=ytok[q][:, k * 128:(k + 1) * 128],
                                                                    identity=consts["id_b"][:]),
                          reads=[t_yt[q], ct], writes=[t_ps_yt], sig=(k == 1))
                kb.op("scalar", lambda e, s=s, h=h, tsl=tsl: e.copy(out=yst[s][:, 2 * h:2 * h + 2, tsl],
                                                                    in_=ps_t[:, 0:2, :]),
                      reads=[t_ps_yt], writes=[t_yst[s]])
                kb.op("tensor", lambda e, kT=kT: e.transpose(out=ps_t[:, 2, :], in_=kT, identity=consts["id_b"][:]),
                      reads=[t_qk[4 + h], ct], writes=[t_ps_kt])
                kb.op("scalar", lambda e, q=q, G=G, h=h: e.activation(out=khat[q][:], in_=ps_t[:, 2, :],
                                                                      func=AF.Copy, scale=G[:, 40 + h:41 + h]),
                      reads=[t_ps_kt, tg], writes=[t_kh[q]])
                _mm(kb, ps_u[:], khat[q][:], Vp[vs][:, h, :], True, True, [t_kh[q], t_vp[vs]], [t_ps_u])
                kb.op("vector", lambda e, G=G, h=h: e.scalar_tensor_tensor(
                    out=Zf[:, h, :], in0=Zf[:, h, :], scalar=G[:, 44 + h:45 + h], in1=ps_u[:],
                    op0=ALU.mult, op1=ALU.add), reads=[t_zf[h], tg, t_ps_u], writes=[t_zf[h]])
                kb.op("gpsimd", lambda e, h=h: e.tensor_copy(out=Zb[:, h, :], in_=Zf[:, h, :]),
                      reads=[t_zf[h]], writes=[t_zb[h]])
        kb.dma("gpsimd", ymT[:, :, tt * 512:(tt + 1) * 512].rearrange("c p t -> p c t"), yst[s][:],
               reads=[t_yst[s]], writes=[t_ymT])
    kb.release(mk)


def emit_mla_proj_pass(kb, consts, S, hT, t_hT, w, scr):
    NT = S // 512
    mk = kb.mark()
    ct = consts["tok"]
    wmla = kb.sbuf("b_wmla", [128, 8, 768], BF16)
    wuqn = kb.sbuf("b_wuqn", [128, 3, 1024], BF16)
    wuqr = kb.sbuf("b_wuqr", [128, 3, 512], BF16)
    wuqs = kb.sbuf("b_wuqs", [128, 3, 512], BF16)
    wuk = kb.sbuf("b_wuk", [128, 2, 1024], BF16)
    wuv = kb.sbuf("b_wuv", [128, 2, 1024], BF16)
    gq = kb.sbuf("b_gq", [128, 5], F32)
    t_w = Tok("b_w")
    load_w_bf16(kb, wmla, w["wmla"], t_w, 8)
    load_w_bf16(kb, wuqn, w["wuqn"], t_w, 3)
    load_w_bf16(kb, wuqr, w["wuqr"], t_w, 3)
    load_w_bf16(kb, wuqs, w["wuqs"], t_w, 3)
    load_w_bf16(kb, wuk, w["wuk"], t_w, 2)
    load_w_bf16(kb, wuv, w["wuv"], t_w, 2)
    kb.dma("sync", gq[:], w["gqkv"], writes=[t_w])
    epsq = consts["eps"]

    hTs = [kb.sbuf("b_hT%d" % i, [128, 8, 512], BF16) for i in range(2)]
    t_hTs = [Tok("b_hT%d" % i) for i in range(2)]
    cs = [kb.sbuf("b_cs%d" % i, [64, 2, 512], F32) for i in range(2)]
    t_cs = [Tok("b_cs%d" % i) for i in range(2)]
    raw = kb.sbuf("b_raw", [128, 5, 512], F32)
    t_raw = [Tok("b_raw%d" % g) for g in range(5)]
    sq = kb.sbuf("b_sq", [128, 5, 512], BF16)
    t_sq = [Tok("b_sq%d" % g) for g in range(5)]
    rs = kb.sbuf("b_rs", [128, 2, 512], F32)
    t_rs = [Tok("b_rs%d" % g) for g in range(2)]
    cn = kb.sbuf("b_cn", [128, 5, 512], BF16)
    t_cn = [Tok("b_cn%d" % g) for g in range(5)]
    rt = [kb.sbuf("b_rt%d" % i, [64, 2, 512], F32) for i in range(2)]
    t_rt = [Tok("b_rt%d" % i) for i in range(2)]
    krs = kb.sbuf("b_krs", [64, 512], BF16)
    t_krs = Tok("b_krs")
    qn_st = kb.sbuf("b_qn", [128, 8, 512], BF16)
    qr_st = kb.sbuf("b_qr", [64, 8, 512], BF16)
    kn_st = kb.sbuf("b_kn", [128, 8, 512], BF16)
    t_qn, t_qr, t_kn = Tok("b_qn"), Tok("b_qr"), Tok("b_kn")
    v_st = [kb.sbuf("b_v%d" % i, [128, 1024], BF16) for i in range(2)]
    t_v = [Tok("b_v%d" % i) for i in range(2)]

    ps_fm = [kb.psum("b_psfm%d" % i, [128, 512], F32) for i in range(2)]
    t_psfm = [Tok("b_psfm%d" % i) for i in range(2)]
    ps_ss = [kb.psum("b_psss%d" % i, [128, 512], F32) for i in range(2)]
    t_psss = [Tok("b_psss%d" % i) for i in range(2)]
    ps_r = [kb.psum("b_psr%d" % i, [128, 512], F32) for i in range(2)]
    t_psr = [Tok("b_psr%d" % i) for i in range(2)]
    ps_tm = [kb.psum("b_pstm%d" % i, [128, 512], F32) for i in range(2)]
    t_pstm = [Tok("b_pstm%d" % i) for i in range(2)]
    d_QTn, d_QTr, d_KTn, d_KrT, d_Vd = scr["QTn"], scr["QTr"], scr["KTn"], scr["KrT"], scr["Vd"]
    t_scr = scr["tok"]

    fmi = 0
    ri = 0
    vi = 0

    def rope(psa, psb, t_pa, t_pb, cst, t_cst, out_ap, t_out):
        nonlocal ri
        k = ri % 2
        ri += 1
        kb.op("vector", lambda e: e.tensor_tensor(out=rt[k][:, 0, :], in0=psa, in1=cst[:, 0, :], op=ALU.mult),
              reads=[t_pa, t_cst], writes=[t_rt[k]])
        kb.op("vector", lambda e: e.tensor_tensor(out=rt[k][:, 1, :], in0=psb, in1=cst[:, 1, :], op=ALU.mult),
              reads=[t_pb, t_cst], writes=[t_rt[k]])
        kb.op("gpsimd", lambda e: e.tensor_tensor(out=out_ap, in0=rt[k][:, 0, :], in1=rt[k][:, 1, :], op=ALU.add),
              reads=[t_rt[k]], writes=[t_out])

    for tt in range(NT):
        s = tt % 2
        tok_sl = slice(tt * 512, (tt + 1) * 512)
        kb.dma("sync", hTs[s][:], hT[:, :, tok_sl].rearrange("c p t -> p c t"), reads=[t_hT], writes=[t_hTs[s]])
        kb.dma("sync", cs[s][:], w["cs"][:, :, tok_sl], writes=[t_cs[s]])
        for g in range(5):
            p = fmi % 2
            fmi += 1
            for c in range(8):
                _mm(kb, ps_fm[p][:], wmla[:, c, g * 128:(g + 1) * 128], hTs[s][:, c, :], c == 0, c == 7,
                    [t_w, t_hTs[s]], [t_psfm[p]])
            kb.op("scalar", lambda e, p=p, g=g: e.copy(out=raw[:, g, :], in_=ps_fm[p][:]),
                  reads=[t_psfm[p]], writes=[t_raw[g]])
            kb.op("gpsimd", lambda e, g=g: e.tensor_tensor(out=sq[:, g, :], in0=raw[:, g, :], in1=raw[:, g, :],
                                                           op=ALU.mult), reads=[t_raw[g]], writes=[t_sq[g]])
        for n, (g0, g1, dim) in enumerate(((0, 3, 384.0), (3, 5, 256.0))):
            for g in range(g0, g1):
                _mm(kb, ps_ss[n][:], consts["ones_b"][:], sq[:, g, :], g == g0, g == g1 - 1,
                    [ct, t_sq[g]], [t_psss[n]])
            kb.op("scalar", lambda e, n=n, dim=dim: e.activation(out=rs[:, n, :], in_=ps_ss[n][:], func=AF.Sqrt,
                                                                 bias=epsq[:, 0:1], scale=1.0 / dim),
                  reads=[t_psss[n], ct], writes=[t_rs[n]])
            kb.op("vector", lambda e, n=n: e.reciprocal(out=rs[:, n, :], in_=rs[:, n, :]),
                  reads=[t_rs[n]], writes=[t_rs[n]])
            for g in range(g0, g1):
                kb.op("vector", lambda e, n=n, g=g: e.scalar_tensor_tensor(
                    out=cn[:, g, :], in0=raw[:, g, :], scalar=gq[:, g:g + 1], in1=rs[:, n, :],
                    op0=ALU.mult, op1=ALU.mult), reads=[t_raw[g], t_w, t_rs[n]], writes=[t_cn[g]])
        for k2 in range(2):
            for c in range(8):
                _mm(kb, ps_r[k2][0:64, :], wmla[:, c, 640 + 64 * k2:704 + 64 * k2], hTs[s][:, c, :], c == 0, c == 7,
                    [t_w, t_hTs[s]], [t_psr[k2]])
        rope(ps_r[0][0:64, :], ps_r[1][0:64, :], t_psr[0], t_psr[1], cs[s], t_cs[s], krs[:], t_krs)
        kb.dma("gpsimd", d_KrT[:, tok_sl], krs[:], reads=[t_krs], writes=[t_scr])
        for h in range(8):
            p = fmi % 2
            fmi += 1
            for c in range(3):
                _mm(kb, ps_fm[p][:], wuqn[:, c, h * 128:(h + 1) * 128], cn[:, c, :], c == 0, c == 2,
                    [t_w, t_cn[c]], [t_psfm[p]])
            kb.op("scalar", lambda e, p=p, h=h: e.copy(out=qn_st[:, h, :], in_=ps_fm[p][:]),
                  reads=[t_psfm[p]], writes=[t_qn])
            for k2, wsrc in enumerate((wuqr, wuqs)):
                for c in range(3):
                    _mm(kb, ps_r[k2][0:64, :], wsrc[:, c, h * 64:(h + 1) * 64], cn[:, c, :], c == 0, c == 2,
                        [t_w, t_cn[c]], [t_psr[k2]])
            rope(ps_r[0][0:64, :], ps_r[1][0:64, :], t_psr[0], t_psr[1], cs[s], t_cs[s], qr_st[:, h, :], t_qr)
            p = fmi % 2
            fmi += 1
            for c in range(2):
                _mm(kb, ps_fm[p][:], wuk[:, c, h * 128:(h + 1) * 128], cn[:, 3 + c, :], c == 0, c == 1,
                    [t_w, t_cn[3 + c]], [t_psfm[p]])
            kb.op("scalar", lambda e, p=p, h=h: e.copy(out=kn_st[:, h, :], in_=ps_fm[p][:]),
                  reads=[t_psfm[p]], writes=[t_kn])
        kb.dma("gpsimd", d_QTn[:, :, tok_sl].rearrange("h p t -> p h t"), qn_st[:], reads=[t_qn], writes=[t_scr])
        kb.dma("gpsimd", d_QTr[:, :, tok_sl].rearrange("h p t -> p h t"), qr_st[:], reads=[t_qr], writes=[t_scr])
        kb.dma("gpsimd", d_KTn[:, :, tok_sl].rearrange("h p t -> p h t"), kn_st[:], reads=[t_kn], writes=[t_scr])
        for j in range(4):
            q = vi % 2
            vi += 1
            for half in range(2):
                p = half
                for c in range(2):
                    _mm(kb, ps_tm[p][:], cn[:, 3 + c, j * 128:(j + 1) * 128], wuv[:, c, half * 512:(half + 1) * 512],
                        c == 0, c == 1, [t_w, t_cn[3 + c]], [t_pstm[p]])
                kb.op("scalar", lambda e, p=p, q=q, half=half: e.copy(out=v_st[q][:, half * 512:(half + 1) * 512],
                                                                      in_=ps_tm[p][:]),
                      reads=[t_pstm[p]], writes=[t_v[q]])
            kb.dma("gpsimd", d_Vd[:, :, tt * 4 + j, :].rearrange("h p d -> p h d"),
                   v_st[q][:].rearrange("p (h d) -> p h d", h=8), reads=[t_v[q]], writes=[t_scr])
    kb.release(mk)


def emit_attn_pass(kb, consts, S, scr, yaT, t_yaT):
    NQ = S // 512
    NK = S // 128
    mk = kb.mark()
    ct = consts["tok"]
    scale = 192.0 ** -0.5
    t_scr = scr["tok"]
    masks = kb.sbuf("c_mask", [128, 4, 512], BF16)
    t_mask = Tok("c_mask")
    for r in range(4):
        for kbk in range(2):
            kb.op("gpsimd", lambda e, r=r, kbk=kbk: e.affine_select(
                out=masks[64 * kbk:64 * kbk + 64, r, :].rearrange("p (a b) -> p a b", a=8),
                in_=consts["ones_b512"][64 * kbk:64 * kbk + 64, :].rearrange("p (a b) -> p a b", a=8),
                pattern=[[1, 8], [0, 64]], compare_op=ALU.is_ge, fill=0.0, base=-(2 * r + kbk),
                channel_multiplier=0), reads=[ct], writes=[t_mask])
    krt = kb.sbuf("c_krt", [64, S], BF16)
    t_krt = Tok("c_krt")
    kb.dma("sync", krt[:], scr["KrT"][:, :], reads=[t_scr], writes=[t_krt])
    ktn = [kb.sbuf("c_ktn%d" % i, [128, S], BF16) for i in range(2)]
    t_ktn = [Tok("c_ktn%d" % i) for i in range(2)]
    vsb = [kb.sbuf("c_v%d" % i, [128, NK, 128], BF16) for i in range(2)]
    t_vsb = [Tok("c_v%d" % i) for i in range(2)]
    qn = [kb.sbuf("c_qn%d" % i, [128, 512], BF16) for i in range(2)]
    qr = [kb.sbuf("c_qr%d" % i, [64, 512], BF16) for i in range(2)]
    t_q = [Tok("c_q%d" % i) for i in range(2)]
    NPT = 4
    NPS = 3
    pT = [kb.sbuf("c_pT%d" % i, [128, 512], BF16) for i in range(NPT)]
    t_pT = [Tok("c_pT%d" % i) for i in range(NPT)]
    rden = [kb.sbuf("c_rd%d" % i, [128, 512], F32) for i in range(2)]
    t_rden = [Tok("c_rd%d" % i) for i in range(2)]
    yst = [kb.sbuf("c_y%d" % i, [128, 512], BF16) for i in range(2)]
    t_yst = [Tok("c_y%d" % i) for i in range(2)]
    ps_s = [kb.psum("c_pss%d" % i, [128, 512], F32) for i in range(NPS)]
    t_pss = [Tok("c_pss%d" % i) for i in range(NPS)]
    ps_a = [kb.psum("c_psa%d" % i, [128, 512], F32) for i in range(2)]
    t_psa = [Tok("c_psa%d" % i) for i in range(2)]
    ps_d = [kb.psum("c_psd%d" % i, [128, 512], F32) for i in range(2)]
    t_psd = [Tok("c_psd%d" % i) for i in range(2)]
    DEPTH_PIPE = 2
    gi = 0
    qi = 0
    for h in range(8):
        hs = h % 2
        kb.dma("sync", ktn[hs][:], scr["KTn"][h, :, :], reads=[t_scr], writes=[t_ktn[hs]])
        kb.dma("sync", vsb[hs][:], scr["Vd"][h, :, :, :], reads=[t_scr], writes=[t_vsb[hs]])
        for j in range(NQ):
            a = qi % 2
            qi += 1
            qsl = slice(j * 512, (j + 1) * 512)
            kb.dma("sync", qn[a][:], scr["QTn"][h, :, qsl], reads=[t_scr], writes=[t_q[a]])
            kb.dma("sync", qr[a][:], scr["QTr"][h, :, qsl], reads=[t_scr], writes=[t_q[a]])
            nkt = 4 * (j + 1)
            base = gi
            gi += nkt

            def qk(kt):
                p = (base + kt) % NPS
                ksl = slice(kt * 128, (kt + 1) * 128)
                _mm(kb, ps_s[p][:], ktn[hs][:, ksl], qn[a][:], True, False, [t_ktn[hs], t_q[a]], [t_pss[p]])
                _mm(kb, ps_s[p][:], krt[:, ksl], qr[a][:], False, True, [t_krt, t_q[a]], [t_pss[p]])

            def soft(kt):
                p = (base + kt) % NPS
                u = (base + kt) % NPT
                kb.op("scalar", lambda e: e.activation(out=pT[u][:], in_=ps_s[p][:], func=AF.Exp, scale=scale),
                      reads=[t_pss[p]], writes=[t_pT[u]])
                if kt >= 4 * j:
                    r = kt - 4 * j
                    kb.op("vector", lambda e: e.tensor_tensor(out=pT[u][:], in0=pT[u][:], in1=masks[:, r, :],
                                                              op=ALU.mult),
                          reads=[t_pT[u], t_mask], writes=[t_pT[u]])

            def pv(kt):
                u = (base + kt) % NPT
                _mm(kb, ps_a[a][:], vsb[hs][:, kt, :], pT[u][:], kt == 0, kt == nkt - 1,
                    [t_vsb[hs], t_pT[u]], [t_psa[a]])
                _mm(kb, ps_d[a][:], consts["ones_b"][:], pT[u][:], kt == 0, kt == nkt - 1,
                    [ct, t_pT[u]], [t_psd[a]])

            for kt in range(min(DEPTH_PIPE, nkt)):
                qk(kt)
            for kt in range(nkt):
                soft(kt)
                if kt + DEPTH_PIPE < nkt:
                    qk(kt + DEPTH_PIPE)
                pv(kt)
            kb.op("vector", lambda e, a=a: e.reciprocal(out=rden[a][:], in_=ps_d[a][:]),
                  reads=[t_psd[a]], writes=[t_rden[a]])
            kb.op("vector", lambda e, a=a: e.tensor_tensor(out=yst[a][:], in0=ps_a[a][:], in1=rden[a][:],
                                                           op=ALU.mult),
                  reads=[t_psa[a], t_rden[a]], writes=[t_yst[a]])
            kb.dma("gpsimd", yaT[h, :, qsl], yst[a][:], reads=[t_yst[a]], writes=[t_yaT])
    kb.release(mk)


def alloc_mla_scratch(kb, S):
    return dict(QTn=kb.dram_tmp("s_QTn", [8, 128, S], BF16), QTr=kb.dram_tmp("s_QTr", [8, 64, S], BF16),
                KTn=kb.dram_tmp("s_KTn", [8, 128, S], BF16), KrT=kb.dram_tmp("s_KrT", [64, S], BF16),
                Vd=kb.dram_tmp("s_Vd", [8, 128, S // 128, 128], BF16), tok=Tok("dram:mla_scr"))


def emit_merge_pass(kb, consts, S, hT, ymT, yaT, t_in, x, t_x, w, gate1, t_gate1, xmid, t_xmid):
    NT = S // 512
    mk = kb.mark()
    wg = kb.sbuf("d_wg", [128, 8, 2048], BF16)
    wbm = kb.sbuf("d_wbm", [128, 8, 1024], BF16)
    wba = kb.sbuf("d_wba", [128, 8, 1024], BF16)
    wo = kb.sbuf("d_wo", [128, 8, 1024], BF16)
    t_w = Tok("d_w")
    load_w_bf16(kb, wg, w["wgate"], t_w, 8)
    load_w_bf16(kb, wbm, w["wbm"], t_w, 8)
    load_w_bf16(kb, wba, w["wba"], t_w, 8)
    load_w_bf16(kb, wo, w["wout"], t_w, 8)
    hTs = [kb.sbuf("d_hT%d" % i, [128, 8, 512], BF16) for i in range(2)]
    ymS = [kb.sbuf("d_ym%d" % i, [128, 8, 512], BF16) for i in range(2)]
    yaS = [kb.sbuf("d_ya%d" % i, [128, 8, 512], BF16) for i in range(2)]
    t_ld = [Tok("d_ld%d" % i) for i in range(2)]
    sg = [kb.sbuf("d_sg%d" % i, [128, 2, 512], F32) for i in range(2)]
    t_sg = [Tok("d_sg%d" % i) for i in range(2)]
    mm_ = [kb.sbuf("d_mm%d" % i, [128, 2, 512], F32) for i in range(2)]
    t_mm = [Tok("d_mm%d" % i) for i in range(2)]
    mg = [kb.sbuf("d_mg%d" % i, [128, 8, 512], BF16) for i in range(2)]
    t_mg = [Tok("d_mg%d" % i) for i in range(2)]
    xt = [kb.sbuf("d_x%d" % i, [128, 1024], F32) for i in range(2)]
    t_xt = [Tok("d_x%d" % i) for i in range(2)]
    tmp = [kb.sbuf("d_tmp%d" % i, [128, 512], F32) for i in range(2)]
    t_tmp = [Tok("d_tmp%d" % i) for i in range(2)]
    ps4 = [kb.psum("d_ps%d" % i, [128, 512], F32) for i in range(4)]
    t_ps4 = [Tok("d_ps%d" % i) for i in range(4)]
    pso = [kb.psum("d_pso%d" % i, [128, 512], F32) for i in range(2)]
    t_pso = [Tok("d_pso%d" % i) for i in range(2)]
    oi = 0
    xi = 0
    for tt in range(NT):
        s = tt % 2
        tsl = slice(tt * 512, (tt + 1) * 512)
        kb.dma("sync", hTs[s][:], hT[:, :, tsl].rearrange("c p t -> p c t"), reads=[t_in], writes=[t_ld[s]])
        kb.dma("sync", ymS[s][:], ymT[:, :, tsl].rearrange("c p t -> p c t"), reads=[t_in], writes=[t_ld[s]])
        kb.dma("sync", yaS[s][:], yaT[:, :, tsl].rearrange("c p t -> p c t"), reads=[t_in], writes=[t_ld[s]])
        for oc in range(8):
            a = oc % 2
            osl = slice(oc * 128, (oc + 1) * 128)
            for c in range(8):
                _mm(kb, ps4[0][:], wbm[:, c, osl], ymS[s][:, c, :], c == 0, c == 7, [t_w, t_ld[s]], [t_ps4[0]])
            for c in range(8):
                _mm(kb, ps4[1][:], wba[:, c, osl], yaS[s][:, c, :], c == 0, c == 7, [t_w, t_ld[s]], [t_ps4[1]])
            for k in range(2):
                for c in range(8):
                    _mm(kb, ps4[2 + k][:], wg[:, c, k * 1024 + oc * 128:k * 1024 + (oc + 1) * 128], hTs[s][:, c, :],
                        c == 0, c == 7, [t_w, t_ld[s]], [t_ps4[2 + k]])
                kb.op("scalar", lambda e, a=a, k=k: e.activation(out=sg[a][:, k, :], in_=ps4[2 + k][:],
                                                                 func=AF.Sigmoid),
                      reads=[t_ps4[2 + k]], writes=[t_sg[a]])
            for k in range(2):
                kb.op("vector", lambda e, a=a, k=k: e.tensor_tensor(out=mm_[a][:, k, :], in0=ps4[k][:],
                                                                    in1=sg[a][:, k, :], op=ALU.mult),
                      reads=[t_ps4[k], t_sg[a]], writes=[t_mm[a]])
            kb.op("gpsimd", lambda e, a=a, s=s, oc=oc: e.tensor_tensor(out=mg[s][:, oc, :], in0=mm_[a][:, 0, :],
                                                                       in1=mm_[a][:, 1, :], op=ALU.add),
                  reads=[t_mm[a]], writes=[t_mg[s]])
        for j in range(4):
            xs = xi % 2
            xi += 1
            t0 = tt * 512 + j * 128
            kb.dma("sync", xt[xs][:], x[t0:t0 + 128, :], reads=[t_x], writes=[t_xt[xs]])
            for half in range(2):
                p = oi % 2
                oi += 1
                hsl = slice(half * 512, (half + 1) * 512)
                for c in range(8):
                    _mm(kb, pso[p][:], mg[s][:, c, j * 128:(j + 1) * 128], wo[:, c, hsl], c == 0, c == 7,
                        [t_w, t_mg[s]], [t_pso[p]])
                kb.op("vector", lambda e, p=p, hsl=hsl: e.tensor_tensor(out=tmp[p][:], in0=pso[p][:],
                                                                        in1=gate1[:, hsl], op=ALU.mult),
                      reads=[t_pso[p], t_gate1], writes=[t_tmp[p]])
                kb.op("gpsimd", lambda e, p=p, xs=xs, hsl=hsl: e.tensor_tensor(out=xt[xs][:, hsl], in0=xt[xs][:, hsl],
                                                                               in1=tmp[p][:], op=ALU.add),
                      reads=[t_tmp[p], t_xt[xs]], writes=[t_xt[xs]])
            kb.dma("gpsimd", xmid[t0:t0 + 128, :], xt[xs][:], reads=[t_xt[xs]], writes=[t_xmid])
    kb.release(mk)


def emit_moe_pass(kb, consts, S, xmid, t_xmid, w, gmul2, t_gmul2, shift2, t_shift2, gate2, t_gate2,
                  xout, t_xout, final_g=None):
    TS = min(2048, S)
    NST = S // TS
    NTL = TS // 128
    mk = kb.mark()
    ct = consts["tok"]
    h2T = kb.sbuf("e_h2T", [128, 8, TS], BF16)
    t_h2T = Tok("e_h2T")
    yacc = kb.sbuf("e_yacc", [128, NTL, 1024], F32)
    t_yacc = [Tok("e_yacc%d" % i) for i in range(NTL)]
    Wt = kb.sbuf("e_Wt", [128, NTL, 32], F32)
    t_Wt = [Tok("e_Wt%d" % i) for i in range(NTL)]
    wr = kb.sbuf("e_wr", [128, 8, 36], F32)
    rb = kb.sbuf("e_rb", [128, 36], F32)
    t_wr = Tok("e_wr")
    kb.dma("sync", wr[:], w["wr"].rearrange("(c p) n -> p c n", p=128), writes=[t_wr])
    kb.dma("sync", rb[:], w["rb"].partition_broadcast(128), writes=[t_wr])
    fg = None
    if final_g is not None:
        fg = kb.sbuf("e_fg", [128, 1024], F32)
        kb.dma("sync", fg[:], final_g.partition_broadcast(128), writes=[t_wr])
    weg = [kb.sbuf("e_weg%d" % i, [128, 8, 256], BF16) for i in range(2)]
    weu = [kb.sbuf("e_weu%d" % i, [128, 8, 256], BF16) for i in range(2)]
    wed = [kb.sbuf("e_wed%d" % i, [128, 2, 1024], BF16) for i in range(2)]
    t_we = [Tok("e_we%d" % i) for i in range(2)]
    xt = [kb.sbuf("e_x%d" % i, [128, 1024], F32) for i in range(2)]
    t_xt = [Tok("e_x%d" % i) for i in range(2)]
    scr = [kb.sbuf("e_scr%d" % i, [128, 1024], F32) for i in range(2)]
    t_scr = [Tok("e_scr%d" % i) for i in range(2)]
    h2f = [kb.sbuf("e_h2f%d" % i, [128, 1024], F32) for i in range(2)]
    t_h2f = [Tok("e_h2f%d" % i) for i in range(2)]
    stat = [kb.sbuf("e_st%d" % i, [128, 4], F32) for i in range(2)]
    t_st = [Tok("e_st%d" % i) for i in range(2)]
    h2Tf = kb.sbuf("e_h2Tf", [128, 8, 128], F32)
    t_h2Tf = Tok("e_h2Tf")
    R = [kb.sbuf("e_R%d" % i, [128, 160], F32) for i in range(2)]
    t_R = [Tok("e_R%d" % i) for i in range(2)]
    sgl = [kb.sbuf("e_sg%d" % i, [128, 512], F32) for i in range(2)]
    t_sgl = [Tok("e_sg%d" % i) for i in range(2)]
    aT = [kb.sbuf("e_aT%d" % i, [128, 2, 512], BF16) for i in range(2)]
    t_aT = [Tok("e_aT%d" % i) for i in range(2)]
    bank = [kb.psum("e_bank%d" % i, [128, 512], F32) for i in range(8)]
    t_bank = [Tok("e_bank%d" % i) for i in range(8)]
    wload = 0

    def load_expert(e):
        nonlocal wload
        k = wload % 2
        wload += 1
        for c in range(8):
            kb.dma("gpsimd", weg[k][:, c, :], w["weg"][e, c * 128:(c + 1) * 128, :], writes=[t_we[k]])
        for c in range(8):
            kb.dma("gpsimd", weu[k][:, c, :], w["weu"][e, c * 128:(c + 1) * 128, :], writes=[t_we[k]])
        for c in range(2):
            kb.dma("gpsimd", wed[k][:, c, :], w["wed"][e, c * 128:(c + 1) * 128, :], writes=[t_we[k]])
        return k

    for st in range(NST):
        for i in range(NTL):
            s = i % 2
            t0 = st * TS + i * 128
            kb.dma("sync", xt[s][:], xmid[t0:t0 + 128, :], reads=[t_xmid], writes=[t_xt[s]])
            emit_norm_mod(kb, xt[s], t_xt[s], gmul2, t_gmul2, shift2, t_shift2, h2f[s], t_h2f[s],
                          scr[s], t_scr[s], stat[s], t_st[s], consts["eps"][:, 0:1])
            for half in range(2):
                for c4 in range(4):
                    c = half * 4 + c4
                    kb.op("tensor", lambda e, s=s, c=c, half=half, c4=c4: e.transpose(
                        out=bank[half][:, c4 * 128:(c4 + 1) * 128], in_=h2f[s][:, c * 128:(c + 1) * 128],
                        identity=consts["id_f"][:]), reads=[t_h2f[s], ct], writes=[t_bank[half]], sig=(c4 == 3))
                kb.op("scalar", lambda e, half=half: e.copy(
                    out=h2Tf[:, half * 4:half * 4 + 4, :], in_=bank[half][:].rearrange("p (c t) -> p c t", c=4)),
                    reads=[t_bank[half]], writes=[t_h2Tf])
            kb.op("gpsimd", lambda e, i=i: e.tensor_copy(out=h2T[:, :, i * 128:(i + 1) * 128], in_=h2Tf[:]),
                  reads=[t_h2Tf], writes=[t_h2T])
            for c in range(8):
                _mm(kb, bank[2][:, 0:36], h2Tf[:, c, :], wr[:, c, :], c == 0, c == 7, [t_h2Tf, t_wr], [t_bank[2]])
            r = R[s]
            tr = t_R[s]
            V = lambda fn, reads, writes: kb.op("vector", fn, reads=reads, writes=writes)
            V(lambda e: e.tensor_tensor(out=r[:, 0:36], in0=bank[2][:, 0:36], in1=rb[:], op=ALU.add),
              [t_bank[2], t_wr], [tr])
            V(lambda e: e.reduce_max(out=r[:, 36:37], in_=r[:, 0:4], axis=AX.X), [tr], [tr])
            V(lambda e: e.tensor_scalar_mul(out=r[:, 37:38], in0=r[:, 36:37], scalar1=-1.0), [tr], [tr])
            kb.op("scalar", lambda e: e.activation(out=r[:, 40:44], in_=r[:, 0:4], func=AF.Exp, bias=r[:, 37:38]),
                  reads=[tr], writes=[tr])
            V(lambda e: e.reduce_sum(out=r[:, 44:45], in_=r[:, 40:44], axis=AX.X), [tr], [tr])
            V(lambda e: e.reciprocal(out=r[:, 45:46], in_=r[:, 44:45]), [tr], [tr])
            V(lambda e: e.tensor_scalar(out=r[:, 48:52], in0=r[:, 0:4], scalar1=r[:, 36:37], scalar2=None,
                                        op0=ALU.is_equal), [tr], [tr])
            V(lambda e: e.tensor_scalar(out=r[:, 52:56], in0=r[:, 48:52], scalar1=-1.0, scalar2=BIG,
                                        op0=ALU.add, op1=ALU.mult), [tr], [tr])
            V(lambda e: e.tensor_tensor(out=r[:, 64:96].rearrange("p (g k) -> p g k", g=4),
                                        in0=r[:, 4:36].rearrange("p (g k) -> p g k", g=4),
                                        in1=r[:, 52:56].unsqueeze(2).to_broadcast([128, 4, 8]), op=ALU.add),
              [tr], [tr])
            V(lambda e: e.reduce_max(out=r[:, 96:97], in_=r[:, 64:96], axis=AX.X), [tr], [tr])
            V(lambda e: e.tensor_scalar(out=r[:, 104:136], in0=r[:, 64:96], scalar1=r[:, 96:97], scalar2=None,
                                        op0=ALU.is_equal), [tr], [tr])
            V(lambda e: e.scalar_tensor_tensor(out=r[:, 64:96], in0=r[:, 104:136], scalar=-BIG, in1=r[:, 64:96],
                                               op0=ALU.mult, op1=ALU.add), [tr], [tr])
            V(lambda e: e.reduce_max(out=r[:, 97:98], in_=r[:, 64:96], axis=AX.X), [tr], [tr])
            V(lambda e: e.tensor_tensor(out=r[:, 98:99], in0=r[:, 97:98], in1=r[:, 96:97], op=ALU.subtract),
              [tr], [tr])
            kb.op("scalar", lambda e: e.activation(out=r[:, 99:100], in_=r[:, 98:99], func=AF.Exp),
                  reads=[tr], writes=[tr])
            V(lambda e: e.tensor_scalar_add(out=r[:, 100:101], in0=r[:, 99:100], scalar1=1.0), [tr], [tr])
            V(lambda e: e.reciprocal(out=r[:, 101:102], in_=r[:, 100:101]), [tr], [tr])
            V(lambda e: e.tensor_tensor(out=r[:, 102:103], in0=r[:, 101:102], in1=r[:, 45:46], op=ALU.mult),
              [tr], [tr])
            V(lambda e: e.tensor_tensor(out=r[:, 103:104], in0=r[:, 102:103], in1=r[:, 99:100], op=ALU.mult),
              [tr], [tr])
            V(lambda e, i=i: e.tensor_scalar_mul(out=Wt[:, i, :], in0=r[:, 104:136], scalar1=r[:, 102:103]),
              [tr], [t_Wt[i]])
            V(lambda e: e.tensor_scalar(out=r[:, 104:136], in0=r[:, 64:96], scalar1=r[:, 97:98], scalar2=None,
                                        op0=ALU.is_equal), [tr], [tr])
            V(lambda e, i=i: e.scalar_tensor_tensor(out=Wt[:, i, :], in0=r[:, 104:136], scalar=r[:, 103:104],
                                                    in1=Wt[:, i, :], op0=ALU.mult, op1=ALU.add),
              [tr, t_Wt[i]], [t_Wt[i]])
        steps = [(ex, tl) for ex in range(32) for tl in range(TS // 512)]
        wslot = {}
        gcount = [0]
        dcount = [0]

        def gu_group(si, hc, which):
            ex, tl = steps[si]
            k = wslot[ex]
            tsl = slice(tl * 512, (tl + 1) * 512)
            hsl = slice(hc * 128, (hc + 1) * 128)
            pg = hc
            if which == 0:
                for c in range(8):
                    _mm(kb, bank[pg][:], weg[k][:, c, hsl], h2T[:, c, tsl], c == 0, c == 7,
                        [t_we[k], t_h2T], [t_bank[pg]])
                kb.op("scalar", lambda e: e.activation(out=sgl[pg][:], in_=bank[pg][:], func=AF.Silu),
                      reads=[t_bank[pg]], writes=[t_sgl[pg]])
            else:
                a = si % 2
                for c in range(8):
                    _mm(kb, bank[2 + pg][:], weu[k][:, c, hsl], h2T[:, c, tsl], c == 0, c == 7,
                        [t_we[k], t_h2T], [t_bank[2 + pg]])
                kb.op("vector", lambda e: e.tensor_tensor(out=aT[a][:, hc, :], in0=bank[2 + pg][:], in1=sgl[pg][:],
                                                          op=ALU.mult),
                      reads=[t_bank[2 + pg], t_sgl[pg]], writes=[t_aT[a]])

        def d_group(si, j, half):
            ex, tl = steps[si]
            k = wslot[ex]
            a = si % 2
            ti = tl * 4 + j
            pd = 4 + dcount[0] % 4
            dcount[0] += 1
            osl = slice(half * 512, (half + 1) * 512)
            for hc in range(2):
                _mm(kb, bank[pd][:], aT[a][:, hc, j * 128:(j + 1) * 128], wed[k][:, hc, osl],
                    hc == 0, hc == 1, [t_aT[a], t_we[k]], [t_bank[pd]])
            if ex == 0:
                kb.op("vector", lambda e: e.tensor_scalar_mul(out=yacc[:, ti, osl], in0=bank[pd][:],
                                                              scalar1=Wt[:, ti, ex:ex + 1]),
                      reads=[t_bank[pd], t_Wt[ti]], writes=[t_yacc[ti]])
            else:
                kb.op("vector", lambda e: e.scalar_tensor_tensor(out=yacc[:, ti, osl], in0=bank[pd][:],
                                                                 scalar=Wt[:, ti, ex:ex + 1], in1=yacc[:, ti, osl],
                                                                 op0=ALU.mult, op1=ALU.add),
                      reads=[t_bank[pd], t_Wt[ti], t_yacc[ti]], writes=[t_yacc[ti]])

        dlist = [(j, half) for j in range(4) for half in range(2)]
        for si in range(len(steps) + 1):
            if si < len(steps):
                ex, tl = steps[si]
                if tl == 0:
                    wslot[ex] = load_expert(ex)
            gul = [(0, 0), (0, 1), (1, 0), (1, 1)]
            for gidx in range(4):
                if si < len(steps):
                    gu_group(si, gul[gidx][0], gul[gidx][1])
                if si > 0:
                    for (j, half) in dlist[2 * gidx:2 * gidx + 2]:
                        d_group(si - 1, j, half)
        for i in range(NTL):
            s = i % 2
            t0 = st * TS + i * 128
            kb.dma("sync", xt[s][:], xmid[t0:t0 + 128, :], reads=[t_xmid], writes=[t_xt[s]])
            kb.op("vector", lambda e, i=i: e.tensor_tensor(out=yacc[:, i, :], in0=yacc[:, i, :], in1=gate2[:],
                                                           op=ALU.mult),
                  reads=[t_yacc[i], t_gate2], writes=[t_yacc[i]])
            kb.op("gpsimd", lambda e, s=s, i=i: e.tensor_tensor(out=xt[s][:], in0=xt[s][:], in1=yacc[:, i, :],
                                                                op=ALU.add),
                  reads=[t_xt[s], t_yacc[i]], writes=[t_xt[s]])
            if final_g is None:
                kb.dma("gpsimd", xout[t0:t0 + 128, :], xt[s][:], reads=[t_xt[s]], writes=[t_xout])
            else:
                emit_norm_mod(kb, xt[s], t_xt[s], fg, t_wr, None, None, h2f[s], t_h2f[s],
                              scr[s], t_scr[s], stat[s], t_st[s], consts["eps"][:, 0:1])
                kb.dma("gpsimd", xout[t0:t0 + 128, :], h2f[s][:], reads=[t_h2f[s]], writes=[t_xout])
    kb.release(mk)


LAYER_SHAPES = dict(
    modw=[D, 6144], modb=[6144], g1=[D],
    wqk=[D, 1024], wtm=[D, 2056], cw=[128, 8, 4], cb=[128, 8], ifb=[8],
    wmla=[D, 768], gqkv=[128, 5], wuqn=[384, 1024], wuqr=[384, 512], wuqs=[384, 512],
    wuk=[256, 1024], wuv=[256, 1024],
    wgate=[D, 2048], wbm=[D, D], wba=[D, D], wout=[D, D], g2=[D], wr=[D, 36], rb=[36],
    weg=[32, D, 256], weu=[32, D, 256], wed=[32, 256, D])
DEPTH = 2


def build_full(S):
    kb = KB()
    x = kb.dram_in("x", [S, D], F32)
    cvec = kb.dram_in("cvec", [D], F32)
    cs = kb.dram_in("cs", [64, 2, S], F32)
    fg = kb.dram_in("fg", [D], F32)
    W = []
    for l in range(DEPTH):
        W.append({k: kb.dram_in("%s_%d" % (k, l), v, F32) for k, v in LAYER_SHAPES.items()})
        W[l]["cs"] = cs
    out = kb.dram_out("out", [S, D], F32)
    hT = kb.dram_tmp("s_hT", [8, 128, S], BF16)
    ymT = kb.dram_tmp("s_ymT", [8, 128, S], BF16)
    yaT = kb.dram_tmp("s_yaT", [8, 128, S], BF16)
    xmid = kb.dram_tmp("s_xmid", [S, D], F32)
    xnext = kb.dram_tmp("s_xnext", [S, D], F32)
    scr = alloc_mla_scratch(kb, S)
    t_hT, t_ymT, t_yaT = Tok("dram:hT"), Tok("dram:ymT"), Tok("dram:yaT")
    t_xmid, t_xnext, t_out, t_x = Tok("dram:xmid"), Tok("dram:xnext"), Tok("dram:out"), Tok("dram:x")
    consts = load_consts(kb)
    x_cur, t_xcur = x, t_x
    for l in range(DEPTH):
        w = W[l]
        mk = kb.mark()
        emit_k1_body(kb, consts, x_cur, cvec, w["modw"][:, 0:2048], w["modb"][0:2048], w["g1"], hT, S,
                     hT_tok=t_hT, x_tok=t_xcur)
        kb.release(mk)
        emit_mlstm_pass(kb, consts, S, hT, t_hT, w, ymT, t_ymT)
        emit_mla_proj_pass(kb, consts, S, hT, t_hT, w, scr)
        emit_attn_pass(kb, consts, S, scr, yaT, t_yaT)
        mk = kb.mark()
        mods = [kb.sbuf("mod%d" % i, [128, D], F32) for i in range(4)]
        tm = [Tok("mod%d" % i) for i in range(4)]
        g2t = kb.sbuf("g2t", [128, D], F32)
        tg2 = Tok("g2t")
        emit_mod(kb, consts, w["modw"][:, 2048:6144], w["modb"][2048:6144], cvec, 4, list(zip(mods, tm)))
        kb.dma("sync", g2t[:], w["g2"].partition_broadcast(128), writes=[tg2])
        kb.op("vector", lambda e: e.scalar_tensor_tensor(out=mods[2][:], in0=mods[2][:], scalar=1.0, in1=g2t[:],
                                                         op0=ALU.add, op1=ALU.mult),
              reads=[tm[2], tg2], writes=[tm[2]])
        t_in_all = _MultiTok([t_hT, t_ymT, t_yaT])
        emit_merge_pass(kb, consts, S, hT, ymT, yaT, t_in_all, x_cur, t_xcur, w, mods[0], tm[0], xmid, t_xmid)
        last = (l == DEPTH - 1)
        emit_moe_pass(kb, consts, S, xmid, t_xmid, w, mods[2], tm[2], mods[1], tm[1], mods[3], tm[3],
                      out if last else xnext, t_out if last else t_xnext, final_g=fg if last else None)
        kb.release(mk)
        x_cur, t_xcur = xnext, t_xnext
    kb.finish([t_out])
    return kb


class _MultiTok:
    def __init__(self, toks):
        self.name = "dram:multi"
        self._toks = toks


def _rope_tables(S):
    pos = np.arange(S, dtype=np.float32)
    inv_freq = (1.0 / (np.float32(10000.0) ** (np.arange(0, 64, 2, dtype=np.float32) / np.float32(64)))).astype(np.float32)
    ang = pos[:, None] * inv_freq[None, :]
    cos = np.cos(ang).astype(np.float32)
    sin = np.sin(ang).astype(np.float32)
    CC = np.concatenate([cos, cos], 1).T
    SS = np.concatenate([-sin, sin], 1).T
    return np.ascontiguousarray(np.stack([CC, SS], 1))


def _prep_layer(z, l):
    c = np.ascontiguousarray
    win = z["w_in"][l]
    d = {}
    d["modw"] = c(z["mod_w"][l])
    d["modb"] = c(z["mod_b"][l])
    d["g1"] = c(z["norm1_g"][l])
    d["wqk"] = c(win[:, :1024])
    d["wtm"] = c(win[:, 1024:3080])
    d["cw"] = c(z["conv_w"][l].reshape(4, 8, 128).transpose(2, 1, 0))
    d["cb"] = c(z["conv_b"][l].reshape(8, 128).T)
    d["ifb"] = c(np.concatenate([z["igate_b"][l], z["fgate_b"][l]]))
    kr = win[:, 3720:3784]
    d["wmla"] = c(np.concatenate([win[:, 3080:3784], kr[:, 32:], kr[:, :32]], 1))
    d["gqkv"] = c(np.concatenate([z["q_norm_g"][l].reshape(3, 128).T, z["kv_norm_g"][l].reshape(2, 128).T], 1))
    uq = z["w_uq"][l].reshape(384, 8, 192)
    d["wuqn"] = c(uq[:, :, :128].reshape(384, 1024))
    d["wuqr"] = c(uq[:, :, 128:].reshape(384, 512))
    d["wuqs"] = c(np.concatenate([uq[:, :, 160:], uq[:, :, 128:160]], 2).reshape(384, 512))
    ukv = z["w_ukv"][l].reshape(256, 8, 256)
    d["wuk"] = c(ukv[:, :, :128].reshape(256, 1024))
    d["wuv"] = c(ukv[:, :, 128:].reshape(256, 1024))
    d["wgate"] = c(win[:, 3784:5832])
    d["wbm"] = c(z["w_branch_m"][l])
    d["wba"] = c(z["w_branch_a"][l])
    d["wout"] = c(z["w_out"][l])
    d["g2"] = c(z["norm2_g"][l])
    d["wr"] = c(np.concatenate([z["w_group"][l], z["w_router"][l]], 1))
    d["rb"] = c(np.concatenate([z["b_group"][l], z["b_router"][l]]))
    d["weg"] = c(z["w_expert_gate"][l])
    d["weu"] = c(z["w_expert_up"][l])
    d["wed"] = c(z["w_expert_down"][l])
    return d


_PROGRAMS = {}


def kernel(**inputs):
    z = {k: np.asarray(v) for k, v in inputs.items()}
    x = z["x"].astype(np.float32, copy=False)
    B, S, _ = x.shape
    if S not in _PROGRAMS:
        _PROGRAMS[S] = build_full(S)
    kb = _PROGRAMS[S]
    shared = {"cs": _rope_tables(S), "fg": np.ascontiguousarray(z["final_norm_g"].astype(np.float32))}
    for l in range(DEPTH):
        for k, v in _prep_layer(z, l).items():
            assert list(v.shape) == LAYER_SHAPES[k], (k, v.shape)
            shared["%s_%d" % (k, l)] = v.astype(np.float32, copy=False)
    in_maps = []
    for b in range(B):
        m = dict(shared)
        m["x"] = np.ascontiguousarray(x[b])
        m["cvec"] = np.ascontiguousarray(z["c"][b].astype(np.float32))
        in_maps.append(m)
    res = run_bass_kernel_spmd(kb.nc, in_maps, core_ids=list(range(B)))
    return np.stack([np.asarray(r["out"]) for r in res.results], 0).astype(np.float32)
```

```python
import math
import numpy as np
import ml_dtypes
import concourse.bass as bass
import concourse.mybir as mybir
from concourse.bass_utils import run_bass_kernel_spmd

F32 = mybir.dt.float32
BF16 = mybir.dt.bfloat16
AF = mybir.ActivationFunctionType
ALU = mybir.AluOpType
AX = mybir.AxisListType

D = 1024
NCORES = 8
EPS = 1e-6
BIG = 30000.0


class Tok:
    __slots__ = ("w", "r", "wsem", "rsem", "name")

    def __init__(self, name=""):
        self.w = None
        self.r = []
        self.wsem = None
        self.rsem = None
        self.name = name


class _Sem:
    __slots__ = ("h", "cnt", "key")

    def __init__(self, h, key):
        self.h = h
        self.cnt = 0
        self.key = key


class _Eng:
    def __init__(self, name, e, sem):
        self.name = name
        self.e = e
        self.sem = sem
        self.seen = {}


class KB:
    def __init__(self):
        self.nc = bass.Bass("TRN2", target_bir_lowering=False)
        nc = self.nc
        self._nsem = 0
        self.eng = {}
        for name in ("tensor", "vector", "scalar", "gpsimd", "sync"):
            self.eng[name] = _Eng(name, getattr(nc, name), self._newsem("e_" + name))
        self._stack = []
        self._dsems = []
        self._free_dsems = []
        self._live_dsems = []
        self.ninst = 0

    def _newsem(self, name):
        if not name.startswith("e_") and self._free_dsems:
            sem = self._free_dsems.pop()
            self._live_dsems.append(sem)
            return sem
        self._nsem += 1
        h = self.nc.semaphore(name + "_%d" % self._nsem).__enter__()
        sem = _Sem(h, self._nsem)
        if not name.startswith("e_"):
            self._dsems.append(sem)
            self._live_dsems.append(sem)
        return sem

    def dram_in(self, name, shape, dt):
        return self.nc.dram_tensor(name, list(shape), dt, kind="ExternalInput").ap()

    def dram_out(self, name, shape, dt):
        return self.nc.dram_tensor(name, list(shape), dt, kind="ExternalOutput").ap()

    def dram_tmp(self, name, shape, dt):
        return self.nc.dram_tensor(name, list(shape), dt, kind="Internal").ap()

    def sbuf(self, name, shape, dt):
        self._uid = getattr(self, "_uid", 0) + 1
        cm = self.nc.sbuf_tensor("%s_u%d" % (name, self._uid), list(shape), dt)
        t = cm.__enter__()
        self._stack.append(cm)
        return t

    def psum(self, name, shape, dt):
        self._uid = getattr(self, "_uid", 0) + 1
        cm = self.nc.psum_tensor("%s_u%d" % (name, self._uid), list(shape), dt)
        t = cm.__enter__()
        self._stack.append(cm)
        return t

    def mark(self):
        return (len(self._stack), len(self._live_dsems))

    def release(self, mark):
        self.barrier()
        while len(self._stack) > mark[0]:
            self._stack.pop().__exit__(None, None, None)
        while len(self._live_dsems) > mark[1]:
            self._free_dsems.append(self._live_dsems.pop())

    def barrier(self):
        sems = [E.sem for E in self.eng.values()] + self._dsems
        for E in self.eng.values():
            for sem in sems:
                if sem is E.sem or sem.cnt == 0:
                    continue
                if E.seen.get(sem.key, 0) >= sem.cnt:
                    continue
                E.e.wait_ge(sem.h, sem.cnt)
                E.seen[sem.key] = sem.cnt
                self.ninst += 1

    def _wait(self, E, evs):
        need = {}
        for ev in evs:
            if ev is None:
                continue
            sem, val = ev
            if sem is E.sem and E.name == "tensor":
                continue
            if need.get(sem.key, (None, 0))[1] < val:
                need[sem.key] = (sem, val)
        for key, (sem, val) in need.items():
            if E.seen.get(key, 0) >= val:
                continue
            assert val <= sem.cnt, "waiting on an event that is never signalled"
            E.e.wait_ge(sem.h, val)
            E.seen[key] = val
            self.ninst += 1

    def _deps(self, reads, writes):
        evs = []
        for t in reads:
            if hasattr(t, "_toks"):
                evs.extend(x.w for x in t._toks)
            else:
                evs.append(t.w)
        for t in writes:
            evs.append(t.w)
            evs.extend(t.r)
        return evs

    def op(self, engname, fn, reads=(), writes=(), sig=True):
        E = self.eng[engname]
        self._wait(E, self._deps(reads, writes))
        ins = fn(E.e)
        self.ninst += 1
        if sig:
            E.sem.cnt += 1
            ins.then_inc(E.sem.h, 1)
            ev = (E.sem, E.sem.cnt)
        else:
            assert engname == "tensor"
            ev = (E.sem, E.sem.cnt + 1)
        for t in reads:
            for tt_ in (t._toks if hasattr(t, "_toks") else (t,)):
                tt_.r.append(ev)
                if len(tt_.r) > 24:
                    tt_.r = self._compact(tt_.r)
        for t in writes:
            t.w = ev
            t.r = []
        return ins

    @staticmethod
    def _compact(r):
        best = {}
        for sem, val in r:
            if best.get(sem.key, (None, 0))[1] < val:
                best[sem.key] = (sem, val)
        return list(best.values())

    def dma(self, queue, out, in_, reads=(), writes=(), **kw):
        E = self.eng[queue]
        evs = self._deps(reads, [])
        for t in writes:
            isdram = getattr(t, "name", "").startswith("dram:")
            if t.w is not None and not isdram and not (t.wsem is not None and t.w[0] is t.wsem):
                evs.append(t.w)
            evs.extend(t.r)
        self._wait(E, evs)
        ins = E.e.dma_start(out=out, in_=in_, **kw)
        self.ninst += 1
        if writes and writes[0].wsem is None and not getattr(writes[0], "_dram", False):
            pass
        tok = None
        for t in writes:
            if not getattr(t, "name", "").startswith("dram:"):
                tok = t
                if tok.wsem is None:
                    tok.wsem = self._newsem("dw")
                sem = tok.wsem
                break
        if tok is None:
            for t in reads:
                if not getattr(t, "name", "").startswith("dram:"):
                    tok = t
                    if tok.rsem is None:
                        tok.rsem = self._newsem("dr")
                    sem = tok.rsem
                    break
        assert tok is not None, "dma needs an sbuf-side token"
        sem.cnt += 16
        ins.then_inc(sem.h, 16)
        ev = (sem, sem.cnt)
        for t in reads:
            for tt_ in (t._toks if hasattr(t, "_toks") else (t,)):
                tt_.r.append(ev)
                if len(tt_.r) > 24:
                    tt_.r = self._compact(tt_.r)
        for t in writes:
            t.w = ev
            t.r = []
        return ins

    def finish(self, toks):
        E = self.eng["sync"]
        evs = []
        for t in toks:
            evs.append(t.w)
            evs.extend(t.r)
        self._wait(E, evs)


def load_consts(kb):
    nc = kb.nc
    c = {}
    c["tok"] = Tok("consts")
    ones_f = kb.sbuf("c_ones_f", [128, 128], F32)
    ones_b = kb.sbuf("c_ones_b", [128, 128], BF16)
    id_f = kb.sbuf("c_id_f", [128, 128], F32)
    id_b = kb.sbuf("c_id_b", [128, 128], BF16)
    tri_f = kb.sbuf("c_tri_f", [128, 128], F32)
    tris_f = kb.sbuf("c_tris_f", [128, 128], F32)
    tri_b = kb.sbuf("c_tri_b", [128, 128], BF16)
    t = c["tok"]
    kb.op("gpsimd", lambda e: e.memset(ones_f[:], 1.0), writes=[t])
    kb.op("gpsimd", lambda e: e.memset(ones_b[:], 1.0), writes=[t])
    kb.op("gpsimd", lambda e: e.affine_select(out=id_f[:], in_=ones_f[:], pattern=[[-1, 128]],
                                             compare_op=ALU.is_equal, fill=0.0, base=0,
                                             channel_multiplier=1), reads=[t], writes=[t])
    kb.op("gpsimd", lambda e: e.tensor_copy(out=id_b[:], in_=id_f[:]), reads=[t], writes=[t])
    kb.op("gpsimd", lambda e: e.affine_select(out=tri_f[:], in_=ones_f[:], pattern=[[1, 128]],
                                             compare_op=ALU.is_ge, fill=0.0, base=0,
                                             channel_multiplier=-1), reads=[t], writes=[t])
    kb.op("gpsimd", lambda e: e.tensor_copy(out=tri_b[:], in_=tri_f[:]), reads=[t], writes=[t])
    kb.op("gpsimd", lambda e: e.affine_select(out=tris_f[:], in_=ones_f[:], pattern=[[-1, 128]],
                                             compare_op=ALU.is_gt, fill=0.0, base=0,
                                             channel_multiplier=1), reads=[t], writes=[t])
    eps = kb.sbuf("c_eps", [128, 1], F32)
    kb.op("gpsimd", lambda e: e.memset(eps[:], EPS), writes=[t])
    c["eps"] = eps
    one = kb.sbuf("c_one", [128, 1], F32)
    kb.op("gpsimd", lambda e: e.memset(one[:], 1.0), writes=[t])
    c["one"] = one
    ones_b512 = kb.sbuf("c_ones_b512", [128, 512], BF16)
    kb.op("gpsimd", lambda e: e.memset(ones_b512[:], 1.0), writes=[t])
    c["ones_b512"] = ones_b512
    c.update(ones_f=ones_f, ones_b=ones_b, id_f=id_f, id_b=id_b, tri_f=tri_f, tris_f=tris_f,
             tri_b=tri_b)
    return c


def emit_mod(kb, consts, modw, modb, cvec, nvec, outs, ps_tag="modps"):
    nc = kb.nc
    mk = kb.mark()
    cpc = kb.sbuf("mod_cpc", [128, 8], F32)
    cond = kb.sbuf("mod_cond", [128, 8], F32)
    bc = kb.sbuf("mod_bc", [128, 8, 128], F32)
    wt = kb.sbuf("mod_w", [128, 8, 1024], F32)
    bt = kb.sbuf("mod_b", [128, 1024], F32)
    ps = kb.psum(ps_tag, [128, 512], F32)
    t_c, t_cond, t_bc, t_w, t_b, t_ps = (Tok("m%d" % i) for i in range(6))
    kb.dma("sync", cpc[:], cvec.rearrange("(p c) -> p c", c=8), writes=[t_c])
    kb.op("scalar", lambda e: e.activation(out=cond[:], in_=cpc[:], func=AF.Silu),
          reads=[t_c], writes=[t_cond])
    for c in range(8):
        kb.op("vector", lambda e, c=c: e.tensor_scalar_mul(out=bc[:, c, :], in0=consts["ones_f"][:],
                                                           scalar1=cond[:, c:c + 1]),
              reads=[t_cond, consts["tok"]], writes=[t_bc])
    for v in range(nvec):
        out_t, out_tok = outs[v]
        kb.dma("sync", wt[:], modw[:, v * 1024:(v + 1) * 1024].rearrange("(p c) n -> p c n", c=8),
               writes=[t_w])
        kb.dma("sync", bt[:], modb[v * 1024:(v + 1) * 1024].partition_broadcast(128), writes=[t_b])
        for n2 in range(2):
            for c in range(8):
                kb.op("tensor", lambda e, c=c, n2=n2: e.matmul(ps[:], lhsT=bc[:, c, :],
                                                               rhs=wt[:, c, n2 * 512:(n2 + 1) * 512],
                                                               start=(c == 0), stop=(c == 7)),
                      reads=[t_bc, t_w], writes=[t_ps], sig=(c == 7))
            kb.op("vector", lambda e, n2=n2: e.tensor_tensor(out=out_t[:, n2 * 512:(n2 + 1) * 512],
                                                             in0=ps[:], in1=bt[:, n2 * 512:(n2 + 1) * 512],
                                                             op=ALU.add),
                  reads=[t_ps, t_b], writes=[out_tok])
    kb.release(mk)


def emit_norm_mod(kb, xt, t_x, gmul, t_g, shift, t_s, hb, t_h, scr, t_scr, stat, t_stat, eps_ap):
    kb.op("gpsimd", lambda e: e.memset(stat[:, 0:1], 0.0), writes=[t_stat])
    kb.op("scalar", lambda e: e.activation(out=scr[:], in_=xt[:], func=AF.Square,
                                           accum_out=stat[:, 0:1]),
          reads=[t_x], writes=[t_scr, t_stat])
    kb.op("scalar", lambda e: e.activation(out=stat[:, 1:2], in_=stat[:, 0:1], func=AF.Sqrt,
                                           bias=eps_ap, scale=1.0 / D),
          reads=[t_stat], writes=[t_stat])
    kb.op("vector", lambda e: e.reciprocal(out=stat[:, 2:3], in_=stat[:, 1:2]),
          reads=[t_stat], writes=[t_stat])
    if shift is None:
        kb.op("vector", lambda e: e.scalar_tensor_tensor(out=hb[:], in0=xt[:], scalar=stat[:, 2:3],
                                                         in1=gmul[:], op0=ALU.mult, op1=ALU.mult),
              reads=[t_x, t_stat, t_g], writes=[t_h])
        return
    kb.op("vector", lambda e: e.scalar_tensor_tensor(out=scr[:], in0=xt[:], scalar=stat[:, 2:3],
                                                     in1=gmul[:], op0=ALU.mult, op1=ALU.mult),
          reads=[t_x, t_stat, t_g], writes=[t_scr])
    kb.op("gpsimd", lambda e: e.tensor_tensor(out=hb[:], in0=scr[:], in1=shift[:], op=ALU.add),
          reads=[t_scr, t_s], writes=[t_h])


def build_k1(Sh):
    kb = KB()
    nc = kb.nc
    x = kb.dram_in("x", [Sh, D], F32)
    cvec = kb.dram_in("cvec", [D], F32)
    modw = kb.dram_in("modw", [D, 2 * D], F32)
    modb = kb.dram_in("modb", [2 * D], F32)
    g = kb.dram_in("g", [D], F32)
    hT = kb.dram_out("hT", [8, 128, Sh], BF16)
    consts = load_consts(kb)
    emit_k1_body(kb, consts, x, cvec, modw, modb, g, hT, Sh)
    kb.finish([kb._out_tok])
    return kb


def emit_k1_body(kb, consts, x, cvec, modw, modb, g, hT, Sh, hT_tok=None, x_tok=None):
    shift = kb.sbuf("k1_shift", [128, D], F32)
    gmul = kb.sbuf("k1_gmul", [128, D], F32)
    gt = kb.sbuf("k1_g", [128, D], F32)
    t_shift, t_gmul, t_g = Tok("shift"), Tok("gmul"), Tok("g")
    emit_mod(kb, consts, modw, modb, cvec, 2, [(shift, t_shift), (gmul, t_gmul)])
    kb.dma("sync", gt[:], g.partition_broadcast(128), writes=[t_g])
    kb.op("vector", lambda e: e.scalar_tensor_tensor(out=gmul[:], in0=gmul[:], scalar=1.0, in1=gt[:],
                                                     op0=ALU.add, op1=ALU.mult),
          reads=[t_gmul, t_g], writes=[t_gmul])
    NB = 2
    xt = [kb.sbuf("k1_x%d" % i, [128, D], F32) for i in range(NB)]
    scr = [kb.sbuf("k1_scr%d" % i, [128, D], F32) for i in range(NB)]
    hb = [kb.sbuf("k1_hb%d" % i, [128, D], BF16) for i in range(NB)]
    stat = [kb.sbuf("k1_st%d" % i, [128, 4], F32) for i in range(NB)]
    hTs = [kb.sbuf("k1_hT%d" % i, [128, 8, 512], BF16) for i in range(2)]
    pst = [kb.psum("k1_pst%d" % i, [128, 8, 128], BF16) for i in range(2)]
    t_x = [Tok("x%d" % i) for i in range(NB)]
    t_scr = [Tok("scr%d" % i) for i in range(NB)]
    t_hb = [Tok("hb%d" % i) for i in range(NB)]
    t_st = [Tok("st%d" % i) for i in range(NB)]
    t_hT = [Tok("hT%d" % i) for i in range(2)]
    t_ps = [Tok("ps%d" % i) for i in range(2)]
    t_out = hT_tok if hT_tok is not None else Tok("dram:hT")
    kb._out_tok = t_out
    ntile = Sh // 128
    for i in range(ntile):
        s = i % NB
        grp = (i // 4) % 2
        kb.dma("sync", xt[s][:], x[i * 128:(i + 1) * 128, :], reads=([x_tok] if x_tok is not None else []), writes=[t_x[s]])
        emit_norm_mod(kb, xt[s], t_x[s], gmul, t_gmul, shift, t_shift, hb[s], t_hb[s],
                      scr[s], t_scr[s], stat[s], t_st[s], consts["eps"][:, 0:1])
        p = i % 2
        for c in range(8):
            kb.op("tensor", lambda e, c=c, s=s, p=p: e.transpose(out=pst[p][:, c, :],
                                                                 in_=hb[s][:, c * 128:(c + 1) * 128],
                                                                 identity=consts["id_b"][:]),
                  reads=[t_hb[s], consts["tok"]], writes=[t_ps[p]], sig=(c == 7))
        j = i % 4
        kb.op("scalar", lambda e, p=p, grp=grp, j=j: e.copy(out=hTs[grp][:, :, j * 128:(j + 1) * 128],
                                                            in_=pst[p][:]),
              reads=[t_ps[p]], writes=[t_hT[grp]])
        if j == 3:
            t0 = (i - 3) * 128
            kb.dma("gpsimd", hT[:, :, t0:t0 + 512].rearrange("c p t -> p c t"), hTs[grp][:],
                   reads=[t_hT[grp]], writes=[t_out])


def _mm(kb, out, lhsT, rhs, start, stop, reads, writes):
    return kb.op("tensor", lambda e: e.matmul(out, lhsT=lhsT, rhs=rhs, start=start, stop=stop),
                 reads=reads, writes=writes, sig=stop)


def load_w_bf16(kb, dst, src_rows_cols, tok, nchunk, queue="gpsimd"):
    for c in range(nchunk):
        kb.dma(queue, dst[:, c, :], src_rows_cols[c * 128:(c + 1) * 128, :], writes=[tok])


def emit_mlstm_pass(kb, consts, S, hT, t_hT, w, ymT, t_ymT):
    NT = S // 512
    mk = kb.mark()
    ct = consts["tok"]
    wqk = kb.sbuf("a_wqk", [128, 8, 1024], BF16)
    wtm = kb.sbuf("a_wtm", [128, 8, 2056], BF16)
    cw = kb.sbuf("a_cw", [128, 8, 4], F32)
    cb = kb.sbuf("a_cb", [128, 8], F32)
    ifb = kb.sbuf("a_ifb", [128, 8], F32)
    t_w = Tok("a_w")
    t_wt = Tok("a_wt")
    t_cw = Tok("a_cw")
    load_w_bf16(kb, wqk, w["wqk"], t_w, 8)
    load_w_bf16(kb, wtm, w["wtm"], t_wt, 8)
    kb.dma("sync", cw[:], w["cw"], writes=[t_cw])
    kb.dma("sync", cb[:], w["cb"], writes=[t_cw])
    kb.dma("sync", ifb[:], w["ifb"].partition_broadcast(128), writes=[t_cw])

    hTs = [kb.sbuf("a_hT%d" % i, [128, 8, 512], BF16) for i in range(2)]
    t_hTs = [Tok("a_hT%d" % i) for i in range(2)]
    pre = kb.sbuf("a_pre", [128, 8, 515], F32)
    t_pre = [Tok("a_pre%d" % g) for g in range(8)]
    cacc = [kb.sbuf("a_cacc%d" % i, [128, 512], F32) for i in range(2)]
    t_cacc = [Tok("a_cacc%d" % i) for i in range(2)]
    qkT = kb.sbuf("a_qkT", [128, 8, 512], BF16)
    t_qk = [Tok("a_qk%d" % g) for g in range(8)]
    Vp = [kb.sbuf("a_vp%d" % i, [128, 4, 257], BF16) for i in range(2)]
    t_vp = [Tok("a_vp%d" % i) for i in range(2)]
    sgo = [kb.sbuf("a_sgo%d" % i, [128, 1024], F32) for i in range(2)]
    t_sgo = [Tok("a_sgo%d" % i) for i in range(2)]
    gsb = [kb.sbuf("a_g%d" % i, [128, 48], F32) for i in range(2)]
    t_g = [Tok("a_g%d" % i) for i in range(2)]
    Zf = kb.sbuf("a_zf", [128, 4, 257], F32)
    Zb = kb.sbuf("a_zb", [128, 4, 257], BF16)
    t_zf = [Tok("a_zf%d" % h) for h in range(4)]
    t_zb = [Tok("a_zb%d" % h) for h in range(4)]
    s0sb = [kb.sbuf("a_s0%d" % i, [128, 128], BF16) for i in range(2)]
    t_s0 = [Tok("a_s0%d" % i) for i in range(2)]
    ytok = [kb.sbuf("a_yt%d" % i, [128, 256], BF16) for i in range(2)]
    t_yt = [Tok("a_yt%d" % i) for i in range(2)]
    khat = [kb.sbuf("a_kh%d" % i, [128, 128], BF16) for i in range(2)]
    t_kh = [Tok("a_kh%d" % i) for i in range(2)]
    dsm = [kb.sbuf("a_d%d" % i, [128, 8], F32) for i in range(2)]
    t_d = [Tok("a_d%d" % i) for i in range(2)]
    yst = [kb.sbuf("a_yst%d" % i, [128, 8, 512], BF16) for i in range(2)]
    t_yst = [Tok("a_yst%d" % i) for i in range(2)]

    ps_fm = [kb.psum("a_psfm%d" % i, [128, 512], F32) for i in range(2)]
    t_psfm = [Tok("a_psfm%d" % i) for i in range(2)]
    ps_tm = [kb.psum("a_pstm%d" % i, [128, 512], F32) for i in range(2)]
    t_pstm = [Tok("a_pstm%d" % i) for i in range(2)]
    ps_m = kb.psum("a_psm", [128, 512], F32)
    t_ps_s0, t_ps_o, t_ps_if, t_ps_cs = Tok("ps_s0"), Tok("ps_o"), Tok("ps_if"), Tok("ps_cs")
    ps_u = kb.psum("a_psu", [128, 257], F32)
    t_ps_u = Tok("ps_u")
    ps_t = kb.psum("a_pst", [128, 3, 128], BF16)
    t_ps_yt, t_ps_kt = Tok("ps_yt"), Tok("ps_kt")

    kb.op("gpsimd", lambda e: e.memset(pre[:, :, 0:3], 0.0), writes=t_pre)
    kb.op("gpsimd", lambda e: e.memset(Zf[:], 0.0), writes=t_zf)
    kb.op("gpsimd", lambda e: e.memset(Zb[:], 0.0), writes=t_zb)
    for i in range(2):
        kb.op("gpsimd", lambda e, i=i: e.memset(Vp[i][:, :, 256:257], 1.0), writes=[t_vp[i]])

    fmi = 0
    tmi = 0
    hi = 0
    for tt in range(NT):
        s = tt % 2
        kb.dma("sync", hTs[s][:], hT[:, :, tt * 512:(tt + 1) * 512].rearrange("c p t -> p c t"),
               reads=[t_hT], writes=[t_hTs[s]])
        for g in range(8):
            p = fmi % 2
            fmi += 1
            for c in range(8):
                _mm(kb, ps_fm[p][:], wqk[:, c, g * 128:(g + 1) * 128], hTs[s][:, c, :], c == 0, c == 7,
                    [t_w, t_hTs[s]], [t_psfm[p]])
            kb.op("scalar", lambda e, p=p, g=g: e.copy(out=pre[:, g, 3:515], in_=ps_fm[p][:]),
                  reads=[t_psfm[p]], writes=[t_pre[g]])
            a = g % 2
            kb.op("vector", lambda e, a=a, g=g: e.tensor_scalar_mul(out=cacc[a][:], in0=pre[:, g, 0:512],
                                                                    scalar1=cw[:, g, 0:1]),
                  reads=[t_pre[g], t_cw], writes=[t_cacc[a]])
            for j in range(1, 4):
                kb.op("vector", lambda e, a=a, g=g, j=j: e.scalar_tensor_tensor(
                    out=cacc[a][:], in0=pre[:, g, j:j + 512], scalar=cw[:, g, j:j + 1], in1=cacc[a][:],
                    op0=ALU.mult, op1=ALU.add), reads=[t_pre[g], t_cw, t_cacc[a]], writes=[t_cacc[a]])
            kb.op("scalar", lambda e, a=a, g=g: e.activation(out=qkT[:, g, :], in_=cacc[a][:], func=AF.Silu,
                                                             bias=cb[:, g:g + 1]),
                  reads=[t_cacc[a], t_cw], writes=[t_qk[g]])
            kb.op("gpsimd", lambda e, g=g: e.tensor_copy(out=pre[:, g, 0:3], in_=pre[:, g, 512:515]),
                  reads=[t_pre[g]], writes=[t_pre[g]])
        for j in range(4):
            vs = (tt * 4 + j) % 2
            tsl = slice(j * 128, (j + 1) * 128)
            for half in range(2):
                p = tmi % 2
                tmi += 1
                for c in range(8):
                    _mm(kb, ps_tm[p][:], hTs[s][:, c, tsl], wtm[:, c, half * 512:(half + 1) * 512],
                        c == 0, c == 7, [t_wt, t_hTs[s]], [t_pstm[p]])
                kb.op("scalar", lambda e, p=p, vs=vs, half=half: e.copy(
                    out=Vp[vs][:, 2 * half:2 * half + 2, 0:256],
                    in_=ps_tm[p][:].rearrange("p (h v) -> p h v", h=2)),
                    reads=[t_pstm[p]], writes=[t_vp[vs]])
            for half in range(2):
                p = tmi % 2
                tmi += 1
                for c in range(8):
                    _mm(kb, ps_tm[p][:], hTs[s][:, c, tsl], wtm[:, c, 1024 + half * 512:1024 + (half + 1) * 512],
                        c == 0, c == 7, [t_wt, t_hTs[s]], [t_pstm[p]])
                kb.op("scalar", lambda e, p=p, vs=vs, half=half: e.activation(
                    out=sgo[vs][:, half * 512:(half + 1) * 512], in_=ps_tm[p][:], func=AF.Sigmoid),
                    reads=[t_pstm[p]], writes=[t_sgo[vs]])
            for c in range(8):
                _mm(kb, ps_m[:, 385:393], hTs[s][:, c, tsl], wtm[:, c, 2048:2056], c == 0, c == 7,
                    [t_wt, t_hTs[s]], [t_ps_if])
            G = gsb[vs]
            tg = t_g[vs]
            kb.op("vector", lambda e, G=G: e.tensor_tensor(out=G[:, 0:8], in0=ps_m[:, 385:393], in1=ifb[:],
                                                           op=ALU.add),
                  reads=[t_ps_if, t_cw], writes=[tg])
            kb.op("scalar", lambda e, G=G: e.activation(out=G[:, 8:12], in_=G[:, 4:8], func=AF.Exp, scale=-1.0),
                  reads=[tg], writes=[tg])
            kb.op("scalar", lambda e, G=G: e.activation(out=G[:, 8:12], in_=G[:, 8:12], func=AF.Ln,
                                                        bias=consts["one"][:, 0:1]),
                  reads=[tg, ct], writes=[tg])
            kb.op("vector", lambda e, G=G: e.tensor_scalar_mul(out=G[:, 8:12], in0=G[:, 8:12], scalar1=-1.0),
                  reads=[tg], writes=[tg])
            _mm(kb, ps_m[:, 393:397], consts["tri_f"][:], G[:, 8:12], True, True, [ct, tg], [t_ps_cs])
            _mm(kb, ps_m[:, 397:401], consts["tris_f"][:], G[:, 8:12], True, True, [ct, tg], [t_ps_cs])
            _mm(kb, ps_m[:, 401:405], consts["ones_f"][:], G[:, 8:12], True, True, [ct, tg], [t_ps_cs])
            kb.op("vector", lambda e, G=G: e.tensor_copy(out=G[:, 16:20], in_=ps_m[:, 393:397]),
                  reads=[t_ps_cs], writes=[tg])
            kb.op("vector", lambda e, G=G: e.tensor_tensor(out=G[:, 20:24], in0=G[:, 0:4], in1=ps_m[:, 393:397],
                                                           op=ALU.subtract),
                  reads=[t_ps_cs, tg], writes=[tg])
            kb.op("vector", lambda e, G=G: e.tensor_tensor(out=G[:, 24:28], in0=G[:, 0:4], in1=ps_m[:, 397:401],
                                                           op=ALU.add),
                  reads=[t_ps_cs, tg], writes=[tg])
            kb.op("vector", lambda e, G=G: e.tensor_copy(out=G[:, 28:32], in_=ps_m[:, 401:405]),
                  reads=[t_ps_cs], writes=[tg])
            kb.op("scalar", lambda e, G=G: e.activation(out=G[:, 32:48], in_=G[:, 16:32], func=AF.Exp),
                  reads=[tg], writes=[tg])
            kb.op("vector", lambda e, G=G: e.tensor_scalar_mul(out=G[:, 12:16], in0=G[:, 32:36],
                                                               scalar1=128.0 ** -0.5),
                  reads=[tg], writes=[tg])
            for h in range(4):
                q = hi % 2
                hi += 1
                qT = qkT[:, h, tsl]
                kT = qkT[:, 4 + h, tsl]
                _mm(kb, ps_m[:, 0:128], kT, qT, True, True, [t_qk[h], t_qk[4 + h]], [t_ps_s0])
                kb.op("vector", lambda e, q=q, G=G, h=h: e.scalar_tensor_tensor(
                    out=s0sb[q][:], in0=ps_m[:, 0:128], scalar=G[:, 36 + h:37 + h], in1=consts["tri_f"][:],
                    op0=ALU.mult, op1=ALU.mult), reads=[t_ps_s0, tg, ct], writes=[t_s0[q]])
                _mm(kb, ps_m[:, 128:385], s0sb[q][:], Vp[vs][:, h, :], True, False, [t_s0[q], t_vp[vs]], [t_ps_o])
                _mm(kb, ps_m[:, 128:385], qT, Zb[:, h, :], False, True, [t_qk[h], t_zb[h]], [t_ps_o])
                dd = dsm[q]
                td = t_d[q]
                kb.op("vector", lambda e, dd=dd, G=G, h=h: e.tensor_tensor(
                    out=dd[:, 0:1], in0=ps_m[:, 384:385], in1=G[:, 12 + h:13 + h], op=ALU.mult),
                    reads=[t_ps_o, tg], writes=[td])
                kb.op("vector", lambda e, dd=dd: e.tensor_scalar(out=dd[:, 1:2], in0=dd[:, 0:1], scalar1=-1.0,
                                                                 scalar2=1.0, op0=ALU.mult, op1=ALU.max),
                      reads=[td], writes=[td])
                kb.op("vector", lambda e, dd=dd: e.tensor_tensor(out=dd[:, 2:3], in0=dd[:, 1:2], in1=dd[:, 0:1],
                                                                 op=ALU.max), reads=[td], writes=[td])
                kb.op("vector", lambda e, dd=dd: e.reciprocal(out=dd[:, 3:4], in_=dd[:, 2:3]),
                      reads=[td], writes=[td])
                kb.op("vector", lambda e, dd=dd, G=G, h=h: e.tensor_tensor(
                    out=dd[:, 4:5], in0=dd[:, 3:4], in1=G[:, 12 + h:13 + h], op=ALU.mult),
                    reads=[td, tg], writes=[td])
                kb.op("vector", lambda e, q=q, dd=dd, vs=vs, h=h: e.scalar_tensor_tensor(
                    out=ytok[q][:], in0=ps_m[:, 128:384], scalar=dd[:, 4:5],
                    in1=sgo[vs][:, h * 256:(h + 1) * 256], op0=ALU.mult, op1=ALU.mult),
                    reads=[t_ps_o, td, t_sgo[vs]], writes=[t_yt[q]])
                for k in range(2):
                    kb.op("tensor", lambda e, q=q, k=k: e.transpose(out=ps_t[:, k, :],
                                                                    in_=ytok[q][:, k * 128:(k + 1) * 128],
                                                                    identity=consts["id_b"][:]),
                          reads=[t_yt[q], ct], writes=[t_ps_yt], sig=(k == 1))
                kb.op("scalar", lambda e, s=s, h=h, tsl=tsl: e.copy(out=yst[s][:, 2 * h:2 * h + 2, tsl],
                                                                    in_=ps_t[:, 0:2, :]),
                      reads=[t_ps_yt], writes=[t_yst[s]])
                kb.op("tensor", lambda e, kT=kT: e.transpose(out=ps_t[:, 2, :], in_=kT, identity=consts["id_b"][:]),
                      reads=[t_qk[4 + h], ct], writes=[t_ps_kt])
                kb.op("scalar", lambda e, q=q, G=G, h=h: e.activation(out=khat[q][:], in_=ps_t[:, 2, :],
                                                                      func=AF.Copy, scale=G[:, 40 + h:41 + h]),
                      reads=[t_ps_kt, tg], writes=[t_kh[q]])
                _mm(kb, ps_u[:], khat[q][:], Vp[vs][:, h, :], True, True, [t_kh[q], t_vp[vs]], [t_ps_u])
                kb.op("vector", lambda e, G=G, h=h: e.scalar_tensor_tensor(
                    out=Zf[:, h, :], in0=Zf[:, h, :], scalar=G[:, 44 + h:45 + h], in1=ps_u[:],
                    op0=ALU.mult, op1=ALU.add), reads=[t_zf[h], tg, t_ps_u], writes=[t_zf[h]])
                kb.op("gpsimd", lambda e, h=h: e.tensor_copy(out=Zb[:, h, :], in_=Zf[:, h, :]),
                      reads=[t_zf[h]], writes=[t_zb[h]])
        kb.dma("gpsimd", ymT[:, :, tt * 512:(tt + 1) * 512].rearrange("c p t -> p c t"), yst[s][:],
               reads=[t_yst[s]], writes=[t_ymT])
    kb.release(mk)


def emit_mla_proj_pass(kb, consts, S, hT, t_hT, w, scr):
    NT = S // 512
    mk = kb.mark()
    ct = consts["tok"]
    wmla = kb.sbuf("b_wmla", [128, 8, 768], BF16)
    wuqn = kb.sbuf("b_wuqn", [128, 3, 1024], BF16)
    wuqr = kb.sbuf("b_wuqr", [128, 3, 512], BF16)
    wuqs = kb.sbuf("b_wuqs", [128, 3, 512], BF16)
    wuk = kb.sbuf("b_wuk", [128, 2, 1024], BF16)
    wuv = kb.sbuf("b_wuv", [128, 2, 1024], BF16)
    gq = kb.sbuf("b_gq", [128, 5], F32)
    t_w = Tok("b_w")
    load_w_bf16(kb, wmla, w["wmla"], t_w, 8)
    load_w_bf16(kb, wuqn, w["wuqn"], t_w, 3)
    load_w_bf16(kb, wuqr, w["wuqr"], t_w, 3)
    load_w_bf16(kb, wuqs, w["wuqs"], t_w, 3)
    load_w_bf16(kb, wuk, w["wuk"], t_w, 2)
    load_w_bf16(kb, wuv, w["wuv"], t_w, 2)
    kb.dma("sync", gq[:], w["gqkv"], writes=[t_w])
    epsq = consts["eps"]

    hTs = [kb.sbuf("b_hT%d" % i, [128, 8, 512], BF16) for i in range(2)]
    t_hTs = [Tok("b_hT%d" % i) for i in range(2)]
    cs = [kb.sbuf("b_cs%d" % i, [64, 2, 512], F32) for i in range(2)]
    t_cs = [Tok("b_cs%d" % i) for i in range(2)]
    raw = kb.sbuf("b_raw", [128, 5, 512], F32)
    t_raw = [Tok("b_raw%d" % g) for g in range(5)]
    sq = kb.sbuf("b_sq", [128, 5, 512], BF16)
    t_sq = [Tok("b_sq%d" % g) for g in range(5)]
    rs = kb.sbuf("b_rs", [128, 2, 512], F32)
    t_rs = [Tok("b_rs%d" % g) for g in range(2)]
    cn = kb.sbuf("b_cn", [128, 5, 512], BF16)
    t_cn = [Tok("b_cn%d" % g) for g in range(5)]
    rt = [kb.sbuf("b_rt%d" % i, [64, 2, 512], F32) for i in range(2)]
    t_rt = [Tok("b_rt%d" % i) for i in range(2)]
    krs = kb.sbuf("b_krs", [64, 512], BF16)
    t_krs = Tok("b_krs")
    qn_st = kb.sbuf("b_qn", [128, 8, 512], BF16)
    qr_st = kb.sbuf("b_qr", [64, 8, 512], BF16)
    kn_st = kb.sbuf("b_kn", [128, 8, 512], BF16)
    t_qn, t_qr, t_kn = Tok("b_qn"), Tok("b_qr"), Tok("b_kn")
    v_st = [kb.sbuf("b_v%d" % i, [128, 1024], BF16) for i in range(2)]
    t_v = [Tok("b_v%d" % i) for i in range(2)]

    ps_fm = [kb.psum("b_psfm%d" % i, [128, 512], F32) for i in range(2)]
    t_psfm = [Tok("b_psfm%d" % i) for i in range(2)]
    ps_ss = [kb.psum("b_psss%d" % i, [128, 512], F32) for i in range(2)]
    t_psss = [Tok("b_psss%d" % i) for i in range(2)]
    ps_r = [kb.psum("b_psr%d" % i, [128, 512], F32) for i in range(2)]
    t_psr = [Tok("b_psr%d" % i) for i in range(2)]
    ps_tm = [kb.psum("b_pstm%d" % i, [128, 512], F32) for i in range(2)]
    t_pstm = [Tok("b_pstm%d" % i) for i in range(2)]
    d_QTn, d_QTr, d_KTn, d_KrT, d_Vd = scr["QTn"], scr["QTr"], scr["KTn"], scr["KrT"], scr["Vd"]
    t_scr = scr["tok"]

    fmi = 0
    ri = 0
    vi = 0

    def rope(psa, psb, t_pa, t_pb, cst, t_cst, out_ap, t_out):
        nonlocal ri
        k = ri % 2
        ri += 1
        kb.op("vector", lambda e: e.tensor_tensor(out=rt[k][:, 0, :], in0=psa, in1=cst[:, 0, :], op=ALU.mult),
              reads=[t_pa, t_cst], writes=[t_rt[k]])
        kb.op("vector", lambda e: e.tensor_tensor(out=rt[k][:, 1, :], in0=psb, in1=cst[:, 1, :], op=ALU.mult),
              reads=[t_pb, t_cst], writes=[t_rt[k]])
        kb.op("gpsimd", lambda e: e.tensor_tensor(out=out_ap, in0=rt[k][:, 0, :], in1=rt[k][:, 1, :], op=ALU.add),
              reads=[t_rt[k]], writes=[t_out])

    for tt in range(NT):
        s = tt % 2
        tok_sl = slice(tt * 512, (tt + 1) * 512)
        kb.dma("sync", hTs[s][:], hT[:, :, tok_sl].rearrange("c p t -> p c t"), reads=[t_hT], writes=[t_hTs[s]])
        kb.dma("sync", cs[s][:], w["cs"][:, :, tok_sl], writes=[t_cs[s]])
        for g in range(5):
            p = fmi % 2
            fmi += 1
            for c in range(8):
                _mm(kb, ps_fm[p][:], wmla[:, c, g * 128:(g + 1) * 128], hTs[s][:, c, :], c == 0, c == 7,
                    [t_w, t_hTs[s]], [t_psfm[p]])
            kb.op("scalar", lambda e, p=p, g=g: e.copy(out=raw[:, g, :], in_=ps_fm[p][:]),
                  reads=[t_psfm[p]], writes=[t_raw[g]])
            kb.op("gpsimd", lambda e, g=g: e.tensor_tensor(out=sq[:, g, :], in0=raw[:, g, :], in1=raw[:, g, :],
                                                           op=ALU.mult), reads=[t_raw[g]], writes=[t_sq[g]])
        for n, (g0, g1, dim) in enumerate(((0, 3, 384.0), (3, 5, 256.0))):
            for g in range(g0, g1):
                _mm(kb, ps_ss[n][:], consts["ones_b"][:], sq[:, g, :], g == g0, g == g1 - 1,
                    [ct, t_sq[g]], [t_psss[n]])
            kb.op("scalar", lambda e, n=n, dim=dim: e.activation(out=rs[:, n, :], in_=ps_ss[n][:], func=AF.Sqrt,
                                                                 bias=epsq[:, 0:1], scale=1.0 / dim),
                  reads=[t_psss[n], ct], writes=[t_rs[n]])
            kb.op("vector", lambda e, n=n: e.reciprocal(out=rs[:, n, :], in_=rs[:, n, :]),
                  reads=[t_rs[n]], writes=[t_rs[n]])
            for g in range(g0, g1):
                kb.op("vector", lambda e, n=n, g=g: e.scalar_tensor_tensor(
                    out=cn[:, g, :], in0=raw[:, g, :], scalar=gq[:, g:g + 1], in1=rs[:, n, :],
                    op0=ALU.mult, op1=ALU.mult), reads=[t_raw[g], t_w, t_rs[n]], writes=[t_cn[g]])
        for k2 in range(2):
            for c in range(8):
                _mm(kb, ps_r[k2][0:64, :], wmla[:, c, 640 + 64 * k2:704 + 64 * k2], hTs[s][:, c, :], c == 0, c == 7,
                    [t_w, t_hTs[s]], [t_psr[k2]])
        rope(ps_r[0][0:64, :], ps_r[1][0:64, :], t_psr[0], t_psr[1], cs[s], t_cs[s], krs[:], t_krs)
        kb.dma("gpsimd", d_KrT[:, tok_sl], krs[:], reads=[t_krs], writes=[t_scr])
        for h in range(8):
            p = fmi % 2
            fmi += 1
            for c in range(3):
                _mm(kb, ps_fm[p][:], wuqn[:, c, h * 128:(h + 1) * 128], cn[:, c, :], c == 0, c == 2,
                    [t_w, t_cn[c]], [t_psfm[p]])
            kb.op("scalar", lambda e, p=p, h=h: e.copy(out=qn_st[:, h, :], in_=ps_fm[p][:]),
                  reads=[t_psfm[p]], writes=[t_qn])
            for k2, wsrc in enumerate((wuqr, wuqs)):
                for c in range(3):
                    _mm(kb, ps_r[k2][0:64, :], wsrc[:, c, h * 64:(h + 1) * 64], cn[:, c, :], c == 0, c == 2,
                        [t_w, t_cn[c]], [t_psr[k2]])
            rope(ps_r[0][0:64, :], ps_r[1][0:64, :], t_psr[0], t_psr[1], cs[s], t_cs[s], qr_st[:, h, :], t_qr)
            p = fmi % 2
            fmi += 1
            for c in range(2):
                _mm(kb, ps_fm[p][:], wuk[:, c, h * 128:(h + 1) * 128], cn[:, 3 + c, :], c == 0, c == 1,
                    [t_w, t_cn[3 + c]], [t_psfm[p]])
            kb.op("scalar", lambda e, p=p, h=h: e.copy(out=kn_st[:, h, :], in_=ps_fm[p][:]),
                  reads=[t_psfm[p]], writes=[t_kn])
        kb.dma("gpsimd", d_QTn[:, :, tok_sl].rearrange("h p t -> p h t"), qn_st[:], reads=[t_qn], writes=[t_scr])
        kb.dma("gpsimd", d_QTr[:, :, tok_sl].rearrange("h p t -> p h t"), qr_st[:], reads=[t_qr], writes=[t_scr])
        kb.dma("gpsimd", d_KTn[:, :, tok_sl].rearrange("h p t -> p h t"), kn_st[:], reads=[t_kn], writes=[t_scr])
        for j in range(4):
            q = vi % 2
            vi += 1
            for half in range(2):
                p = half
                for c in range(2):
                    _mm(kb, ps_tm[p][:], cn[:, 3 + c, j * 128:(j + 1) * 128], wuv[:, c, half * 512:(half + 1) * 512],
                        c == 0, c == 1, [t_w, t_cn[3 + c]], [t_pstm[p]])
                kb.op("scalar", lambda e, p=p, q=q, half=half: e.copy(out=v_st[q][:, half * 512:(half + 1) * 512],
                                                                      in_=ps_tm[p][:]),
                      reads=[t_pstm[p]], writes=[t_v[q]])
            kb.dma("gpsimd", d_Vd[:, :, tt * 4 + j, :].rearrange("h p d -> p h d"),
                   v_st[q][:].rearrange("p (h d) -> p h d", h=8), reads=[t_v[q]], writes=[t_scr])
    kb.release(mk)


def emit_attn_pass(kb, consts, S, scr, yaT, t_yaT):
    NQ = S // 512
    NK = S // 128
    mk = kb.mark()
    ct = consts["tok"]
    scale = 192.0 ** -0.5
    t_scr = scr["tok"]
    masks = kb.sbuf("c_mask", [128, 4, 512], BF16)
    t_mask = Tok("c_mask")
    for r in range(4):
        for kbk in range(2):
            kb.op("gpsimd", lambda e, r=r, kbk=kbk: e.affine_select(
                out=masks[64 * kbk:64 * kbk + 64, r, :].rearrange("p (a b) -> p a b", a=8),
                in_=consts["ones_b512"][64 * kbk:64 * kbk + 64, :].rearrange("p (a b) -> p a b", a=8),
                pattern=[[1, 8], [0, 64]], compare_op=ALU.is_ge, fill=0.0, base=-(2 * r + kbk),
                channel_multiplier=0), reads=[ct], writes=[t_mask])
    krt = kb.sbuf("c_krt", [64, S], BF16)
    t_krt = Tok("c_krt")
    kb.dma("sync", krt[:], scr["KrT"][:, :], reads=[t_scr], writes=[t_krt])
    ktn = [kb.sbuf("c_ktn%d" % i, [128, S], BF16) for i in range(2)]
    t_ktn = [Tok("c_ktn%d" % i) for i in range(2)]
    vsb = [kb.sbuf("c_v%d" % i, [128, NK, 128], BF16) for i in range(2)]
    t_vsb = [Tok("c_v%d" % i) for i in range(2)]
    qn = [kb.sbuf("c_qn%d" % i, [128, 512], BF16) for i in range(2)]
    qr = [kb.sbuf("c_qr%d" % i, [64, 512], BF16) for i in range(2)]
    t_q = [Tok("c_q%d" % i) for i in range(2)]
    NPT = 4
    NPS = 3
    pacc = [kb.sbuf("c_pacc%d" % i, [128, 512], F32) for i in range(2)]
    t_pacc = [Tok("c_pacc%d" % i) for i in range(2)]
    pacc2 = [kb.sbuf("c_pacd%d" % i, [128, 512], F32) for i in range(2)]
    t_pacc2 = [Tok("c_pacd%d" % i) for i in range(2)]
    pT = [kb.sbuf("c_pT%d" % i, [128, 512], BF16) for i in range(NPT)]
    t_pT = [Tok("c_pT%d" % i) for i in range(NPT)]
    rden = [kb.sbuf("c_rd%d" % i, [128, 512], F32) for i in range(2)]
    t_rden = [Tok("c_rd%d" % i) for i in range(2)]
    yst = [kb.sbuf("c_y%d" % i, [128, 512], BF16) for i in range(2)]
    t_yst = [Tok("c_y%d" % i) for i in range(2)]
    ps_s = [kb.psum("c_pss%d" % i, [128, 512], F32) for i in range(NPS)]
    t_pss = [Tok("c_pss%d" % i) for i in range(NPS)]
    ps_a = [kb.psum("c_psa%d" % i, [128, 512], F32) for i in range(2)]
    t_psa = [Tok("c_psa%d" % i) for i in range(2)]
    ps_d = [kb.psum("c_psd%d" % i, [128, 512], F32) for i in range(2)]
    t_psd = [Tok("c_psd%d" % i) for i in range(2)]
    DEPTH_PIPE = 2
    gi = 0
    qi = 0
    for h in range(8):
        hs = h % 2
        kb.dma("sync", ktn[hs][:], scr["KTn"][h, :, :], reads=[t_scr], writes=[t_ktn[hs]])
        kb.dma("sync", vsb[hs][:], scr["Vd"][h, :, :, :], reads=[t_scr], writes=[t_vsb[hs]])
        for j in range(NQ):
            a = qi % 2
            qi += 1
            qsl = slice(j * 512, (j + 1) * 512)
            kb.dma("sync", qn[a][:], scr["QTn"][h, :, qsl], reads=[t_scr], writes=[t_q[a]])
            kb.dma("sync", qr[a][:], scr["QTr"][h, :, qsl], reads=[t_scr], writes=[t_q[a]])
            nkt = 4 * (j + 1)
            base = gi
            gi += nkt

            def qk(kt):
                p = (base + kt) % NPS
                ksl = slice(kt * 128, (kt + 1) * 128)
                _mm(kb, ps_s[p][:], ktn[hs][:, ksl], qn[a][:], True, False, [t_ktn[hs], t_q[a]], [t_pss[p]])
                _mm(kb, ps_s[p][:], krt[:, ksl], qr[a][:], False, True, [t_krt, t_q[a]], [t_pss[p]])

            def soft(kt):
                p = (base + kt) % NPS
                u = (base + kt) % NPT
                kb.op("scalar", lambda e: e.activation(out=pT[u][:], in_=ps_s[p][:], func=AF.Exp, scale=scale),
                      reads=[t_pss[p]], writes=[t_pT[u]])
                if kt >= 4 * j:
                    r = kt - 4 * j
                    kb.op("vector", lambda e: e.tensor_tensor(out=pT[u][:], in0=pT[u][:], in1=masks[:, r, :],
                                                              op=ALU.mult),
                          reads=[t_pT[u], t_mask], writes=[t_pT[u]])

            def pv(kt):
                u = (base + kt) % NPT
                _mm(kb, ps_a[a][:], vsb[hs][:, kt, :], pT[u][:], kt == 0, kt == nkt - 1,
                    [t_vsb[hs], t_pT[u]], [t_psa[a]])
                if kt == 0:
                    kb.op("vector", lambda e: e.tensor_copy(out=pacc[a][:], in_=pT[u][:]),
                          reads=[t_pT[u]], writes=[t_pacc[a]])
                elif kt == 2:
                    kb.op("gpsimd", lambda e: e.tensor_copy(out=pacc2[a][:], in_=pT[u][:]),
                          reads=[t_pT[u]], writes=[t_pacc2[a]])
                elif kt % 3 == 2:
                    kb.op("gpsimd", lambda e: e.tensor_tensor(out=pacc2[a][:], in0=pacc2[a][:], in1=pT[u][:],
                                                              op=ALU.add),
                          reads=[t_pT[u], t_pacc2[a]], writes=[t_pacc2[a]])
                else:
                    kb.op("vector", lambda e: e.tensor_tensor(out=pacc[a][:], in0=pacc[a][:], in1=pT[u][:],
                                                              op=ALU.add),
                          reads=[t_pT[u], t_pacc[a]], writes=[t_pacc[a]])

            for kt in range(min(DEPTH_PIPE, nkt)):
                qk(kt)
            for kt in range(nkt):
                soft(kt)
                if kt + DEPTH_PIPE < nkt:
                    qk(kt + DEPTH_PIPE)
                pv(kt)
            _mm(kb, ps_d[a][:], consts["ones_f"][:], pacc[a][:], True, False, [ct, t_pacc[a]], [t_psd[a]])
            _mm(kb, ps_d[a][:], consts["ones_f"][:], pacc2[a][:], False, True, [ct, t_pacc2[a]], [t_psd[a]])
            kb.op("vector", lambda e, a=a: e.reciprocal(out=rden[a][:], in_=ps_d[a][:]),
                  reads=[t_psd[a]], writes=[t_rden[a]])
            kb.op("vector", lambda e, a=a: e.tensor_tensor(out=yst[a][:], in0=ps_a[a][:], in1=rden[a][:],
                                                           op=ALU.mult),
                  reads=[t_psa[a], t_rden[a]], writes=[t_yst[a]])
            kb.dma("gpsimd", yaT[h, :, qsl], yst[a][:], reads=[t_yst[a]], writes=[t_yaT])
    kb.release(mk)


def alloc_mla_scratch(kb, S):
    return dict(QTn=kb.dram_tmp("s_QTn", [8, 128, S], BF16), QTr=kb.dram_tmp("s_QTr", [8, 64, S], BF16),
                KTn=kb.dram_tmp("s_KTn", [8, 128, S], BF16), KrT=kb.dram_tmp("s_KrT", [64, S], BF16),
                Vd=kb.dram_tmp("s_Vd", [8, 128, S // 128, 128], BF16), tok=Tok("dram:mla_scr"))


def emit_merge_pass(kb, consts, S, hT, ymT, yaT, t_in, x, t_x, w, gate1, t_gate1, xmid, t_xmid):
    NT = S // 512
    mk = kb.mark()
    wg = kb.sbuf("d_wg", [128, 8, 2048], BF16)
    wbm = kb.sbuf("d_wbm", [128, 8, 1024], BF16)
    wba = kb.sbuf("d_wba", [128, 8, 1024], BF16)
    wo = kb.sbuf("d_wo", [128, 8, 1024], BF16)
    t_w = Tok("d_w")
    load_w_bf16(kb, wg, w["wgate"], t_w, 8)
    load_w_bf16(kb, wbm, w["wbm"], t_w, 8)
    load_w_bf16(kb, wba, w["wba"], t_w, 8)
    load_w_bf16(kb, wo, w["wout"], t_w, 8)
    hTs = [kb.sbuf("d_hT%d" % i, [128, 8, 512], BF16) for i in range(2)]
    ymS = [kb.sbuf("d_ym%d" % i, [128, 8, 512], BF16) for i in range(2)]
    yaS = [kb.sbuf("d_ya%d" % i, [128, 8, 512], BF16) for i in range(2)]
    t_ld = [Tok("d_ld%d" % i) for i in range(2)]
    sg = [kb.sbuf("d_sg%d" % i, [128, 2, 512], F32) for i in range(2)]
    t_sg = [Tok("d_sg%d" % i) for i in range(2)]
    mm_ = [kb.sbuf("d_mm%d" % i, [128, 2, 512], F32) for i in range(2)]
    t_mm = [Tok("d_mm%d" % i) for i in range(2)]
    mg = [kb.sbuf("d_mg%d" % i, [128, 8, 512], BF16) for i in range(2)]
    t_mg = [Tok("d_mg%d" % i) for i in range(2)]
    xt = [kb.sbuf("d_x%d" % i, [128, 1024], F32) for i in range(2)]
    t_xt = [Tok("d_x%d" % i) for i in range(2)]
    tmp = [kb.sbuf("d_tmp%d" % i, [128, 512], F32) for i in range(2)]
    t_tmp = [Tok("d_tmp%d" % i) for i in range(2)]
    ps4 = [kb.psum("d_ps%d" % i, [128, 512], F32) for i in range(4)]
    t_ps4 = [Tok("d_ps%d" % i) for i in range(4)]
    pso = [kb.psum("d_pso%d" % i, [128, 512], F32) for i in range(2)]
    t_pso = [Tok("d_pso%d" % i) for i in range(2)]
    oi = 0
    xi = 0
    for tt in range(NT):
        s = tt % 2
        tsl = slice(tt * 512, (tt + 1) * 512)
        kb.dma("sync", hTs[s][:], hT[:, :, tsl].rearrange("c p t -> p c t"), reads=[t_in], writes=[t_ld[s]])
        kb.dma("sync", ymS[s][:], ymT[:, :, tsl].rearrange("c p t -> p c t"), reads=[t_in], writes=[t_ld[s]])
        kb.dma("sync", yaS[s][:], yaT[:, :, tsl].rearrange("c p t -> p c t"), reads=[t_in], writes=[t_ld[s]])
        for oc in range(8):
            a = oc % 2
            osl = slice(oc * 128, (oc + 1) * 128)
            for c in range(8):
                _mm(kb, ps4[0][:], wbm[:, c, osl], ymS[s][:, c, :], c == 0, c == 7, [t_w, t_ld[s]], [t_ps4[0]])
            for c in range(8):
                _mm(kb, ps4[1][:], wba[:, c, osl], yaS[s][:, c, :], c == 0, c == 7, [t_w, t_ld[s]], [t_ps4[1]])
            for k in range(2):
                for c in range(8):
                    _mm(kb, ps4[2 + k][:], wg[:, c, k * 1024 + oc * 128:k * 1024 + (oc + 1) * 128], hTs[s][:, c, :],
                        c == 0, c == 7, [t_w, t_ld[s]], [t_ps4[2 + k]])
                kb.op("scalar", lambda e, a=a, k=k: e.activation(out=sg[a][:, k, :], in_=ps4[2 + k][:],
                                                                 func=AF.Sigmoid),
                      reads=[t_ps4[2 + k]], writes=[t_sg[a]])
            for k in range(2):
                kb.op("vector", lambda e, a=a, k=k: e.tensor_tensor(out=mm_[a][:, k, :], in0=ps4[k][:],
                                                                    in1=sg[a][:, k, :], op=ALU.mult),
                      reads=[t_ps4[k], t_sg[a]], writes=[t_mm[a]])
            kb.op("gpsimd", lambda e, a=a, s=s, oc=oc: e.tensor_tensor(out=mg[s][:, oc, :], in0=mm_[a][:, 0, :],
                                                                       in1=mm_[a][:, 1, :], op=ALU.add),
                  reads=[t_mm[a]], writes=[t_mg[s]])
        for j in range(4):
            xs = xi % 2
            xi += 1
            t0 = tt * 512 + j * 128
            kb.dma("sync", xt[xs][:], x[t0:t0 + 128, :], reads=[t_x], writes=[t_xt[xs]])
            for half in range(2):
                p = oi % 2
                oi += 1
                hsl = slice(half * 512, (half + 1) * 512)
                for c in range(8):
                    _mm(kb, pso[p][:], mg[s][:, c, j * 128:(j + 1) * 128], wo[:, c, hsl], c == 0, c == 7,
                        [t_w, t_mg[s]], [t_pso[p]])
                kb.op("vector", lambda e, p=p, hsl=hsl: e.tensor_tensor(out=tmp[p][:], in0=pso[p][:],
                                                                        in1=gate1[:, hsl], op=ALU.mult),
                      reads=[t_pso[p], t_gate1], writes=[t_tmp[p]])
                kb.op("gpsimd", lambda e, p=p, xs=xs, hsl=hsl: e.tensor_tensor(out=xt[xs][:, hsl], in0=xt[xs][:, hsl],
                                                                               in1=tmp[p][:], op=ALU.add),
                      reads=[t_tmp[p], t_xt[xs]], writes=[t_xt[xs]])
            kb.dma("gpsimd", xmid[t0:t0 + 128, :], xt[xs][:], reads=[t_xt[xs]], writes=[t_xmid])
    kb.release(mk)


def emit_moe_pass(kb, consts, S, xmid, t_xmid, w, gmul2, t_gmul2, shift2, t_shift2, gate2, t_gate2,
                  xout, t_xout, final_g=None):
    TS = min(2048, S)
    NST = S // TS
    NTL = TS // 128
    mk = kb.mark()
    ct = consts["tok"]
    h2T = kb.sbuf("e_h2T", [128, 8, TS], BF16)
    t_h2T = Tok("e_h2T")
    yacc = kb.sbuf("e_yacc", [128, NTL, 1024], F32)
    t_yacc = [Tok("e_yacc%d" % i) for i in range(NTL)]
    Wt = kb.sbuf("e_Wt", [128, NTL, 32], F32)
    t_Wt = [Tok("e_Wt%d" % i) for i in range(NTL)]
    wr = kb.sbuf("e_wr", [128, 8, 36], F32)
    rb = kb.sbuf("e_rb", [128, 36], F32)
    t_wr = Tok("e_wr")
    kb.dma("sync", wr[:], w["wr"].rearrange("(c p) n -> p c n", p=128), writes=[t_wr])
    kb.dma("sync", rb[:], w["rb"].partition_broadcast(128), writes=[t_wr])
    fg = None
    if final_g is not None:
        fg = kb.sbuf("e_fg", [128, 1024], F32)
        kb.dma("sync", fg[:], final_g.partition_broadcast(128), writes=[t_wr])
    weg = [kb.sbuf("e_weg%d" % i, [128, 8, 256], BF16) for i in range(2)]
    weu = [kb.sbuf("e_weu%d" % i, [128, 8, 256], BF16) for i in range(2)]
    wed = [kb.sbuf("e_wed%d" % i, [128, 2, 1024], BF16) for i in range(2)]
    t_we = [Tok("e_we%d" % i) for i in range(2)]
    xt = [kb.sbuf("e_x%d" % i, [128, 1024], F32) for i in range(2)]
    t_xt = [Tok("e_x%d" % i) for i in range(2)]
    scr = [kb.sbuf("e_scr%d" % i, [128, 1024], F32) for i in range(2)]
    t_scr = [Tok("e_scr%d" % i) for i in range(2)]
    h2f = [kb.sbuf("e_h2f%d" % i, [128, 1024], F32) for i in range(2)]
    t_h2f = [Tok("e_h2f%d" % i) for i in range(2)]
    stat = [kb.sbuf("e_st%d" % i, [128, 4], F32) for i in range(2)]
    t_st = [Tok("e_st%d" % i) for i in range(2)]
    h2Tf = kb.sbuf("e_h2Tf", [128, 8, 128], F32)
    t_h2Tf = Tok("e_h2Tf")
    R = [kb.sbuf("e_R%d" % i, [128, 160], F32) for i in range(2)]
    t_R = [Tok("e_R%d" % i) for i in range(2)]
    sgl = [kb.sbuf("e_sg%d" % i, [128, 512], F32) for i in range(2)]
    t_sgl = [Tok("e_sg%d" % i) for i in range(2)]
    aT = [kb.sbuf("e_aT%d" % i, [128, 2, 512], BF16) for i in range(2)]
    t_aT = [Tok("e_aT%d" % i) for i in range(2)]
    bank = [kb.psum("e_bank%d" % i, [128, 512], F32) for i in range(8)]
    t_bank = [Tok("e_bank%d" % i) for i in range(8)]
    wload = 0

    def load_expert(e):
        nonlocal wload
        k = wload % 2
        wload += 1
        for c in range(8):
            kb.dma("gpsimd", weg[k][:, c, :], w["weg"][e, c * 128:(c + 1) * 128, :], writes=[t_we[k]])
        for c in range(8):
            kb.dma("gpsimd", weu[k][:, c, :], w["weu"][e, c * 128:(c + 1) * 128, :], writes=[t_we[k]])
        for c in range(2):
            kb.dma("gpsimd", wed[k][:, c, :], w["wed"][e, c * 128:(c + 1) * 128, :], writes=[t_we[k]])
        return k

    for st in range(NST):
        for i in range(NTL):
            s = i % 2
            t0 = st * TS + i * 128
            kb.dma("sync", xt[s][:], xmid[t0:t0 + 128, :], reads=[t_xmid], writes=[t_xt[s]])
            emit_norm_mod(kb, xt[s], t_xt[s], gmul2, t_gmul2, shift2, t_shift2, h2f[s], t_h2f[s],
                          scr[s], t_scr[s], stat[s], t_st[s], consts["eps"][:, 0:1])
            for half in range(2):
                for c4 in range(4):
                    c = half * 4 + c4
                    kb.op("tensor", lambda e, s=s, c=c, half=half, c4=c4: e.transpose(
                        out=bank[half][:, c4 * 128:(c4 + 1) * 128], in_=h2f[s][:, c * 128:(c + 1) * 128],
                        identity=consts["id_f"][:]), reads=[t_h2f[s], ct], writes=[t_bank[half]], sig=(c4 == 3))
                kb.op("scalar", lambda e, half=half: e.copy(
                    out=h2Tf[:, half * 4:half * 4 + 4, :], in_=bank[half][:].rearrange("p (c t) -> p c t", c=4)),
                    reads=[t_bank[half]], writes=[t_h2Tf])
            kb.op("gpsimd", lambda e, i=i: e.tensor_copy(out=h2T[:, :, i * 128:(i + 1) * 128], in_=h2Tf[:]),
                  reads=[t_h2Tf], writes=[t_h2T])
            for c in range(8):
                _mm(kb, bank[2][:, 0:36], h2Tf[:, c, :], wr[:, c, :], c == 0, c == 7, [t_h2Tf, t_wr], [t_bank[2]])
            r = R[s]
            tr = t_R[s]
            V = lambda fn, reads, writes: kb.op("vector", fn, reads=reads, writes=writes)
            V(lambda e: e.tensor_tensor(out=r[:, 0:36], in0=bank[2][:, 0:36], in1=rb[:], op=ALU.add),
              [t_bank[2], t_wr], [tr])
            V(lambda e: e.reduce_max(out=r[:, 36:37], in_=r[:, 0:4], axis=AX.X), [tr], [tr])
            V(lambda e: e.tensor_scalar_mul(out=r[:, 37:38], in0=r[:, 36:37], scalar1=-1.0), [tr], [tr])
            kb.op("scalar", lambda e: e.activation(out=r[:, 40:44], in_=r[:, 0:4], func=AF.Exp, bias=r[:, 37:38]),
                  reads=[tr], writes=[tr])
            V(lambda e: e.reduce_sum(out=r[:, 44:45], in_=r[:, 40:44], axis=AX.X), [tr], [tr])
            V(lambda e: e.reciprocal(out=r[:, 45:46], in_=r[:, 44:45]), [tr], [tr])
            V(lambda e: e.tensor_scalar(out=r[:, 48:52], in0=r[:, 0:4], scalar1=r[:, 36:37], scalar2=None,
                                        op0=ALU.is_equal), [tr], [tr])
            V(lambda e: e.tensor_scalar(out=r[:, 52:56], in0=r[:, 48:52], scalar1=-1.0, scalar2=BIG,
                                        op0=ALU.add, op1=ALU.mult), [tr], [tr])
            V(lambda e: e.tensor_tensor(out=r[:, 64:96].rearrange("p (g k) -> p g k", g=4),
                                        in0=r[:, 4:36].rearrange("p (g k) -> p g k", g=4),
                                        in1=r[:, 52:56].unsqueeze(2).to_broadcast([128, 4, 8]), op=ALU.add),
              [tr], [tr])
            V(lambda e: e.reduce_max(out=r[:, 96:97], in_=r[:, 64:96], axis=AX.X), [tr], [tr])
            V(lambda e: e.tensor_scalar(out=r[:, 104:136], in0=r[:, 64:96], scalar1=r[:, 96:97], scalar2=None,
                                        op0=ALU.is_equal), [tr], [tr])
            V(lambda e: e.scalar_tensor_tensor(out=r[:, 64:96], in0=r[:, 104:136], scalar=-BIG, in1=r[:, 64:96],
                                               op0=ALU.mult, op1=ALU.add), [tr], [tr])
            V(lambda e: e.reduce_max(out=r[:, 97:98], in_=r[:, 64:96], axis=AX.X), [tr], [tr])
            V(lambda e: e.tensor_tensor(out=r[:, 98:99], in0=r[:, 97:98], in1=r[:, 96:97], op=ALU.subtract),
              [tr], [tr])
            kb.op("scalar", lambda e: e.activation(out=r[:, 99:100], in_=r[:, 98:99], func=AF.Exp),
                  reads=[tr], writes=[tr])
            V(lambda e: e.tensor_scalar_add(out=r[:, 100:101], in0=r[:, 99:100], scalar1=1.0), [tr], [tr])
            V(lambda e: e.reciprocal(out=r[:, 101:102], in_=r[:, 100:101]), [tr], [tr])
            V(lambda e: e.tensor_tensor(out=r[:, 102:103], in0=r[:, 101:102], in1=r[:, 45:46], op=ALU.mult),
              [tr], [tr])
            V(lambda e: e.tensor_tensor(out=r[:, 103:104], in0=r[:, 102:103], in1=r[:, 99:100], op=ALU.mult),
              [tr], [tr])
            V(lambda e, i=i: e.tensor_scalar_mul(out=Wt[:, i, :], in0=r[:, 104:136], scalar1=r[:, 102:103]),
              [tr], [t_Wt[i]])
            V(lambda e: e.tensor_scalar(out=r[:, 104:136], in0=r[:, 64:96], scalar1=r[:, 97:98], scalar2=None,
                                        op0=ALU.is_equal), [tr], [tr])
            V(lambda e, i=i: e.scalar_tensor_tensor(out=Wt[:, i, :], in0=r[:, 104:136], scalar=r[:, 103:104],
                                                    in1=Wt[:, i, :], op0=ALU.mult, op1=ALU.add),
              [tr, t_Wt[i]], [t_Wt[i]])
        steps = [(ex, tl) for ex in range(32) for tl in range(TS // 512)]
        wslot = {}
        gcount = [0]
        dcount = [0]

        def gu_group(si, hc, which):
            ex, tl = steps[si]
            k = wslot[ex]
            tsl = slice(tl * 512, (tl + 1) * 512)
            hsl = slice(hc * 128, (hc + 1) * 128)
            pg = hc
            if which == 0:
                for c in range(8):
                    _mm(kb, bank[pg][:], weg[k][:, c, hsl], h2T[:, c, tsl], c == 0, c == 7,
                        [t_we[k], t_h2T], [t_bank[pg]])
                kb.op("scalar", lambda e: e.activation(out=sgl[pg][:], in_=bank[pg][:], func=AF.Silu),
                      reads=[t_bank[pg]], writes=[t_sgl[pg]])
            else:
                a = si % 2
                for c in range(8):
                    _mm(kb, bank[2 + pg][:], weu[k][:, c, hsl], h2T[:, c, tsl], c == 0, c == 7,
                        [t_we[k], t_h2T], [t_bank[2 + pg]])
                kb.op("vector", lambda e: e.tensor_tensor(out=aT[a][:, hc, :], in0=bank[2 + pg][:], in1=sgl[pg][:],
                                                          op=ALU.mult),
                      reads=[t_bank[2 + pg], t_sgl[pg]], writes=[t_aT[a]])

        def d_group(si, j, half):
            ex, tl = steps[si]
            k = wslot[ex]
            a = si % 2
            ti = tl * 4 + j
            pd = 4 + dcount[0] % 4
            dcount[0] += 1
            osl = slice(half * 512, (half + 1) * 512)
            for hc in range(2):
                _mm(kb, bank[pd][:], aT[a][:, hc, j * 128:(j + 1) * 128], wed[k][:, hc, osl],
                    hc == 0, hc == 1, [t_aT[a], t_we[k]], [t_bank[pd]])
            if ex == 0:
                kb.op("vector", lambda e: e.tensor_scalar_mul(out=yacc[:, ti, osl], in0=bank[pd][:],
                                                              scalar1=Wt[:, ti, ex:ex + 1]),
                      reads=[t_bank[pd], t_Wt[ti]], writes=[t_yacc[ti]])
            else:
                kb.op("vector", lambda e: e.scalar_tensor_tensor(out=yacc[:, ti, osl], in0=bank[pd][:],
                                                                 scalar=Wt[:, ti, ex:ex + 1], in1=yacc[:, ti, osl],
                                                                 op0=ALU.mult, op1=ALU.add),
                      reads=[t_bank[pd], t_Wt[ti], t_yacc[ti]], writes=[t_yacc[ti]])

        dlist = [(j, half) for j in range(4) for half in range(2)]
        for si in range(len(steps) + 1):
            if si < len(steps):
                ex, tl = steps[si]
                if tl == 0:
                    wslot[ex] = load_expert(ex)
            gul = [(0, 0), (0, 1), (1, 0), (1, 1)]
            for gidx in range(4):
                if si < len(steps):
                    gu_group(si, gul[gidx][0], gul[gidx][1])
                if si > 0:
                    for (j, half) in dlist[2 * gidx:2 * gidx + 2]:
                        d_group(si - 1, j, half)
        for i in range(NTL):
            s = i % 2
            t0 = st * TS + i * 128
            kb.dma("sync", xt[s][:], xmid[t0:t0 + 128, :], reads=[t_xmid], writes=[t_xt[s]])
            kb.op("vector", lambda e, i=i: e.tensor_tensor(out=yacc[:, i, :], in0=yacc[:, i, :], in1=gate2[:],
                                                           op=ALU.mult),
                  reads=[t_yacc[i], t_gate2], writes=[t_yacc[i]])
            kb.op("gpsimd", lambda e, s=s, i=i: e.tensor_tensor(out=xt[s][:], in0=xt[s][:], in1=yacc[:, i, :],
                                                                op=ALU.add),
                  reads=[t_xt[s], t_yacc[i]], writes=[t_xt[s]])
            if final_g is None:
                kb.dma("gpsimd", xout[t0:t0 + 128, :], xt[s][:], reads=[t_xt[s]], writes=[t_xout])
            else:
                emit_norm_mod(kb, xt[s], t_xt[s], fg, t_wr, None, None, h2f[s], t_h2f[s],
                              scr[s], t_scr[s], stat[s], t_st[s], consts["eps"][:, 0:1])
                kb.dma("gpsimd", xout[t0:t0 + 128, :], h2f[s][:], reads=[t_h2f[s]], writes=[t_xout])
    kb.release(mk)


LAYER_SHAPES = dict(
    modw=[D, 6144], modb=[6144], g1=[D],
    wqk=[D, 1024], wtm=[D, 2056], cw=[128, 8, 4], cb=[128, 8], ifb=[8],
    wmla=[D, 768], gqkv=[128, 5], wuqn=[384, 1024], wuqr=[384, 512], wuqs=[384, 512],
    wuk=[256, 1024], wuv=[256, 1024],
    wgate=[D, 2048], wbm=[D, D], wba=[D, D], wout=[D, D], g2=[D], wr=[D, 36], rb=[36],
    weg=[32, D, 256], weu=[32, D, 256], wed=[32, 256, D])
DEPTH = 2


def build_full(S):
    kb = KB()
    x = kb.dram_in("x", [S, D], F32)
    cvec = kb.dram_in("cvec", [D], F32)
    cs = kb.dram_in("cs", [64, 2, S], F32)
    fg = kb.dram_in("fg", [D], F32)
    W = []
    for l in range(DEPTH):
        W.append({k: kb.dram_in("%s_%d" % (k, l), v, F32) for k, v in LAYER_SHAPES.items()})
        W[l]["cs"] = cs
    out = kb.dram_out("out", [S, D], F32)
    hT = kb.dram_tmp("s_hT", [8, 128, S], BF16)
    ymT = kb.dram_tmp("s_ymT", [8, 128, S], BF16)
    yaT = kb.dram_tmp("s_yaT", [8, 128, S], BF16)
    xmid = kb.dram_tmp("s_xmid", [S, D], F32)
    xnext = kb.dram_tmp("s_xnext", [S, D], F32)
    scr = alloc_mla_scratch(kb, S)
    t_hT, t_ymT, t_yaT = Tok("dram:hT"), Tok("dram:ymT"), Tok("dram:yaT")
    t_xmid, t_xnext, t_out, t_x = Tok("dram:xmid"), Tok("dram:xnext"), Tok("dram:out"), Tok("dram:x")
    consts = load_consts(kb)
    x_cur, t_xcur = x, t_x
    for l in range(DEPTH):
        w = W[l]
        mk = kb.mark()
        emit_k1_body(kb, consts, x_cur, cvec, w["modw"][:, 0:2048], w["modb"][0:2048], w["g1"], hT, S,
                     hT_tok=t_hT, x_tok=t_xcur)
        kb.release(mk)
        emit_mlstm_pass(kb, consts, S, hT, t_hT, w, ymT, t_ymT)
        emit_mla_proj_pass(kb, consts, S, hT, t_hT, w, scr)
        emit_attn_pass(kb, consts, S, scr, yaT, t_yaT)
        mk = kb.mark()
        mods = [kb.sbuf("mod%d" % i, [128, D], F32) for i in range(4)]
        tm = [Tok("mod%d" % i) for i in range(4)]
        g2t = kb.sbuf("g2t", [128, D], F32)
        tg2 = Tok("g2t")
        emit_mod(kb, consts, w["modw"][:, 2048:6144], w["modb"][2048:6144], cvec, 4, list(zip(mods, tm)))
        kb.dma("sync", g2t[:], w["g2"].partition_broadcast(128), writes=[tg2])
        kb.op("vector", lambda e: e.scalar_tensor_tensor(out=mods[2][:], in0=mods[2][:], scalar=1.0, in1=g2t[:],
                                                         op0=ALU.add, op1=ALU.mult),
              reads=[tm[2], tg2], writes=[tm[2]])
        t_in_all = _MultiTok([t_hT, t_ymT, t_yaT])
        emit_merge_pass(kb, consts, S, hT, ymT, yaT, t_in_all, x_cur, t_xcur, w, mods[0], tm[0], xmid, t_xmid)
        last = (l == DEPTH - 1)
        emit_moe_pass(kb, consts, S, xmid, t_xmid, w, mods[2], tm[2], mods[1], tm[1], mods[3], tm[3],
                      out if last else xnext, t_out if last else t_xnext, final_g=fg if last else None)
        kb.release(mk)
        x_cur, t_xcur = xnext, t_xnext
    kb.finish([t_out])
    return kb


class _MultiTok:
    def __init__(self, toks):
        self.name = "dram:multi"
        self._toks = toks


def _rope_tables(S):
    pos = np.arange(S, dtype=np.float32)
    inv_freq = (1.0 / (np.float32(10000.0) ** (np.arange(0, 64, 2, dtype=np.float32) / np.float32(64)))).astype(np.float32)
    ang = pos[:, None] * inv_freq[None, :]
    cos = np.cos(ang).astype(np.float32)
    sin = np.sin(ang).astype(np.float32)
    CC = np.concatenate([cos, cos], 1).T
    SS = np.concatenate([-sin, sin], 1).T
    return np.ascontiguousarray(np.stack([CC, SS], 1))


def _prep_layer(z, l):
    c = np.ascontiguousarray
    win = z["w_in"][l]
    d = {}
    d["modw"] = c(z["mod_w"][l])
    d["modb"] = c(z["mod_b"][l])
    d["g1"] = c(z["norm1_g"][l])
    d["wqk"] = c(win[:, :1024])
    d["wtm"] = c(win[:, 1024:3080])
    d["cw"] = c(z["conv_w"][l].reshape(4, 8, 128).transpose(2, 1, 0))
    d["cb"] = c(z["conv_b"][l].reshape(8, 128).T)
    d["ifb"] = c(np.concatenate([z["igate_b"][l], z["fgate_b"][l]]))
    kr = win[:, 3720:3784]
    d["wmla"] = c(np.concatenate([win[:, 3080:3784], kr[:, 32:], kr[:, :32]], 1))
    d["gqkv"] = c(np.concatenate([z["q_norm_g"][l].reshape(3, 128).T, z["kv_norm_g"][l].reshape(2, 128).T], 1))
    uq = z["w_uq"][l].reshape(384, 8, 192)
    d["wuqn"] = c(uq[:, :, :128].reshape(384, 1024))
    d["wuqr"] = c(uq[:, :, 128:].reshape(384, 512))
    d["wuqs"] = c(np.concatenate([uq[:, :, 160:], uq[:, :, 128:160]], 2).reshape(384, 512))
    ukv = z["w_ukv"][l].reshape(256, 8, 256)
    d["wuk"] = c(ukv[:, :, :128].reshape(256, 1024))
    d["wuv"] = c(ukv[:, :, 128:].reshape(256, 1024))
    d["wgate"] = c(win[:, 3784:5832])
    d["wbm"] = c(z["w_branch_m"][l])
    d["wba"] = c(z["w_branch_a"][l])
    d["wout"] = c(z["w_out"][l])
    d["g2"] = c(z["norm2_g"][l])
    d["wr"] = c(np.concatenate([z["w_group"][l], z["w_router"][l]], 1))
    d["rb"] = c(np.concatenate([z["b_group"][l], z["b_router"][l]]))
    d["weg"] = c(z["w_expert_gate"][l])
    d["weu"] = c(z["w_expert_up"][l])
    d["wed"] = c(z["w_expert_down"][l])
    return d


_PROGRAMS = {}


def kernel(**inputs):
    z = {k: np.asarray(v) for k, v in inputs.items()}
    x = z["x"].astype(np.float32, copy=False)
    B, S, _ = x.shape
    if S not in _PROGRAMS:
        _PROGRAMS[S] = build_full(S)
    kb = _PROGRAMS[S]
    shared = {"cs": _rope_tables(S), "fg": np.ascontiguousarray(z["final_norm_g"].astype(np.float32))}
    for l in range(DEPTH):
        for k, v in _prep_layer(z, l).items():
            assert list(v.shape) == LAYER_SHAPES[k], (k, v.shape)
            shared["%s_%d" % (k, l)] = v.astype(np.float32, copy=False)
    in_maps = []
    for b in range(B):
        m = dict(shared)
        m["x"] = np.ascontiguousarray(x[b])
        m["cvec"] = np.ascontiguousarray(z["c"][b].astype(np.float32))
        in_maps.append(m)
    res = run_bass_kernel_spmd(kb.nc, in_maps, core_ids=list(range(B)))
    return np.stack([np.asarray(r["out"]) for r in res.results], 0).astype(np.float32)
```

```python
import math
import numpy as np
import ml_dtypes
import concourse.bass as bass
import concourse.mybir as mybir
from concourse.bass_utils import run_bass_kernel_spmd

F32 = mybir.dt.float32
BF16 = mybir.dt.bfloat16
AF = mybir.ActivationFunctionType
ALU = mybir.AluOpType
AX = mybir.AxisListType

D = 1024
NCORES = 8
EPS = 1e-6
BIG = 30000.0


class Tok:
    __slots__ = ("w", "r", "wsem", "rsem", "name")

    def __init__(self, name=""):
        self.w = None
        self.r = []
        self.wsem = None
        self.rsem = None
        self.name = name


class _Sem:
    __slots__ = ("h", "cnt", "key")

    def __init__(self, h, key):
        self.h = h
        self.cnt = 0
        self.key = key


class _Eng:
    def __init__(self, name, e, sem):
        self.name = name
        self.e = e
        self.sem = sem
        self.seen = {}


class KB:
    def __init__(self):
        self.nc = bass.Bass("TRN2", target_bir_lowering=False)
        nc = self.nc
        self._nsem = 0
        self.eng = {}
        for name in ("tensor", "vector", "scalar", "gpsimd", "sync"):
            self.eng[name] = _Eng(name, getattr(nc, name), self._newsem("e_" + name))
        self._stack = []
        self._dsems = []
        self._free_dsems = []
        self._live_dsems = []
        self.ninst = 0

    def _newsem(self, name):
        if not name.startswith("e_") and self._free_dsems:
            sem = self._free_dsems.pop()
            self._live_dsems.append(sem)
            return sem
        self._nsem += 1
        h = self.nc.semaphore(name + "_%d" % self._nsem).__enter__()
        sem = _Sem(h, self._nsem)
        if not name.startswith("e_"):
            self._dsems.append(sem)
            self._live_dsems.append(sem)
        return sem

    def dram_in(self, name, shape, dt):
        return self.nc.dram_tensor(name, list(shape), dt, kind="ExternalInput").ap()

    def dram_out(self, name, shape, dt):
        return self.nc.dram_tensor(name, list(shape), dt, kind="ExternalOutput").ap()

    def dram_tmp(self, name, shape, dt):
        return self.nc.dram_tensor(name, list(shape), dt, kind="Internal").ap()

    def sbuf(self, name, shape, dt):
        self._uid = getattr(self, "_uid", 0) + 1
        cm = self.nc.sbuf_tensor("%s_u%d" % (name, self._uid), list(shape), dt)
        t = cm.__enter__()
        self._stack.append(cm)
        return t

    def psum(self, name, shape, dt):
        self._uid = getattr(self, "_uid", 0) + 1
        cm = self.nc.psum_tensor("%s_u%d" % (name, self._uid), list(shape), dt)
        t = cm.__enter__()
        self._stack.append(cm)
        return t

    def mark(self):
        return (len(self._stack), len(self._live_dsems))

    def release(self, mark):
        self.barrier()
        while len(self._stack) > mark[0]:
            self._stack.pop().__exit__(None, None, None)
        while len(self._live_dsems) > mark[1]:
            self._free_dsems.append(self._live_dsems.pop())

    def barrier(self):
        sems = [E.sem for E in self.eng.values()] + self._dsems
        for E in self.eng.values():
            for sem in sems:
                if sem is E.sem or sem.cnt == 0:
                    continue
                if E.seen.get(sem.key, 0) >= sem.cnt:
                    continue
                E.e.wait_ge(sem.h, sem.cnt)
                E.seen[sem.key] = sem.cnt
                self.ninst += 1

    def _wait(self, E, evs):
        need = {}
        for ev in evs:
            if ev is None:
                continue
            sem, val = ev
            if sem is E.sem and E.name == "tensor":
                continue
            if need.get(sem.key, (None, 0))[1] < val:
                need[sem.key] = (sem, val)
        for key, (sem, val) in need.items():
            if E.seen.get(key, 0) >= val:
                continue
            assert val <= sem.cnt, "waiting on an event that is never signalled"
            E.e.wait_ge(sem.h, val)
            E.seen[key] = val
            self.ninst += 1

    def _deps(self, reads, writes):
        evs = []
        for t in reads:
            if hasattr(t, "_toks"):
                evs.extend(x.w for x in t._toks)
            else:
                evs.append(t.w)
        for t in writes:
            evs.append(t.w)
            evs.extend(t.r)
        return evs

    def op(self, engname, fn, reads=(), writes=(), sig=True):
        E = self.eng[engname]
        self._wait(E, self._deps(reads, writes))
        ins = fn(E.e)
        self.ninst += 1
        if sig:
            E.sem.cnt += 1
            ins.then_inc(E.sem.h, 1)
            ev = (E.sem, E.sem.cnt)
        else:
            assert engname == "tensor"
            ev = (E.sem, E.sem.cnt + 1)
        for t in reads:
            for tt_ in (t._toks if hasattr(t, "_toks") else (t,)):
                tt_.r.append(ev)
                if len(tt_.r) > 24:
                    tt_.r = self._compact(tt_.r)
        for t in writes:
            t.w = ev
            t.r = []
        return ins

    @staticmethod
    def _compact(r):
        best = {}
        for sem, val in r:
            if best.get(sem.key, (None, 0))[1] < val:
                best[sem.key] = (sem, val)
        return list(best.values())

    def dma(self, queue, out, in_, reads=(), writes=(), **kw):
        E = self.eng[queue]
        evs = self._deps(reads, [])
        for t in writes:
            isdram = getattr(t, "name", "").startswith("dram:")
            if t.w is not None and not isdram and not (t.wsem is not None and t.w[0] is t.wsem):
                evs.append(t.w)
            evs.extend(t.r)
        self._wait(E, evs)
        ins = E.e.dma_start(out=out, in_=in_, **kw)
        self.ninst += 1
        if writes and writes[0].wsem is None and not getattr(writes[0], "_dram", False):
            pass
        tok = None
        for t in writes:
            if not getattr(t, "name", "").startswith("dram:"):
                tok = t
                if tok.wsem is None:
                    tok.wsem = self._newsem("dw")
                sem = tok.wsem
                break
        if tok is None:
            for t in reads:
                if not getattr(t, "name", "").startswith("dram:"):
                    tok = t
                    if tok.rsem is None:
                        tok.rsem = self._newsem("dr")
                    sem = tok.rsem
                    break
        assert tok is not None, "dma needs an sbuf-side token"
        sem.cnt += 16
        ins.then_inc(sem.h, 16)
        ev = (sem, sem.cnt)
        for t in reads:
            for tt_ in (t._toks if hasattr(t, "_toks") else (t,)):
                tt_.r.append(ev)
                if len(tt_.r) > 24:
                    tt_.r = self._compact(tt_.r)
        for t in writes:
            t.w = ev
            t.r = []
        return ins

    def finish(self, toks):
        E = self.eng["sync"]
        evs = []
        for t in toks:
            evs.append(t.w)
            evs.extend(t.r)
        self._wait(E, evs)


def load_consts(kb):
    nc = kb.nc
    c = {}
    c["tok"] = Tok("consts")
    ones_f = kb.sbuf("c_ones_f", [128, 128], F32)
    ones_b = kb.sbuf("c_ones_b", [128, 128], BF16)
    id_f = kb.sbuf("c_id_f", [128, 128], F32)
    id_b = kb.sbuf("c_id_b", [128, 128], BF16)
    tri_f = kb.sbuf("c_tri_f", [128, 128], F32)
    tris_f = kb.sbuf("c_tris_f", [128, 128], F32)
    tri_b = kb.sbuf("c_tri_b", [128, 128], BF16)
    t = c["tok"]
    kb.op("gpsimd", lambda e: e.memset(ones_f[:], 1.0), writes=[t])
    kb.op("gpsimd", lambda e: e.memset(ones_b[:], 1.0), writes=[t])
    kb.op("gpsimd", lambda e: e.affine_select(out=id_f[:], in_=ones_f[:], pattern=[[-1, 128]],
                                             compare_op=ALU.is_equal, fill=0.0, base=0,
                                             channel_multiplier=1), reads=[t], writes=[t])
    kb.op("gpsimd", lambda e: e.tensor_copy(out=id_b[:], in_=id_f[:]), reads=[t], writes=[t])
    kb.op("gpsimd", lambda e: e.affine_select(out=tri_f[:], in_=ones_f[:], pattern=[[1, 128]],
                                             compare_op=ALU.is_ge, fill=0.0, base=0,
                                             channel_multiplier=-1), reads=[t], writes=[t])
    kb.op("gpsimd", lambda e: e.tensor_copy(out=tri_b[:], in_=tri_f[:]), reads=[t], writes=[t])
    kb.op("gpsimd", lambda e: e.affine_select(out=tris_f[:], in_=ones_f[:], pattern=[[-1, 128]],
                                             compare_op=ALU.is_gt, fill=0.0, base=0,
                                             channel_multiplier=1), reads=[t], writes=[t])
    eps = kb.sbuf("c_eps", [128, 1], F32)
    kb.op("gpsimd", lambda e: e.memset(eps[:], EPS), writes=[t])
    c["eps"] = eps
    one = kb.sbuf("c_one", [128, 1], F32)
    kb.op("gpsimd", lambda e: e.memset(one[:], 1.0), writes=[t])
    c["one"] = one
    ones_b512 = kb.sbuf("c_ones_b512", [128, 512], BF16)
    kb.op("gpsimd", lambda e: e.memset(ones_b512[:], 1.0), writes=[t])
    c["ones_b512"] = ones_b512
    c.update(ones_f=ones_f, ones_b=ones_b, id_f=id_f, id_b=id_b, tri_f=tri_f, tris_f=tris_f,
             tri_b=tri_b)
    return c


def emit_mod(kb, consts, modw, modb, cvec, nvec, outs, ps_tag="modps"):
    nc = kb.nc
    mk = kb.mark()
    cpc = kb.sbuf("mod_cpc", [128, 8], F32)
    cond = kb.sbuf("mod_cond", [128, 8], F32)
    bc = kb.sbuf("mod_bc", [128, 8, 128], F32)
    wt = kb.sbuf("mod_w", [128, 8, 1024], F32)
    bt = kb.sbuf("mod_b", [128, 1024], F32)
    ps = kb.psum(ps_tag, [128, 512], F32)
    t_c, t_cond, t_bc, t_w, t_b, t_ps = (Tok("m%d" % i) for i in range(6))
    kb.dma("sync", cpc[:], cvec.rearrange("(p c) -> p c", c=8), writes=[t_c])
    kb.op("scalar", lambda e: e.activation(out=cond[:], in_=cpc[:], func=AF.Silu),
          reads=[t_c], writes=[t_cond])
    for c in range(8):
        kb.op("vector", lambda e, c=c: e.tensor_scalar_mul(out=bc[:, c, :], in0=consts["ones_f"][:],
                                                           scalar1=cond[:, c:c + 1]),
              reads=[t_cond, consts["tok"]], writes=[t_bc])
    for v in range(nvec):
        out_t, out_tok = outs[v]
        kb.dma("sync", wt[:], modw[:, v * 1024:(v + 1) * 1024].rearrange("(p c) n -> p c n", c=8),
               writes=[t_w])
        kb.dma("sync", bt[:], modb[v * 1024:(v + 1) * 1024].partition_broadcast(128), writes=[t_b])
        for n2 in range(2):
            for c in range(8):
                kb.op("tensor", lambda e, c=c, n2=n2: e.matmul(ps[:], lhsT=bc[:, c, :],
                                                               rhs=wt[:, c, n2 * 512:(n2 + 1) * 512],
                                                               start=(c == 0), stop=(c == 7)),
                      reads=[t_bc, t_w], writes=[t_ps], sig=(c == 7))
            kb.op("vector", lambda e, n2=n2: e.tensor_tensor(out=out_t[:, n2 * 512:(n2 + 1) * 512],
                                                             in0=ps[:], in1=bt[:, n2 * 512:(n2 + 1) * 512],
                                                             op=ALU.add),
                  reads=[t_ps, t_b], writes=[out_tok])
    kb.release(mk)


def emit_norm_mod(kb, xt, t_x, gmul, t_g, shift, t_s, hb, t_h, scr, t_scr, stat, t_stat, eps_ap):
    kb.op("gpsimd", lambda e: e.memset(stat[:, 0:1], 0.0), writes=[t_stat])
    kb.op("scalar", lambda e: e.activation(out=scr[:], in_=xt[:], func=AF.Square,
                                           accum_out=stat[:, 0:1]),
          reads=[t_x], writes=[t_scr, t_stat])
    kb.op("scalar", lambda e: e.activation(out=stat[:, 1:2], in_=stat[:, 0:1], func=AF.Sqrt,
                                           bias=eps_ap, scale=1.0 / D),
          reads=[t_stat], writes=[t_stat])
    kb.op("vector", lambda e: e.reciprocal(out=stat[:, 2:3], in_=stat[:, 1:2]),
          reads=[t_stat], writes=[t_stat])
    if shift is None:
        kb.op("vector", lambda e: e.scalar_tensor_tensor(out=hb[:], in0=xt[:], scalar=stat[:, 2:3],
                                                         in1=gmul[:], op0=ALU.mult, op1=ALU.mult),
              reads=[t_x, t_stat, t_g], writes=[t_h])
        return
    kb.op("vector", lambda e: e.scalar_tensor_tensor(out=scr[:], in0=xt[:], scalar=stat[:, 2:3],
                                                     in1=gmul[:], op0=ALU.mult, op1=ALU.mult),
          reads=[t_x, t_stat, t_g], writes=[t_scr])
    kb.op("gpsimd", lambda e: e.tensor_tensor(out=hb[:], in0=scr[:], in1=shift[:], op=ALU.add),
          reads=[t_scr, t_s], writes=[t_h])


def build_k1(Sh):
    kb = KB()
    nc = kb.nc
    x = kb.dram_in("x", [Sh, D], F32)
    cvec = kb.dram_in("cvec", [D], F32)
    modw = kb.dram_in("modw", [D, 2 * D], F32)
    modb = kb.dram_in("modb", [2 * D], F32)
    g = kb.dram_in("g", [D], F32)
    hT = kb.dram_out("hT", [8, 128, Sh], BF16)
    consts = load_consts(kb)
    emit_k1_body(kb, consts, x, cvec, modw, modb, g, hT, Sh)
    kb.finish([kb._out_tok])
    return kb


def emit_k1_body(kb, consts, x, cvec, modw, modb, g, hT, Sh, hT_tok=None, x_tok=None):
    shift = kb.sbuf("k1_shift", [128, D], F32)
    gmul = kb.sbuf("k1_gmul", [128, D], F32)
    gt = kb.sbuf("k1_g", [128, D], F32)
    t_shift, t_gmul, t_g = Tok("shift"), Tok("gmul"), Tok("g")
    emit_mod(kb, consts, modw, modb, cvec, 2, [(shift, t_shift), (gmul, t_gmul)])
    kb.dma("sync", gt[:], g.partition_broadcast(128), writes=[t_g])
    kb.op("vector", lambda e: e.scalar_tensor_tensor(out=gmul[:], in0=gmul[:], scalar=1.0, in1=gt[:],
                                                     op0=ALU.add, op1=ALU.mult),
          reads=[t_gmul, t_g], writes=[t_gmul])
    NB = 2
    xt = [kb.sbuf("k1_x%d" % i, [128, D], F32) for i in range(NB)]
    scr = [kb.sbuf("k1_scr%d" % i, [128, D], F32) for i in range(NB)]
    hb = [kb.sbuf("k1_hb%d" % i, [128, D], BF16) for i in range(NB)]
    stat = [kb.sbuf("k1_st%d" % i, [128, 4], F32) for i in range(NB)]
    hTs = [kb.sbuf("k1_hT%d" % i, [128, 8, 512], BF16) for i in range(2)]
    pst = [kb.psum("k1_pst%d" % i, [128, 8, 128], BF16) for i in range(2)]
    t_x = [Tok("x%d" % i) for i in range(NB)]
    t_scr = [Tok("scr%d" % i) for i in range(NB)]
    t_hb = [Tok("hb%d" % i) for i in range(NB)]
    t_st = [Tok("st%d" % i) for i in range(NB)]
    t_hT = [Tok("hT%d" % i) for i in range(2)]
    t_ps = [Tok("ps%d" % i) for i in range(2)]
    t_out = hT_tok if hT_tok is not None else Tok("dram:hT")
    kb._out_tok = t_out
    ntile = Sh // 128
    for i in range(ntile):
        s = i % NB
        grp = (i // 4) % 2
        kb.dma("sync", xt[s][:], x[i * 128:(i + 1) * 128, :], reads=([x_tok] if x_tok is not None else []), writes=[t_x[s]])
        emit_norm_mod(kb, xt[s], t_x[s], gmul, t_gmul, shift, t_shift, hb[s], t_hb[s],
                      scr[s], t_scr[s], stat[s], t_st[s], consts["eps"][:, 0:1])
        p = i % 2
        for c in range(8):
            kb.op("tensor", lambda e, c=c, s=s, p=p: e.transpose(out=pst[p][:, c, :],
                                                                 in_=hb[s][:, c * 128:(c + 1) * 128],
                                                                 identity=consts["id_b"][:]),
                  reads=[t_hb[s], consts["tok"]], writes=[t_ps[p]], sig=(c == 7))
        j = i % 4
        kb.op("scalar", lambda e, p=p, grp=grp, j=j: e.copy(out=hTs[grp][:, :, j * 128:(j + 1) * 128],
                                                            in_=pst[p][:]),
              reads=[t_ps[p]], writes=[t_hT[grp]])
        if j == 3:
            t0 = (i - 3) * 128
            kb.dma("gpsimd", hT[:, :, t0:t0 + 512].rearrange("c p t -> p c t"), hTs[grp][:],
                   reads=[t_hT[grp]], writes=[t_out])


def _mm(kb, out, lhsT, rhs, start, stop, reads, writes):
    return kb.op("tensor", lambda e: e.matmul(out, lhsT=lhsT, rhs=rhs, start=start, stop=stop),
                 reads=reads, writes=writes, sig=stop)


def load_w_bf16(kb, dst, src_rows_cols, tok, nchunk, queue="gpsimd"):
    for c in range(nchunk):
        kb.dma(queue, dst[:, c, :], src_rows_cols[c * 128:(c + 1) * 128, :], writes=[tok])


def emit_mlstm_pass(kb, consts, S, hT, t_hT, w, ymT, t_ymT):
    NT = S // 512
    mk = kb.mark()
    ct = consts["tok"]
    wqk = kb.sbuf("a_wqk", [128, 8, 1024], BF16)
    wtm = kb.sbuf("a_wtm", [128, 8, 2056], BF16)
    cw = kb.sbuf("a_cw", [128, 8, 4], F32)
    cb = kb.sbuf("a_cb", [128, 8], F32)
    ifb = kb.sbuf("a_ifb", [128, 8], F32)
    t_w = Tok("a_w")
    t_wt = Tok("a_wt")
    t_cw = Tok("a_cw")
    load_w_bf16(kb, wqk, w["wqk"], t_w, 8)
    load_w_bf16(kb, wtm, w["wtm"], t_wt, 8)
    kb.dma("sync", cw[:], w["cw"], writes=[t_cw])
    kb.dma("sync", cb[:], w["cb"], writes=[t_cw])
    kb.dma("sync", ifb[:], w["ifb"].partition_broadcast(128), writes=[t_cw])

    hTs = [kb.sbuf("a_hT%d" % i, [128, 8, 512], BF16) for i in range(2)]
    t_hTs = [Tok("a_hT%d" % i) for i in range(2)]
    pre = kb.sbuf("a_pre", [128, 8, 515], F32)
    t_pre = [Tok("a_pre%d" % g) for g in range(8)]
    cacc = [kb.sbuf("a_cacc%d" % i, [128, 512], F32) for i in range(2)]
    t_cacc = [Tok("a_cacc%d" % i) for i in range(2)]
    qkT = kb.sbuf("a_qkT", [128, 8, 512], BF16)
    t_qk = [Tok("a_qk%d" % g) for g in range(8)]
    Vp = [kb.sbuf("a_vp%d" % i, [128, 4, 257], BF16) for i in range(2)]
    t_vp = [Tok("a_vp%d" % i) for i in range(2)]
    sgo = [kb.sbuf("a_sgo%d" % i, [128, 1024], F32) for i in range(2)]
    t_sgo = [Tok("a_sgo%d" % i) for i in range(2)]
    gsb = [kb.sbuf("a_g%d" % i, [128, 48], F32) for i in range(2)]
    t_g = [Tok("a_g%d" % i) for i in range(2)]
    Zf = kb.sbuf("a_zf", [128, 4, 257], F32)
    Zb = kb.sbuf("a_zb", [128, 4, 257], BF16)
    t_zf = [Tok("a_zf%d" % h) for h in range(4)]
    t_zb = [Tok("a_zb%d" % h) for h in range(4)]
    s0sb = [kb.sbuf("a_s0%d" % i, [128, 128], BF16) for i in range(2)]
    t_s0 = [Tok("a_s0%d" % i) for i in range(2)]
    ytok = [kb.sbuf("a_yt%d" % i, [128, 256], BF16) for i in range(2)]
    t_yt = [Tok("a_yt%d" % i) for i in range(2)]
    khat = [kb.sbuf("a_kh%d" % i, [128, 128], BF16) for i in range(2)]
    t_kh = [Tok("a_kh%d" % i) for i in range(2)]
    dsm = [kb.sbuf("a_d%d" % i, [128, 8], F32) for i in range(2)]
    t_d = [Tok("a_d%d" % i) for i in range(2)]
    yst = [kb.sbuf("a_yst%d" % i, [128, 8, 512], BF16) for i in range(2)]
    t_yst = [Tok("a_yst%d" % i) for i in range(2)]

    ps_fm = [kb.psum("a_psfm%d" % i, [128, 512], F32) for i in range(2)]
    t_psfm = [Tok("a_psfm%d" % i) for i in range(2)]
    ps_tm = [kb.psum("a_pstm%d" % i, [128, 512], F32) for i in range(2)]
    t_pstm = [Tok("a_pstm%d" % i) for i in range(2)]
    ps_m = kb.psum("a_psm", [128, 512], F32)
    t_ps_s0, t_ps_o, t_ps_if, t_ps_cs = Tok("ps_s0"), Tok("ps_o"), Tok("ps_if"), Tok("ps_cs")
    ps_u = kb.psum("a_psu", [128, 257], F32)
    t_ps_u = Tok("ps_u")
    ps_t = kb.psum("a_pst", [128, 3, 128], BF16)
    t_ps_yt, t_ps_kt = Tok("ps_yt"), Tok("ps_kt")

    kb.op("gpsimd", lambda e: e.memset(pre[:, :, 0:3], 0.0), writes=t_pre)
    kb.op("gpsimd", lambda e: e.memset(Zf[:], 0.0), writes=t_zf)
    kb.op("gpsimd", lambda e: e.memset(Zb[:], 0.0), writes=t_zb)
    for i in range(2):
        kb.op("gpsimd", lambda e, i=i: e.memset(Vp[i][:, :, 256:257], 1.0), writes=[t_vp[i]])

    fmi = 0
    tmi = 0
    hi = 0
    for tt in range(NT):
        s = tt % 2
        kb.dma("sync", hTs[s][:], hT[:, :, tt * 512:(tt + 1) * 512].rearrange("c p t -> p c t"),
               reads=[t_hT], writes=[t_hTs[s]])
        for g in range(8):
            p = fmi % 2
            fmi += 1
            for c in range(8):
                _mm(kb, ps_fm[p][:], wqk[:, c, g * 128:(g + 1) * 128], hTs[s][:, c, :], c == 0, c == 7,
                    [t_w, t_hTs[s]], [t_psfm[p]])
            kb.op("scalar", lambda e, p=p, g=g: e.copy(out=pre[:, g, 3:515], in_=ps_fm[p][:]),
                  reads=[t_psfm[p]], writes=[t_pre[g]])
            a = g % 2
            kb.op("vector", lambda e, a=a, g=g: e.tensor_scalar_mul(out=cacc[a][:], in0=pre[:, g, 0:512],
                                                                    scalar1=cw[:, g, 0:1]),
                  reads=[t_pre[g], t_cw], writes=[t_cacc[a]])
            for j in range(1, 4):
                kb.op("vector", lambda e, a=a, g=g, j=j: e.scalar_tensor_tensor(
                    out=cacc[a][:], in0=pre[:, g, j:j + 512], scalar=cw[:, g, j:j + 1], in1=cacc[a][:],
                    op0=ALU.mult, op1=ALU.add), reads=[t_pre[g], t_cw, t_cacc[a]], writes=[t_cacc[a]])
            kb.op("scalar", lambda e, a=a, g=g: e.activation(out=qkT[:, g, :], in_=cacc[a][:], func=AF.Silu,
                                                             bias=cb[:, g:g + 1]),
                  reads=[t_cacc[a], t_cw], writes=[t_qk[g]])
            kb.op("gpsimd", lambda e, g=g: e.tensor_copy(out=pre[:, g, 0:3], in_=pre[:, g, 512:515]),
                  reads=[t_pre[g]], writes=[t_pre[g]])
        for j in range(4):
            vs = (tt * 4 + j) % 2
            tsl = slice(j * 128, (j + 1) * 128)
            for half in range(2):
                p = tmi % 2
                tmi += 1
                for c in range(8):
                    _mm(kb, ps_tm[p][:], hTs[s][:, c, tsl], wtm[:, c, half * 512:(half + 1) * 512],
                        c == 0, c == 7, [t_wt, t_hTs[s]], [t_pstm[p]])
                kb.op("scalar", lambda e, p=p, vs=vs, half=half: e.copy(
                    out=Vp[vs][:, 2 * half:2 * half + 2, 0:256],
                    in_=ps_tm[p][:].rearrange("p (h v) -> p h v", h=2)),
                    reads=[t_pstm[p]], writes=[t_vp[vs]])
            for half in range(2):
                p = tmi % 2
                tmi += 1
                for c in range(8):
                    _mm(kb, ps_tm[p][:], hTs[s][:, c, tsl], wtm[:, c, 1024 + half * 512:1024 + (half + 1) * 512],
                        c == 0, c == 7, [t_wt, t_hTs[s]], [t_pstm[p]])
                kb.op("scalar", lambda e, p=p, vs=vs, half=half: e.activation(
                    out=sgo[vs][:, half * 512:(half + 1) * 512], in_=ps_tm[p][:], func=AF.Sigmoid),
                    reads=[t_pstm[p]], writes=[t_sgo[vs]])
            for c in range(8):
                _mm(kb, ps_m[:, 385:393], hTs[s][:, c, tsl], wtm[:, c, 2048:2056], c == 0, c == 7,
                    [t_wt, t_hTs[s]], [t_ps_if])
            G = gsb[vs]
            tg = t_g[vs]
            kb.op("vector", lambda e, G=G: e.tensor_tensor(out=G[:, 0:8], in0=ps_m[:, 385:393], in1=ifb[:],
                                                           op=ALU.add),
                  reads=[t_ps_if, t_cw], writes=[tg])
            kb.op("scalar", lambda e, G=G: e.activation(out=G[:, 8:12], in_=G[:, 4:8], func=AF.Exp, scale=-1.0),
                  reads=[tg], writes=[tg])
            kb.op("scalar", lambda e, G=G: e.activation(out=G[:, 8:12], in_=G[:, 8:12], func=AF.Ln,
                                                        bias=consts["one"][:, 0:1]),
                  reads=[tg, ct], writes=[tg])
            kb.op("vector", lambda e, G=G: e.tensor_scalar_mul(out=G[:, 8:12], in0=G[:, 8:12], scalar1=-1.0),
                  reads=[tg], writes=[tg])
            _mm(kb, ps_m[:, 393:397], consts["tri_f"][:], G[:, 8:12], True, True, [ct, tg], [t_ps_cs])
            _mm(kb, ps_m[:, 397:401], consts["tris_f"][:], G[:, 8:12], True, True, [ct, tg], [t_ps_cs])
            _mm(kb, ps_m[:, 401:405], consts["ones_f"][:], G[:, 8:12], True, True, [ct, tg], [t_ps_cs])
            kb.op("vector", lambda e, G=G: e.tensor_copy(out=G[:, 16:20], in_=ps_m[:, 393:397]),
                  reads=[t_ps_cs], writes=[tg])
            kb.op("vector", lambda e, G=G: e.tensor_tensor(out=G[:, 20:24], in0=G[:, 0:4], in1=ps_m[:, 393:397],
                                                           op=ALU.subtract),
                  reads=[t_ps_cs, tg], writes=[tg])
            kb.op("vector", lambda e, G=G: e.tensor_tensor(out=G[:, 24:28], in0=G[:, 0:4], in1=ps_m[:, 397:401],
                                                           op=ALU.add),
                  reads=[t_ps_cs, tg], writes=[tg])
            kb.op("vector", lambda e, G=G: e.tensor_copy(out=G[:, 28:32], in_=ps_m[:, 401:405]),
                  reads=[t_ps_cs], writes=[tg])
            kb.op("scalar", lambda e, G=G: e.activation(out=G[:, 32:48], in_=G[:, 16:32], func=AF.Exp),
                  reads=[tg], writes=[tg])
            kb.op("vector", lambda e, G=G: e.tensor_scalar_mul(out=G[:, 12:16], in0=G[:, 32:36],
                                                               scalar1=128.0 ** -0.5),
                  reads=[tg], writes=[tg])
            for h in range(4):
                q = hi % 2
                hi += 1
                qT = qkT[:, h, tsl]
                kT = qkT[:, 4 + h, tsl]
                _mm(kb, ps_m[:, 0:128], kT, qT, True, True, [t_qk[h], t_qk[4 + h]], [t_ps_s0])
                kb.op("vector", lambda e, q=q, G=G, h=h: e.scalar_tensor_tensor(
                    out=s0sb[q][:], in0=ps_m[:, 0:128], scalar=G[:, 36 + h:37 + h], in1=consts["tri_f"][:],
                    op0=ALU.mult, op1=ALU.mult), reads=[t_ps_s0, tg, ct], writes=[t_s0[q]])
                _mm(kb, ps_m[:, 128:385], s0sb[q][:], Vp[vs][:, h, :], True, False, [t_s0[q], t_vp[vs]], [t_ps_o])
                _mm(kb, ps_m[:, 128:385], qT, Zb[:, h, :], False, True, [t_qk[h], t_zb[h]], [t_ps_o])
                dd = dsm[q]
                td = t_d[q]
                kb.op("vector", lambda e, dd=dd, G=G, h=h: e.tensor_tensor(
                    out=dd[:, 0:1], in0=ps_m[:, 384:385], in1=G[:, 12 + h:13 + h], op=ALU.mult),
                    reads=[t_ps_o, tg], writes=[td])
                kb.op("vector", lambda e, dd=dd: e.tensor_scalar(out=dd[:, 1:2], in0=dd[:, 0:1], scalar1=-1.0,
                                                                 scalar2=1.0, op0=ALU.mult, op1=ALU.max),
                      reads=[td], writes=[td])
                kb.op("vector", lambda e, dd=dd: e.tensor_tensor(out=dd[:, 2:3], in0=dd[:, 1:2], in1=dd[:, 0:1],
                                                                 op=ALU.max), reads=[td], writes=[td])
                kb.op("vector", lambda e, dd=dd: e.reciprocal(out=dd[:, 3:4], in_=dd[:, 2:3]),
                      reads=[td], writes=[td])
                kb.op("vector", lambda e, dd=dd, G=G, h=h: e.tensor_tensor(
                    out=dd[:, 4:5], in0=dd[:, 3:4], in1=G[:, 12 + h:13 + h], op=ALU.mult),
                    reads=[td, tg], writes=[td])
                kb.op("vector", lambda e, q=q, dd=dd, vs=vs, h=h: e.scalar_tensor_tensor(
                    out=ytok[q][:], in0=ps_m[:, 128:384], scalar=dd[:, 4:5],
                    in1=sgo[vs][:, h * 256:(h + 1) * 256], op0=ALU.mult, op1=ALU.mult),
                    reads=[t_ps_o, td, t_sgo[vs]], writes=[t_yt[q]])
                for k in range(2):
                    kb.op("tensor", lambda e, q=q, k=k: e.transpose(out=ps_t[:, k, :],
                                                                    in_=ytok[q][:, k * 128:(k + 1) * 128],
                                                                    identity=consts["id_b"][:]),
                          reads=[t_yt[q], ct], writes=[t_ps_yt], sig=(k == 1))
                kb.op("scalar", lambda e, s=s, h=h, tsl=tsl: e.copy(out=yst[s][:, 2 * h:2 * h + 2, tsl],
                                                                    in_=ps_t[:, 0:2, :]),
                      reads=[t_ps_yt], writes=[t_yst[s]])
                kb.op("tensor", lambda e, kT=kT: e.transpose(out=ps_t[:, 2, :], in_=kT, identity=consts["id_b"][:]),
                      reads=[t_qk[4 + h], ct], writes=[t_ps_kt])
                kb.op("scalar", lambda e, q=q, G=G, h=h: e.activation(out=khat[q][:], in_=ps_t[:, 2, :],
                                                                      func=AF.Copy, scale=G[:, 40 + h:41 + h]),
                      reads=[t_ps_kt, tg], writes=[t_kh[q]])
                _mm(kb, ps_u[:], khat[q][:], Vp[vs][:, h, :], True, True, [t_kh[q], t_vp[vs]], [t_ps_u])
                kb.op("vector", lambda e, G=G, h=h: e.scalar_tensor_tensor(
                    out=Zf[:, h, :], in0=Zf[:, h, :], scalar=G[:, 44 + h:45 + h], in1=ps_u[:],
                    op0=ALU.mult, op1=ALU.add), reads=[t_zf[h], tg, t_ps_u], writes=[t_zf[h]])
                kb.op("gpsimd", lambda e, h=h: e.tensor_copy(out=Zb[:, h, :], in_=Zf[:, h, :]),
                      reads=[t_zf[h]], writes=[t_zb[h]])
        kb.dma("gpsimd", ymT[:, :, tt * 512:(tt + 1) * 512].rearrange("c p t -> p c t"), yst[s][:],
               reads=[t_yst[s]], writes=[t_ymT])
    kb.release(mk)


def emit_mla_proj_pass(kb, consts, S, hT, t_hT, w, scr):
    NT = S // 512
    mk = kb.mark()
    ct = consts["tok"]
    wmla = kb.sbuf("b_wmla", [128, 8, 768], BF16)
    wuqn = kb.sbuf("b_wuqn", [128, 3, 1024], BF16)
    wuqr = kb.sbuf("b_wuqr", [128, 3, 512], BF16)
    wuqs = kb.sbuf("b_wuqs", [128, 3, 512], BF16)
    wuk = kb.sbuf("b_wuk", [128, 2, 1024], BF16)
    wuv = kb.sbuf("b_wuv", [128, 2, 1024], BF16)
    gq = kb.sbuf("b_gq", [128, 5], F32)
    t_w = Tok("b_w")
    load_w_bf16(kb, wmla, w["wmla"], t_w, 8)
    load_w_bf16(kb, wuqn, w["wuqn"], t_w, 3)
    load_w_bf16(kb, wuqr, w["wuqr"], t_w, 3)
    load_w_bf16(kb, wuqs, w["wuqs"], t_w, 3)
    load_w_bf16(kb, wuk, w["wuk"], t_w, 2)
    load_w_bf16(kb, wuv, w["wuv"], t_w, 2)
    kb.dma("sync", gq[:], w["gqkv"], writes=[t_w])
    epsq = consts["eps"]

    hTs = [kb.sbuf("b_hT%d" % i, [128, 8, 512], BF16) for i in range(2)]
    t_hTs = [Tok("b_hT%d" % i) for i in range(2)]
    cs = [kb.sbuf("b_cs%d" % i, [64, 2, 512], F32) for i in range(2)]
    t_cs = [Tok("b_cs%d" % i) for i in range(2)]
    raw = kb.sbuf("b_raw", [128, 5, 512], F32)
    t_raw = [Tok("b_raw%d" % g) for g in range(5)]
    sq = kb.sbuf("b_sq", [128, 5, 512], BF16)
    t_sq = [Tok("b_sq%d" % g) for g in range(5)]
    rs = kb.sbuf("b_rs", [128, 2, 512], F32)
    t_rs = [Tok("b_rs%d" % g) for g in range(2)]
    cn = kb.sbuf("b_cn", [128, 5, 512], BF16)
    t_cn = [Tok("b_cn%d" % g) for g in range(5)]
    rt = [kb.sbuf("b_rt%d" % i, [64, 2, 512], F32) for i in range(2)]
    t_rt = [Tok("b_rt%d" % i) for i in range(2)]
    krs = kb.sbuf("b_krs", [64, 512], BF16)
    t_krs = Tok("b_krs")
    qn_st = kb.sbuf("b_qn", [128, 8, 512], BF16)
    qr_st = kb.sbuf("b_qr", [64, 8, 512], BF16)
    kn_st = kb.sbuf("b_kn", [128, 8, 512], BF16)
    t_qn, t_qr, t_kn = Tok("b_qn"), Tok("b_qr"), Tok("b_kn")
    v_st = [kb.sbuf("b_v%d" % i, [128, 1024], BF16) for i in range(2)]
    t_v = [Tok("b_v%d" % i) for i in range(2)]

    ps_fm = [kb.psum("b_psfm%d" % i, [128, 512], F32) for i in range(2)]
    t_psfm = [Tok("b_psfm%d" % i) for i in range(2)]
    ps_ss = [kb.psum("b_psss%d" % i, [128, 512], F32) for i in range(2)]
    t_psss = [Tok("b_psss%d" % i) for i in range(2)]
    ps_r = [kb.psum("b_psr%d" % i, [128, 512], F32) for i in range(2)]
    t_psr = [Tok("b_psr%d" % i) for i in range(2)]
    ps_tm = [kb.psum("b_pstm%d" % i, [128, 512], F32) for i in range(2)]
    t_pstm = [Tok("b_pstm%d" % i) for i in range(2)]
    d_QTn, d_QTr, d_KTn, d_KrT, d_Vd = scr["QTn"], scr["QTr"], scr["KTn"], scr["KrT"], scr["Vd"]
    t_scr = scr["tok"]

    fmi = 0
    ri = 0
    vi = 0

    def rope(psa, psb, t_pa, t_pb, cst, t_cst, out_ap, t_out):
        nonlocal ri
        k = ri % 2
        ri += 1
        kb.op("vector", lambda e: e.tensor_tensor(out=rt[k][:, 0, :], in0=psa, in1=cst[:, 0, :], op=ALU.mult),
              reads=[t_pa, t_cst], writes=[t_rt[k]])
        kb.op("vector", lambda e: e.tensor_tensor(out=rt[k][:, 1, :], in0=psb, in1=cst[:, 1, :], op=ALU.mult),
              reads=[t_pb, t_cst], writes=[t_rt[k]])
        kb.op("gpsimd", lambda e: e.tensor_tensor(out=out_ap, in0=rt[k][:, 0, :], in1=rt[k][:, 1, :], op=ALU.add),
              reads=[t_rt[k]], writes=[t_out])

    for tt in range(NT):
        s = tt % 2
        tok_sl = slice(tt * 512, (tt + 1) * 512)
        kb.dma("sync", hTs[s][:], hT[:, :, tok_sl].rearrange("c p t -> p c t"), reads=[t_hT], writes=[t_hTs[s]])
        kb.dma("sync", cs[s][:], w["cs"][:, :, tok_sl], writes=[t_cs[s]])
        for g in range(5):
            p = fmi % 2
            fmi += 1
            for c in range(8):
                _mm(kb, ps_fm[p][:], wmla[:, c, g * 128:(g + 1) * 128], hTs[s][:, c, :], c == 0, c == 7,
                    [t_w, t_hTs[s]], [t_psfm[p]])
            kb.op("scalar", lambda e, p=p, g=g: e.copy(out=raw[:, g, :], in_=ps_fm[p][:]),
                  reads=[t_psfm[p]], writes=[t_raw[g]])
            kb.op("gpsimd", lambda e, g=g: e.tensor_tensor(out=sq[:, g, :], in0=raw[:, g, :], in1=raw[:, g, :],
                                                           op=ALU.mult), reads=[t_raw[g]], writes=[t_sq[g]])
        for n, (g0, g1, dim) in enumerate(((0, 3, 384.0), (3, 5, 256.0))):
            for g in range(g0, g1):
                _mm(kb, ps_ss[n][:], consts["ones_b"][:], sq[:, g, :], g == g0, g == g1 - 1,
                    [ct, t_sq[g]], [t_psss[n]])
            kb.op("scalar", lambda e, n=n, dim=dim: e.activation(out=rs[:, n, :], in_=ps_ss[n][:], func=AF.Sqrt,
                                                                 bias=epsq[:, 0:1], scale=1.0 / dim),
                  reads=[t_psss[n], ct], writes=[t_rs[n]])
            kb.op("vector", lambda e, n=n: e.reciprocal(out=rs[:, n, :], in_=rs[:, n, :]),
                  reads=[t_rs[n]], writes=[t_rs[n]])
            for g in range(g0, g1):
                kb.op("vector", lambda e, n=n, g=g: e.scalar_tensor_tensor(
                    out=cn[:, g, :], in0=raw[:, g, :], scalar=gq[:, g:g + 1], in1=rs[:, n, :],
                    op0=ALU.mult, op1=ALU.mult), reads=[t_raw[g], t_w, t_rs[n]], writes=[t_cn[g]])
        for k2 in range(2):
            for c in range(8):
                _mm(kb, ps_r[k2][0:64, :], wmla[:, c, 640 + 64 * k2:704 + 64 * k2], hTs[s][:, c, :], c == 0, c == 7,
                    [t_w, t_hTs[s]], [t_psr[k2]])
        rope(ps_r[0][0:64, :], ps_r[1][0:64, :], t_psr[0], t_psr[1], cs[s], t_cs[s], krs[:], t_krs)
        kb.dma("gpsimd", d_KrT[:, tok_sl], krs[:], reads=[t_krs], writes=[t_scr])
        for h in range(8):
            p = fmi % 2
            fmi += 1
            for c in range(3):
                _mm(kb, ps_fm[p][:], wuqn[:, c, h * 128:(h + 1) * 128], cn[:, c, :], c == 0, c == 2,
                    [t_w, t_cn[c]], [t_psfm[p]])
            kb.op("scalar", lambda e, p=p, h=h: e.copy(out=qn_st[:, h, :], in_=ps_fm[p][:]),
                  reads=[t_psfm[p]], writes=[t_qn])
            for k2, wsrc in enumerate((wuqr, wuqs)):
                for c in range(3):
                    _mm(kb, ps_r[k2][0:64, :], wsrc[:, c, h * 64:(h + 1) * 64], cn[:, c, :], c == 0, c == 2,
                        [t_w, t_cn[c]], [t_psr[k2]])
            rope(ps_r[0][0:64, :], ps_r[1][0:64, :], t_psr[0], t_psr[1], cs[s], t_cs[s], qr_st[:, h, :], t_qr)
            p = fmi % 2
            fmi += 1
            for c in range(2):
                _mm(kb, ps_fm[p][:], wuk[:, c, h * 128:(h + 1) * 128], cn[:, 3 + c, :], c == 0, c == 1,
                    [t_w, t_cn[3 + c]], [t_psfm[p]])
            kb.op("scalar", lambda e, p=p, h=h: e.copy(out=kn_st[:, h, :], in_=ps_fm[p][:]),
                  reads=[t_psfm[p]], writes=[t_kn])
        kb.dma("gpsimd", d_QTn[:, :, tok_sl].rearrange("h p t -> p h t"), qn_st[:], reads=[t_qn], writes=[t_scr])
        kb.dma("gpsimd", d_QTr[:, :, tok_sl].rearrange("h p t -> p h t"), qr_st[:], reads=[t_qr], writes=[t_scr])
        kb.dma("gpsimd", d_KTn[:, :, tok_sl].rearrange("h p t -> p h t"), kn_st[:], reads=[t_kn], writes=[t_scr])
        for j in range(4):
            q = vi % 2
            vi += 1
            for half in range(2):
                p = half
                for c in range(2):
                    _mm(kb, ps_tm[p][:], cn[:, 3 + c, j * 128:(j + 1) * 128], wuv[:, c, half * 512:(half + 1) * 512],
                        c == 0, c == 1, [t_w, t_cn[3 + c]], [t_pstm[p]])
                kb.op("scalar", lambda e, p=p, q=q, half=half: e.copy(out=v_st[q][:, half * 512:(half + 1) * 512],
                                                                      in_=ps_tm[p][:]),
                      reads=[t_pstm[p]], writes=[t_v[q]])
            kb.dma("gpsimd", d_Vd[:, :, tt * 4 + j, :].rearrange("h p d -> p h d"),
                   v_st[q][:].rearrange("p (h d) -> p h d", h=8), reads=[t_v[q]], writes=[t_scr])
    kb.release(mk)


def emit_attn_pass(kb, consts, S, scr, yaT, t_yaT):
    NQ = S // 512
    NK = S // 128
    mk = kb.mark()
    ct = consts["tok"]
    scale = 192.0 ** -0.5
    t_scr = scr["tok"]
    masks = kb.sbuf("c_mask", [128, 4, 512], BF16)
    t_mask = Tok("c_mask")
    for r in range(4):
        for kbk in range(2):
            kb.op("gpsimd", lambda e, r=r, kbk=kbk: e.affine_select(
                out=masks[64 * kbk:64 * kbk + 64, r, :].rearrange("p (a b) -> p a b", a=8),
                in_=consts["ones_b512"][64 * kbk:64 * kbk + 64, :].rearrange("p (a b) -> p a b", a=8),
                pattern=[[1, 8], [0, 64]], compare_op=ALU.is_ge, fill=0.0, base=-(2 * r + kbk),
                channel_multiplier=0), reads=[ct], writes=[t_mask])
    krt = kb.sbuf("c_krt", [64, S], BF16)
    t_krt = Tok("c_krt")
    kb.dma("sync", krt[:], scr["KrT"][:, :], reads=[t_scr], writes=[t_krt])
    ktn = [kb.sbuf("c_ktn%d" % i, [128, S], BF16) for i in range(2)]
    t_ktn = [Tok("c_ktn%d" % i) for i in range(2)]
    vsb = [kb.sbuf("c_v%d" % i, [128, NK, 128], BF16) for i in range(2)]
    t_vsb = [Tok("c_v%d" % i) for i in range(2)]
    qn = [kb.sbuf("c_qn%d" % i, [128, 512], BF16) for i in range(2)]
    qr = [kb.sbuf("c_qr%d" % i, [64, 512], BF16) for i in range(2)]
    t_q = [Tok("c_q%d" % i) for i in range(2)]
    NPT = 4
    NPS = 3
    pacc = [kb.sbuf("c_pacc%d" % i, [128, 512], F32) for i in range(2)]
    t_pacc = [Tok("c_pacc%d" % i) for i in range(2)]
    pacc2 = [kb.sbuf("c_pacd%d" % i, [128, 512], F32) for i in range(2)]
    t_pacc2 = [Tok("c_pacd%d" % i) for i in range(2)]
    pT = [kb.sbuf("c_pT%d" % i, [128, 512], BF16) for i in range(NPT)]
    t_pT = [Tok("c_pT%d" % i) for i in range(NPT)]
    rden = [kb.sbuf("c_rd%d" % i, [128, 512], F32) for i in range(2)]
    t_rden = [Tok("c_rd%d" % i) for i in range(2)]
    yst = [kb.sbuf("c_y%d" % i, [128, 512], BF16) for i in range(2)]
    t_yst = [Tok("c_y%d" % i) for i in range(2)]
    ps_s = [kb.psum("c_pss%d" % i, [128, 512], F32) for i in range(NPS)]
    t_pss = [Tok("c_pss%d" % i) for i in range(NPS)]
    ps_a = [kb.psum("c_psa%d" % i, [128, 512], F32) for i in range(2)]
    t_psa = [Tok("c_psa%d" % i) for i in range(2)]
    ps_d = [kb.psum("c_psd%d" % i, [128, 512], F32) for i in range(2)]
    t_psd = [Tok("c_psd%d" % i) for i in range(2)]
    DEPTH_PIPE = 2
    gi = 0
    qi = 0
    for h in range(8):
        hs = h % 2
        kb.dma("sync", ktn[hs][:], scr["KTn"][h, :, :], reads=[t_scr], writes=[t_ktn[hs]])
        kb.dma("sync", vsb[hs][:], scr["Vd"][h, :, :, :], reads=[t_scr], writes=[t_vsb[hs]])
        for j in range(NQ):
            a = qi % 2
            qi += 1
            qsl = slice(j * 512, (j + 1) * 512)
            kb.dma("sync", qn[a][:], scr["QTn"][h, :, qsl], reads=[t_scr], writes=[t_q[a]])
            kb.dma("sync", qr[a][:], scr["QTr"][h, :, qsl], reads=[t_scr], writes=[t_q[a]])
            nkt = 4 * (j + 1)
            base = gi
            gi += nkt

            def qk(kt):
                p = (base + kt) % NPS
                ksl = slice(kt * 128, (kt + 1) * 128)
                _mm(kb, ps_s[p][:], ktn[hs][:, ksl], qn[a][:], True, False, [t_ktn[hs], t_q[a]], [t_pss[p]])
                _mm(kb, ps_s[p][:], krt[:, ksl], qr[a][:], False, True, [t_krt, t_q[a]], [t_pss[p]])

            def soft(kt):
                p = (base + kt) % NPS
                u = (base + kt) % NPT
                kb.op("scalar", lambda e: e.activation(out=pT[u][:], in_=ps_s[p][:], func=AF.Exp, scale=scale),
                      reads=[t_pss[p]], writes=[t_pT[u]])
                if kt >= 4 * j:
                    r = kt - 4 * j
                    kb.op("vector", lambda e: e.tensor_tensor(out=pT[u][:], in0=pT[u][:], in1=masks[:, r, :],
                                                              op=ALU.mult),
                          reads=[t_pT[u], t_mask], writes=[t_pT[u]])

            def pv(kt):
                u = (base + kt) % NPT
                _mm(kb, ps_a[a][:], vsb[hs][:, kt, :], pT[u][:], kt == 0, kt == nkt - 1,
                    [t_vsb[hs], t_pT[u]], [t_psa[a]])
                if kt == 0:
                    kb.op("vector", lambda e: e.tensor_copy(out=pacc[a][:], in_=pT[u][:]),
                          reads=[t_pT[u]], writes=[t_pacc[a]])
                else:
                    kb.op("vector", lambda e: e.tensor_tensor(out=pacc[a][:], in0=pacc[a][:], in1=pT[u][:],
                                                              op=ALU.add),
                          reads=[t_pT[u], t_pacc[a]], writes=[t_pacc[a]])

            for kt in range(min(DEPTH_PIPE, nkt)):
                qk(kt)
            for kt in range(nkt):
                soft(kt)
                if kt + DEPTH_PIPE < nkt:
                    qk(kt + DEPTH_PIPE)
                pv(kt)
            _mm(kb, ps_d[a][:], consts["ones_f"][:], pacc[a][:], True, True, [ct, t_pacc[a]], [t_psd[a]])
            kb.op("vector", lambda e, a=a: e.reciprocal(out=rden[a][:], in_=ps_d[a][:]),
                  reads=[t_psd[a]], writes=[t_rden[a]])
            kb.op("vector", lambda e, a=a: e.tensor_tensor(out=yst[a][:], in0=ps_a[a][:], in1=rden[a][:],
                                                           op=ALU.mult),
                  reads=[t_psa[a], t_rden[a]], writes=[t_yst[a]])
            kb.dma("gpsimd", yaT[h, :, qsl], yst[a][:], reads=[t_yst[a]], writes=[t_yaT])
    kb.release(mk)


def alloc_mla_scratch(kb, S):
    return dict(QTn=kb.dram_tmp("s_QTn", [8, 128, S], BF16), QTr=kb.dram_tmp("s_QTr", [8, 64, S], BF16),
                KTn=kb.dram_tmp("s_KTn", [8, 128, S], BF16), KrT=kb.dram_tmp("s_KrT", [64, S], BF16),
                Vd=kb.dram_tmp("s_Vd", [8, 128, S // 128, 128], BF16), tok=Tok("dram:mla_scr"))


def emit_merge_pass(kb, consts, S, hT, ymT, yaT, t_in, x, t_x, w, gate1, t_gate1, xmid, t_xmid):
    NT = S // 512
    mk = kb.mark()
    wg = kb.sbuf("d_wg", [128, 8, 2048], BF16)
    wbm = kb.sbuf("d_wbm", [128, 8, 1024], BF16)
    wba = kb.sbuf("d_wba", [128, 8, 1024], BF16)
    wo = kb.sbuf("d_wo", [128, 8, 1024], BF16)
    t_w = Tok("d_w")
    load_w_bf16(kb, wg, w["wgate"], t_w, 8)
    load_w_bf16(kb, wbm, w["wbm"], t_w, 8)
    load_w_bf16(kb, wba, w["wba"], t_w, 8)
    load_w_bf16(kb, wo, w["wout"], t_w, 8)
    hTs = [kb.sbuf("d_hT%d" % i, [128, 8, 512], BF16) for i in range(2)]
    ymS = [kb.sbuf("d_ym%d" % i, [128, 8, 512], BF16) for i in range(2)]
    yaS = [kb.sbuf("d_ya%d" % i, [128, 8, 512], BF16) for i in range(2)]
    t_ld = [Tok("d_ld%d" % i) for i in range(2)]
    sg = [kb.sbuf("d_sg%d" % i, [128, 2, 512], F32) for i in range(2)]
    t_sg = [Tok("d_sg%d" % i) for i in range(2)]
    mm_ = [kb.sbuf("d_mm%d" % i, [128, 2, 512], F32) for i in range(2)]
    t_mm = [Tok("d_mm%d" % i) for i in range(2)]
    mg = [kb.sbuf("d_mg%d" % i, [128, 8, 512], BF16) for i in range(2)]
    t_mg = [Tok("d_mg%d" % i) for i in range(2)]
    xt = [kb.sbuf("d_x%d" % i, [128, 1024], F32) for i in range(2)]
    t_xt = [Tok("d_x%d" % i) for i in range(2)]
    tmp = [kb.sbuf("d_tmp%d" % i, [128, 512], F32) for i in range(2)]
    t_tmp = [Tok("d_tmp%d" % i) for i in range(2)]
    ps4 = [kb.psum("d_ps%d" % i, [128, 512], F32) for i in range(4)]
    t_ps4 = [Tok("d_ps%d" % i) for i in range(4)]
    pso = [kb.psum("d_pso%d" % i, [128, 512], F32) for i in range(2)]
    t_pso = [Tok("d_pso%d" % i) for i in range(2)]
    oi = 0
    xi = 0
    for tt in range(NT):
        s = tt % 2
        tsl = slice(tt * 512, (tt + 1) * 512)
        kb.dma("sync", hTs[s][:], hT[:, :, tsl].rearrange("c p t -> p c t"), reads=[t_in], writes=[t_ld[s]])
        kb.dma("sync", ymS[s][:], ymT[:, :, tsl].rearrange("c p t -> p c t"), reads=[t_in], writes=[t_ld[s]])
        kb.dma("sync", yaS[s][:], yaT[:, :, tsl].rearrange("c p t -> p c t"), reads=[t_in], writes=[t_ld[s]])
        for oc in range(8):
            a = oc % 2
            osl = slice(oc * 128, (oc + 1) * 128)
            for c in range(8):
                _mm(kb, ps4[0][:], wbm[:, c, osl], ymS[s][:, c, :], c == 0, c == 7, [t_w, t_ld[s]], [t_ps4[0]])
            for c in range(8):
                _mm(kb, ps4[1][:], wba[:, c, osl], yaS[s][:, c, :], c == 0, c == 7, [t_w, t_ld[s]], [t_ps4[1]])
            for k in range(2):
                for c in range(8):
                    _mm(kb, ps4[2 + k][:], wg[:, c, k * 1024 + oc * 128:k * 1024 + (oc + 1) * 128], hTs[s][:, c, :],
                        c == 0, c == 7, [t_w, t_ld[s]], [t_ps4[2 + k]])
                kb.op("scalar", lambda e, a=a, k=k: e.activation(out=sg[a][:, k, :], in_=ps4[2 + k][:],
                                                                 func=AF.Sigmoid),
                      reads=[t_ps4[2 + k]], writes=[t_sg[a]])
            for k in range(2):
                kb.op("vector", lambda e, a=a, k=k: e.tensor_tensor(out=mm_[a][:, k, :], in0=ps4[k][:],
                                                                    in1=sg[a][:, k, :], op=ALU.mult),
                      reads=[t_ps4[k], t_sg[a]], writes=[t_mm[a]])
            kb.op("gpsimd", lambda e, a=a, s=s, oc=oc: e.tensor_tensor(out=mg[s][:, oc, :], in0=mm_[a][:, 0, :],
                                                                       in1=mm_[a][:, 1, :], op=ALU.add),
                  reads=[t_mm[a]], writes=[t_mg[s]])
        for j in range(4):
            xs = xi % 2
            xi += 1
            t0 = tt * 512 + j * 128
            kb.dma("sync", xt[xs][:], x[t0:t0 + 128, :], reads=[t_x], writes=[t_xt[xs]])
            for half in range(2):
                p = oi % 2
                oi += 1
                hsl = slice(half * 512, (half + 1) * 512)
                for c in range(8):
                    _mm(kb, pso[p][:], mg[s][:, c, j * 128:(j + 1) * 128], wo[:, c, hsl], c == 0, c == 7,
                        [t_w, t_mg[s]], [t_pso[p]])
                kb.op("vector", lambda e, p=p, hsl=hsl: e.tensor_tensor(out=tmp[p][:], in0=pso[p][:],
                                                                        in1=gate1[:, hsl], op=ALU.mult),
                      reads=[t_pso[p], t_gate1], writes=[t_tmp[p]])
                kb.op("vector", lambda e, p=p, xs=xs, hsl=hsl: e.tensor_tensor(out=xt[xs][:, hsl], in0=xt[xs][:, hsl],
                                                                               in1=tmp[p][:], op=ALU.add),
                      reads=[t_tmp[p], t_xt[xs]], writes=[t_xt[xs]])
            kb.dma("gpsimd", xmid[t0:t0 + 128, :], xt[xs][:], reads=[t_xt[xs]], writes=[t_xmid])
    kb.release(mk)


def emit_moe_pass(kb, consts, S, xmid, t_xmid, w, gmul2, t_gmul2, shift2, t_shift2, gate2, t_gate2,
                  xout, t_xout, final_g=None):
    TS = min(2048, S)
    NST = S // TS
    NTL = TS // 128
    mk = kb.mark()
    ct = consts["tok"]
    h2T = kb.sbuf("e_h2T", [128, 8, TS], BF16)
    t_h2T = Tok("e_h2T")
    yacc = kb.sbuf("e_yacc", [128, NTL, 1024], F32)
    t_yacc = [Tok("e_yacc%d" % i) for i in range(NTL)]
    Wt = kb.sbuf("e_Wt", [128, NTL, 32], F32)
    t_Wt = [Tok("e_Wt%d" % i) for i in range(NTL)]
    wr = kb.sbuf("e_wr", [128, 8, 36], F32)
    rb = kb.sbuf("e_rb", [128, 36], F32)
    t_wr = Tok("e_wr")
    kb.dma("sync", wr[:], w["wr"].rearrange("(c p) n -> p c n", p=128), writes=[t_wr])
    kb.dma("sync", rb[:], w["rb"].partition_broadcast(128), writes=[t_wr])
    fg = None
    if final_g is not None:
        fg = kb.sbuf("e_fg", [128, 1024], F32)
        kb.dma("sync", fg[:], final_g.partition_broadcast(128), writes=[t_wr])
    weg = [kb.sbuf("e_weg%d" % i, [128, 8, 256], BF16) for i in range(2)]
    weu = [kb.sbuf("e_weu%d" % i, [128, 8, 256], BF16) for i in range(2)]
    wed = [kb.sbuf("e_wed%d" % i, [128, 2, 1024], BF16) for i in range(2)]
    t_we = [Tok("e_we%d" % i) for i in range(2)]
    xt = [kb.sbuf("e_x%d" % i, [128, 1024], F32) for i in range(2)]
    t_xt = [Tok("e_x%d" % i) for i in range(2)]
    scr = [kb.sbuf("e_scr%d" % i, [128, 1024], F32) for i in range(2)]
    t_scr = [Tok("e_scr%d" % i) for i in range(2)]
    h2f = [kb.sbuf("e_h2f%d" % i, [128, 1024], F32) for i in range(2)]
    t_h2f = [Tok("e_h2f%d" % i) for i in range(2)]
    stat = [kb.sbuf("e_st%d" % i, [128, 4], F32) for i in range(2)]
    t_st = [Tok("e_st%d" % i) for i in range(2)]
    h2Tf = kb.sbuf("e_h2Tf", [128, 8, 128], F32)
    t_h2Tf = Tok("e_h2Tf")
    R = [kb.sbuf("e_R%d" % i, [128, 160], F32) for i in range(2)]
    t_R = [Tok("e_R%d" % i) for i in range(2)]
    sgl = [kb.sbuf("e_sg%d" % i, [128, 512], F32) for i in range(2)]
    t_sgl = [Tok("e_sg%d" % i) for i in range(2)]
    aT = [kb.sbuf("e_aT%d" % i, [128, 2, 512], BF16) for i in range(2)]
    t_aT = [Tok("e_aT%d" % i) for i in range(2)]
    bank = [kb.psum("e_bank%d" % i, [128, 512], F32) for i in range(8)]
    t_bank = [Tok("e_bank%d" % i) for i in range(8)]
    wload = 0

    def load_expert(e):
        nonlocal wload
        k = wload % 2
        wload += 1
        for c in range(8):
            kb.dma("gpsimd", weg[k][:, c, :], w["weg"][e, c * 128:(c + 1) * 128, :], writes=[t_we[k]])
        for c in range(8):
            kb.dma("gpsimd", weu[k][:, c, :], w["weu"][e, c * 128:(c + 1) * 128, :], writes=[t_we[k]])
        for c in range(2):
            kb.dma("gpsimd", wed[k][:, c, :], w["wed"][e, c * 128:(c + 1) * 128, :], writes=[t_we[k]])
        return k

    for st in range(NST):
        for i in range(NTL):
            s = i % 2
            t0 = st * TS + i * 128
            kb.dma("sync", xt[s][:], xmid[t0:t0 + 128, :], reads=[t_xmid], writes=[t_xt[s]])
            emit_norm_mod(kb, xt[s], t_xt[s], gmul2, t_gmul2, shift2, t_shift2, h2f[s], t_h2f[s],
                          scr[s], t_scr[s], stat[s], t_st[s], consts["eps"][:, 0:1])
            for half in range(2):
                for c4 in range(4):
                    c = half * 4 + c4
                    kb.op("tensor", lambda e, s=s, c=c, half=half, c4=c4: e.transpose(
                        out=bank[half][:, c4 * 128:(c4 + 1) * 128], in_=h2f[s][:, c * 128:(c + 1) * 128],
                        identity=consts["id_f"][:]), reads=[t_h2f[s], ct], writes=[t_bank[half]], sig=(c4 == 3))
                kb.op("scalar", lambda e, half=half: e.copy(
                    out=h2Tf[:, half * 4:half * 4 + 4, :], in_=bank[half][:].rearrange("p (c t) -> p c t", c=4)),
                    reads=[t_bank[half]], writes=[t_h2Tf])
            kb.op("gpsimd", lambda e, i=i: e.tensor_copy(out=h2T[:, :, i * 128:(i + 1) * 128], in_=h2Tf[:]),
                  reads=[t_h2Tf], writes=[t_h2T])
            for c in range(8):
                _mm(kb, bank[2][:, 0:36], h2Tf[:, c, :], wr[:, c, :], c == 0, c == 7, [t_h2Tf, t_wr], [t_bank[2]])
            r = R[s]
            tr = t_R[s]
            V = lambda fn, reads, writes: kb.op("vector", fn, reads=reads, writes=writes)
            V(lambda e: e.tensor_tensor(out=r[:, 0:36], in0=bank[2][:, 0:36], in1=rb[:], op=ALU.add),
              [t_bank[2], t_wr], [tr])
            V(lambda e: e.reduce_max(out=r[:, 36:37], in_=r[:, 0:4], axis=AX.X), [tr], [tr])
            V(lambda e: e.tensor_scalar_mul(out=r[:, 37:38], in0=r[:, 36:37], scalar1=-1.0), [tr], [tr])
            kb.op("scalar", lambda e: e.activation(out=r[:, 40:44], in_=r[:, 0:4], func=AF.Exp, bias=r[:, 37:38]),
                  reads=[tr], writes=[tr])
            V(lambda e: e.reduce_sum(out=r[:, 44:45], in_=r[:, 40:44], axis=AX.X), [tr], [tr])
            V(lambda e: e.reciprocal(out=r[:, 45:46], in_=r[:, 44:45]), [tr], [tr])
            V(lambda e: e.tensor_scalar(out=r[:, 48:52], in0=r[:, 0:4], scalar1=r[:, 36:37], scalar2=None,
                                        op0=ALU.is_equal), [tr], [tr])
            V(lambda e: e.tensor_scalar(out=r[:, 52:56], in0=r[:, 48:52], scalar1=-1.0, scalar2=BIG,
                                        op0=ALU.add, op1=ALU.mult), [tr], [tr])
            V(lambda e: e.tensor_tensor(out=r[:, 64:96].rearrange("p (g k) -> p g k", g=4),
                                        in0=r[:, 4:36].rearrange("p (g k) -> p g k", g=4),
                                        in1=r[:, 52:56].unsqueeze(2).to_broadcast([128, 4, 8]), op=ALU.add),
              [tr], [tr])
            V(lambda e: e.reduce_max(out=r[:, 96:97], in_=r[:, 64:96], axis=AX.X), [tr], [tr])
            V(lambda e: e.tensor_scalar(out=r[:, 104:136], in0=r[:, 64:96], scalar1=r[:, 96:97], scalar2=None,
                                        op0=ALU.is_equal), [tr], [tr])
            V(lambda e: e.scalar_tensor_tensor(out=r[:, 64:96], in0=r[:, 104:136], scalar=-BIG, in1=r[:, 64:96],
                                               op0=ALU.mult, op1=ALU.add), [tr], [tr])
            V(lambda e: e.reduce_max(out=r[:, 97:98], in_=r[:, 64:96], axis=AX.X), [tr], [tr])
            V(lambda e: e.tensor_tensor(out=r[:, 98:99], in0=r[:, 97:98], in1=r[:, 96:97], op=ALU.subtract),
              [tr], [tr])
            kb.op("scalar", lambda e: e.activation(out=r[:, 99:100], in_=r[:, 98:99], func=AF.Exp),
                  reads=[tr], writes=[tr])
            V(lambda e: e.tensor_scalar_add(out=r[:, 100:101], in0=r[:, 99:100], scalar1=1.0), [tr], [tr])
            V(lambda e: e.reciprocal(out=r[:, 101:102], in_=r[:, 100:101]), [tr], [tr])
            V(lambda e: e.tensor_tensor(out=r[:, 102:103], in0=r[:, 101:102], in1=r[:, 45:46], op=ALU.mult),
              [tr], [tr])
            V(lambda e: e.tensor_tensor(out=r[:, 103:104], in0=r[:, 102:103], in1=r[:, 99:100], op=ALU.mult),
              [tr], [tr])
            V(lambda e, i=i: e.tensor_scalar_mul(out=Wt[:, i, :], in0=r[:, 104:136], scalar1=r[:, 102:103]),
              [tr], [t_Wt[i]])
            V(lambda e: e.tensor_scalar(out=r[:, 104:136], in0=r[:, 64:96], scalar1=r[:, 97:98], scalar2=None,
                                        op0=ALU.is_equal), [tr], [tr])
            V(lambda e, i=i: e.scalar_tensor_tensor(out=Wt[:, i, :], in0=r[:, 104:136], scalar=r[:, 103:104],
                                                    in1=Wt[:, i, :], op0=ALU.mult, op1=ALU.add),
              [tr, t_Wt[i]], [t_Wt[i]])
        steps = [(ex, tl) for ex in range(32) for tl in range(TS // 512)]
        wslot = {}
        gcount = [0]
        dcount = [0]

        def gu_group(si, hc, which):
            ex, tl = steps[si]
            k = wslot[ex]
            tsl = slice(tl * 512, (tl + 1) * 512)
            hsl = slice(hc * 128, (hc + 1) * 128)
            pg = hc
            if which == 0:
                for c in range(8):
                    _mm(kb, bank[pg][:], weg[k][:, c, hsl], h2T[:, c, tsl], c == 0, c == 7,
                        [t_we[k], t_h2T], [t_bank[pg]])
                kb.op("scalar", lambda e: e.activation(out=sgl[pg][:], in_=bank[pg][:], func=AF.Silu),
                      reads=[t_bank[pg]], writes=[t_sgl[pg]])
            else:
                a = si % 2
                for c in range(8):
                    _mm(kb, bank[2 + pg][:], weu[k][:, c, hsl], h2T[:, c, tsl], c == 0, c == 7,
                        [t_we[k], t_h2T], [t_bank[2 + pg]])
                kb.op("vector", lambda e: e.tensor_tensor(out=aT[a][:, hc, :], in0=bank[2 + pg][:], in1=sgl[pg][:],
                                                          op=ALU.mult),
                      reads=[t_bank[2 + pg], t_sgl[pg]], writes=[t_aT[a]])

        def d_group(si, j, half):
            ex, tl = steps[si]
            k = wslot[ex]
            a = si % 2
            ti = tl * 4 + j
            pd = 4 + dcount[0] % 4
            dcount[0] += 1
            osl = slice(half * 512, (half + 1) * 512)
            for hc in range(2):
                _mm(kb, bank[pd][:], aT[a][:, hc, j * 128:(j + 1) * 128], wed[k][:, hc, osl],
                    hc == 0, hc == 1, [t_aT[a], t_we[k]], [t_bank[pd]])
            if ex == 0:
                kb.op("vector", lambda e: e.tensor_scalar_mul(out=yacc[:, ti, osl], in0=bank[pd][:],
                                                              scalar1=Wt[:, ti, ex:ex + 1]),
                      reads=[t_bank[pd], t_Wt[ti]], writes=[t_yacc[ti]])
            else:
                kb.op("vector", lambda e: e.scalar_tensor_tensor(out=yacc[:, ti, osl], in0=bank[pd][:],
                                                                 scalar=Wt[:, ti, ex:ex + 1], in1=yacc[:, ti, osl],
                                                                 op0=ALU.mult, op1=ALU.add),
                      reads=[t_bank[pd], t_Wt[ti], t_yacc[ti]], writes=[t_yacc[ti]])

        dlist = [(j, half) for j in range(4) for half in range(2)]
        for si in range(len(steps) + 1):
            if si < len(steps):
                ex, tl = steps[si]
                if tl == 0:
                    wslot[ex] = load_expert(ex)
            gul = [(0, 0), (0, 1), (1, 0), (1, 1)]
            for gidx in range(4):
                if si < len(steps):
                    gu_group(si, gul[gidx][0], gul[gidx][1])
                if si > 0:
                    for (j, half) in dlist[2 * gidx:2 * gidx + 2]:
                        d_group(si - 1, j, half)
        for i in range(NTL):
            s = i % 2
            t0 = st * TS + i * 128
            kb.dma("sync", xt[s][:], xmid[t0:t0 + 128, :], reads=[t_xmid], writes=[t_xt[s]])
            kb.op("vector", lambda e, i=i: e.tensor_tensor(out=yacc[:, i, :], in0=yacc[:, i, :], in1=gate2[:],
                                                           op=ALU.mult),
                  reads=[t_yacc[i], t_gate2], writes=[t_yacc[i]])
            kb.op("gpsimd", lambda e, s=s, i=i: e.tensor_tensor(out=xt[s][:], in0=xt[s][:], in1=yacc[:, i, :],
                                                                op=ALU.add),
                  reads=[t_xt[s], t_yacc[i]], writes=[t_xt[s]])
            if final_g is None:
                kb.dma("gpsimd", xout[t0:t0 + 128, :], xt[s][:], reads=[t_xt[s]], writes=[t_xout])
            else:
                emit_norm_mod(kb, xt[s], t_xt[s], fg, t_wr, None, None, h2f[s], t_h2f[s],
                              scr[s], t_scr[s], stat[s], t_st[s], consts["eps"][:, 0:1])
                kb.dma("gpsimd", xout[t0:t0 + 128, :], h2f[s][:], reads=[t_h2f[s]], writes=[t_xout])
    kb.release(mk)


LAYER_SHAPES = dict(
    modw=[D, 6144], modb=[6144], g1=[D],
    wqk=[D, 1024], wtm=[D, 2056], cw=[128, 8, 4], cb=[128, 8], ifb=[8],
    wmla=[D, 768], gqkv=[128, 5], wuqn=[384, 1024], wuqr=[384, 512], wuqs=[384, 512],
    wuk=[256, 1024], wuv=[256, 1024],
    wgate=[D, 2048], wbm=[D, D], wba=[D, D], wout=[D, D], g2=[D], wr=[D, 36], rb=[36],
    weg=[32, D, 256], weu=[32, D, 256], wed=[32, 256, D])
DEPTH = 2


def build_full(S):
    kb = KB()
    x = kb.dram_in("x", [S, D], F32)
    cvec = kb.dram_in("cvec", [D], F32)
    cs = kb.dram_in("cs", [64, 2, S], F32)
    fg = kb.dram_in("fg", [D], F32)
    W = []
    for l in range(DEPTH):
        W.append({k: kb.dram_in("%s_%d" % (k, l), v, F32) for k, v in LAYER_SHAPES.items()})
        W[l]["cs"] = cs
    out = kb.dram_out("out", [S, D], F32)
    hT = kb.dram_tmp("s_hT", [8, 128, S], BF16)
    ymT = kb.dram_tmp("s_ymT", [8, 128, S], BF16)
    yaT = kb.dram_tmp("s_yaT", [8, 128, S], BF16)
    xmid = kb.dram_tmp("s_xmid", [S, D], F32)
    xnext = kb.dram_tmp("s_xnext", [S, D], F32)
    scr = alloc_mla_scratch(kb, S)
    t_hT, t_ymT, t_yaT = Tok("dram:hT"), Tok("dram:ymT"), Tok("dram:yaT")
    t_xmid, t_xnext, t_out, t_x = Tok("dram:xmid"), Tok("dram:xnext"), Tok("dram:out"), Tok("dram:x")
    consts = load_consts(kb)
    x_cur, t_xcur = x, t_x
    for l in range(DEPTH):
        w = W[l]
        mk = kb.mark()
        emit_k1_body(kb, consts, x_cur, cvec, w["modw"][:, 0:2048], w["modb"][0:2048], w["g1"], hT, S,
                     hT_tok=t_hT, x_tok=t_xcur)
        kb.release(mk)
        emit_mlstm_pass(kb, consts, S, hT, t_hT, w, ymT, t_ymT)
        emit_mla_proj_pass(kb, consts, S, hT, t_hT, w, scr)
        emit_attn_pass(kb, consts, S, scr, yaT, t_yaT)
        mk = kb.mark()
        mods = [kb.sbuf("mod%d" % i, [128, D], F32) for i in range(4)]
        tm = [Tok("mod%d" % i) for i in range(4)]
        g2t = kb.sbuf("g2t", [128, D], F32)
        tg2 = Tok("g2t")
        emit_mod(kb, consts, w["modw"][:, 2048:6144], w["modb"][2048:6144], cvec, 4, list(zip(mods, tm)))
        kb.dma("sync", g2t[:], w["g2"].partition_broadcast(128), writes=[tg2])
        kb.op("vector", lambda e: e.scalar_tensor_tensor(out=mods[2][:], in0=mods[2][:], scalar=1.0, in1=g2t[:],
                                                         op0=ALU.add, op1=ALU.mult),
              reads=[tm[2], tg2], writes=[tm[2]])
        t_in_all = _MultiTok([t_hT, t_ymT, t_yaT])
        emit_merge_pass(kb, consts, S, hT, ymT, yaT, t_in_all, x_cur, t_xcur, w, mods[0], tm[0], xmid, t_xmid)
        last = (l == DEPTH - 1)
        emit_moe_pass(kb, consts, S, xmid, t_xmid, w, mods[2], tm[2], mods[1], tm[1], mods[3], tm[3],
                      out if last else xnext, t_out if last else t_xnext, final_g=fg if last else None)
        kb.release(mk)
        x_cur, t_xcur = xnext, t_xnext
    kb.finish([t_out])
    return kb


class _MultiTok:
    def __init__(self, toks):
        self.name = "dram:multi"
        self._toks = toks


def _rope_tables(S):
    pos = np.arange(S, dtype=np.float32)
    inv_freq = (1.0 / (np.float32(10000.0) ** (np.arange(0, 64, 2, dtype=np.float32) / np.float32(64)))).astype(np.float32)
    ang = pos[:, None] * inv_freq[None, :]
    cos = np.cos(ang).astype(np.float32)
    sin = np.sin(ang).astype(np.float32)
    CC = np.concatenate([cos, cos], 1).T
    SS = np.concatenate([-sin, sin], 1).T
    return np.ascontiguousarray(np.stack([CC, SS], 1))


def _prep_layer(z, l):
    c = np.ascontiguousarray
    win = z["w_in"][l]
    d = {}
    d["modw"] = c(z["mod_w"][l])
    d["modb"] = c(z["mod_b"][l])
    d["g1"] = c(z["norm1_g"][l])
    d["wqk"] = c(win[:, :1024])
    d["wtm"] = c(win[:, 1024:3080])
    d["cw"] = c(z["conv_w"][l].reshape(4, 8, 128).transpose(2, 1, 0))
    d["cb"] = c(z["conv_b"][l].reshape(8, 128).T)
    d["ifb"] = c(np.concatenate([z["igate_b"][l], z["fgate_b"][l]]))
    kr = win[:, 3720:3784]
    d["wmla"] = c(np.concatenate([win[:, 3080:3784], kr[:, 32:], kr[:, :32]], 1))
    d["gqkv"] = c(np.concatenate([z["q_norm_g"][l].reshape(3, 128).T, z["kv_norm_g"][l].reshape(2, 128).T], 1))
    uq = z["w_uq"][l].reshape(384, 8, 192)
    d["wuqn"] = c(uq[:, :, :128].reshape(384, 1024))
    d["wuqr"] = c(uq[:, :, 128:].reshape(384, 512))
    d["wuqs"] = c(np.concatenate([uq[:, :, 160:], uq[:, :, 128:160]], 2).reshape(384, 512))
    ukv = z["w_ukv"][l].reshape(256, 8, 256)
    d["wuk"] = c(ukv[:, :, :128].reshape(256, 1024))
    d["wuv"] = c(ukv[:, :, 128:].reshape(256, 1024))
    d["wgate"] = c(win[:, 3784:5832])
    d["wbm"] = c(z["w_branch_m"][l])
    d["wba"] = c(z["w_branch_a"][l])
    d["wout"] = c(z["w_out"][l])
    d["g2"] = c(z["norm2_g"][l])
    d["wr"] = c(np.concatenate([z["w_group"][l], z["w_router"][l]], 1))
    d["rb"] = c(np.concatenate([z["b_group"][l], z["b_router"][l]]))
    d["weg"] = c(z["w_expert_gate"][l])
    d["weu"] = c(z["w_expert_up"][l])
    d["wed"] = c(z["w_expert_down"][l])
    return d


_PROGRAMS = {}


def kernel(**inputs):
    z = {k: np.asarray(v) for k, v in inputs.items()}
    x = z["x"].astype(np.float32, copy=False)
    B, S, _ = x.shape
    if S not in _PROGRAMS:
        _PROGRAMS[S] = build_full(S)
    kb = _PROGRAMS[S]
    shared = {"cs": _rope_tables(S), "fg": np.ascontiguousarray(z["final_norm_g"].astype(np.float32))}
    for l in range(DEPTH):
        for k, v in _prep_layer(z, l).items():
            assert list(v.shape) == LAYER_SHAPES[k], (k, v.shape)
            shared["%s_%d" % (k, l)] = v.astype(np.float32, copy=False)
    in_maps = []
    for b in range(B):
        m = dict(shared)
        m["x"] = np.ascontiguousarray(x[b])
        m["cvec"] = np.ascontiguousarray(z["c"][b].astype(np.float32))
        in_maps.append(m)
    res = run_bass_kernel_spmd(kb.nc, in_maps, core_ids=list(range(B)))
    return np.stack([np.asarray(r["out"]) for r in res.results], 0).astype(np.float32)
```

```python
import math
import numpy as np
import ml_dtypes
import concourse.bass as bass
import concourse.mybir as mybir
from concourse.bass_utils import run_bass_kernel_spmd

F32 = mybir.dt.float32
BF16 = mybir.dt.bfloat16
AF = mybir.ActivationFunctionType
ALU = mybir.AluOpType
AX = mybir.AxisListType

D = 1024
NCORES = 8
EPS = 1e-6
BIG = 30000.0


class Tok:
    __slots__ = ("w", "r", "wsem", "rsem", "name")

    def __init__(self, name=""):
        self.w = None
        self.r = []
        self.wsem = None
        self.rsem = None
        self.name = name


class _Sem:
    __slots__ = ("h", "cnt", "key")

    def __init__(self, h, key):
        self.h = h
        self.cnt = 0
        self.key = key


class _Eng:
    def __init__(self, name, e, sem):
        self.name = name
        self.e = e
        self.sem = sem
        self.seen = {}


class KB:
    def __init__(self):
        self.nc = bass.Bass("TRN2", target_bir_lowering=False)
        nc = self.nc
        self._nsem = 0
        self.eng = {}
        for name in ("tensor", "vector", "scalar", "gpsimd", "sync"):
            self.eng[name] = _Eng(name, getattr(nc, name), self._newsem("e_" + name))
        self._stack = []
        self._dsems = []
        self._free_dsems = []
        self._live_dsems = []
        self.ninst = 0

    def _newsem(self, name):
        if not name.startswith("e_") and self._free_dsems:
            sem = self._free_dsems.pop()
            self._live_dsems.append(sem)
            return sem
        self._nsem += 1
        h = self.nc.semaphore(name + "_%d" % self._nsem).__enter__()
        sem = _Sem(h, self._nsem)
        if not name.startswith("e_"):
            self._dsems.append(sem)
            self._live_dsems.append(sem)
        return sem

    def dram_in(self, name, shape, dt):
        return self.nc.dram_tensor(name, list(shape), dt, kind="ExternalInput").ap()

    def dram_out(self, name, shape, dt):
        return self.nc.dram_tensor(name, list(shape), dt, kind="ExternalOutput").ap()

    def dram_tmp(self, name, shape, dt):
        return self.nc.dram_tensor(name, list(shape), dt, kind="Internal").ap()

    def sbuf(self, name, shape, dt):
        self._uid = getattr(self, "_uid", 0) + 1
        cm = self.nc.sbuf_tensor("%s_u%d" % (name, self._uid), list(shape), dt)
        t = cm.__enter__()
        self._stack.append(cm)
        return t

    def psum(self, name, shape, dt):
        self._uid = getattr(self, "_uid", 0) + 1
        cm = self.nc.psum_tensor("%s_u%d" % (name, self._uid), list(shape), dt)
        t = cm.__enter__()
        self._stack.append(cm)
        return t

    def mark(self):
        return (len(self._stack), len(self._live_dsems))

    def release(self, mark):
        self.barrier()
        while len(self._stack) > mark[0]:
            self._stack.pop().__exit__(None, None, None)
        while len(self._live_dsems) > mark[1]:
            self._free_dsems.append(self._live_dsems.pop())

    def barrier(self):
        sems = [E.sem for E in self.eng.values()] + self._dsems
        for E in self.eng.values():
            for sem in sems:
                if sem is E.sem or sem.cnt == 0:
                    continue
                if E.seen.get(sem.key, 0) >= sem.cnt:
                    continue
                E.e.wait_ge(sem.h, sem.cnt)
                E.seen[sem.key] = sem.cnt
                self.ninst += 1

    def _wait(self, E, evs):
        need = {}
        for ev in evs:
            if ev is None:
                continue
            sem, val = ev
            if sem is E.sem and E.name == "tensor":
                continue
            if need.get(sem.key, (None, 0))[1] < val:
                need[sem.key] = (sem, val)
        for key, (sem, val) in need.items():
            if E.seen.get(key, 0) >= val:
                continue
            assert val <= sem.cnt, "waiting on an event that is never signalled"
            E.e.wait_ge(sem.h, val)
            E.seen[key] = val
            self.ninst += 1

    def _deps(self, reads, writes):
        evs = []
        for t in reads:
            if hasattr(t, "_toks"):
                evs.extend(x.w for x in t._toks)
            else:
                evs.append(t.w)
        for t in writes:
            evs.append(t.w)
            evs.extend(t.r)
        return evs

    def op(self, engname, fn, reads=(), writes=(), sig=True):
        E = self.eng[engname]
        self._wait(E, self._deps(reads, writes))
        ins = fn(E.e)
        self.ninst += 1
        if sig:
            E.sem.cnt += 1
            ins.then_inc(E.sem.h, 1)
            ev = (E.sem, E.sem.cnt)
        else:
            assert engname == "tensor"
            ev = (E.sem, E.sem.cnt + 1)
        for t in reads:
            for tt_ in (t._toks if hasattr(t, "_toks") else (t,)):
                tt_.r.append(ev)
                if len(tt_.r) > 24:
                    tt_.r = self._compact(tt_.r)
        for t in writes:
            t.w = ev
            t.r = []
        return ins

    @staticmethod
    def _compact(r):
        best = {}
        for sem, val in r:
            if best.get(sem.key, (None, 0))[1] < val:
                best[sem.key] = (sem, val)
        return list(best.values())

    def dma(self, queue, out, in_, reads=(), writes=(), **kw):
        E = self.eng[queue]
        evs = self._deps(reads, [])
        for t in writes:
            isdram = getattr(t, "name", "").startswith("dram:")
            if t.w is not None and not isdram and not (t.wsem is not None and t.w[0] is t.wsem):
                evs.append(t.w)
            evs.extend(t.r)
        self._wait(E, evs)
        ins = E.e.dma_start(out=out, in_=in_, **kw)
        self.ninst += 1
        if writes and writes[0].wsem is None and not getattr(writes[0], "_dram", False):
            pass
        tok = None
        for t in writes:
            if not getattr(t, "name", "").startswith("dram:"):
                tok = t
                if tok.wsem is None:
                    tok.wsem = self._newsem("dw")
                sem = tok.wsem
                break
        if tok is None:
            for t in reads:
                if not getattr(t, "name", "").startswith("dram:"):
                    tok = t
                    if tok.rsem is None:
                        tok.rsem = self._newsem("dr")
                    sem = tok.rsem
                    break
        assert tok is not None, "dma needs an sbuf-side token"
        sem.cnt += 16
        ins.then_inc(sem.h, 16)
        ev = (sem, sem.cnt)
        for t in reads:
            for tt_ in (t._toks if hasattr(t, "_toks") else (t,)):
                tt_.r.append(ev)
                if len(tt_.r) > 24:
                    tt_.r = self._compact(tt_.r)
        for t in writes:
            t.w = ev
            t.r = []
        return ins

    def finish(self, toks):
        E = self.eng["sync"]
        evs = []
        for t in toks:
            evs.append(t.w)
            evs.extend(t.r)
        self._wait(E, evs)


def load_consts(kb):
    nc = kb.nc
    c = {}
    c["tok"] = Tok("consts")
    ones_f = kb.sbuf("c_ones_f", [128, 128], F32)
    ones_b = kb.sbuf("c_ones_b", [128, 128], BF16)
    id_f = kb.sbuf("c_id_f", [128, 128], F32)
    id_b = kb.sbuf("c_id_b", [128, 128], BF16)
    tri_f = kb.sbuf("c_tri_f", [128, 128], F32)
    tris_f = kb.sbuf("c_tris_f", [128, 128], F32)
    tri_b = kb.sbuf("c_tri_b", [128, 128], BF16)
    t = c["tok"]
    kb.op("gpsimd", lambda e: e.memset(ones_f[:], 1.0), writes=[t])
    kb.op("gpsimd", lambda e: e.memset(ones_b[:], 1.0), writes=[t])
    kb.op("gpsimd", lambda e: e.affine_select(out=id_f[:], in_=ones_f[:], pattern=[[-1, 128]],
                                             compare_op=ALU.is_equal, fill=0.0, base=0,
                                             channel_multiplier=1), reads=[t], writes=[t])
    kb.op("gpsimd", lambda e: e.tensor_copy(out=id_b[:], in_=id_f[:]), reads=[t], writes=[t])
    kb.op("gpsimd", lambda e: e.affine_select(out=tri_f[:], in_=ones_f[:], pattern=[[1, 128]],
                                             compare_op=ALU.is_ge, fill=0.0, base=0,
                                             channel_multiplier=-1), reads=[t], writes=[t])
    kb.op("gpsimd", lambda e: e.tensor_copy(out=tri_b[:], in_=tri_f[:]), reads=[t], writes=[t])
    kb.op("gpsimd", lambda e: e.affine_select(out=tris_f[:], in_=ones_f[:], pattern=[[-1, 128]],
                                             compare_op=ALU.is_gt, fill=0.0, base=0,
                                             channel_multiplier=1), reads=[t], writes=[t])
    eps = kb.sbuf("c_eps", [128, 1], F32)
    kb.op("gpsimd", lambda e: e.memset(eps[:], EPS), writes=[t])
    c["eps"] = eps
    one = kb.sbuf("c_one", [128, 1], F32)
    kb.op("gpsimd", lambda e: e.memset(one[:], 1.0), writes=[t])
    c["one"] = one
    ones_b512 = kb.sbuf("c_ones_b512", [128, 512], BF16)
    kb.op("gpsimd", lambda e: e.memset(ones_b512[:], 1.0), writes=[t])
    c["ones_b512"] = ones_b512
    c.update(ones_f=ones_f, ones_b=ones_b, id_f=id_f, id_b=id_b, tri_f=tri_f, tris_f=tris_f,
             tri_b=tri_b)
    return c


def emit_mod(kb, consts, modw, modb, cvec, nvec, outs, ps_tag="modps"):
    nc = kb.nc
    mk = kb.mark()
    cpc = kb.sbuf("mod_cpc", [128, 8], F32)
    cond = kb.sbuf("mod_cond", [128, 8], F32)
    bc = kb.sbuf("mod_bc", [128, 8, 128], F32)
    wt = kb.sbuf("mod_w", [128, 8, 1024], F32)
    bt = kb.sbuf("mod_b", [128, 1024], F32)
    ps = kb.psum(ps_tag, [128, 512], F32)
    t_c, t_cond, t_bc, t_w, t_b, t_ps = (Tok("m%d" % i) for i in range(6))
    kb.dma("sync", cpc[:], cvec.rearrange("(p c) -> p c", c=8), writes=[t_c])
    kb.op("scalar", lambda e: e.activation(out=cond[:], in_=cpc[:], func=AF.Silu),
          reads=[t_c], writes=[t_cond])
    for c in range(8):
        kb.op("vector", lambda e, c=c: e.tensor_scalar_mul(out=bc[:, c, :], in0=consts["ones_f"][:],
                                                           scalar1=cond[:, c:c + 1]),
              reads=[t_cond, consts["tok"]], writes=[t_bc])
    for v in range(nvec):
        out_t, out_tok = outs[v]
        kb.dma("sync", wt[:], modw[:, v * 1024:(v + 1) * 1024].rearrange("(p c) n -> p c n", c=8),
               writes=[t_w])
        kb.dma("sync", bt[:], modb[v * 1024:(v + 1) * 1024].partition_broadcast(128), writes=[t_b])
        for n2 in range(2):
            for c in range(8):
                kb.op("tensor", lambda e, c=c, n2=n2: e.matmul(ps[:], lhsT=bc[:, c, :],
                                                               rhs=wt[:, c, n2 * 512:(n2 + 1) * 512],
                                                               start=(c == 0), stop=(c == 7)),
                      reads=[t_bc, t_w], writes=[t_ps], sig=(c == 7))
            kb.op("vector", lambda e, n2=n2: e.tensor_tensor(out=out_t[:, n2 * 512:(n2 + 1) * 512],
                                                             in0=ps[:], in1=bt[:, n2 * 512:(n2 + 1) * 512],
                                                             op=ALU.add),
                  reads=[t_ps, t_b], writes=[out_tok])
    kb.release(mk)


def emit_norm_mod(kb, xt, t_x, gmul, t_g, shift, t_s, hb, t_h, scr, t_scr, stat, t_stat, eps_ap):
    kb.op("gpsimd", lambda e: e.memset(stat[:, 0:1], 0.0), writes=[t_stat])
    kb.op("scalar", lambda e: e.activation(out=scr[:], in_=xt[:], func=AF.Square,
                                           accum_out=stat[:, 0:1]),
          reads=[t_x], writes=[t_scr, t_stat])
    kb.op("scalar", lambda e: e.activation(out=stat[:, 1:2], in_=stat[:, 0:1], func=AF.Sqrt,
                                           bias=eps_ap, scale=1.0 / D),
          reads=[t_stat], writes=[t_stat])
    kb.op("vector", lambda e: e.reciprocal(out=stat[:, 2:3], in_=stat[:, 1:2]),
          reads=[t_stat], writes=[t_stat])
    if shift is None:
        kb.op("vector", lambda e: e.scalar_tensor_tensor(out=hb[:], in0=xt[:], scalar=stat[:, 2:3],
                                                         in1=gmul[:], op0=ALU.mult, op1=ALU.mult),
              reads=[t_x, t_stat, t_g], writes=[t_h])
        return
    kb.op("vector", lambda e: e.scalar_tensor_tensor(out=scr[:], in0=xt[:], scalar=stat[:, 2:3],
                                                     in1=gmul[:], op0=ALU.mult, op1=ALU.mult),
          reads=[t_x, t_stat, t_g], writes=[t_scr])
    kb.op("gpsimd", lambda e: e.tensor_tensor(out=hb[:], in0=scr[:], in1=shift[:], op=ALU.add),
          reads=[t_scr, t_s], writes=[t_h])


def build_k1(Sh):
    kb = KB()
    nc = kb.nc
    x = kb.dram_in("x", [Sh, D], F32)
    cvec = kb.dram_in("cvec", [D], F32)
    modw = kb.dram_in("modw", [D, 2 * D], F32)
    modb = kb.dram_in("modb", [2 * D], F32)
    g = kb.dram_in("g", [D], F32)
    hT = kb.dram_out("hT", [8, 128, Sh], BF16)
    consts = load_consts(kb)
    emit_k1_body(kb, consts, x, cvec, modw, modb, g, hT, Sh)
    kb.finish([kb._out_tok])
    return kb


def emit_k1_body(kb, consts, x, cvec, modw, modb, g, hT, Sh, hT_tok=None, x_tok=None):
    shift = kb.sbuf("k1_shift", [128, D], F32)
    gmul = kb.sbuf("k1_gmul", [128, D], F32)
    gt = kb.sbuf("k1_g", [128, D], F32)
    t_shift, t_gmul, t_g = Tok("shift"), Tok("gmul"), Tok("g")
    emit_mod(kb, consts, modw, modb, cvec, 2, [(shift, t_shift), (gmul, t_gmul)])
    kb.dma("sync", gt[:], g.partition_broadcast(128), writes=[t_g])
    kb.op("vector", lambda e: e.scalar_tensor_tensor(out=gmul[:], in0=gmul[:], scalar=1.0, in1=gt[:],
                                                     op0=ALU.add, op1=ALU.mult),
          reads=[t_gmul, t_g], writes=[t_gmul])
    NB = 2
    xt = [kb.sbuf("k1_x%d" % i, [128, D], F32) for i in range(NB)]
    scr = [kb.sbuf("k1_scr%d" % i, [128, D], F32) for i in range(NB)]
    hb = [kb.sbuf("k1_hb%d" % i, [128, D], BF16) for i in range(NB)]
    stat = [kb.sbuf("k1_st%d" % i, [128, 4], F32) for i in range(NB)]
    hTs = [kb.sbuf("k1_hT%d" % i, [128, 8, 512], BF16) for i in range(2)]
    pst = [kb.psum("k1_pst%d" % i, [128, 8, 128], BF16) for i in range(2)]
    t_x = [Tok("x%d" % i) for i in range(NB)]
    t_scr = [Tok("scr%d" % i) for i in range(NB)]
    t_hb = [Tok("hb%d" % i) for i in range(NB)]
    t_st = [Tok("st%d" % i) for i in range(NB)]
    t_hT = [Tok("hT%d" % i) for i in range(2)]
    t_ps = [Tok("ps%d" % i) for i in range(2)]
    t_out = hT_tok if hT_tok is not None else Tok("dram:hT")
    kb._out_tok = t_out
    ntile = Sh // 128
    for i in range(ntile):
        s = i % NB
        grp = (i // 4) % 2
        kb.dma("sync", xt[s][:], x[i * 128:(i + 1) * 128, :], reads=([x_tok] if x_tok is not None else []), writes=[t_x[s]])
        emit_norm_mod(kb, xt[s], t_x[s], gmul, t_gmul, shift, t_shift, hb[s], t_hb[s],
                      scr[s], t_scr[s], stat[s], t_st[s], consts["eps"][:, 0:1])
        p = i % 2
        for c in range(8):
            kb.op("tensor", lambda e, c=c, s=s, p=p: e.transpose(out=pst[p][:, c, :],
                                                                 in_=hb[s][:, c * 128:(c + 1) * 128],
                                                                 identity=consts["id_b"][:]),
                  reads=[t_hb[s], consts["tok"]], writes=[t_ps[p]], sig=(c == 7))
        j = i % 4
        kb.op("scalar", lambda e, p=p, grp=grp, j=j: e.copy(out=hTs[grp][:, :, j * 128:(j + 1) * 128],
                                                            in_=pst[p][:]),
              reads=[t_ps[p]], writes=[t_hT[grp]])
        if j == 3:
            t0 = (i - 3) * 128
            kb.dma("gpsimd", hT[:, :, t0:t0 + 512].rearrange("c p t -> p c t"), hTs[grp][:],
                   reads=[t_hT[grp]], writes=[t_out])


def _mm(kb, out, lhsT, rhs, start, stop, reads, writes):
    return kb.op("tensor", lambda e: e.matmul(out, lhsT=lhsT, rhs=rhs, start=start, stop=stop),
                 reads=reads, writes=writes, sig=stop)


def load_w_bf16(kb, dst, src_rows_cols, tok, nchunk, queue="gpsimd"):
    for c in range(nchunk):
        kb.dma(queue, dst[:, c, :], src_rows_cols[c * 128:(c + 1) * 128, :], writes=[tok])


def emit_mlstm_pass(kb, consts, S, hT, t_hT, w, ymT, t_ymT):
    NT = S // 512
    mk = kb.mark()
    ct = consts["tok"]
    wqk = kb.sbuf("a_wqk", [128, 8, 1024], BF16)
    wtm = kb.sbuf("a_wtm", [128, 8, 2056], BF16)
    cw = kb.sbuf("a_cw", [128, 8, 4], F32)
    cb = kb.sbuf("a_cb", [128, 8], F32)
    ifb = kb.sbuf("a_ifb", [128, 8], F32)
    t_w = Tok("a_w")
    t_wt = Tok("a_wt")
    t_cw = Tok("a_cw")
    load_w_bf16(kb, wqk, w["wqk"], t_w, 8)
    load_w_bf16(kb, wtm, w["wtm"], t_wt, 8)
    kb.dma("sync", cw[:], w["cw"], writes=[t_cw])
    kb.dma("sync", cb[:], w["cb"], writes=[t_cw])
    kb.dma("sync", ifb[:], w["ifb"].partition_broadcast(128), writes=[t_cw])

    hTs = [kb.sbuf("a_hT%d" % i, [128, 8, 512], BF16) for i in range(2)]
    t_hTs = [Tok("a_hT%d" % i) for i in range(2)]
    pre = kb.sbuf("a_pre", [128, 8, 515], F32)
    t_pre = [Tok("a_pre%d" % g) for g in range(8)]
    cacc = [kb.sbuf("a_cacc%d" % i, [128, 512], F32) for i in range(2)]
    t_cacc = [Tok("a_cacc%d" % i) for i in range(2)]
    qkT = kb.sbuf("a_qkT", [128, 8, 512], BF16)
    t_qk = [Tok("a_qk%d" % g) for g in range(8)]
    Vp = [kb.sbuf("a_vp%d" % i, [128, 4, 257], BF16) for i in range(2)]
    t_vp = [Tok("a_vp%d" % i) for i in range(2)]
    sgo = [kb.sbuf("a_sgo%d" % i, [128, 1024], F32) for i in range(2)]
    t_sgo = [Tok("a_sgo%d" % i) for i in range(2)]
    gsb = [kb.sbuf("a_g%d" % i, [128, 48], F32) for i in range(2)]
    t_g = [Tok("a_g%d" % i) for i in range(2)]
    Zf = kb.sbuf("a_zf", [128, 4, 257], F32)
    Zb = kb.sbuf("a_zb", [128, 4, 257], BF16)
    t_zf = [Tok("a_zf%d" % h) for h in range(4)]
    t_zb = [Tok("a_zb%d" % h) for h in range(4)]
    s0sb = [kb.sbuf("a_s0%d" % i, [128, 128], BF16) for i in range(2)]
    t_s0 = [Tok("a_s0%d" % i) for i in range(2)]
    ytok = [kb.sbuf("a_yt%d" % i, [128, 256], BF16) for i in range(2)]
    t_yt = [Tok("a_yt%d" % i) for i in range(2)]
    khat = [kb.sbuf("a_kh%d" % i, [128, 128], BF16) for i in range(2)]
    t_kh = [Tok("a_kh%d" % i) for i in range(2)]
    dsm = [kb.sbuf("a_d%d" % i, [128, 8], F32) for i in range(2)]
    t_d = [Tok("a_d%d" % i) for i in range(2)]
    yst = [kb.sbuf("a_yst%d" % i, [128, 8, 512], BF16) for i in range(2)]
    t_yst = [Tok("a_yst%d" % i) for i in range(2)]

    ps_fm = [kb.psum("a_psfm%d" % i, [128, 512], F32) for i in range(2)]
    t_psfm = [Tok("a_psfm%d" % i) for i in range(2)]
    ps_tm = [kb.psum("a_pstm%d" % i, [128, 512], F32) for i in range(2)]
    t_pstm = [Tok("a_pstm%d" % i) for i in range(2)]
    ps_m = kb.psum("a_psm", [128, 512], F32)
    t_ps_s0, t_ps_o, t_ps_if, t_ps_cs = Tok("ps_s0"), Tok("ps_o"), Tok("ps_if"), Tok("ps_cs")
    ps_u = kb.psum("a_psu", [128, 257], F32)
    t_ps_u = Tok("ps_u")
    ps_t = kb.psum("a_pst", [128, 3, 128], BF16)
    t_ps_yt, t_ps_kt = Tok("ps_yt"), Tok("ps_kt")

    kb.op("gpsimd", lambda e: e.memset(pre[:, :, 0:3], 0.0), writes=t_pre)
    kb.op("gpsimd", lambda e: e.memset(Zf[:], 0.0), writes=t_zf)
    kb.op("gpsimd", lambda e: e.memset(Zb[:], 0.0), writes=t_zb)
    for i in range(2):
        kb.op("gpsimd", lambda e, i=i: e.memset(Vp[i][:, :, 256:257], 1.0), writes=[t_vp[i]])

    fmi = 0
    tmi = 0
    hi = 0
    for tt in range(NT):
        s = tt % 2
        kb.dma("sync", hTs[s][:], hT[:, :, tt * 512:(tt + 1) * 512].rearrange("c p t -> p c t"),
               reads=[t_hT], writes=[t_hTs[s]])
        for g in range(8):
            p = fmi % 2
            fmi += 1
            for c in range(8):
                _mm(kb, ps_fm[p][:], wqk[:, c, g * 128:(g + 1) * 128], hTs[s][:, c, :], c == 0, c == 7,
                    [t_w, t_hTs[s]], [t_psfm[p]])
            kb.op("scalar", lambda e, p=p, g=g: e.copy(out=pre[:, g, 3:515], in_=ps_fm[p][:]),
                  reads=[t_psfm[p]], writes=[t_pre[g]])
            a = g % 2
            kb.op("vector", lambda e, a=a, g=g: e.tensor_scalar_mul(out=cacc[a][:], in0=pre[:, g, 0:512],
                                                                    scalar1=cw[:, g, 0:1]),
                  reads=[t_pre[g], t_cw], writes=[t_cacc[a]])
            for j in range(1, 4):
                kb.op("vector", lambda e, a=a, g=g, j=j: e.scalar_tensor_tensor(
                    out=cacc[a][:], in0=pre[:, g, j:j + 512], scalar=cw[:, g, j:j + 1], in1=cacc[a][:],
                    op0=ALU.mult, op1=ALU.add), reads=[t_pre[g], t_cw, t_cacc[a]], writes=[t_cacc[a]])
            kb.op("scalar", lambda e, a=a, g=g: e.activation(out=qkT[:, g, :], in_=cacc[a][:], func=AF.Silu,
                                                             bias=cb[:, g:g + 1]),
                  reads=[t_cacc[a], t_cw], writes=[t_qk[g]])
            kb.op("gpsimd", lambda e, g=g: e.tensor_copy(out=pre[:, g, 0:3], in_=pre[:, g, 512:515]),
                  reads=[t_pre[g]], writes=[t_pre[g]])
        for j in range(4):
            vs = (tt * 4 + j) % 2
            tsl = slice(j * 128, (j + 1) * 128)
            for half in range(2):
                p = tmi % 2
                tmi += 1
                for c in range(8):
                    _mm(kb, ps_tm[p][:], hTs[s][:, c, tsl], wtm[:, c, half * 512:(half + 1) * 512],
                        c == 0, c == 7, [t_wt, t_hTs[s]], [t_pstm[p]])
                kb.op("scalar", lambda e, p=p, vs=vs, half=half: e.copy(
                    out=Vp[vs][:, 2 * half:2 * half + 2, 0:256],
                    in_=ps_tm[p][:].rearrange("p (h v) -> p h v", h=2)),
                    reads=[t_pstm[p]], writes=[t_vp[vs]])
            for half in range(2):
                p = tmi % 2
                tmi += 1
                for c in range(8):
                    _mm(kb, ps_tm[p][:], hTs[s][:, c, tsl], wtm[:, c, 1024 + half * 512:1024 + (half + 1) * 512],
                        c == 0, c == 7, [t_wt, t_hTs[s]], [t_pstm[p]])
                kb.op("scalar", lambda e, p=p, vs=vs, half=half: e.activation(
                    out=sgo[vs][:, half * 512:(half + 1) * 512], in_=ps_tm[p][:], func=AF.Sigmoid),
                    reads=[t_pstm[p]], writes=[t_sgo[vs]])
            for c in range(8):
                _mm(kb, ps_m[:, 385:393], hTs[s][:, c, tsl], wtm[:, c, 2048:2056], c == 0, c == 7,
                    [t_wt, t_hTs[s]], [t_ps_if])
            G = gsb[vs]
            tg = t_g[vs]
            kb.op("vector", lambda e, G=G: e.tensor_tensor(out=G[:, 0:8], in0=ps_m[:, 385:393], in1=ifb[:],
                                                           op=ALU.add),
                  reads=[t_ps_if, t_cw], writes=[tg])
            kb.op("scalar", lambda e, G=G: e.activation(out=G[:, 8:12], in_=G[:, 4:8], func=AF.Exp, scale=-1.0),
                  reads=[tg], writes=[tg])
            kb.op("scalar", lambda e, G=G: e.activation(out=G[:, 8:12], in_=G[:, 8:12], func=AF.Ln,
                                                        bias=consts["one"][:, 0:1]),
                  reads=[tg, ct], writes=[tg])
            kb.op("vector", lambda e, G=G: e.tensor_scalar_mul(out=G[:, 8:12], in0=G[:, 8:12], scalar1=-1.0),
                  reads=[tg], writes=[tg])
            _mm(kb, ps_m[:, 393:397], consts["tri_f"][:], G[:, 8:12], True, True, [ct, tg], [t_ps_cs])
            _mm(kb, ps_m[:, 397:401], consts["tris_f"][:], G[:, 8:12], True, True, [ct, tg], [t_ps_cs])
            _mm(kb, ps_m[:, 401:405], consts["ones_f"][:], G[:, 8:12], True, True, [ct, tg], [t_ps_cs])
            kb.op("vector", lambda e, G=G: e.tensor_copy(out=G[:, 16:20], in_=ps_m[:, 393:397]),
                  reads=[t_ps_cs], writes=[tg])
            kb.op("vector", lambda e, G=G: e.tensor_tensor(out=G[:, 20:24], in0=G[:, 0:4], in1=ps_m[:, 393:397],
                                                           op=ALU.subtract),
                  reads=[t_ps_cs, tg], writes=[tg])
            kb.op("vector", lambda e, G=G: e.tensor_tensor(out=G[:, 24:28], in0=G[:, 0:4], in1=ps_m[:, 397:401],
                                                           op=ALU.add),
                  reads=[t_ps_cs, tg], writes=[tg])
            kb.op("vector", lambda e, G=G: e.tensor_copy(out=G[:, 28:32], in_=ps_m[:, 401:405]),
                  reads=[t_ps_cs], writes=[tg])
            kb.op("scalar", lambda e, G=G: e.activation(out=G[:, 32:48], in_=G[:, 16:32], func=AF.Exp),
                  reads=[tg], writes=[tg])
            kb.op("vector", lambda e, G=G: e.tensor_scalar_mul(out=G[:, 12:16], in0=G[:, 32:36],
                                                               scalar1=128.0 ** -0.5),
                  reads=[tg], writes=[tg])
            for h in range(4):
                q = hi % 2
                hi += 1
                qT = qkT[:, h, tsl]
                kT = qkT[:, 4 + h, tsl]
                _mm(kb, ps_m[:, 0:128], kT, qT, True, True, [t_qk[h], t_qk[4 + h]], [t_ps_s0])
                kb.op("vector", lambda e, q=q, G=G, h=h: e.scalar_tensor_tensor(
                    out=s0sb[q][:], in0=ps_m[:, 0:128], scalar=G[:, 36 + h:37 + h], in1=consts["tri_f"][:],
                    op0=ALU.mult, op1=ALU.mult), reads=[t_ps_s0, tg, ct], writes=[t_s0[q]])
                _mm(kb, ps_m[:, 128:385], s0sb[q][:], Vp[vs][:, h, :], True, False, [t_s0[q], t_vp[vs]], [t_ps_o])
                _mm(kb, ps_m[:, 128:385], qT, Zb[:, h, :], False, True, [t_qk[h], t_zb[h]], [t_ps_o])
                dd = dsm[q]
                td = t_d[q]
                kb.op("vector", lambda e, dd=dd, G=G, h=h: e.tensor_tensor(
                    out=dd[:, 0:1], in0=ps_m[:, 384:385], in1=G[:, 12 + h:13 + h], op=ALU.mult),
                    reads=[t_ps_o, tg], writes=[td])
                kb.op("vector", lambda e, dd=dd: e.tensor_scalar(out=dd[:, 1:2], in0=dd[:, 0:1], scalar1=-1.0,
                                                                 scalar2=1.0, op0=ALU.mult, op1=ALU.max),
                      reads=[td], writes=[td])
                kb.op("vector", lambda e, dd=dd: e.tensor_tensor(out=dd[:, 2:3], in0=dd[:, 1:2], in1=dd[:, 0:1],
                                                                 op=ALU.max), reads=[td], writes=[td])
                kb.op("vector", lambda e, dd=dd: e.reciprocal(out=dd[:, 3:4], in_=dd[:, 2:3]),
                      reads=[td], writes=[td])
                kb.op("vector", lambda e, dd=dd, G=G, h=h: e.tensor_tensor(
                    out=dd[:, 4:5], in0=dd[:, 3:4], in1=G[:, 12 + h:13 + h], op=ALU.mult),
                    reads=[td, tg], writes=[td])
                kb.op("vector", lambda e, q=q, dd=dd, vs=vs, h=h: e.scalar_tensor_tensor(
                    out=ytok[q][:], in0=ps_m[:, 128:384], scalar=dd[:, 4:5],
                    in1=sgo[vs][:, h * 256:(h + 1) * 256], op0=ALU.mult, op1=ALU.mult),
                    reads=[t_ps_o, td, t_sgo[vs]], writes=[t_yt[q]])
                for k in range(2):
                    kb.op("tensor", lambda e, q=q, k=k: e.transpose(out=ps_t[:, k, :],
                                                                    in_=ytok[q][:, k * 128:(k + 1) * 128],
                                                                    identity=consts["id_b"][:]),
                          reads=[t_yt[q], ct], writes=[t_ps_yt], sig=(k == 1))
                kb.op("scalar", lambda e, s=s, h=h, tsl=tsl: e.copy(out=yst[s][:, 2 * h:2 * h + 2, tsl],
                                                                    in_=ps_t[:, 0:2, :]),
                      reads=[t_ps_yt], writes=[t_yst[s]])
                kb.op("tensor", lambda e, kT=kT: e.transpose(out=ps_t[:, 2, :], in_=kT, identity=consts["id_b"][:]),
                      reads=[t_qk[4 + h], ct], writes=[t_ps_kt])
                kb.op("scalar", lambda e, q=q, G=G, h=h: e.activation(out=khat[q][:], in_=ps_t[:, 2, :],
                                                                      func=AF.Copy, scale=G[:, 40 + h:41 + h]),
                      reads=[t_ps_kt, tg], writes=[t_kh[q]])
                _mm(kb, ps_u[:], khat[q][:], Vp[vs][:, h, :], True, True, [t_kh[q], t_vp[vs]], [t_ps_u])
                kb.op("vector", lambda e, G=G, h=h: e.scalar_tensor_tensor(
                    out=Zf[:, h, :], in0=Zf[:, h, :], scalar=G[:, 44 + h:45 + h], in1=ps_u[:],
                    op0=ALU.mult, op1=ALU.add), reads=[t_zf[h], tg, t_ps_u], writes=[t_zf[h]])
                kb.op("gpsimd", lambda e, h=h: e.tensor_copy(out=Zb[:, h, :], in_=Zf[:, h, :]),
                      reads=[t_zf[h]], writes=[t_zb[h]])
        kb.dma("gpsimd", ymT[:, :, tt * 512:(tt + 1) * 512].rearrange("c p t -> p c t"), yst[s][:],
               reads=[t_yst[s]], writes=[t_ymT])
    kb.release(mk)


def emit_mla_proj_pass(kb, consts, S, hT, t_hT, w, scr):
    NT = S // 512
    mk = kb.mark()
    ct = consts["tok"]
    wmla = kb.sbuf("b_wmla", [128, 8, 768], BF16)
    wuqn = kb.sbuf("b_wuqn", [128, 3, 1024], BF16)
    wuqr = kb.sbuf("b_wuqr", [128, 3, 512], BF16)
    wuqs = kb.sbuf("b_wuqs", [128, 3, 512], BF16)
    wuk = kb.sbuf("b_wuk", [128, 2, 1024], BF16)
    wuv = kb.sbuf("b_wuv", [128, 2, 1024], BF16)
    gq = kb.sbuf("b_gq", [128, 5], F32)
    t_w = Tok("b_w")
    load_w_bf16(kb, wmla, w["wmla"], t_w, 8)
    load_w_bf16(kb, wuqn, w["wuqn"], t_w, 3)
    load_w_bf16(kb, wuqr, w["wuqr"], t_w, 3)
    load_w_bf16(kb, wuqs, w["wuqs"], t_w, 3)
    load_w_bf16(kb, wuk, w["wuk"], t_w, 2)
    load_w_bf16(kb, wuv, w["wuv"], t_w, 2)
    kb.dma("sync", gq[:], w["gqkv"], writes=[t_w])
    epsq = consts["eps"]

    hTs = [kb.sbuf("b_hT%d" % i, [128, 8, 512], BF16) for i in range(2)]
    t_hTs = [Tok("b_hT%d" % i) for i in range(2)]
    cs = [kb.sbuf("b_cs%d" % i, [64, 2, 512], F32) for i in range(2)]
    t_cs = [Tok("b_cs%d" % i) for i in range(2)]
    raw = kb.sbuf("b_raw", [128, 5, 512], F32)
    t_raw = [Tok("b_raw%d" % g) for g in range(5)]
    sq = kb.sbuf("b_sq", [128, 5, 512], BF16)
    t_sq = [Tok("b_sq%d" % g) for g in range(5)]
    rs = kb.sbuf("b_rs", [128, 2, 512], F32)
    t_rs = [Tok("b_rs%d" % g) for g in range(2)]
    cn = kb.sbuf("b_cn", [128, 5, 512], BF16)
    t_cn = [Tok("b_cn%d" % g) for g in range(5)]
    rt = [kb.sbuf("b_rt%d" % i, [64, 2, 512], F32) for i in range(2)]
    t_rt = [Tok("b_rt%d" % i) for i in range(2)]
    krs = kb.sbuf("b_krs", [64, 512], BF16)
    t_krs = Tok("b_krs")
    qn_st = kb.sbuf("b_qn", [128, 8, 512], BF16)
    qr_st = kb.sbuf("b_qr", [64, 8, 512], BF16)
    kn_st = kb.sbuf("b_kn", [128, 8, 512], BF16)
    t_qn, t_qr, t_kn = Tok("b_qn"), Tok("b_qr"), Tok("b_kn")
    v_st = [kb.sbuf("b_v%d" % i, [128, 1024], BF16) for i in range(2)]
    t_v = [Tok("b_v%d" % i) for i in range(2)]

    ps_fm = [kb.psum("b_psfm%d" % i, [128, 512], F32) for i in range(2)]
    t_psfm = [Tok("b_psfm%d" % i) for i in range(2)]
    ps_ss = [kb.psum("b_psss%d" % i, [128, 512], F32) for i in range(2)]
    t_psss = [Tok("b_psss%d" % i) for i in range(2)]
    ps_r = [kb.psum("b_psr%d" % i, [128, 512], F32) for i in range(2)]
    t_psr = [Tok("b_psr%d" % i) for i in range(2)]
    ps_tm = [kb.psum("b_pstm%d" % i, [128, 512], F32) for i in range(2)]
    t_pstm = [Tok("b_pstm%d" % i) for i in range(2)]
    d_QTn, d_QTr, d_KTn, d_KrT, d_Vd = scr["QTn"], scr["QTr"], scr["KTn"], scr["KrT"], scr["Vd"]
    t_scr = scr["tok"]

    fmi = 0
    ri = 0
    vi = 0

    def rope(psa, psb, t_pa, t_pb, cst, t_cst, out_ap, t_out):
        nonlocal ri
        k = ri % 2
        ri += 1
        kb.op("vector", lambda e: e.tensor_tensor(out=rt[k][:, 0, :], in0=psa, in1=cst[:, 0, :], op=ALU.mult),
              reads=[t_pa, t_cst], writes=[t_rt[k]])
        kb.op("vector", lambda e: e.tensor_tensor(out=rt[k][:, 1, :], in0=psb, in1=cst[:, 1, :], op=ALU.mult),
              reads=[t_pb, t_cst], writes=[t_rt[k]])
        kb.op("gpsimd", lambda e: e.tensor_tensor(out=out_ap, in0=rt[k][:, 0, :], in1=rt[k][:, 1, :], op=ALU.add),
              reads=[t_rt[k]], writes=[t_out])

    for tt in range(NT):
        s = tt % 2
        tok_sl = slice(tt * 512, (tt + 1) * 512)
        kb.dma("sync", hTs[s][:], hT[:, :, tok_sl].rearrange("c p t -> p c t"), reads=[t_hT], writes=[t_hTs[s]])
        kb.dma("sync", cs[s][:], w["cs"][:, :, tok_sl], writes=[t_cs[s]])
        for g in range(5):
            p = fmi % 2
            fmi += 1
            for c in range(8):
                _mm(kb, ps_fm[p][:], wmla[:, c, g * 128:(g + 1) * 128], hTs[s][:, c, :], c == 0, c == 7,
                    [t_w, t_hTs[s]], [t_psfm[p]])
            kb.op("scalar", lambda e, p=p, g=g: e.copy(out=raw[:, g, :], in_=ps_fm[p][:]),
                  reads=[t_psfm[p]], writes=[t_raw[g]])
            kb.op("gpsimd", lambda e, g=g: e.tensor_tensor(out=sq[:, g, :], in0=raw[:, g, :], in1=raw[:, g, :],
                                                           op=ALU.mult), reads=[t_raw[g]], writes=[t_sq[g]])
        for n, (g0, g1, dim) in enumerate(((0, 3, 384.0), (3, 5, 256.0))):
            for g in range(g0, g1):
                _mm(kb, ps_ss[n][:], consts["ones_b"][:], sq[:, g, :], g == g0, g == g1 - 1,
                    [ct, t_sq[g]], [t_psss[n]])
            kb.op("scalar", lambda e, n=n, dim=dim: e.activation(out=rs[:, n, :], in_=ps_ss[n][:], func=AF.Sqrt,
                                                                 bias=epsq[:, 0:1], scale=1.0 / dim),
                  reads=[t_psss[n], ct], writes=[t_rs[n]])
            kb.op("vector", lambda e, n=n: e.reciprocal(out=rs[:, n, :], in_=rs[:, n, :]),
                  reads=[t_rs[n]], writes=[t_rs[n]])
            for g in range(g0, g1):
                kb.op("vector", lambda e, n=n, g=g: e.scalar_tensor_tensor(
                    out=cn[:, g, :], in0=raw[:, g, :], scalar=gq[:, g:g + 1], in1=rs[:, n, :],
                    op0=ALU.mult, op1=ALU.mult), reads=[t_raw[g], t_w, t_rs[n]], writes=[t_cn[g]])
        for k2 in range(2):
            for c in range(8):
                _mm(kb, ps_r[k2][0:64, :], wmla[:, c, 640 + 64 * k2:704 + 64 * k2], hTs[s][:, c, :], c == 0, c == 7,
                    [t_w, t_hTs[s]], [t_psr[k2]])
        rope(ps_r[0][0:64, :], ps_r[1][0:64, :], t_psr[0], t_psr[1], cs[s], t_cs[s], krs[:], t_krs)
        kb.dma("gpsimd", d_KrT[:, tok_sl], krs[:], reads=[t_krs], writes=[t_scr])
        for h in range(8):
            p = fmi % 2
            fmi += 1
            for c in range(3):
                _mm(kb, ps_fm[p][:], wuqn[:, c, h * 128:(h + 1) * 128], cn[:, c, :], c == 0, c == 2,
                    [t_w, t_cn[c]], [t_psfm[p]])
            kb.op("scalar", lambda e, p=p, h=h: e.copy(out=qn_st[:, h, :], in_=ps_fm[p][:]),
                  reads=[t_psfm[p]], writes=[t_qn])
            for k2, wsrc in enumerate((wuqr, wuqs)):
                for c in range(3):
                    _mm(kb, ps_r[k2][0:64, :], wsrc[:, c, h * 64:(h + 1) * 64], cn[:, c, :], c == 0, c == 2,
                        [t_w, t_cn[c]], [t_psr[k2]])
            rope(ps_r[0][0:64, :], ps_r[1][0:64, :], t_psr[0], t_psr[1], cs[s], t_cs[s], qr_st[:, h, :], t_qr)
            p = fmi % 2
            fmi += 1
            for c in range(2):
                _mm(kb, ps_fm[p][:], wuk[:, c, h * 128:(h + 1) * 128], cn[:, 3 + c, :], c == 0, c == 1,
                    [t_w, t_cn[3 + c]], [t_psfm[p]])
            kb.op("scalar", lambda e, p=p, h=h: e.copy(out=kn_st[:, h, :], in_=ps_fm[p][:]),
                  reads=[t_psfm[p]], writes=[t_kn])
        kb.dma("gpsimd", d_QTn[:, :, tok_sl].rearrange("h p t -> p h t"), qn_st[:], reads=[t_qn], writes=[t_scr])
        kb.dma("gpsimd", d_QTr[:, :, tok_sl].rearrange("h p t -> p h t"), qr_st[:], reads=[t_qr], writes=[t_scr])
        kb.dma("gpsimd", d_KTn[:, :, tok_sl].rearrange("h p t -> p h t"), kn_st[:], reads=[t_kn], writes=[t_scr])
        for j in range(4):
            q = vi % 2
            vi += 1
            for half in range(2):
                p = half
                for c in range(2):
                    _mm(kb, ps_tm[p][:], cn[:, 3 + c, j * 128:(j + 1) * 128], wuv[:, c, half * 512:(half + 1) * 512],
                        c == 0, c == 1, [t_w, t_cn[3 + c]], [t_pstm[p]])
                kb.op("scalar", lambda e, p=p, q=q, half=half: e.copy(out=v_st[q][:, half * 512:(half + 1) * 512],
                                                                      in_=ps_tm[p][:]),
                      reads=[t_pstm[p]], writes=[t_v[q]])
            kb.dma("gpsimd", d_Vd[:, :, tt * 4 + j, :].rearrange("h p d -> p h d"),
                   v_st[q][:].rearrange("p (h d) -> p h d", h=8), reads=[t_v[q]], writes=[t_scr])
    kb.release(mk)


def emit_attn_pass(kb, consts, S, scr, yaT, t_yaT):
    NQ = S // 512
    NK = S // 128
    mk = kb.mark()
    ct = consts["tok"]
    scale = 192.0 ** -0.5
    t_scr = scr["tok"]
    masks = kb.sbuf("c_mask", [128, 4, 512], BF16)
    t_mask = Tok("c_mask")
    for r in range(4):
        for kbk in range(2):
            kb.op("gpsimd", lambda e, r=r, kbk=kbk: e.affine_select(
                out=masks[64 * kbk:64 * kbk + 64, r, :].rearrange("p (a b) -> p a b", a=8),
                in_=consts["ones_b512"][64 * kbk:64 * kbk + 64, :].rearrange("p (a b) -> p a b", a=8),
                pattern=[[1, 8], [0, 64]], compare_op=ALU.is_ge, fill=0.0, base=-(2 * r + kbk),
                channel_multiplier=0), reads=[ct], writes=[t_mask])
    krt = kb.sbuf("c_krt", [64, S], BF16)
    t_krt = Tok("c_krt")
    kb.dma("sync", krt[:], scr["KrT"][:, :], reads=[t_scr], writes=[t_krt])
    ktn = [kb.sbuf("c_ktn%d" % i, [128, S], BF16) for i in range(2)]
    t_ktn = [Tok("c_ktn%d" % i) for i in range(2)]
    vsb = [kb.sbuf("c_v%d" % i, [128, NK, 128], BF16) for i in range(2)]
    t_vsb = [Tok("c_v%d" % i) for i in range(2)]
    qn = [kb.sbuf("c_qn%d" % i, [128, 512], BF16) for i in range(2)]
    qr = [kb.sbuf("c_qr%d" % i, [64, 512], BF16) for i in range(2)]
    t_q = [Tok("c_q%d" % i) for i in range(2)]
    NPT = 3
    NPS = 2
    pacc = [kb.sbuf("c_pacc%d" % i, [128, 1024], F32) for i in range(2)]
    t_pacc = [Tok("c_pacc%d" % i) for i in range(2)]
    pT = [kb.sbuf("c_pT%d" % i, [128, 1024], BF16) for i in range(NPT)]
    t_pT = [Tok("c_pT%d" % i) for i in range(NPT)]
    rden = [kb.sbuf("c_rd%d" % i, [128, 512], F32) for i in range(2)]
    t_rden = [Tok("c_rd%d" % i) for i in range(2)]
    yst = [kb.sbuf("c_y%d" % i, [128, 512], BF16) for i in range(2)]
    t_yst = [Tok("c_y%d" % i) for i in range(2)]
    ps_s = [kb.psum("c_pss%d" % i, [128, 1024], F32) for i in range(NPS)]
    t_pss = [Tok("c_pss%d" % i) for i in range(NPS)]
    ps_a = [kb.psum("c_psa%d" % i, [128, 512], F32) for i in range(2)]
    t_psa = [Tok("c_psa%d" % i) for i in range(2)]
    ps_d = [kb.psum("c_psd%d" % i, [128, 512], F32) for i in range(1)]
    t_psd = [Tok("c_psd%d" % i) for i in range(1)]
    gi = 0
    qi = 0
    for h in range(8):
        hs = h % 2
        kb.dma("sync", ktn[hs][:], scr["KTn"][h, :, :], reads=[t_scr], writes=[t_ktn[hs]])
        kb.dma("sync", vsb[hs][:], scr["Vd"][h, :, :, :], reads=[t_scr], writes=[t_vsb[hs]])
        for j in range(NQ):
            a = qi % 2
            qi += 1
            qsl = slice(j * 512, (j + 1) * 512)
            kb.dma("sync", qn[a][:], scr["QTn"][h, :, qsl], reads=[t_scr], writes=[t_q[a]])
            kb.dma("sync", qr[a][:], scr["QTr"][h, :, qsl], reads=[t_scr], writes=[t_q[a]])
            nkt = 4 * (j + 1)
            npair = nkt // 2
            base = gi
            gi += npair

            def qk(pr):
                p = (base + pr) % NPS
                for half in range(2):
                    kt = 2 * pr + half
                    ksl = slice(kt * 128, (kt + 1) * 128)
                    osl = slice(half * 512, (half + 1) * 512)
                    _mm(kb, ps_s[p][:, osl], ktn[hs][:, ksl], qn[a][:], True, False, [t_ktn[hs], t_q[a]], [t_pss[p]])
                    _mm(kb, ps_s[p][:, osl], krt[:, ksl], qr[a][:], False, True, [t_krt, t_q[a]], [t_pss[p]])

            def soft(pr):
                p = (base + pr) % NPS
                u = (base + pr) % NPT
                kb.op("scalar", lambda e: e.activation(out=pT[u][:], in_=ps_s[p][:], func=AF.Exp, scale=scale),
                      reads=[t_pss[p]], writes=[t_pT[u]])
                if 2 * pr >= 4 * j:
                    r = 2 * pr - 4 * j
                    kb.op("vector", lambda e: e.tensor_tensor(
                        out=pT[u][:], in0=pT[u][:], in1=masks[:, r:r + 2, :].rearrange("p a b -> p (a b)"),
                        op=ALU.mult), reads=[t_pT[u], t_mask], writes=[t_pT[u]])
                if pr == 0:
                    kb.op("vector", lambda e: e.tensor_copy(out=pacc[a][:], in_=pT[u][:]),
                          reads=[t_pT[u]], writes=[t_pacc[a]])
                else:
                    kb.op("vector", lambda e: e.tensor_tensor(out=pacc[a][:], in0=pacc[a][:], in1=pT[u][:],
                                                              op=ALU.add),
                          reads=[t_pT[u], t_pacc[a]], writes=[t_pacc[a]])

            def pv(pr):
                u = (base + pr) % NPT
                for half in range(2):
                    kt = 2 * pr + half
                    _mm(kb, ps_a[a][:], vsb[hs][:, kt, :], pT[u][:, half * 512:(half + 1) * 512],
                        kt == 0, kt == nkt - 1, [t_vsb[hs], t_pT[u]], [t_psa[a]])

            qk(0)
            for pr in range(npair):
                soft(pr)
                if pr + 1 < npair:
                    qk(pr + 1)
                pv(pr)
            _mm(kb, ps_d[0][:], consts["ones_f"][:], pacc[a][:, 0:512], True, False, [ct, t_pacc[a]], [t_psd[0]])
            _mm(kb, ps_d[0][:], consts["ones_f"][:], pacc[a][:, 512:1024], False, True, [ct, t_pacc[a]], [t_psd[0]])
            kb.op("vector", lambda e, a=a: e.reciprocal(out=rden[a][:], in_=ps_d[0][:]),
                  reads=[t_psd[0]], writes=[t_rden[a]])
            kb.op("vector", lambda e, a=a: e.tensor_tensor(out=yst[a][:], in0=ps_a[a][:], in1=rden[a][:],
                                                           op=ALU.mult),
                  reads=[t_psa[a], t_rden[a]], writes=[t_yst[a]])
            kb.dma("gpsimd", yaT[h, :, qsl], yst[a][:], reads=[t_yst[a]], writes=[t_yaT])
    kb.release(mk)


def alloc_mla_scratch(kb, S):
    return dict(QTn=kb.dram_tmp("s_QTn", [8, 128, S], BF16), QTr=kb.dram_tmp("s_QTr", [8, 64, S], BF16),
                KTn=kb.dram_tmp("s_KTn", [8, 128, S], BF16), KrT=kb.dram_tmp("s_KrT", [64, S], BF16),
                Vd=kb.dram_tmp("s_Vd", [8, 128, S // 128, 128], BF16), tok=Tok("dram:mla_scr"))


def emit_merge_pass(kb, consts, S, hT, ymT, yaT, t_in, x, t_x, w, gate1, t_gate1, xmid, t_xmid):
    NT = S // 512
    mk = kb.mark()
    wg = kb.sbuf("d_wg", [128, 8, 2048], BF16)
    wbm = kb.sbuf("d_wbm", [128, 8, 1024], BF16)
    wba = kb.sbuf("d_wba", [128, 8, 1024], BF16)
    wo = kb.sbuf("d_wo", [128, 8, 1024], BF16)
    t_w = Tok("d_w")
    load_w_bf16(kb, wg, w["wgate"], t_w, 8)
    load_w_bf16(kb, wbm, w["wbm"], t_w, 8)
    load_w_bf16(kb, wba, w["wba"], t_w, 8)
    load_w_bf16(kb, wo, w["wout"], t_w, 8)
    hTs = [kb.sbuf("d_hT%d" % i, [128, 8, 512], BF16) for i in range(2)]
    ymS = [kb.sbuf("d_ym%d" % i, [128, 8, 512], BF16) for i in range(2)]
    yaS = [kb.sbuf("d_ya%d" % i, [128, 8, 512], BF16) for i in range(2)]
    t_ld = [Tok("d_ld%d" % i) for i in range(2)]
    sg = [kb.sbuf("d_sg%d" % i, [128, 2, 512], F32) for i in range(2)]
    t_sg = [Tok("d_sg%d" % i) for i in range(2)]
    mm_ = [kb.sbuf("d_mm%d" % i, [128, 2, 512], F32) for i in range(2)]
    t_mm = [Tok("d_mm%d" % i) for i in range(2)]
    mg = [kb.sbuf("d_mg%d" % i, [128, 8, 512], BF16) for i in range(2)]
    t_mg = [Tok("d_mg%d" % i) for i in range(2)]
    xt = [kb.sbuf("d_x%d" % i, [128, 1024], F32) for i in range(2)]
    t_xt = [Tok("d_x%d" % i) for i in range(2)]
    tmp = [kb.sbuf("d_tmp%d" % i, [128, 512], F32) for i in range(2)]
    t_tmp = [Tok("d_tmp%d" % i) for i in range(2)]
    ps4 = [kb.psum("d_ps%d" % i, [128, 512], F32) for i in range(4)]
    t_ps4 = [Tok("d_ps%d" % i) for i in range(4)]
    pso = [kb.psum("d_pso%d" % i, [128, 512], F32) for i in range(2)]
    t_pso = [Tok("d_pso%d" % i) for i in range(2)]
    oi = 0
    xi = 0
    for tt in range(NT):
        s = tt % 2
        tsl = slice(tt * 512, (tt + 1) * 512)
        kb.dma("sync", hTs[s][:], hT[:, :, tsl].rearrange("c p t -> p c t"), reads=[t_in], writes=[t_ld[s]])
        kb.dma("sync", ymS[s][:], ymT[:, :, tsl].rearrange("c p t -> p c t"), reads=[t_in], writes=[t_ld[s]])
        kb.dma("sync", yaS[s][:], yaT[:, :, tsl].rearrange("c p t -> p c t"), reads=[t_in], writes=[t_ld[s]])
        for oc in range(8):
            a = oc % 2
            osl = slice(oc * 128, (oc + 1) * 128)
            for c in range(8):
                _mm(kb, ps4[0][:], wbm[:, c, osl], ymS[s][:, c, :], c == 0, c == 7, [t_w, t_ld[s]], [t_ps4[0]])
            for c in range(8):
                _mm(kb, ps4[1][:], wba[:, c, osl], yaS[s][:, c, :], c == 0, c == 7, [t_w, t_ld[s]], [t_ps4[1]])
            for k in range(2):
                for c in range(8):
                    _mm(kb, ps4[2 + k][:], wg[:, c, k * 1024 + oc * 128:k * 1024 + (oc + 1) * 128], hTs[s][:, c, :],
                        c == 0, c == 7, [t_w, t_ld[s]], [t_ps4[2 + k]])
                kb.op("scalar", lambda e, a=a, k=k: e.activation(out=sg[a][:, k, :], in_=ps4[2 + k][:],
                                                                 func=AF.Sigmoid),
                      reads=[t_ps4[2 + k]], writes=[t_sg[a]])
            for k in range(2):
                kb.op("vector", lambda e, a=a, k=k: e.tensor_tensor(out=mm_[a][:, k, :], in0=ps4[k][:],
                                                                    in1=sg[a][:, k, :], op=ALU.mult),
                      reads=[t_ps4[k], t_sg[a]], writes=[t_mm[a]])
            kb.op("gpsimd", lambda e, a=a, s=s, oc=oc: e.tensor_tensor(out=mg[s][:, oc, :], in0=mm_[a][:, 0, :],
                                                                       in1=mm_[a][:, 1, :], op=ALU.add),
                  reads=[t_mm[a]], writes=[t_mg[s]])
        for j in range(4):
            xs = xi % 2
            xi += 1
            t0 = tt * 512 + j * 128
            kb.dma("sync", xt[xs][:], x[t0:t0 + 128, :], reads=[t_x], writes=[t_xt[xs]])
            for half in range(2):
                p = oi % 2
                oi += 1
                hsl = slice(half * 512, (half + 1) * 512)
                for c in range(8):
                    _mm(kb, pso[p][:], mg[s][:, c, j * 128:(j + 1) * 128], wo[:, c, hsl], c == 0, c == 7,
                        [t_w, t_mg[s]], [t_pso[p]])
                kb.op("vector", lambda e, p=p, hsl=hsl: e.tensor_tensor(out=tmp[p][:], in0=pso[p][:],
                                                                        in1=gate1[:, hsl], op=ALU.mult),
                      reads=[t_pso[p], t_gate1], writes=[t_tmp[p]])
                kb.op("vector", lambda e, p=p, xs=xs, hsl=hsl: e.tensor_tensor(out=xt[xs][:, hsl], in0=xt[xs][:, hsl],
                                                                               in1=tmp[p][:], op=ALU.add),
                      reads=[t_tmp[p], t_xt[xs]], writes=[t_xt[xs]])
            kb.dma("gpsimd", xmid[t0:t0 + 128, :], xt[xs][:], reads=[t_xt[xs]], writes=[t_xmid])
    kb.release(mk)


def emit_moe_pass(kb, consts, S, xmid, t_xmid, w, gmul2, t_gmul2, shift2, t_shift2, gate2, t_gate2,
                  xout, t_xout, final_g=None):
    TS = min(2048, S)
    NST = S // TS
    NTL = TS // 128
    mk = kb.mark()
    ct = consts["tok"]
    h2T = kb.sbuf("e_h2T", [128, 8, TS], BF16)
    t_h2T = Tok("e_h2T")
    yacc = kb.sbuf("e_yacc", [128, NTL, 1024], F32)
    t_yacc = [Tok("e_yacc%d" % i) for i in range(NTL)]
    Wt = kb.sbuf("e_Wt", [128, NTL, 32], F32)
    t_Wt = [Tok("e_Wt%d" % i) for i in range(NTL)]
    wr = kb.sbuf("e_wr", [128, 8, 36], F32)
    rb = kb.sbuf("e_rb", [128, 36], F32)
    t_wr = Tok("e_wr")
    kb.dma("sync", wr[:], w["wr"].rearrange("(c p) n -> p c n", p=128), writes=[t_wr])
    kb.dma("sync", rb[:], w["rb"].partition_broadcast(128), writes=[t_wr])
    fg = None
    if final_g is not None:
        fg = kb.sbuf("e_fg", [128, 1024], F32)
        kb.dma("sync", fg[:], final_g.partition_broadcast(128), writes=[t_wr])
    weg = [kb.sbuf("e_weg%d" % i, [128, 8, 256], BF16) for i in range(2)]
    weu = [kb.sbuf("e_weu%d" % i, [128, 8, 256], BF16) for i in range(2)]
    wed = [kb.sbuf("e_wed%d" % i, [128, 2, 1024], BF16) for i in range(2)]
    t_we = [Tok("e_we%d" % i) for i in range(2)]
    xt = [kb.sbuf("e_x%d" % i, [128, 1024], F32) for i in range(2)]
    t_xt = [Tok("e_x%d" % i) for i in range(2)]
    scr = [kb.sbuf("e_scr%d" % i, [128, 1024], F32) for i in range(2)]
    t_scr = [Tok("e_scr%d" % i) for i in range(2)]
    h2f = [kb.sbuf("e_h2f%d" % i, [128, 1024], F32) for i in range(2)]
    t_h2f = [Tok("e_h2f%d" % i) for i in range(2)]
    stat = [kb.sbuf("e_st%d" % i, [128, 4], F32) for i in range(2)]
    t_st = [Tok("e_st%d" % i) for i in range(2)]
    h2Tf = kb.sbuf("e_h2Tf", [128, 8, 128], F32)
    t_h2Tf = Tok("e_h2Tf")
    R = [kb.sbuf("e_R%d" % i, [128, 160], F32) for i in range(2)]
    t_R = [Tok("e_R%d" % i) for i in range(2)]
    sgl = [kb.sbuf("e_sg%d" % i, [128, 512], F32) for i in range(2)]
    t_sgl = [Tok("e_sg%d" % i) for i in range(2)]
    aT = [kb.sbuf("e_aT%d" % i, [128, 2, 512], BF16) for i in range(2)]
    t_aT = [Tok("e_aT%d" % i) for i in range(2)]
    bank = [kb.psum("e_bank%d" % i, [128, 512], F32) for i in range(8)]
    t_bank = [Tok("e_bank%d" % i) for i in range(8)]
    wload = 0

    def load_expert(e):
        nonlocal wload
        k = wload % 2
        wload += 1
        for c in range(8):
            kb.dma("gpsimd", weg[k][:, c, :], w["weg"][e, c * 128:(c + 1) * 128, :], writes=[t_we[k]])
        for c in range(8):
            kb.dma("gpsimd", weu[k][:, c, :], w["weu"][e, c * 128:(c + 1) * 128, :], writes=[t_we[k]])
        for c in range(2):
            kb.dma("gpsimd", wed[k][:, c, :], w["wed"][e, c * 128:(c + 1) * 128, :], writes=[t_we[k]])
        return k

    for st in range(NST):
        for i in range(NTL):
            s = i % 2
            t0 = st * TS + i * 128
            kb.dma("sync", xt[s][:], xmid[t0:t0 + 128, :], reads=[t_xmid], writes=[t_xt[s]])
            emit_norm_mod(kb, xt[s], t_xt[s], gmul2, t_gmul2, shift2, t_shift2, h2f[s], t_h2f[s],
                          scr[s], t_scr[s], stat[s], t_st[s], consts["eps"][:, 0:1])
            for half in range(2):
                for c4 in range(4):
                    c = half * 4 + c4
                    kb.op("tensor", lambda e, s=s, c=c, half=half, c4=c4: e.transpose(
                        out=bank[half][:, c4 * 128:(c4 + 1) * 128], in_=h2f[s][:, c * 128:(c + 1) * 128],
                        identity=consts["id_f"][:]), reads=[t_h2f[s], ct], writes=[t_bank[half]], sig=(c4 == 3))
                kb.op("scalar", lambda e, half=half: e.copy(
                    out=h2Tf[:, half * 4:half * 4 + 4, :], in_=bank[half][:].rearrange("p (c t) -> p c t", c=4)),
                    reads=[t_bank[half]], writes=[t_h2Tf])
            kb.op("gpsimd", lambda e, i=i: e.tensor_copy(out=h2T[:, :, i * 128:(i + 1) * 128], in_=h2Tf[:]),
                  reads=[t_h2Tf], writes=[t_h2T])
            for c in range(8):
                _mm(kb, bank[2][:, 0:36], h2Tf[:, c, :], wr[:, c, :], c == 0, c == 7, [t_h2Tf, t_wr], [t_bank[2]])
            r = R[s]
            tr = t_R[s]
            V = lambda fn, reads, writes: kb.op("vector", fn, reads=reads, writes=writes)
            V(lambda e: e.tensor_tensor(out=r[:, 0:36], in0=bank[2][:, 0:36], in1=rb[:], op=ALU.add),
              [t_bank[2], t_wr], [tr])
            V(lambda e: e.reduce_max(out=r[:, 36:37], in_=r[:, 0:4], axis=AX.X), [tr], [tr])
            V(lambda e: e.tensor_scalar_mul(out=r[:, 37:38], in0=r[:, 36:37], scalar1=-1.0), [tr], [tr])
            kb.op("scalar", lambda e: e.activation(out=r[:, 40:44], in_=r[:, 0:4], func=AF.Exp, bias=r[:, 37:38]),
                  reads=[tr], writes=[tr])
            V(lambda e: e.reduce_sum(out=r[:, 44:45], in_=r[:, 40:44], axis=AX.X), [tr], [tr])
            V(lambda e: e.reciprocal(out=r[:, 45:46], in_=r[:, 44:45]), [tr], [tr])
            V(lambda e: e.tensor_scalar(out=r[:, 48:52], in0=r[:, 0:4], scalar1=r[:, 36:37], scalar2=None,
                                        op0=ALU.is_equal), [tr], [tr])
            V(lambda e: e.tensor_scalar(out=r[:, 52:56], in0=r[:, 48:52], scalar1=-1.0, scalar2=BIG,
                                        op0=ALU.add, op1=ALU.mult), [tr], [tr])
            V(lambda e: e.tensor_tensor(out=r[:, 64:96].rearrange("p (g k) -> p g k", g=4),
                                        in0=r[:, 4:36].rearrange("p (g k) -> p g k", g=4),
                                        in1=r[:, 52:56].unsqueeze(2).to_broadcast([128, 4, 8]), op=ALU.add),
              [tr], [tr])
            V(lambda e: e.reduce_max(out=r[:, 96:97], in_=r[:, 64:96], axis=AX.X), [tr], [tr])
            V(lambda e: e.tensor_scalar(out=r[:, 104:136], in0=r[:, 64:96], scalar1=r[:, 96:97], scalar2=None,
                                        op0=ALU.is_equal), [tr], [tr])
            V(lambda e: e.scalar_tensor_tensor(out=r[:, 64:96], in0=r[:, 104:136], scalar=-BIG, in1=r[:, 64:96],
                                               op0=ALU.mult, op1=ALU.add), [tr], [tr])
            V(lambda e: e.reduce_max(out=r[:, 97:98], in_=r[:, 64:96], axis=AX.X), [tr], [tr])
            V(lambda e: e.tensor_tensor(out=r[:, 98:99], in0=r[:, 97:98], in1=r[:, 96:97], op=ALU.subtract),
              [tr], [tr])
            kb.op("scalar", lambda e: e.activation(out=r[:, 99:100], in_=r[:, 98:99], func=AF.Exp),
                  reads=[tr], writes=[tr])
            V(lambda e: e.tensor_scalar_add(out=r[:, 100:101], in0=r[:, 99:100], scalar1=1.0), [tr], [tr])
            V(lambda e: e.reciprocal(out=r[:, 101:102], in_=r[:, 100:101]), [tr], [tr])
            V(lambda e: e.tensor_tensor(out=r[:, 102:103], in0=r[:, 101:102], in1=r[:, 45:46], op=ALU.mult),
              [tr], [tr])
            V(lambda e: e.tensor_tensor(out=r[:, 103:104], in0=r[:, 102:103], in1=r[:, 99:100], op=ALU.mult),
              [tr], [tr])
            V(lambda e, i=i: e.tensor_scalar_mul(out=Wt[:, i, :], in0=r[:, 104:136], scalar1=r[:, 102:103]),
              [tr], [t_Wt[i]])
            V(lambda e: e.tensor_scalar(out=r[:, 104:136], in0=r[:, 64:96], scalar1=r[:, 97:98], scalar2=None,
                                        op0=ALU.is_equal), [tr], [tr])
            V(lambda e, i=i: e.scalar_tensor_tensor(out=Wt[:, i, :], in0=r[:, 104:136], scalar=r[:, 103:104],
                                                    in1=Wt[:, i, :], op0=ALU.mult, op1=ALU.add),
              [tr, t_Wt[i]], [t_Wt[i]])
        steps = [(ex, tl) for ex in range(32) for tl in range(TS // 512)]
        wslot = {}
        gcount = [0]
        dcount = [0]

        def gu_group(si, hc, which):
            ex, tl = steps[si]
            k = wslot[ex]
            tsl = slice(tl * 512, (tl + 1) * 512)
            hsl = slice(hc * 128, (hc + 1) * 128)
            pg = hc
            if which == 0:
                for c in range(8):
                    _mm(kb, bank[pg][:], weg[k][:, c, hsl], h2T[:, c, tsl], c == 0, c == 7,
                        [t_we[k], t_h2T], [t_bank[pg]])
                kb.op("scalar", lambda e: e.activation(out=sgl[pg][:], in_=bank[pg][:], func=AF.Silu),
                      reads=[t_bank[pg]], writes=[t_sgl[pg]])
            else:
                a = si % 2
                for c in range(8):
                    _mm(kb, bank[2 + pg][:], weu[k][:, c, hsl], h2T[:, c, tsl], c == 0, c == 7,
                        [t_we[k], t_h2T], [t_bank[2 + pg]])
                kb.op("vector", lambda e: e.tensor_tensor(out=aT[a][:, hc, :], in0=bank[2 + pg][:], in1=sgl[pg][:],
                                                          op=ALU.mult),
                      reads=[t_bank[2 + pg], t_sgl[pg]], writes=[t_aT[a]])

        def d_group(si, j, half):
            ex, tl = steps[si]
            k = wslot[ex]
            a = si % 2
            ti = tl * 4 + j
            pd = 4 + dcount[0] % 4
            dcount[0] += 1
            osl = slice(half * 512, (half + 1) * 512)
            for hc in range(2):
                _mm(kb, bank[pd][:], aT[a][:, hc, j * 128:(j + 1) * 128], wed[k][:, hc, osl],
                    hc == 0, hc == 1, [t_aT[a], t_we[k]], [t_bank[pd]])
            if ex == 0:
                kb.op("vector", lambda e: e.tensor_scalar_mul(out=yacc[:, ti, osl], in0=bank[pd][:],
                                                              scalar1=Wt[:, ti, ex:ex + 1]),
                      reads=[t_bank[pd], t_Wt[ti]], writes=[t_yacc[ti]])
            else:
                kb.op("vector", lambda e: e.scalar_tensor_tensor(out=yacc[:, ti, osl], in0=bank[pd][:],
                                                                 scalar=Wt[:, ti, ex:ex + 1], in1=yacc[:, ti, osl],
                                                                 op0=ALU.mult, op1=ALU.add),
                      reads=[t_bank[pd], t_Wt[ti], t_yacc[ti]], writes=[t_yacc[ti]])

        dlist = [(j, half) for j in range(4) for half in range(2)]
        for si in range(len(steps) + 1):
            if si < len(steps):
                ex, tl = steps[si]
                if tl == 0:
                    wslot[ex] = load_expert(ex)
            gul = [(0, 0), (0, 1), (1, 0), (1, 1)]
            for gidx in range(4):
                if si < len(steps):
                    gu_group(si, gul[gidx][0], gul[gidx][1])
                if si > 0:
                    for (j, half) in dlist[2 * gidx:2 * gidx + 2]:
                        d_group(si - 1, j, half)
        for i in range(NTL):
            s = i % 2
            t0 = st * TS + i * 128
            kb.dma("sync", xt[s][:], xmid[t0:t0 + 128, :], reads=[t_xmid], writes=[t_xt[s]])
            kb.op("vector", lambda e, i=i: e.tensor_tensor(out=yacc[:, i, :], in0=yacc[:, i, :], in1=gate2[:],
                                                           op=ALU.mult),
                  reads=[t_yacc[i], t_gate2], writes=[t_yacc[i]])
            kb.op("gpsimd", lambda e, s=s, i=i: e.tensor_tensor(out=xt[s][:], in0=xt[s][:], in1=yacc[:, i, :],
                                                                op=ALU.add),
                  reads=[t_xt[s], t_yacc[i]], writes=[t_xt[s]])
            if final_g is None:
                kb.dma("gpsimd", xout[t0:t0 + 128, :], xt[s][:], reads=[t_xt[s]], writes=[t_xout])
            else:
                emit_norm_mod(kb, xt[s], t_xt[s], fg, t_wr, None, None, h2f[s], t_h2f[s],
                              scr[s], t_scr[s], stat[s], t_st[s], consts["eps"][:, 0:1])
                kb.dma("gpsimd", xout[t0:t0 + 128, :], h2f[s][:], reads=[t_h2f[s]], writes=[t_xout])
    kb.release(mk)


LAYER_SHAPES = dict(
    modw=[D, 6144], modb=[6144], g1=[D],
    wqk=[D, 1024], wtm=[D, 2056], cw=[128, 8, 4], cb=[128, 8], ifb=[8],
    wmla=[D, 768], gqkv=[128, 5], wuqn=[384, 1024], wuqr=[384, 512], wuqs=[384, 512],
    wuk=[256, 1024], wuv=[256, 1024],
    wgate=[D, 2048], wbm=[D, D], wba=[D, D], wout=[D, D], g2=[D], wr=[D, 36], rb=[36],
    weg=[32, D, 256], weu=[32, D, 256], wed=[32, 256, D])
DEPTH = 2


def build_full(S):
    kb = KB()
    x = kb.dram_in("x", [S, D], F32)
    cvec = kb.dram_in("cvec", [D], F32)
    cs = kb.dram_in("cs", [64, 2, S], F32)
    fg = kb.dram_in("fg", [D], F32)
    W = []
    for l in range(DEPTH):
        W.append({k: kb.dram_in("%s_%d" % (k, l), v, F32) for k, v in LAYER_SHAPES.items()})
        W[l]["cs"] = cs
    out = kb.dram_out("out", [S, D], F32)
    hT = kb.dram_tmp("s_hT", [8, 128, S], BF16)
    ymT = kb.dram_tmp("s_ymT", [8, 128, S], BF16)
    yaT = kb.dram_tmp("s_yaT", [8, 128, S], BF16)
    xmid = kb.dram_tmp("s_xmid", [S, D], F32)
    xnext = kb.dram_tmp("s_xnext", [S, D], F32)
    scr = alloc_mla_scratch(kb, S)
    t_hT, t_ymT, t_yaT = Tok("dram:hT"), Tok("dram:ymT"), Tok("dram:yaT")
    t_xmid, t_xnext, t_out, t_x = Tok("dram:xmid"), Tok("dram:xnext"), Tok("dram:out"), Tok("dram:x")
    consts = load_consts(kb)
    x_cur, t_xcur = x, t_x
    for l in range(DEPTH):
        w = W[l]
        mk = kb.mark()
        emit_k1_body(kb, consts, x_cur, cvec, w["modw"][:, 0:2048], w["modb"][0:2048], w["g1"], hT, S,
                     hT_tok=t_hT, x_tok=t_xcur)
        kb.release(mk)
        emit_mlstm_pass(kb, consts, S, hT, t_hT, w, ymT, t_ymT)
        emit_mla_proj_pass(kb, consts, S, hT, t_hT, w, scr)
        emit_attn_pass(kb, consts, S, scr, yaT, t_yaT)
        mk = kb.mark()
        mods = [kb.sbuf("mod%d" % i, [128, D], F32) for i in range(4)]
        tm = [Tok("mod%d" % i) for i in range(4)]
        g2t = kb.sbuf("g2t", [128, D], F32)
        tg2 = Tok("g2t")
        emit_mod(kb, consts, w["modw"][:, 2048:6144], w["modb"][2048:6144], cvec, 4, list(zip(mods, tm)))
        kb.dma("sync", g2t[:], w["g2"].partition_broadcast(128), writes=[tg2])
        kb.op("vector", lambda e: e.scalar_tensor_tensor(out=mods[2][:], in0=mods[2][:], scalar=1.0, in1=g2t[:],
                                                         op0=ALU.add, op1=ALU.mult),
              reads=[tm[2], tg2], writes=[tm[2]])
        t_in_all = _MultiTok([t_hT, t_ymT, t_yaT])
        emit_merge_pass(kb, consts, S, hT, ymT, yaT, t_in_all, x_cur, t_xcur, w, mods[0], tm[0], xmid, t_xmid)
        last = (l == DEPTH - 1)
        emit_moe_pass(kb, consts, S, xmid, t_xmid, w, mods[2], tm[2], mods[1], tm[1], mods[3], tm[3],
                      out if last else xnext, t_out if last else t_xnext, final_g=fg if last else None)
        kb.release(mk)
        x_cur, t_xcur = xnext, t_xnext
    kb.finish([t_out])
    return kb


class _MultiTok:
    def __init__(self, toks):
        self.name = "dram:multi"
        self._toks = toks


def _rope_tables(S):
    pos = np.arange(S, dtype=np.float32)
    inv_freq = (1.0 / (np.float32(10000.0) ** (np.arange(0, 64, 2, dtype=np.float32) / np.float32(64)))).astype(np.float32)
    ang = pos[:, None] * inv_freq[None, :]
    cos = np.cos(ang).astype(np.float32)
    sin = np.sin(ang).astype(np.float32)
    CC = np.concatenate([cos, cos], 1).T
    SS = np.concatenate([-sin, sin], 1).T
    return np.ascontiguousarray(np.stack([CC, SS], 1))


def _prep_layer(z, l):
    c = np.ascontiguousarray
    win = z["w_in"][l]
    d = {}
    d["modw"] = c(z["mod_w"][l])
    d["modb"] = c(z["mod_b"][l])
    d["g1"] = c(z["norm1_g"][l])
    d["wqk"] = c(win[:, :1024])
    d["wtm"] = c(win[:, 1024:3080])
    d["cw"] = c(z["conv_w"][l].reshape(4, 8, 128).transpose(2, 1, 0))
    d["cb"] = c(z["conv_b"][l].reshape(8, 128).T)
    d["ifb"] = c(np.concatenate([z["igate_b"][l], z["fgate_b"][l]]))
    kr = win[:, 3720:3784]
    d["wmla"] = c(np.concatenate([win[:, 3080:3784], kr[:, 32:], kr[:, :32]], 1))
    d["gqkv"] = c(np.concatenate([z["q_norm_g"][l].reshape(3, 128).T, z["kv_norm_g"][l].reshape(2, 128).T], 1))
    uq = z["w_uq"][l].reshape(384, 8, 192)
    d["wuqn"] = c(uq[:, :, :128].reshape(384, 1024))
    d["wuqr"] = c(uq[:, :, 128:].reshape(384, 512))
    d["wuqs"] = c(np.concatenate([uq[:, :, 160:], uq[:, :, 128:160]], 2).reshape(384, 512))
    ukv = z["w_ukv"][l].reshape(256, 8, 256)
    d["wuk"] = c(ukv[:, :, :128].reshape(256, 1024))
    d["wuv"] = c(ukv[:, :, 128:].reshape(256, 1024))
    d["wgate"] = c(win[:, 3784:5832])
    d["wbm"] = c(z["w_branch_m"][l])
    d["wba"] = c(z["w_branch_a"][l])
    d["wout"] = c(z["w_out"][l])
    d["g2"] = c(z["norm2_g"][l])
    d["wr"] = c(np.concatenate([z["w_group"][l], z["w_router"][l]], 1))
    d["rb"] = c(np.concatenate([z["b_group"][l], z["b_router"][l]]))
    d["weg"] = c(z["w_expert_gate"][l])
    d["weu"] = c(z["w_expert_up"][l])
    d["wed"] = c(z["w_expert_down"][l])
    return d


_PROGRAMS = {}


def kernel(**inputs):
    z = {k: np.asarray(v) for k, v in inputs.items()}
    x = z["x"].astype(np.float32, copy=False)
    B, S, _ = x.shape
    if S not in _PROGRAMS:
        _PROGRAMS[S] = build_full(S)
    kb = _PROGRAMS[S]
    shared = {"cs": _rope_tables(S), "fg": np.ascontiguousarray(z["final_norm_g"].astype(np.float32))}
    for l in range(DEPTH):
        for k, v in _prep_layer(z, l).items():
            assert list(v.shape) == LAYER_SHAPES[k], (k, v.shape)
            shared["%s_%d" % (k, l)] = v.astype(np.float32, copy=False)
    in_maps = []
    for b in range(B):
        m = dict(shared)
        m["x"] = np.ascontiguousarray(x[b])
        m["cvec"] = np.ascontiguousarray(z["c"][b].astype(np.float32))
        in_maps.append(m)
    res = run_bass_kernel_spmd(kb.nc, in_maps, core_ids=list(range(B)))
    return np.stack([np.asarray(r["out"]) for r in res.results], 0).astype(np.float32)
```

```python
import math
import numpy as np
import ml_dtypes
import concourse.bass as bass
import concourse.mybir as mybir
from concourse.bass_utils import run_bass_kernel_spmd

F32 = mybir.dt.float32
BF16 = mybir.dt.bfloat16
AF = mybir.ActivationFunctionType
ALU = mybir.AluOpType
AX = mybir.AxisListType

D = 1024
NCORES = 8
EPS = 1e-6
BIG = 30000.0


class Tok:
    __slots__ = ("w", "r", "wsem", "rsem", "name")

    def __init__(self, name=""):
        self.w = None
        self.r = []
        self.wsem = None
        self.rsem = None
        self.name = name


class _Sem:
    __slots__ = ("h", "cnt", "key")

    def __init__(self, h, key):
        self.h = h
        self.cnt = 0
        self.key = key


class _Eng:
    def __init__(self, name, e, sem):
        self.name = name
        self.e = e
        self.sem = sem
        self.seen = {}


class KB:
    def __init__(self):
        self.nc = bass.Bass("TRN2", target_bir_lowering=False)
        nc = self.nc
        self._nsem = 0
        self.eng = {}
        for name in ("tensor", "vector", "scalar", "gpsimd", "sync"):
            self.eng[name] = _Eng(name, getattr(nc, name), self._newsem("e_" + name))
        self._stack = []
        self._dsems = []
        self._free_dsems = []
        self._live_dsems = []
        self.ninst = 0

    def _newsem(self, name):
        if not name.startswith("e_") and self._free_dsems:
            sem = self._free_dsems.pop()
            self._live_dsems.append(sem)
            return sem
        self._nsem += 1
        h = self.nc.semaphore(name + "_%d" % self._nsem).__enter__()
        sem = _Sem(h, self._nsem)
        if not name.startswith("e_"):
            self._dsems.append(sem)
            self._live_dsems.append(sem)
        return sem

    def dram_in(self, name, shape, dt):
        return self.nc.dram_tensor(name, list(shape), dt, kind="ExternalInput").ap()

    def dram_out(self, name, shape, dt):
        return self.nc.dram_tensor(name, list(shape), dt, kind="ExternalOutput").ap()

    def dram_tmp(self, name, shape, dt):
        return self.nc.dram_tensor(name, list(shape), dt, kind="Internal").ap()

    def sbuf(self, name, shape, dt):
        self._uid = getattr(self, "_uid", 0) + 1
        cm = self.nc.sbuf_tensor("%s_u%d" % (name, self._uid), list(shape), dt)
        t = cm.__enter__()
        self._stack.append(cm)
        return t

    def psum(self, name, shape, dt):
        self._uid = getattr(self, "_uid", 0) + 1
        cm = self.nc.psum_tensor("%s_u%d" % (name, self._uid), list(shape), dt)
        t = cm.__enter__()
        self._stack.append(cm)
        return t

    def mark(self):
        return (len(self._stack), len(self._live_dsems))

    def release(self, mark):
        self.barrier()
        while len(self._stack) > mark[0]:
            self._stack.pop().__exit__(None, None, None)
        while len(self._live_dsems) > mark[1]:
            self._free_dsems.append(self._live_dsems.pop())

    def barrier(self):
        sems = [E.sem for E in self.eng.values()] + self._dsems
        for E in self.eng.values():
            for sem in sems:
                if sem is E.sem or sem.cnt == 0:
                    continue
                if E.seen.get(sem.key, 0) >= sem.cnt:
                    continue
                E.e.wait_ge(sem.h, sem.cnt)
                E.seen[sem.key] = sem.cnt
                self.ninst += 1

    def _wait(self, E, evs):
        need = {}
        for ev in evs:
            if ev is None:
                continue
            sem, val = ev
            if sem is E.sem and E.name == "tensor":
                continue
            if need.get(sem.key, (None, 0))[1] < val:
                need[sem.key] = (sem, val)
        for key, (sem, val) in need.items():
            if E.seen.get(key, 0) >= val:
                continue
            assert val <= sem.cnt, "waiting on an event that is never signalled"
            E.e.wait_ge(sem.h, val)
            E.seen[key] = val
            self.ninst += 1

    def _deps(self, reads, writes):
        evs = []
        for t in reads:
            if hasattr(t, "_toks"):
                evs.extend(x.w for x in t._toks)
            else:
                evs.append(t.w)
        for t in writes:
            evs.append(t.w)
            evs.extend(t.r)
        return evs

    def op(self, engname, fn, reads=(), writes=(), sig=True):
        E = self.eng[engname]
        self._wait(E, self._deps(reads, writes))
        ins = fn(E.e)
        self.ninst += 1
        if sig:
            E.sem.cnt += 1
            ins.then_inc(E.sem.h, 1)
            ev = (E.sem, E.sem.cnt)
        else:
            assert engname == "tensor"
            ev = (E.sem, E.sem.cnt + 1)
        for t in reads:
            for tt_ in (t._toks if hasattr(t, "_toks") else (t,)):
                tt_.r.append(ev)
                if len(tt_.r) > 24:
                    tt_.r = self._compact(tt_.r)
        for t in writes:
            t.w = ev
            t.r = []
        return ins

    @staticmethod
    def _compact(r):
        best = {}
        for sem, val in r:
            if best.get(sem.key, (None, 0))[1] < val:
                best[sem.key] = (sem, val)
        return list(best.values())

    def dma(self, queue, out, in_, reads=(), writes=(), **kw):
        E = self.eng[queue]
        evs = self._deps(reads, [])
        for t in writes:
            isdram = getattr(t, "name", "").startswith("dram:")
            if t.w is not None and not isdram and not (t.wsem is not None and t.w[0] is t.wsem):
                evs.append(t.w)
            evs.extend(t.r)
        self._wait(E, evs)
        ins = E.e.dma_start(out=out, in_=in_, **kw)
        self.ninst += 1
        if writes and writes[0].wsem is None and not getattr(writes[0], "_dram", False):
            pass
        tok = None
        for t in writes:
            if not getattr(t, "name", "").startswith("dram:"):
                tok = t
                if tok.wsem is None:
                    tok.wsem = self._newsem("dw")
                sem = tok.wsem
                break
        if tok is None:
            for t in reads:
                if not getattr(t, "name", "").startswith("dram:"):
                    tok = t
                    if tok.rsem is None:
                        tok.rsem = self._newsem("dr")
                    sem = tok.rsem
                    break
        assert tok is not None, "dma needs an sbuf-side token"
        sem.cnt += 16
        ins.then_inc(sem.h, 16)
        ev = (sem, sem.cnt)
        for t in reads:
            for tt_ in (t._toks if hasattr(t, "_toks") else (t,)):
                tt_.r.append(ev)
                if len(tt_.r) > 24:
                    tt_.r = self._compact(tt_.r)
        for t in writes:
            t.w = ev
            t.r = []
        return ins

    def finish(self, toks):
        E = self.eng["sync"]
        evs = []
        for t in toks:
            evs.append(t.w)
            evs.extend(t.r)
        self._wait(E, evs)


def load_consts(kb):
    nc = kb.nc
    c = {}
    c["tok"] = Tok("consts")
    ones_f = kb.sbuf("c_ones_f", [128, 128], F32)
    ones_b = kb.sbuf("c_ones_b", [128, 128], BF16)
    id_f = kb.sbuf("c_id_f", [128, 128], F32)
    id_b = kb.sbuf("c_id_b", [128, 128], BF16)
    tri_f = kb.sbuf("c_tri_f", [128, 128], F32)
    tris_f = kb.sbuf("c_tris_f", [128, 128], F32)
    tri_b = kb.sbuf("c_tri_b", [128, 128], BF16)
    t = c["tok"]
    kb.op("gpsimd", lambda e: e.memset(ones_f[:], 1.0), writes=[t])
    kb.op("gpsimd", lambda e: e.memset(ones_b[:], 1.0), writes=[t])
    kb.op("gpsimd", lambda e: e.affine_select(out=id_f[:], in_=ones_f[:], pattern=[[-1, 128]],
                                             compare_op=ALU.is_equal, fill=0.0, base=0,
                                             channel_multiplier=1), reads=[t], writes=[t])
    kb.op("gpsimd", lambda e: e.tensor_copy(out=id_b[:], in_=id_f[:]), reads=[t], writes=[t])
    kb.op("gpsimd", lambda e: e.affine_select(out=tri_f[:], in_=ones_f[:], pattern=[[1, 128]],
                                             compare_op=ALU.is_ge, fill=0.0, base=0,
                                             channel_multiplier=-1), reads=[t], writes=[t])
    kb.op("gpsimd", lambda e: e.tensor_copy(out=tri_b[:], in_=tri_f[:]), reads=[t], writes=[t])
    kb.op("gpsimd", lambda e: e.affine_select(out=tris_f[:], in_=ones_f[:], pattern=[[-1, 128]],
                                             compare_op=ALU.is_gt, fill=0.0, base=0,
                                             channel_multiplier=1), reads=[t], writes=[t])
    eps = kb.sbuf("c_eps", [128, 1], F32)
    kb.op("gpsimd", lambda e: e.memset(eps[:], EPS), writes=[t])
    c["eps"] = eps
    one = kb.sbuf("c_one", [128, 1], F32)
    kb.op("gpsimd", lambda e: e.memset(one[:], 1.0), writes=[t])
    c["one"] = one
    ones_b512 = kb.sbuf("c_ones_b512", [128, 512], BF16)
    kb.op("gpsimd", lambda e: e.memset(ones_b512[:], 1.0), writes=[t])
    c["ones_b512"] = ones_b512
    c.update(ones_f=ones_f, ones_b=ones_b, id_f=id_f, id_b=id_b, tri_f=tri_f, tris_f=tris_f,
             tri_b=tri_b)
    return c


def emit_mod(kb, consts, modw, modb, cvec, nvec, outs, ps_tag="modps"):
    nc = kb.nc
    mk = kb.mark()
    cpc = kb.sbuf("mod_cpc", [128, 8], F32)
    cond = kb.sbuf("mod_cond", [128, 8], F32)
    bc = kb.sbuf("mod_bc", [128, 8, 128], F32)
    wt = kb.sbuf("mod_w", [128, 8, 1024], F32)
    bt = kb.sbuf("mod_b", [128, 1024], F32)
    ps = kb.psum(ps_tag, [128, 512], F32)
    t_c, t_cond, t_bc, t_w, t_b, t_ps = (Tok("m%d" % i) for i in range(6))
    kb.dma("sync", cpc[:], cvec.rearrange("(p c) -> p c", c=8), writes=[t_c])
    kb.op("scalar", lambda e: e.activation(out=cond[:], in_=cpc[:], func=AF.Silu),
          reads=[t_c], writes=[t_cond])
    for c in range(8):
        kb.op("vector", lambda e, c=c: e.tensor_scalar_mul(out=bc[:, c, :], in0=consts["ones_f"][:],
                                                           scalar1=cond[:, c:c + 1]),
              reads=[t_cond, consts["tok"]], writes=[t_bc])
    for v in range(nvec):
        out_t, out_tok = outs[v]
        kb.dma("sync", wt[:], modw[:, v * 1024:(v + 1) * 1024].rearrange("(p c) n -> p c n", c=8),
               writes=[t_w])
        kb.dma("sync", bt[:], modb[v * 1024:(v + 1) * 1024].partition_broadcast(128), writes=[t_b])
        for n2 in range(2):
            for c in range(8):
                kb.op("tensor", lambda e, c=c, n2=n2: e.matmul(ps[:], lhsT=bc[:, c, :],
                                                               rhs=wt[:, c, n2 * 512:(n2 + 1) * 512],
                                                               start=(c == 0), stop=(c == 7)),
                      reads=[t_bc, t_w], writes=[t_ps], sig=(c == 7))
            kb.op("vector", lambda e, n2=n2: e.tensor_tensor(out=out_t[:, n2 * 512:(n2 + 1) * 512],
                                                             in0=ps[:], in1=bt[:, n2 * 512:(n2 + 1) * 512],
                                                             op=ALU.add),
                  reads=[t_ps, t_b], writes=[out_tok])
    kb.release(mk)


def emit_norm_mod(kb, xt, t_x, gmul, t_g, shift, t_s, hb, t_h, scr, t_scr, stat, t_stat, eps_ap):
    kb.op("gpsimd", lambda e: e.memset(stat[:, 0:1], 0.0), writes=[t_stat])
    kb.op("scalar", lambda e: e.activation(out=scr[:], in_=xt[:], func=AF.Square,
                                           accum_out=stat[:, 0:1]),
          reads=[t_x], writes=[t_scr, t_stat])
    kb.op("scalar", lambda e: e.activation(out=stat[:, 1:2], in_=stat[:, 0:1], func=AF.Sqrt,
                                           bias=eps_ap, scale=1.0 / D),
          reads=[t_stat], writes=[t_stat])
    kb.op("vector", lambda e: e.reciprocal(out=stat[:, 2:3], in_=stat[:, 1:2]),
          reads=[t_stat], writes=[t_stat])
    if shift is None:
        kb.op("vector", lambda e: e.scalar_tensor_tensor(out=hb[:], in0=xt[:], scalar=stat[:, 2:3],
                                                         in1=gmul[:], op0=ALU.mult, op1=ALU.mult),
              reads=[t_x, t_stat, t_g], writes=[t_h])
        return
    kb.op("vector", lambda e: e.scalar_tensor_tensor(out=scr[:], in0=xt[:], scalar=stat[:, 2:3],
                                                     in1=gmul[:], op0=ALU.mult, op1=ALU.mult),
          reads=[t_x, t_stat, t_g], writes=[t_scr])
    kb.op("gpsimd", lambda e: e.tensor_tensor(out=hb[:], in0=scr[:], in1=shift[:], op=ALU.add),
          reads=[t_scr, t_s], writes=[t_h])


def build_k1(Sh):
    kb = KB()
    nc = kb.nc
    x = kb.dram_in("x", [Sh, D], F32)
    cvec = kb.dram_in("cvec", [D], F32)
    modw = kb.dram_in("modw", [D, 2 * D], F32)
    modb = kb.dram_in("modb", [2 * D], F32)
    g = kb.dram_in("g", [D], F32)
    hT = kb.dram_out("hT", [8, 128, Sh], BF16)
    consts = load_consts(kb)
    emit_k1_body(kb, consts, x, cvec, modw, modb, g, hT, Sh)
    kb.finish([kb._out_tok])
    return kb


def emit_k1_body(kb, consts, x, cvec, modw, modb, g, hT, Sh, hT_tok=None, x_tok=None):
    shift = kb.sbuf("k1_shift", [128, D], F32)
    gmul = kb.sbuf("k1_gmul", [128, D], F32)
    gt = kb.sbuf("k1_g", [128, D], F32)
    t_shift, t_gmul, t_g = Tok("shift"), Tok("gmul"), Tok("g")
    emit_mod(kb, consts, modw, modb, cvec, 2, [(shift, t_shift), (gmul, t_gmul)])
    kb.dma("sync", gt[:], g.partition_broadcast(128), writes=[t_g])
    kb.op("vector", lambda e: e.scalar_tensor_tensor(out=gmul[:], in0=gmul[:], scalar=1.0, in1=gt[:],
                                                     op0=ALU.add, op1=ALU.mult),
          reads=[t_gmul, t_g], writes=[t_gmul])
    NB = 4
    xt = [kb.sbuf("k1_x%d" % i, [128, D], F32) for i in range(NB)]
    scr = [kb.sbuf("k1_scr%d" % i, [128, D], F32) for i in range(NB)]
    hb = [kb.sbuf("k1_hb%d" % i, [128, D], BF16) for i in range(NB)]
    stat = [kb.sbuf("k1_st%d" % i, [128, 4], F32) for i in range(NB)]
    hTs = [kb.sbuf("k1_hT%d" % i, [128, 8, 512], BF16) for i in range(2)]
    pst = [kb.psum("k1_pst%d" % i, [128, 8, 128], BF16) for i in range(2)]
    t_x = [Tok("x%d" % i) for i in range(NB)]
    t_scr = [Tok("scr%d" % i) for i in range(NB)]
    t_hb = [Tok("hb%d" % i) for i in range(NB)]
    t_st = [Tok("st%d" % i) for i in range(NB)]
    t_hT = [Tok("hT%d" % i) for i in range(2)]
    t_ps = [Tok("ps%d" % i) for i in range(2)]
    t_out = hT_tok if hT_tok is not None else Tok("dram:hT")
    kb._out_tok = t_out
    ntile = Sh // 128
    for i in range(ntile):
        s = i % NB
        grp = (i // 4) % 2
        kb.dma("sync", xt[s][:], x[i * 128:(i + 1) * 128, :], reads=([x_tok] if x_tok is not None else []), writes=[t_x[s]])
        emit_norm_mod(kb, xt[s], t_x[s], gmul, t_gmul, shift, t_shift, hb[s], t_hb[s],
                      scr[s], t_scr[s], stat[s], t_st[s], consts["eps"][:, 0:1])
        p = i % 2
        for c in range(8):
            kb.op("tensor", lambda e, c=c, s=s, p=p: e.transpose(out=pst[p][:, c, :],
                                                                 in_=hb[s][:, c * 128:(c + 1) * 128],
                                                                 identity=consts["id_b"][:]),
                  reads=[t_hb[s], consts["tok"]], writes=[t_ps[p]], sig=(c == 7))
        j = i % 4
        kb.op("scalar", lambda e, p=p, grp=grp, j=j: e.copy(out=hTs[grp][:, :, j * 128:(j + 1) * 128],
                                                            in_=pst[p][:]),
              reads=[t_ps[p]], writes=[t_hT[grp]])
        if j == 3:
            t0 = (i - 3) * 128
            kb.dma("gpsimd", hT[:, :, t0:t0 + 512].rearrange("c p t -> p c t"), hTs[grp][:],
                   reads=[t_hT[grp]], writes=[t_out])


def _mm(kb, out, lhsT, rhs, start, stop, reads, writes):
    return kb.op("tensor", lambda e: e.matmul(out, lhsT=lhsT, rhs=rhs, start=start, stop=stop),
                 reads=reads, writes=writes, sig=stop)


def load_w_bf16(kb, dst, src_rows_cols, tok, nchunk, queue="gpsimd"):
    for c in range(nchunk):
        kb.dma(queue, dst[:, c, :], src_rows_cols[c * 128:(c + 1) * 128, :], writes=[tok])


def emit_mlstm_pass(kb, consts, S, hT, t_hT, w, ymT, t_ymT):
    NT = S // 512
    mk = kb.mark()
    ct = consts["tok"]
    wqk = kb.sbuf("a_wqk", [128, 8, 1024], BF16)
    wtm = kb.sbuf("a_wtm", [128, 8, 2056], BF16)
    cw = kb.sbuf("a_cw", [128, 8, 4], F32)
    cb = kb.sbuf("a_cb", [128, 8], F32)
    ifb = kb.sbuf("a_ifb", [128, 8], F32)
    t_w = Tok("a_w")
    t_wt = Tok("a_wt")
    t_cw = Tok("a_cw")
    load_w_bf16(kb, wqk, w["wqk"], t_w, 8)
    load_w_bf16(kb, wtm, w["wtm"], t_wt, 8)
    kb.dma("sync", cw[:], w["cw"], writes=[t_cw])
    kb.dma("sync", cb[:], w["cb"], writes=[t_cw])
    kb.dma("sync", ifb[:], w["ifb"].partition_broadcast(128), writes=[t_cw])

    hTs = [kb.sbuf("a_hT%d" % i, [128, 8, 512], BF16) for i in range(2)]
    t_hTs = [Tok("a_hT%d" % i) for i in range(2)]
    pre = kb.sbuf("a_pre", [128, 8, 515], BF16)
    t_pre = [Tok("a_pre%d" % g) for g in range(8)]
    cacc = [kb.sbuf("a_cacc%d" % i, [128, 512], F32) for i in range(2)]
    t_cacc = [Tok("a_cacc%d" % i) for i in range(2)]
    qkT = kb.sbuf("a_qkT", [128, 8, 512], BF16)
    t_qk = [Tok("a_qk%d" % g) for g in range(8)]
    Vp = [kb.sbuf("a_vp%d" % i, [128, 4, 257], BF16) for i in range(2)]
    t_vp = [Tok("a_vp%d" % i) for i in range(2)]
    sgo = [kb.sbuf("a_sgo%d" % i, [128, 1024], F32) for i in range(2)]
    t_sgo = [Tok("a_sgo%d" % i) for i in range(2)]
    gsb = [kb.sbuf("a_g%d" % i, [128, 48], F32) for i in range(2)]
    t_g = [Tok("a_g%d" % i) for i in range(2)]
    Zf = kb.sbuf("a_zf", [128, 4, 257], F32)
    Zb = kb.sbuf("a_zb", [128, 4, 257], BF16)
    t_zf = [Tok("a_zf%d" % h) for h in range(4)]
    t_zb = [Tok("a_zb%d" % h) for h in range(4)]
    s0sb = [kb.sbuf("a_s0%d" % i, [128, 128], BF16) for i in range(2)]
    t_s0 = [Tok("a_s0%d" % i) for i in range(2)]
    ytok = [kb.sbuf("a_yt%d" % i, [128, 256], BF16) for i in range(2)]
    t_yt = [Tok("a_yt%d" % i) for i in range(2)]
    khat = [kb.sbuf("a_kh%d" % i, [128, 128], BF16) for i in range(2)]
    t_kh = [Tok("a_kh%d" % i) for i in range(2)]
    dsm = [kb.sbuf("a_d%d" % i, [128, 8], F32) for i in range(2)]
    t_d = [Tok("a_d%d" % i) for i in range(2)]
    yst = [kb.sbuf("a_yst%d" % i, [128, 8, 512], BF16) for i in range(2)]
    t_yst = [Tok("a_yst%d" % i) for i in range(2)]

    ps_fm = [kb.psum("a_psfm%d" % i, [128, 512], F32) for i in range(2)]
    t_psfm = [Tok("a_psfm%d" % i) for i in range(2)]
    ps_tm = [kb.psum("a_pstm%d" % i, [128, 512], F32) for i in range(2)]
    t_pstm = [Tok("a_pstm%d" % i) for i in range(2)]
    ps_m = kb.psum("a_psm", [128, 512], F32)
    t_ps_s0, t_ps_o, t_ps_if, t_ps_cs = Tok("ps_s0"), Tok("ps_o"), Tok("ps_if"), Tok("ps_cs")
    ps_u = kb.psum("a_psu", [128, 257], F32)
    t_ps_u = Tok("ps_u")
    ps_t = kb.psum("a_pst", [128, 3, 128], BF16)
    t_ps_yt, t_ps_kt = Tok("ps_yt"), Tok("ps_kt")

    dg = kb.sbuf("a_dg", [128, 32, 128], BF16)
    t_dg = Tok("a_dg")
    for g in range(8):
        for j in range(4):
            kb.op("vector", lambda e, g=g, j=j: e.tensor_scalar_mul(out=dg[:, g * 4 + j, :], in0=consts["id_f"][:],
                                                                    scalar1=cw[:, g, j:j + 1]),
                  reads=[t_cw, ct], writes=[t_dg])
    kb.op("gpsimd", lambda e: e.memset(pre[:, :, 0:3], 0.0), writes=t_pre)
    kb.op("gpsimd", lambda e: e.memset(Zf[:], 0.0), writes=t_zf)
    kb.op("gpsimd", lambda e: e.memset(Zb[:], 0.0), writes=t_zb)
    for i in range(2):
        kb.op("gpsimd", lambda e, i=i: e.memset(Vp[i][:, :, 256:257], 1.0), writes=[t_vp[i]])

    fmi = 0
    tmi = 0
    hi = 0
    for tt in range(NT):
        s = tt % 2
        kb.dma("sync", hTs[s][:], hT[:, :, tt * 512:(tt + 1) * 512].rearrange("c p t -> p c t"),
               reads=[t_hT], writes=[t_hTs[s]])
        for g in range(8):
            p = fmi % 2
            fmi += 1
            for c in range(8):
                _mm(kb, ps_fm[p][:], wqk[:, c, g * 128:(g + 1) * 128], hTs[s][:, c, :], c == 0, c == 7,
                    [t_w, t_hTs[s]], [t_psfm[p]])
            kb.op("scalar", lambda e, p=p, g=g: e.copy(out=pre[:, g, 3:515], in_=ps_fm[p][:]),
                  reads=[t_psfm[p]], writes=[t_pre[g]])
            pc = fmi % 2
            fmi += 1
            for j in range(4):
                _mm(kb, ps_fm[pc][:], dg[:, g * 4 + j, :], pre[:, g, j:j + 512], j == 0, j == 3,
                    [t_dg, t_pre[g]], [t_psfm[pc]])
            kb.op("scalar", lambda e, pc=pc, g=g: e.activation(out=qkT[:, g, :], in_=ps_fm[pc][:], func=AF.Silu,
                                                               bias=cb[:, g:g + 1]),
                  reads=[t_psfm[pc], t_cw], writes=[t_qk[g]])
            kb.op("gpsimd", lambda e, g=g: e.tensor_copy(out=pre[:, g, 0:3], in_=pre[:, g, 512:515]),
                  reads=[t_pre[g]], writes=[t_pre[g]])
        for j in range(4):
            vs = (tt * 4 + j) % 2
            tsl = slice(j * 128, (j + 1) * 128)
            for half in range(2):
                p = tmi % 2
                tmi += 1
                for c in range(8):
                    _mm(kb, ps_tm[p][:], hTs[s][:, c, tsl], wtm[:, c, half * 512:(half + 1) * 512],
                        c == 0, c == 7, [t_wt, t_hTs[s]], [t_pstm[p]])
                kb.op("scalar", lambda e, p=p, vs=vs, half=half: e.copy(
                    out=Vp[vs][:, 2 * half:2 * half + 2, 0:256],
                    in_=ps_tm[p][:].rearrange("p (h v) -> p h v", h=2)),
                    reads=[t_pstm[p]], writes=[t_vp[vs]])
            for half in range(2):
                p = tmi % 2
                tmi += 1
                for c in range(8):
                    _mm(kb, ps_tm[p][:], hTs[s][:, c, tsl], wtm[:, c, 1024 + half * 512:1024 + (half + 1) * 512],
                        c == 0, c == 7, [t_wt, t_hTs[s]], [t_pstm[p]])
                kb.op("scalar", lambda e, p=p, vs=vs, half=half: e.activation(
                    out=sgo[vs][:, half * 512:(half + 1) * 512], in_=ps_tm[p][:], func=AF.Sigmoid),
                    reads=[t_pstm[p]], writes=[t_sgo[vs]])
            for c in range(8):
                _mm(kb, ps_m[:, 385:393], hTs[s][:, c, tsl], wtm[:, c, 2048:2056], c == 0, c == 7,
                    [t_wt, t_hTs[s]], [t_ps_if])
            G = gsb[vs]
            tg = t_g[vs]
            kb.op("vector", lambda e, G=G: e.tensor_tensor(out=G[:, 0:8], in0=ps_m[:, 385:393], in1=ifb[:],
                                                           op=ALU.add),
                  reads=[t_ps_if, t_cw], writes=[tg])
            kb.op("scalar", lambda e, G=G: e.activation(out=G[:, 8:12], in_=G[:, 4:8], func=AF.Exp, scale=-1.0),
                  reads=[tg], writes=[tg])
            kb.op("scalar", lambda e, G=G: e.activation(out=G[:, 8:12], in_=G[:, 8:12], func=AF.Ln,
                                                        bias=consts["one"][:, 0:1]),
                  reads=[tg, ct], writes=[tg])
            kb.op("vector", lambda e, G=G: e.tensor_scalar_mul(out=G[:, 8:12], in0=G[:, 8:12], scalar1=-1.0),
                  reads=[tg], writes=[tg])
            _mm(kb, ps_m[:, 393:397], consts["tri_f"][:], G[:, 8:12], True, True, [ct, tg], [t_ps_cs])
            _mm(kb, ps_m[:, 397:401], consts["tris_f"][:], G[:, 8:12], True, True, [ct, tg], [t_ps_cs])
            _mm(kb, ps_m[:, 401:405], consts["ones_f"][:], G[:, 8:12], True, True, [ct, tg], [t_ps_cs])
            kb.op("vector", lambda e, G=G: e.tensor_copy(out=G[:, 16:20], in_=ps_m[:, 393:397]),
                  reads=[t_ps_cs], writes=[tg])
            kb.op("vector", lambda e, G=G: e.tensor_tensor(out=G[:, 20:24], in0=G[:, 0:4], in1=ps_m[:, 393:397],
                                                           op=ALU.subtract),
                  reads=[t_ps_cs, tg], writes=[tg])
            kb.op("vector", lambda e, G=G: e.tensor_tensor(out=G[:, 24:28], in0=G[:, 0:4], in1=ps_m[:, 397:401],
                                                           op=ALU.add),
                  reads=[t_ps_cs, tg], writes=[tg])
            kb.op("vector", lambda e, G=G: e.tensor_copy(out=G[:, 28:32], in_=ps_m[:, 401:405]),
                  reads=[t_ps_cs], writes=[tg])
            kb.op("scalar", lambda e, G=G: e.activation(out=G[:, 32:48], in_=G[:, 16:32], func=AF.Exp),
                  reads=[tg], writes=[tg])
            kb.op("vector", lambda e, G=G: e.tensor_scalar_mul(out=G[:, 12:16], in0=G[:, 32:36],
                                                               scalar1=128.0 ** -0.5),
                  reads=[tg], writes=[tg])
            kb.op("vector", lambda e, G=G: e.reciprocal(out=G[:, 12:16], in_=G[:, 12:16]),
                  reads=[tg], writes=[tg])
            for h in range(4):
                q = hi % 2
                hi += 1
                qT = qkT[:, h, tsl]
                kT = qkT[:, 4 + h, tsl]
                _mm(kb, ps_m[:, 0:128], kT, qT, True, True, [t_qk[h], t_qk[4 + h]], [t_ps_s0])
                kb.op("vector", lambda e, q=q, G=G, h=h: e.scalar_tensor_tensor(
                    out=s0sb[q][:], in0=ps_m[:, 0:128], scalar=G[:, 36 + h:37 + h], in1=consts["tri_f"][:],
                    op0=ALU.mult, op1=ALU.mult), reads=[t_ps_s0, tg, ct], writes=[t_s0[q]])
                _mm(kb, ps_m[:, 128:385], s0sb[q][:], Vp[vs][:, h, :], True, False, [t_s0[q], t_vp[vs]], [t_ps_o])
                _mm(kb, ps_m[:, 128:385], qT, Zb[:, h, :], False, True, [t_qk[h], t_zb[h]], [t_ps_o])
                dd = dsm[q]
                td = t_d[q]
                kb.op("scalar", lambda e, dd=dd: e.activation(out=dd[:, 0:1], in_=ps_m[:, 384:385], func=AF.Abs),
                      reads=[t_ps_o], writes=[td])
                kb.op("vector", lambda e, dd=dd, G=G, h=h: e.tensor_tensor(
                    out=dd[:, 1:2], in0=dd[:, 0:1], in1=G[:, 12 + h:13 + h], op=ALU.max),
                    reads=[td, tg], writes=[td])
                kb.op("vector", lambda e, dd=dd: e.reciprocal(out=dd[:, 4:5], in_=dd[:, 1:2]),
                      reads=[td], writes=[td])
                kb.op("vector", lambda e, q=q, dd=dd, vs=vs, h=h: e.scalar_tensor_tensor(
                    out=ytok[q][:], in0=ps_m[:, 128:384], scalar=dd[:, 4:5],
                    in1=sgo[vs][:, h * 256:(h + 1) * 256], op0=ALU.mult, op1=ALU.mult),
                    reads=[t_ps_o, td, t_sgo[vs]], writes=[t_yt[q]])
                for k in range(2):
                    kb.op("tensor", lambda e, q=q, k=k: e.transpose(out=ps_t[:, k, :],
                                                                    in_=ytok[q][:, k * 128:(k + 1) * 128],
                                                                    identity=consts["id_b"][:]),
                          reads=[t_yt[q], ct], writes=[t_ps_yt], sig=(k == 1))
                kb.op("scalar", lambda e, s=s, h=h, tsl=tsl: e.copy(out=yst[s][:, 2 * h:2 * h + 2, tsl],
                                                                    in_=ps_t[:, 0:2, :]),
                      reads=[t_ps_yt], writes=[t_yst[s]])
                kb.op("tensor", lambda e, kT=kT: e.transpose(out=ps_t[:, 2, :], in_=kT, identity=consts["id_b"][:]),
                      reads=[t_qk[4 + h], ct], writes=[t_ps_kt])
                kb.op("scalar", lambda e, q=q, G=G, h=h: e.activation(out=khat[q][:], in_=ps_t[:, 2, :],
                                                                      func=AF.Copy, scale=G[:, 40 + h:41 + h]),
                      reads=[t_ps_kt, tg], writes=[t_kh[q]])
                _mm(kb, ps_u[:], khat[q][:], Vp[vs][:, h, :], True, True, [t_kh[q], t_vp[vs]], [t_ps_u])
                kb.op("vector", lambda e, G=G, h=h: e.scalar_tensor_tensor(
                    out=Zf[:, h, :], in0=Zf[:, h, :], scalar=G[:, 44 + h:45 + h], in1=ps_u[:],
                    op0=ALU.mult, op1=ALU.add), reads=[t_zf[h], tg, t_ps_u], writes=[t_zf[h]])
                kb.op("gpsimd", lambda e, h=h: e.tensor_copy(out=Zb[:, h, :], in_=Zf[:, h, :]),
                      reads=[t_zf[h]], writes=[t_zb[h]])
        kb.dma("gpsimd", ymT[:, :, tt * 512:(tt + 1) * 512].rearrange("c p t -> p c t"), yst[s][:],
               reads=[t_yst[s]], writes=[t_ymT])
    kb.release(mk)


def emit_mla_proj_pass(kb, consts, S, hT, t_hT, w, scr):
    NT = S // 512
    mk = kb.mark()
    ct = consts["tok"]
    wmla = kb.sbuf("b_wmla", [128, 8, 768], BF16)
    wuqn = kb.sbuf("b_wuqn", [128, 3, 1024], BF16)
    wuqr = kb.sbuf("b_wuqr", [128, 3, 512], BF16)
    wuqs = kb.sbuf("b_wuqs", [128, 3, 512], BF16)
    wuk = kb.sbuf("b_wuk", [128, 2, 1024], BF16)
    wuv = kb.sbuf("b_wuv", [128, 2, 1024], BF16)
    gq = kb.sbuf("b_gq", [128, 5], F32)
    t_w = Tok("b_w")
    load_w_bf16(kb, wmla, w["wmla"], t_w, 8)
    load_w_bf16(kb, wuqn, w["wuqn"], t_w, 3)
    load_w_bf16(kb, wuqr, w["wuqr"], t_w, 3)
    load_w_bf16(kb, wuqs, w["wuqs"], t_w, 3)
    load_w_bf16(kb, wuk, w["wuk"], t_w, 2)
    load_w_bf16(kb, wuv, w["wuv"], t_w, 2)
    kb.dma("sync", gq[:], w["gqkv"], writes=[t_w])
    epsq = consts["eps"]

    hTs = [kb.sbuf("b_hT%d" % i, [128, 8, 512], BF16) for i in range(2)]
    t_hTs = [Tok("b_hT%d" % i) for i in range(2)]
    cs = [kb.sbuf("b_cs%d" % i, [64, 2, 512], F32) for i in range(2)]
    t_cs = [Tok("b_cs%d" % i) for i in range(2)]
    raw = kb.sbuf("b_raw", [128, 5, 512], F32)
    t_raw = [Tok("b_raw%d" % g) for g in range(5)]
    sq = kb.sbuf("b_sq", [128, 5, 512], BF16)
    t_sq = [Tok("b_sq%d" % g) for g in range(5)]
    rs = kb.sbuf("b_rs", [128, 2, 512], F32)
    t_rs = [Tok("b_rs%d" % g) for g in range(2)]
    cn = kb.sbuf("b_cn", [128, 5, 512], BF16)
    t_cn = [Tok("b_cn%d" % g) for g in range(5)]
    rt = [kb.sbuf("b_rt%d" % i, [64, 2, 512], F32) for i in range(2)]
    t_rt = [Tok("b_rt%d" % i) for i in range(2)]
    krs = kb.sbuf("b_krs", [64, 512], BF16)
    t_krs = Tok("b_krs")
    qn_st = kb.sbuf("b_qn", [128, 8, 512], BF16)
    qr_st = kb.sbuf("b_qr", [64, 8, 512], BF16)
    kn_st = kb.sbuf("b_kn", [128, 8, 512], BF16)
    t_qn, t_qr, t_kn = Tok("b_qn"), Tok("b_qr"), Tok("b_kn")
    v_st = [kb.sbuf("b_v%d" % i, [128, 1024], BF16) for i in range(2)]
    t_v = [Tok("b_v%d" % i) for i in range(2)]

    ps_fm = [kb.psum("b_psfm%d" % i, [128, 512], F32) for i in range(2)]
    t_psfm = [Tok("b_psfm%d" % i) for i in range(2)]
    ps_ss = [kb.psum("b_psss%d" % i, [128, 512], F32) for i in range(2)]
    t_psss = [Tok("b_psss%d" % i) for i in range(2)]
    ps_r = [kb.psum("b_psr%d" % i, [128, 512], F32) for i in range(2)]
    t_psr = [Tok("b_psr%d" % i) for i in range(2)]
    ps_tm = [kb.psum("b_pstm%d" % i, [128, 512], F32) for i in range(2)]
    t_pstm = [Tok("b_pstm%d" % i) for i in range(2)]
    d_QTn, d_QTr, d_KTn, d_KrT, d_Vd = scr["QTn"], scr["QTr"], scr["KTn"], scr["KrT"], scr["Vd"]
    t_scr = scr["tok"]

    fmi = 0
    ri = 0
    vi = 0

    def rope(psa, psb, t_pa, t_pb, cst, t_cst, out_ap, t_out):
        nonlocal ri
        k = ri % 2
        ri += 1
        kb.op("vector", lambda e: e.tensor_tensor(out=rt[k][:, 0, :], in0=psa, in1=cst[:, 0, :], op=ALU.mult),
              reads=[t_pa, t_cst], writes=[t_rt[k]])
        kb.op("vector", lambda e: e.tensor_tensor(out=rt[k][:, 1, :], in0=psb, in1=cst[:, 1, :], op=ALU.mult),
              reads=[t_pb, t_cst], writes=[t_rt[k]])
        kb.op("gpsimd", lambda e: e.tensor_tensor(out=out_ap, in0=rt[k][:, 0, :], in1=rt[k][:, 1, :], op=ALU.add),
              reads=[t_rt[k]], writes=[t_out])

    for tt in range(NT):
        s = tt % 2
        tok_sl = slice(tt * 512, (tt + 1) * 512)
        kb.dma("sync", hTs[s][:], hT[:, :, tok_sl].rearrange("c p t -> p c t"), reads=[t_hT], writes=[t_hTs[s]])
        kb.dma("sync", cs[s][:], w["cs"][:, :, tok_sl], writes=[t_cs[s]])
        for g in range(5):
            p = fmi % 2
            fmi += 1
            for c in range(8):
                _mm(kb, ps_fm[p][:], wmla[:, c, g * 128:(g + 1) * 128], hTs[s][:, c, :], c == 0, c == 7,
                    [t_w, t_hTs[s]], [t_psfm[p]])
            kb.op("scalar", lambda e, p=p, g=g: e.copy(out=raw[:, g, :], in_=ps_fm[p][:]),
                  reads=[t_psfm[p]], writes=[t_raw[g]])
            kb.op("gpsimd", lambda e, g=g: e.tensor_tensor(out=sq[:, g, :], in0=raw[:, g, :], in1=raw[:, g, :],
                                                           op=ALU.mult), reads=[t_raw[g]], writes=[t_sq[g]])
        for n, (g0, g1, dim) in enumerate(((0, 3, 384.0), (3, 5, 256.0))):
            for g in range(g0, g1):
                _mm(kb, ps_ss[n][:], consts["ones_b"][:], sq[:, g, :], g == g0, g == g1 - 1,
                    [ct, t_sq[g]], [t_psss[n]])
            kb.op("scalar", lambda e, n=n, dim=dim: e.activation(out=rs[:, n, :], in_=ps_ss[n][:], func=AF.Sqrt,
                                                                 bias=epsq[:, 0:1], scale=1.0 / dim),
                  reads=[t_psss[n], ct], writes=[t_rs[n]])
            kb.op("vector", lambda e, n=n: e.reciprocal(out=rs[:, n, :], in_=rs[:, n, :]),
                  reads=[t_rs[n]], writes=[t_rs[n]])
            for g in range(g0, g1):
                kb.op("vector", lambda e, n=n, g=g: e.scalar_tensor_tensor(
                    out=cn[:, g, :], in0=raw[:, g, :], scalar=gq[:, g:g + 1], in1=rs[:, n, :],
                    op0=ALU.mult, op1=ALU.mult), reads=[t_raw[g], t_w, t_rs[n]], writes=[t_cn[g]])
        for k2 in range(2):
            for c in range(8):
                _mm(kb, ps_r[k2][0:64, :], wmla[:, c, 640 + 64 * k2:704 + 64 * k2], hTs[s][:, c, :], c == 0, c == 7,
                    [t_w, t_hTs[s]], [t_psr[k2]])
        rope(ps_r[0][0:64, :], ps_r[1][0:64, :], t_psr[0], t_psr[1], cs[s], t_cs[s], krs[:], t_krs)
        kb.dma("gpsimd", d_KrT[:, tok_sl], krs[:], reads=[t_krs], writes=[t_scr])
        for h in range(8):
            p = fmi % 2
            fmi += 1
            for c in range(3):
                _mm(kb, ps_fm[p][:], wuqn[:, c, h * 128:(h + 1) * 128], cn[:, c, :], c == 0, c == 2,
                    [t_w, t_cn[c]], [t_psfm[p]])
            kb.op("scalar", lambda e, p=p, h=h: e.copy(out=qn_st[:, h, :], in_=ps_fm[p][:]),
                  reads=[t_psfm[p]], writes=[t_qn])
            for k2, wsrc in enumerate((wuqr, wuqs)):
                for c in range(3):
                    _mm(kb, ps_r[k2][0:64, :], wsrc[:, c, h * 64:(h + 1) * 64], cn[:, c, :], c == 0, c == 2,
                        [t_w, t_cn[c]], [t_psr[k2]])
            rope(ps_r[0][0:64, :], ps_r[1][0:64, :], t_psr[0], t_psr[1], cs[s], t_cs[s], qr_st[:, h, :], t_qr)
            p = fmi % 2
            fmi += 1
            for c in range(2):
                _mm(kb, ps_fm[p][:], wuk[:, c, h * 128:(h + 1) * 128], cn[:, 3 + c, :], c == 0, c == 1,
                    [t_w, t_cn[3 + c]], [t_psfm[p]])
            kb.op("scalar", lambda e, p=p, h=h: e.copy(out=kn_st[:, h, :], in_=ps_fm[p][:]),
                  reads=[t_psfm[p]], writes=[t_kn])
        kb.dma("gpsimd", d_QTn[:, :, tok_sl].rearrange("h p t -> p h t"), qn_st[:], reads=[t_qn], writes=[t_scr])
        kb.dma("gpsimd", d_QTr[:, :, tok_sl].rearrange("h p t -> p h t"), qr_st[:], reads=[t_qr], writes=[t_scr])
        kb.dma("gpsimd", d_KTn[:, :, tok_sl].rearrange("h p t -> p h t"), kn_st[:], reads=[t_kn], writes=[t_scr])
        for j in range(4):
            q = vi % 2
            vi += 1
            for half in range(2):
                p = half
                for c in range(2):
                    _mm(kb, ps_tm[p][:], cn[:, 3 + c, j * 128:(j + 1) * 128], wuv[:, c, half * 512:(half + 1) * 512],
                        c == 0, c == 1, [t_w, t_cn[3 + c]], [t_pstm[p]])
                kb.op("scalar", lambda e, p=p, q=q, half=half: e.copy(out=v_st[q][:, half * 512:(half + 1) * 512],
                                                                      in_=ps_tm[p][:]),
                      reads=[t_pstm[p]], writes=[t_v[q]])
            kb.dma("gpsimd", d_Vd[:, :, tt * 4 + j, :].rearrange("h p d -> p h d"),
                   v_st[q][:].rearrange("p (h d) -> p h d", h=8), reads=[t_v[q]], writes=[t_scr])
    kb.release(mk)


def emit_attn_pass(kb, consts, S, scr, yaT, t_yaT):
    NQ = S // 512
    NK = S // 128
    mk = kb.mark()
    ct = consts["tok"]
    scale = 192.0 ** -0.5
    t_scr = scr["tok"]
    masks = kb.sbuf("c_mask", [128, 4, 512], BF16)
    t_mask = Tok("c_mask")
    for r in range(4):
        for kbk in range(2):
            kb.op("gpsimd", lambda e, r=r, kbk=kbk: e.affine_select(
                out=masks[64 * kbk:64 * kbk + 64, r, :].rearrange("p (a b) -> p a b", a=8),
                in_=consts["ones_b512"][64 * kbk:64 * kbk + 64, :].rearrange("p (a b) -> p a b", a=8),
                pattern=[[1, 8], [0, 64]], compare_op=ALU.is_ge, fill=0.0, base=-(2 * r + kbk),
                channel_multiplier=0), reads=[ct], writes=[t_mask])
    krt = kb.sbuf("c_krt", [64, S], BF16)
    t_krt = Tok("c_krt")
    kb.dma("sync", krt[:], scr["KrT"][:, :], reads=[t_scr], writes=[t_krt])
    ktn = [kb.sbuf("c_ktn%d" % i, [128, S], BF16) for i in range(2)]
    t_ktn = [Tok("c_ktn%d" % i) for i in range(2)]
    vsb = [kb.sbuf("c_v%d" % i, [128, NK, 128], BF16) for i in range(2)]
    t_vsb = [Tok("c_v%d" % i) for i in range(2)]
    qn = [kb.sbuf("c_qn%d" % i, [128, 512], BF16) for i in range(2)]
    qr = [kb.sbuf("c_qr%d" % i, [64, 512], BF16) for i in range(2)]
    t_q = [Tok("c_q%d" % i) for i in range(2)]
    NPT = 3
    NPS = 2
    pacc = [kb.sbuf("c_pacc%d" % i, [128, 1024], F32) for i in range(2)]
    t_pacc = [Tok("c_pacc%d" % i) for i in range(2)]
    pT = [kb.sbuf("c_pT%d" % i, [128, 1024], BF16) for i in range(NPT)]
    t_pT = [Tok("c_pT%d" % i) for i in range(NPT)]
    rden = [kb.sbuf("c_rd%d" % i, [128, 512], F32) for i in range(2)]
    t_rden = [Tok("c_rd%d" % i) for i in range(2)]
    yst = [kb.sbuf("c_y%d" % i, [128, 512], BF16) for i in range(2)]
    t_yst = [Tok("c_y%d" % i) for i in range(2)]
    ps_s = [kb.psum("c_pss%d" % i, [128, 1024], F32) for i in range(NPS)]
    t_pss = [Tok("c_pss%d" % i) for i in range(NPS)]
    ps_a = [kb.psum("c_psa%d" % i, [128, 512], F32) for i in range(2)]
    t_psa = [Tok("c_psa%d" % i) for i in range(2)]
    ps_d = [kb.psum("c_psd%d" % i, [128, 512], F32) for i in range(1)]
    t_psd = [Tok("c_psd%d" % i) for i in range(1)]
    gi = 0
    qi = 0
    for h in range(8):
        hs = h % 2
        kb.dma("sync", ktn[hs][:], scr["KTn"][h, :, :], reads=[t_scr], writes=[t_ktn[hs]])
        kb.dma("sync", vsb[hs][:], scr["Vd"][h, :, :, :], reads=[t_scr], writes=[t_vsb[hs]])
        for j in range(NQ):
            a = qi % 2
            qi += 1
            qsl = slice(j * 512, (j + 1) * 512)
            kb.dma("sync", qn[a][:], scr["QTn"][h, :, qsl], reads=[t_scr], writes=[t_q[a]])
            kb.dma("sync", qr[a][:], scr["QTr"][h, :, qsl], reads=[t_scr], writes=[t_q[a]])
            nkt = 4 * (j + 1)
            npair = nkt // 2
            base = gi
            gi += npair

            def qk(pr):
                p = (base + pr) % NPS
                for half in range(2):
                    kt = 2 * pr + half
                    ksl = slice(kt * 128, (kt + 1) * 128)
                    osl = slice(half * 512, (half + 1) * 512)
                    _mm(kb, ps_s[p][:, osl], ktn[hs][:, ksl], qn[a][:], True, False, [t_ktn[hs], t_q[a]], [t_pss[p]])
                    _mm(kb, ps_s[p][:, osl], krt[:, ksl], qr[a][:], False, True, [t_krt, t_q[a]], [t_pss[p]])

            def soft(pr):
                p = (base + pr) % NPS
                u = (base + pr) % NPT
                kb.op("scalar", lambda e: e.activation(out=pT[u][:], in_=ps_s[p][:], func=AF.Exp, scale=scale),
                      reads=[t_pss[p]], writes=[t_pT[u]])
                if 2 * pr >= 4 * j:
                    r = 2 * pr - 4 * j
                    kb.op("vector", lambda e: e.tensor_tensor(
                        out=pT[u][:], in0=pT[u][:], in1=masks[:, r:r + 2, :].rearrange("p a b -> p (a b)"),
                        op=ALU.mult), reads=[t_pT[u], t_mask], writes=[t_pT[u]])
                if pr == 0:
                    kb.op("vector", lambda e: e.tensor_copy(out=pacc[a][:], in_=pT[u][:]),
                          reads=[t_pT[u]], writes=[t_pacc[a]])
                else:
                    kb.op("vector", lambda e: e.tensor_tensor(out=pacc[a][:], in0=pacc[a][:], in1=pT[u][:],
                                                              op=ALU.add),
                          reads=[t_pT[u], t_pacc[a]], writes=[t_pacc[a]])

            def pv(pr):
                u = (base + pr) % NPT
                for half in range(2):
                    kt = 2 * pr + half
                    _mm(kb, ps_a[a][:], vsb[hs][:, kt, :], pT[u][:, half * 512:(half + 1) * 512],
                        kt == 0, kt == nkt - 1, [t_vsb[hs], t_pT[u]], [t_psa[a]])

            qk(0)
            for pr in range(npair):
                soft(pr)
                if pr + 1 < npair:
                    qk(pr + 1)
                pv(pr)
            _mm(kb, ps_d[0][:], consts["ones_f"][:], pacc[a][:, 0:512], True, False, [ct, t_pacc[a]], [t_psd[0]])
            _mm(kb, ps_d[0][:], consts["ones_f"][:], pacc[a][:, 512:1024], False, True, [ct, t_pacc[a]], [t_psd[0]])
            kb.op("vector", lambda e, a=a: e.reciprocal(out=rden[a][:], in_=ps_d[0][:]),
                  reads=[t_psd[0]], writes=[t_rden[a]])
            kb.op("vector", lambda e, a=a: e.tensor_tensor(out=yst[a][:], in0=ps_a[a][:], in1=rden[a][:],
                                                           op=ALU.mult),
                  reads=[t_psa[a], t_rden[a]], writes=[t_yst[a]])
            kb.dma("gpsimd", yaT[h, :, qsl], yst[a][:], reads=[t_yst[a]], writes=[t_yaT])
    kb.release(mk)


def alloc_mla_scratch(kb, S):
    return dict(QTn=kb.dram_tmp("s_QTn", [8, 128, S], BF16), QTr=kb.dram_tmp("s_QTr", [8, 64, S], BF16),
                KTn=kb.dram_tmp("s_KTn", [8, 128, S], BF16), KrT=kb.dram_tmp("s_KrT", [64, S], BF16),
                Vd=kb.dram_tmp("s_Vd", [8, 128, S // 128, 128], BF16), tok=Tok("dram:mla_scr"))


def emit_merge_pass(kb, consts, S, hT, ymT, yaT, t_in, x, t_x, w, gate1, t_gate1, xmid, t_xmid):
    NT = S // 512
    mk = kb.mark()
    wg = kb.sbuf("d_wg", [128, 8, 2048], BF16)
    wbm = kb.sbuf("d_wbm", [128, 8, 1024], BF16)
    wba = kb.sbuf("d_wba", [128, 8, 1024], BF16)
    wo = kb.sbuf("d_wo", [128, 8, 1024], BF16)
    t_w = Tok("d_w")
    load_w_bf16(kb, wg, w["wgate"], t_w, 8)
    load_w_bf16(kb, wbm, w["wbm"], t_w, 8)
    load_w_bf16(kb, wba, w["wba"], t_w, 8)
    load_w_bf16(kb, wo, w["wout"], t_w, 8)
    hTs = [kb.sbuf("d_hT%d" % i, [128, 8, 512], BF16) for i in range(2)]
    ymS = [kb.sbuf("d_ym%d" % i, [128, 8, 512], BF16) for i in range(2)]
    yaS = [kb.sbuf("d_ya%d" % i, [128, 8, 512], BF16) for i in range(2)]
    t_ld = [Tok("d_ld%d" % i) for i in range(2)]
    sg = [kb.sbuf("d_sg%d" % i, [128, 2, 512], F32) for i in range(2)]
    t_sg = [Tok("d_sg%d" % i) for i in range(2)]
    mm_ = [kb.sbuf("d_mm%d" % i, [128, 2, 512], F32) for i in range(2)]
    t_mm = [Tok("d_mm%d" % i) for i in range(2)]
    mg = [kb.sbuf("d_mg%d" % i, [128, 8, 512], BF16) for i in range(2)]
    t_mg = [Tok("d_mg%d" % i) for i in range(2)]
    xt = [kb.sbuf("d_x%d" % i, [128, 1024], F32) for i in range(2)]
    t_xt = [Tok("d_x%d" % i) for i in range(2)]
    tmp = [kb.sbuf("d_tmp%d" % i, [128, 512], F32) for i in range(2)]
    t_tmp = [Tok("d_tmp%d" % i) for i in range(2)]
    ps4 = [kb.psum("d_ps%d" % i, [128, 512], F32) for i in range(4)]
    t_ps4 = [Tok("d_ps%d" % i) for i in range(4)]
    pso = [kb.psum("d_pso%d" % i, [128, 512], F32) for i in range(2)]
    t_pso = [Tok("d_pso%d" % i) for i in range(2)]
    oi = 0
    xi = 0
    for tt in range(NT):
        s = tt % 2
        tsl = slice(tt * 512, (tt + 1) * 512)
        kb.dma("sync", hTs[s][:], hT[:, :, tsl].rearrange("c p t -> p c t"), reads=[t_in], writes=[t_ld[s]])
        kb.dma("sync", ymS[s][:], ymT[:, :, tsl].rearrange("c p t -> p c t"), reads=[t_in], writes=[t_ld[s]])
        kb.dma("sync", yaS[s][:], yaT[:, :, tsl].rearrange("c p t -> p c t"), reads=[t_in], writes=[t_ld[s]])
        for oc in range(8):
            a = oc % 2
            osl = slice(oc * 128, (oc + 1) * 128)
            for c in range(8):
                _mm(kb, ps4[0][:], wbm[:, c, osl], ymS[s][:, c, :], c == 0, c == 7, [t_w, t_ld[s]], [t_ps4[0]])
            for c in range(8):
                _mm(kb, ps4[1][:], wba[:, c, osl], yaS[s][:, c, :], c == 0, c == 7, [t_w, t_ld[s]], [t_ps4[1]])
            for k in range(2):
                for c in range(8):
                    _mm(kb, ps4[2 + k][:], wg[:, c, k * 1024 + oc * 128:k * 1024 + (oc + 1) * 128], hTs[s][:, c, :],
                        c == 0, c == 7, [t_w, t_ld[s]], [t_ps4[2 + k]])
                kb.op("scalar", lambda e, a=a, k=k: e.activation(out=sg[a][:, k, :], in_=ps4[2 + k][:],
                                                                 func=AF.Sigmoid),
                      reads=[t_ps4[2 + k]], writes=[t_sg[a]])
            for k in range(2):
                kb.op("vector", lambda e, a=a, k=k: e.tensor_tensor(out=mm_[a][:, k, :], in0=ps4[k][:],
                                                                    in1=sg[a][:, k, :], op=ALU.mult),
                      reads=[t_ps4[k], t_sg[a]], writes=[t_mm[a]])
            kb.op("gpsimd", lambda e, a=a, s=s, oc=oc: e.tensor_tensor(out=mg[s][:, oc, :], in0=mm_[a][:, 0, :],
                                                                       in1=mm_[a][:, 1, :], op=ALU.add),
                  reads=[t_mm[a]], writes=[t_mg[s]])
        for j in range(4):
            xs = xi % 2
            xi += 1
            t0 = tt * 512 + j * 128
            kb.dma("sync", xt[xs][:], x[t0:t0 + 128, :], reads=[t_x], writes=[t_xt[xs]])
            for half in range(2):
                p = oi % 2
                oi += 1
                hsl = slice(half * 512, (half + 1) * 512)
                for c in range(8):
                    _mm(kb, pso[p][:], mg[s][:, c, j * 128:(j + 1) * 128], wo[:, c, hsl], c == 0, c == 7,
                        [t_w, t_mg[s]], [t_pso[p]])
                kb.op("vector", lambda e, p=p, hsl=hsl: e.tensor_tensor(out=tmp[p][:], in0=pso[p][:],
                                                                        in1=gate1[:, hsl], op=ALU.mult),
                      reads=[t_pso[p], t_gate1], writes=[t_tmp[p]])
                kb.op("vector", lambda e, p=p, xs=xs, hsl=hsl: e.tensor_tensor(out=xt[xs][:, hsl], in0=xt[xs][:, hsl],
                                                                               in1=tmp[p][:], op=ALU.add),
                      reads=[t_tmp[p], t_xt[xs]], writes=[t_xt[xs]])
            kb.dma("gpsimd", xmid[t0:t0 + 128, :], xt[xs][:], reads=[t_xt[xs]], writes=[t_xmid])
    kb.release(mk)


def emit_moe_pass(kb, consts, S, xmid, t_xmid, w, gmul2, t_gmul2, shift2, t_shift2, gate2, t_gate2,
                  xout, t_xout, final_g=None):
    TS = min(2048, S)
    NST = S // TS
    NTL = TS // 128
    mk = kb.mark()
    ct = consts["tok"]
    h2T = kb.sbuf("e_h2T", [128, 8, TS], BF16)
    t_h2T = Tok("e_h2T")
    yacc = kb.sbuf("e_yacc", [128, NTL, 1024], F32)
    t_yacc = [Tok("e_yacc%d" % i) for i in range(NTL)]
    Wt = kb.sbuf("e_Wt", [128, NTL, 32], F32)
    t_Wt = [Tok("e_Wt%d" % i) for i in range(NTL)]
    wr = kb.sbuf("e_wr", [128, 8, 36], F32)
    rb = kb.sbuf("e_rb", [128, 36], F32)
    t_wr = Tok("e_wr")
    kb.dma("sync", wr[:], w["wr"].rearrange("(c p) n -> p c n", p=128), writes=[t_wr])
    kb.dma("sync", rb[:], w["rb"].partition_broadcast(128), writes=[t_wr])
    fg = None
    if final_g is not None:
        fg = kb.sbuf("e_fg", [128, 1024], F32)
        kb.dma("sync", fg[:], final_g.partition_broadcast(128), writes=[t_wr])
    weg = [kb.sbuf("e_weg%d" % i, [128, 8, 256], BF16) for i in range(2)]
    weu = [kb.sbuf("e_weu%d" % i, [128, 8, 256], BF16) for i in range(2)]
    wed = [kb.sbuf("e_wed%d" % i, [128, 2, 1024], BF16) for i in range(2)]
    t_we = [Tok("e_we%d" % i) for i in range(2)]
    xt = [kb.sbuf("e_x%d" % i, [128, 1024], F32) for i in range(2)]
    t_xt = [Tok("e_x%d" % i) for i in range(2)]
    scr = [kb.sbuf("e_scr%d" % i, [128, 1024], F32) for i in range(2)]
    t_scr = [Tok("e_scr%d" % i) for i in range(2)]
    h2f = [kb.sbuf("e_h2f%d" % i, [128, 1024], F32) for i in range(2)]
    t_h2f = [Tok("e_h2f%d" % i) for i in range(2)]
    stat = [kb.sbuf("e_st%d" % i, [128, 4], F32) for i in range(2)]
    t_st = [Tok("e_st%d" % i) for i in range(2)]
    h2Tf = kb.sbuf("e_h2Tf", [128, 8, 128], F32)
    t_h2Tf = Tok("e_h2Tf")
    R = [kb.sbuf("e_R%d" % i, [128, 160], F32) for i in range(2)]
    t_R = [Tok("e_R%d" % i) for i in range(2)]
    sgl = [kb.sbuf("e_sg%d" % i, [128, 512], F32) for i in range(2)]
    t_sgl = [Tok("e_sg%d" % i) for i in range(2)]
    aT = [kb.sbuf("e_aT%d" % i, [128, 2, 512], BF16) for i in range(2)]
    t_aT = [Tok("e_aT%d" % i) for i in range(2)]
    bank = [kb.psum("e_bank%d" % i, [128, 512], F32) for i in range(8)]
    t_bank = [Tok("e_bank%d" % i) for i in range(8)]
    wload = 0

    def load_expert(e):
        nonlocal wload
        k = wload % 2
        wload += 1
        for c in range(8):
            kb.dma("gpsimd", weg[k][:, c, :], w["weg"][e, c * 128:(c + 1) * 128, :], writes=[t_we[k]])
        for c in range(8):
            kb.dma("gpsimd", weu[k][:, c, :], w["weu"][e, c * 128:(c + 1) * 128, :], writes=[t_we[k]])
        for c in range(2):
            kb.dma("gpsimd", wed[k][:, c, :], w["wed"][e, c * 128:(c + 1) * 128, :], writes=[t_we[k]])
        return k

    for st in range(NST):
        for i in range(NTL):
            s = i % 2
            t0 = st * TS + i * 128
            kb.dma("sync", xt[s][:], xmid[t0:t0 + 128, :], reads=[t_xmid], writes=[t_xt[s]])
            emit_norm_mod(kb, xt[s], t_xt[s], gmul2, t_gmul2, shift2, t_shift2, h2f[s], t_h2f[s],
                          scr[s], t_scr[s], stat[s], t_st[s], consts["eps"][:, 0:1])
            for half in range(2):
                for c4 in range(4):
                    c = half * 4 + c4
                    kb.op("tensor", lambda e, s=s, c=c, half=half, c4=c4: e.transpose(
                        out=bank[half][:, c4 * 128:(c4 + 1) * 128], in_=h2f[s][:, c * 128:(c + 1) * 128],
                        identity=consts["id_f"][:]), reads=[t_h2f[s], ct], writes=[t_bank[half]], sig=(c4 == 3))
                kb.op("scalar", lambda e, half=half: e.copy(
                    out=h2Tf[:, half * 4:half * 4 + 4, :], in_=bank[half][:].rearrange("p (c t) -> p c t", c=4)),
                    reads=[t_bank[half]], writes=[t_h2Tf])
            kb.op("gpsimd", lambda e, i=i: e.tensor_copy(out=h2T[:, :, i * 128:(i + 1) * 128], in_=h2Tf[:]),
                  reads=[t_h2Tf], writes=[t_h2T])
            for c in range(8):
                _mm(kb, bank[2][:, 0:36], h2Tf[:, c, :], wr[:, c, :], c == 0, c == 7, [t_h2Tf, t_wr], [t_bank[2]])
            r = R[s]
            tr = t_R[s]
            V = lambda fn, reads, writes: kb.op("vector", fn, reads=reads, writes=writes)
            V(lambda e: e.tensor_tensor(out=r[:, 0:36], in0=bank[2][:, 0:36], in1=rb[:], op=ALU.add),
              [t_bank[2], t_wr], [tr])
            V(lambda e: e.reduce_max(out=r[:, 36:37], in_=r[:, 0:4], axis=AX.X), [tr], [tr])
            V(lambda e: e.tensor_scalar_mul(out=r[:, 37:38], in0=r[:, 36:37], scalar1=-1.0), [tr], [tr])
            kb.op("scalar", lambda e: e.activation(out=r[:, 40:44], in_=r[:, 0:4], func=AF.Exp, bias=r[:, 37:38]),
                  reads=[tr], writes=[tr])
            V(lambda e: e.reduce_sum(out=r[:, 44:45], in_=r[:, 40:44], axis=AX.X), [tr], [tr])
            V(lambda e: e.reciprocal(out=r[:, 45:46], in_=r[:, 44:45]), [tr], [tr])
            V(lambda e: e.tensor_scalar(out=r[:, 48:52], in0=r[:, 0:4], scalar1=r[:, 36:37], scalar2=None,
                                        op0=ALU.is_equal), [tr], [tr])
            V(lambda e: e.tensor_scalar(out=r[:, 52:56], in0=r[:, 48:52], scalar1=-1.0, scalar2=BIG,
                                        op0=ALU.add, op1=ALU.mult), [tr], [tr])
            V(lambda e: e.tensor_tensor(out=r[:, 64:96].rearrange("p (g k) -> p g k", g=4),
                                        in0=r[:, 4:36].rearrange("p (g k) -> p g k", g=4),
                                        in1=r[:, 52:56].unsqueeze(2).to_broadcast([128, 4, 8]), op=ALU.add),
              [tr], [tr])
            V(lambda e: e.reduce_max(out=r[:, 96:97], in_=r[:, 64:96], axis=AX.X), [tr], [tr])
            V(lambda e: e.tensor_scalar(out=r[:, 104:136], in0=r[:, 64:96], scalar1=r[:, 96:97], scalar2=None,
                                        op0=ALU.is_equal), [tr], [tr])
            V(lambda e: e.scalar_tensor_tensor(out=r[:, 64:96], in0=r[:, 104:136], scalar=-BIG, in1=r[:, 64:96],
                                               op0=ALU.mult, op1=ALU.add), [tr], [tr])
            V(lambda e: e.reduce_max(out=r[:, 97:98], in_=r[:, 64:96], axis=AX.X), [tr], [tr])
            V(lambda e: e.tensor_tensor(out=r[:, 98:99], in0=r[:, 97:98], in1=r[:, 96:97], op=ALU.subtract),
              [tr], [tr])
            kb.op("scalar", lambda e: e.activation(out=r[:, 99:100], in_=r[:, 98:99], func=AF.Exp),
                  reads=[tr], writes=[tr])
            V(lambda e: e.tensor_scalar_add(out=r[:, 100:101], in0=r[:, 99:100], scalar1=1.0), [tr], [tr])
            V(lambda e: e.reciprocal(out=r[:, 101:102], in_=r[:, 100:101]), [tr], [tr])
            V(lambda e: e.tensor_tensor(out=r[:, 102:103], in0=r[:, 101:102], in1=r[:, 45:46], op=ALU.mult),
              [tr], [tr])
            V(lambda e: e.tensor_tensor(out=r[:, 103:104], in0=r[:, 102:103], in1=r[:, 99:100], op=ALU.mult),
              [tr], [tr])
            V(lambda e, i=i: e.tensor_scalar_mul(out=Wt[:, i, :], in0=r[:, 104:136], scalar1=r[:, 102:103]),
              [tr], [t_Wt[i]])
            V(lambda e: e.tensor_scalar(out=r[:, 104:136], in0=r[:, 64:96], scalar1=r[:, 97:98], scalar2=None,
                                        op0=ALU.is_equal), [tr], [tr])
            V(lambda e, i=i: e.scalar_tensor_tensor(out=Wt[:, i, :], in0=r[:, 104:136], scalar=r[:, 103:104],
                                                    in1=Wt[:, i, :], op0=ALU.mult, op1=ALU.add),
              [tr, t_Wt[i]], [t_Wt[i]])
        steps = [(ex, tl) for ex in range(32) for tl in range(TS // 512)]
        wslot = {}
        gcount = [0]
        dcount = [0]

        def gu_group(si, hc, which):
            ex, tl = steps[si]
            k = wslot[ex]
            tsl = slice(tl * 512, (tl + 1) * 512)
            hsl = slice(hc * 128, (hc + 1) * 128)
            pg = hc
            if which == 0:
                for c in range(8):
                    _mm(kb, bank[pg][:], weg[k][:, c, hsl], h2T[:, c, tsl], c == 0, c == 7,
                        [t_we[k], t_h2T], [t_bank[pg]])
                kb.op("scalar", lambda e: e.activation(out=sgl[pg][:], in_=bank[pg][:], func=AF.Silu),
                      reads=[t_bank[pg]], writes=[t_sgl[pg]])
            else:
                a = si % 2
                for c in range(8):
                    _mm(kb, bank[2 + pg][:], weu[k][:, c, hsl], h2T[:, c, tsl], c == 0, c == 7,
                        [t_we[k], t_h2T], [t_bank[2 + pg]])
                kb.op("vector", lambda e: e.tensor_tensor(out=aT[a][:, hc, :], in0=bank[2 + pg][:], in1=sgl[pg][:],
                                                          op=ALU.mult),
                      reads=[t_bank[2 + pg], t_sgl[pg]], writes=[t_aT[a]])

        def d_group(si, j, half):
            ex, tl = steps[si]
            k = wslot[ex]
            a = si % 2
            ti = tl * 4 + j
            pd = 4 + dcount[0] % 4
            dcount[0] += 1
            osl = slice(half * 512, (half + 1) * 512)
            for hc in range(2):
                _mm(kb, bank[pd][:], aT[a][:, hc, j * 128:(j + 1) * 128], wed[k][:, hc, osl],
                    hc == 0, hc == 1, [t_aT[a], t_we[k]], [t_bank[pd]])
            if ex == 0:
                kb.op("vector", lambda e: e.tensor_scalar_mul(out=yacc[:, ti, osl], in0=bank[pd][:],
                                                              scalar1=Wt[:, ti, ex:ex + 1]),
                      reads=[t_bank[pd], t_Wt[ti]], writes=[t_yacc[ti]])
            else:
                kb.op("vector", lambda e: e.scalar_tensor_tensor(out=yacc[:, ti, osl], in0=bank[pd][:],
                                                                 scalar=Wt[:, ti, ex:ex + 1], in1=yacc[:, ti, osl],
                                                                 op0=ALU.mult, op1=ALU.add),
                      reads=[t_bank[pd], t_Wt[ti], t_yacc[ti]], writes=[t_yacc[ti]])

        dlist = [(j, half) for j in range(4) for half in range(2)]
        for si in range(len(steps) + 1):
            if si < len(steps):
                ex, tl = steps[si]
                if tl == 0:
                    wslot[ex] = load_expert(ex)
            gul = [(0, 0), (0, 1), (1, 0), (1, 1)]
            for gidx in range(4):
                if si < len(steps):
                    gu_group(si, gul[gidx][0], gul[gidx][1])
                if si > 0:
                    for (j, half) in dlist[2 * gidx:2 * gidx + 2]:
                        d_group(si - 1, j, half)
        for i in range(NTL):
            s = i % 2
            t0 = st * TS + i * 128
            kb.dma("sync", xt[s][:], xmid[t0:t0 + 128, :], reads=[t_xmid], writes=[t_xt[s]])
            kb.op("vector", lambda e, i=i: e.tensor_tensor(out=yacc[:, i, :], in0=yacc[:, i, :], in1=gate2[:],
                                                           op=ALU.mult),
                  reads=[t_yacc[i], t_gate2], writes=[t_yacc[i]])
            kb.op("gpsimd", lambda e, s=s, i=i: e.tensor_tensor(out=xt[s][:], in0=xt[s][:], in1=yacc[:, i, :],
                                                                op=ALU.add),
                  reads=[t_xt[s], t_yacc[i]], writes=[t_xt[s]])
            if final_g is None:
                kb.dma("gpsimd", xout[t0:t0 + 128, :], xt[s][:], reads=[t_xt[s]], writes=[t_xout])
            else:
                emit_norm_mod(kb, xt[s], t_xt[s], fg, t_wr, None, None, h2f[s], t_h2f[s],
                              scr[s], t_scr[s], stat[s], t_st[s], consts["eps"][:, 0:1])
                kb.dma("gpsimd", xout[t0:t0 + 128, :], h2f[s][:], reads=[t_h2f[s]], writes=[t_xout])
    kb.release(mk)


LAYER_SHAPES = dict(
    modw=[D, 6144], modb=[6144], g1=[D],
    wqk=[D, 1024], wtm=[D, 2056], cw=[128, 8, 4], cb=[128, 8], ifb=[8],
    wmla=[D, 768], gqkv=[128, 5], wuqn=[384, 1024], wuqr=[384, 512], wuqs=[384, 512],
    wuk=[256, 1024], wuv=[256, 1024],
    wgate=[D, 2048], wbm=[D, D], wba=[D, D], wout=[D, D], g2=[D], wr=[D, 36], rb=[36],
    weg=[32, D, 256], weu=[32, D, 256], wed=[32, 256, D])
DEPTH = 2


def build_full(S):
    kb = KB()
    x = kb.dram_in("x", [S, D], F32)
    cvec = kb.dram_in("cvec", [D], F32)
    cs = kb.dram_in("cs", [64, 2, S], F32)
    fg = kb.dram_in("fg", [D], F32)
    W = []
    for l in range(DEPTH):
        W.append({k: kb.dram_in("%s_%d" % (k, l), v, F32) for k, v in LAYER_SHAPES.items()})
        W[l]["cs"] = cs
    out = kb.dram_out("out", [S, D], F32)
    hT = kb.dram_tmp("s_hT", [8, 128, S], BF16)
    ymT = kb.dram_tmp("s_ymT", [8, 128, S], BF16)
    yaT = kb.dram_tmp("s_yaT", [8, 128, S], BF16)
    xmid = kb.dram_tmp("s_xmid", [S, D], F32)
    xnext = kb.dram_tmp("s_xnext", [S, D], F32)
    scr = alloc_mla_scratch(kb, S)
    t_hT, t_ymT, t_yaT = Tok("dram:hT"), Tok("dram:ymT"), Tok("dram:yaT")
    t_xmid, t_xnext, t_out, t_x = Tok("dram:xmid"), Tok("dram:xnext"), Tok("dram:out"), Tok("dram:x")
    consts = load_consts(kb)
    x_cur, t_xcur = x, t_x
    for l in range(DEPTH):
        w = W[l]
        mk = kb.mark()
        emit_k1_body(kb, consts, x_cur, cvec, w["modw"][:, 0:2048], w["modb"][0:2048], w["g1"], hT, S,
                     hT_tok=t_hT, x_tok=t_xcur)
        kb.release(mk)
        emit_mlstm_pass(kb, consts, S, hT, t_hT, w, ymT, t_ymT)
        emit_mla_proj_pass(kb, consts, S, hT, t_hT, w, scr)
        emit_attn_pass(kb, consts, S, scr, yaT, t_yaT)
        mk = kb.mark()
        mods = [kb.sbuf("mod%d" % i, [128, D], F32) for i in range(4)]
        tm = [Tok("mod%d" % i) for i in range(4)]
        g2t = kb.sbuf("g2t", [128, D], F32)
        tg2 = Tok("g2t")
        emit_mod(kb, consts, w["modw"][:, 2048:6144], w["modb"][2048:6144], cvec, 4, list(zip(mods, tm)))
        kb.dma("sync", g2t[:], w["g2"].partition_broadcast(128), writes=[tg2])
        kb.op("vector", lambda e: e.scalar_tensor_tensor(out=mods[2][:], in0=mods[2][:], scalar=1.0, in1=g2t[:],
                                                         op0=ALU.add, op1=ALU.mult),
              reads=[tm[2], tg2], writes=[tm[2]])
        t_in_all = _MultiTok([t_hT, t_ymT, t_yaT])
        emit_merge_pass(kb, consts, S, hT, ymT, yaT, t_in_all, x_cur, t_xcur, w, mods[0], tm[0], xmid, t_xmid)
        last = (l == DEPTH - 1)
        emit_moe_pass(kb, consts, S, xmid, t_xmid, w, mods[2], tm[2], mods[1], tm[1], mods[3], tm[3],
                      out if last else xnext, t_out if last else t_xnext, final_g=fg if last else None)
        kb.release(mk)
        x_cur, t_xcur = xnext, t_xnext
    kb.finish([t_out])
    return kb


class _MultiTok:
    def __init__(self, toks):
        self.name = "dram:multi"
        self._toks = toks


def _rope_tables(S):
    pos = np.arange(S, dtype=np.float32)
    inv_freq = (1.0 / (np.float32(10000.0) ** (np.arange(0, 64, 2, dtype=np.float32) / np.float32(64)))).astype(np.float32)
    ang = pos[:, None] * inv_freq[None, :]
    cos = np.cos(ang).astype(np.float32)
    sin = np.sin(ang).astype(np.float32)
    CC = np.concatenate([cos, cos], 1).T
    SS = np.concatenate([-sin, sin], 1).T
    return np.ascontiguousarray(np.stack([CC, SS], 1))


def _prep_layer(z, l):
    c = np.ascontiguousarray
    win = z["w_in"][l]
    d = {}
    d["modw"] = c(z["mod_w"][l])
    d["modb"] = c(z["mod_b"][l])
    d["g1"] = c(z["norm1_g"][l])
    d["wqk"] = c(win[:, :1024])
    d["wtm"] = c(win[:, 1024:3080])
    d["cw"] = c(z["conv_w"][l].reshape(4, 8, 128).transpose(2, 1, 0))
    d["cb"] = c(z["conv_b"][l].reshape(8, 128).T)
    d["ifb"] = c(np.concatenate([z["igate_b"][l], z["fgate_b"][l]]))
    kr = win[:, 3720:3784]
    d["wmla"] = c(np.concatenate([win[:, 3080:3784], kr[:, 32:], kr[:, :32]], 1))
    d["gqkv"] = c(np.concatenate([z["q_norm_g"][l].reshape(3, 128).T, z["kv_norm_g"][l].reshape(2, 128).T], 1))
    uq = z["w_uq"][l].reshape(384, 8, 192)
    d["wuqn"] = c(uq[:, :, :128].reshape(384, 1024))
    d["wuqr"] = c(uq[:, :, 128:].reshape(384, 512))
    d["wuqs"] = c(np.concatenate([uq[:, :, 160:], uq[:, :, 128:160]], 2).reshape(384, 512))
    ukv = z["w_ukv"][l].reshape(256, 8, 256)
    d["wuk"] = c(ukv[:, :, :128].reshape(256, 1024))
    d["wuv"] = c(ukv[:, :, 128:].reshape(256, 1024))
    d["wgate"] = c(win[:, 3784:5832])
    d["wbm"] = c(z["w_branch_m"][l])
    d["wba"] = c(z["w_branch_a"][l])
    d["wout"] = c(z["w_out"][l])
    d["g2"] = c(z["norm2_g"][l])
    d["wr"] = c(np.concatenate([z["w_group"][l], z["w_router"][l]], 1))
    d["rb"] = c(np.concatenate([z["b_group"][l], z["b_router"][l]]))
    d["weg"] = c(z["w_expert_gate"][l])
    d["weu"] = c(z["w_expert_up"][l])
    d["wed"] = c(z["w_expert_down"][l])
    return d


_PROGRAMS = {}


def kernel(**inputs):
    z = {k: np.asarray(v) for k, v in inputs.items()}
    x = z["x"].astype(np.float32, copy=False)
    B, S, _ = x.shape
    if S not in _PROGRAMS:
        _PROGRAMS[S] = build_full(S)
    kb = _PROGRAMS[S]
    shared = {"cs": _rope_tables(S), "fg": np.ascontiguousarray(z["final_norm_g"].astype(np.float32))}
    for l in range(DEPTH):
        for k, v in _prep_layer(z, l).items():
            assert list(v.shape) == LAYER_SHAPES[k], (k, v.shape)
            shared["%s_%d" % (k, l)] = v.astype(np.float32, copy=False)
    in_maps = []
    for b in range(B):
        m = dict(shared)
        m["x"] = np.ascontiguousarray(x[b])
        m["cvec"] = np.ascontiguousarray(z["c"][b].astype(np.float32))
        in_maps.append(m)
    res = run_bass_kernel_spmd(kb.nc, in_maps, core_ids=list(range(B)))
    return np.stack([np.asarray(r["out"]) for r in res.results], 0).astype(np.float32)
```
